# Optimizing a Trainium2 kernel written in Bass

```python
import math
import jax
import jax.numpy as jnp
from jax import lax
import numpy as np

D_MODEL = 1024
BATCH = 4
SEQ = 8192
DEPTH = 4

GRID_W = 64
CTX_LEN = 256
HEAD_DIM = 64
ROPE_THETA = 10000.0
NORM_EPS = 1e-6
Q_BLOCK = 128
D_FF = 4 * D_MODEL
N_MOD = 6
MIX_HALF = D_MODEL // 2

GLA_HEADS = 4
GLA_DV = MIX_HALF // GLA_HEADS
GLA_DK = GLA_DV // 2
GLA_RANK = 16
GLA_NORMALIZER = 16.0
GLA_CHUNK = 64
GLA_QK = GLA_HEADS * GLA_DK
GLA_V = GLA_HEADS * GLA_DV

GQA_HEADS = MIX_HALF // HEAD_DIM
GQA_KV_HEADS = 2

HY_CH = MIX_HALF
HY_ORDER = 2
HY_SHORT = 3
HY_BANDS = 16
HY_EMB = 2 * HY_BANDS + 1
HY_FFN = 64
HY_TARGET = 1e-2
HY_FAST_PCT = 0.3
HY_SLOW_PCT = 1.5
HY_COLS = (HY_ORDER + 1) * HY_CH

DIFF_HEADS = MIX_HALF // (2 * HEAD_DIM)
DIFF_QK = DIFF_HEADS * 2 * HEAD_DIM
DIFF_V = DIFF_HEADS * 2 * HEAD_DIM

EVEN_COLS = 2 * GLA_QK + 2 * GLA_V + 2 * GLA_RANK + (GQA_HEADS + 2 * GQA_KV_HEADS) * HEAD_DIM
EVEN_MIX = GLA_V + GQA_HEADS * HEAD_DIM
ODD_COLS = HY_COLS + 2 * DIFF_QK + DIFF_V
ODD_MIX = HY_CH + DIFF_V

kernel_name = "hybrid_gla_gqa_hyena_diffattn_dit"


def rmsnorm(x, g):
    xf = x.astype(jnp.float32)
    y = xf * lax.rsqrt(jnp.mean(xf * xf, axis=-1, keepdims=True) + NORM_EPS)
    return (y * g.astype(jnp.float32)).astype(x.dtype)


def modulate(h, shift, scale):
    return h * (1 + scale) + shift


def split_cols(t, sizes):
    return jnp.split(t, np.cumsum(sizes)[:-1].tolist(), axis=-1)


def sq_relu_mlp(h, w1, w2):
    return jnp.square(jax.nn.relu(h @ w1)) @ w2


def axial_rope_tables(rows):
    r = jnp.broadcast_to(jnp.arange(rows, dtype=jnp.float32)[:, None], (rows, GRID_W)).reshape(-1)
    col = jnp.broadcast_to(jnp.arange(GRID_W, dtype=jnp.float32)[None, :], (rows, GRID_W)).reshape(-1)
    n_pairs = HEAD_DIM // 4
    inv = ROPE_THETA ** (-jnp.arange(n_pairs, dtype=jnp.float32) / n_pairs)
    ang = jnp.concatenate([r[:, None] * inv, col[:, None] * inv], axis=-1)
    return jnp.cos(ang), jnp.sin(ang)


def apply_rope(t, cos, sin):
    L = t.shape[1]
    bshape = (L,) + (1,) * (t.ndim - 3) + (HEAD_DIM // 2,)
    cs, sn = cos.reshape(bshape), sin.reshape(bshape)
    tp = t.astype(jnp.float32).reshape(t.shape[:-1] + (HEAD_DIM // 2, 2))
    x1, x2 = tp[..., 0], tp[..., 1]
    out = jnp.stack([x1 * cs - x2 * sn, x1 * sn + x2 * cs], axis=-1)
    return out.reshape(t.shape).astype(t.dtype)


def gla_chunked(q, k, v, la, s0):
    B, L, H, dk = q.shape
    dv = v.shape[-1]
    n = L // GLA_CHUNK

    def blocks(t):
        return t.astype(jnp.float32).reshape(B, n, GLA_CHUNK, H, t.shape[-1]).transpose(1, 0, 3, 2, 4)

    q, k, v, la = blocks(q), blocks(k), blocks(v), blocks(la)
    b = jnp.cumsum(la, axis=3)
    b_end = b[:, :, :, -1:]
    q_in = q * jnp.exp(b)
    k_in = k * jnp.exp(-b)
    k_end = k * jnp.exp(b_end - b)
    mask = jnp.tril(jnp.ones((GLA_CHUNK, GLA_CHUNK), dtype=bool))
    a = jnp.where(mask, jnp.einsum('nbhid,nbhjd->nbhij', q_in, k_in), 0.0)
    o_intra = jnp.einsum('nbhij,nbhjv->nbhiv', a, v)

    def step(s, inp):
        q_c, k_c, v_c, dec = inp
        o_c = jnp.einsum('bhid,bhdv->bhiv', q_c, s)
        s = s * dec[..., None] + jnp.einsum('bhjd,bhjv->bhdv', k_c, v_c)
        return s, o_c

    s_fin, o_inter = lax.scan(step, s0, (q_in, k_end, v, jnp.exp(b_end[:, :, :, 0])))
    o = (o_intra + o_inter).transpose(1, 0, 3, 2, 4).reshape(B, L, H, dv)
    return o, s_fin


def gla_final_state(k, v, la, s0):
    b = jnp.cumsum(la, axis=1)
    tot = b[:, -1]
    kw = k.astype(jnp.float32) * jnp.exp(tot[:, None] - b)
    return s0 * jnp.exp(tot)[..., None] + jnp.einsum('blhd,blhv->bhdv', kw, v.astype(jnp.float32))


def gla_mixer(p, pc, w_lr, b_lr, gain, ctx_out):
    def prep(q, k, v, g, lr):
        B, L, _ = q.shape
        gk = jnp.einsum('blzr,zrk->blzk', lr.reshape(B, L, 2, GLA_RANK), w_lr) + b_lr
        la = (jax.nn.log_sigmoid(gk.astype(jnp.float32)) / GLA_NORMALIZER).reshape(B, L, 2, GLA_HEADS, GLA_DK)
        return (q.reshape(B, L, GLA_HEADS, GLA_DK) * GLA_DK ** -0.5,
                k.reshape(B, L, GLA_HEADS, GLA_DK),
                v.reshape(B, L, GLA_HEADS, GLA_DV),
                g.reshape(B, L, GLA_HEADS, GLA_DV),
                la)

    q, k, v, g, la = prep(*p)
    qc, kc, vc, gc, lac = prep(*pc)
    s0 = jnp.zeros((q.shape[0], GLA_HEADS, GLA_DK, GLA_DV), jnp.float32)
    o, oc = 0.0, 0.0
    for d in range(2):
        rev = (lambda t: t[:, ::-1]) if d == 1 else (lambda t: t)
        if ctx_out:
            oc_d, s_ctx = gla_chunked(rev(qc), rev(kc), rev(vc), rev(lac[:, :, d]), s0)
            oc = oc + rev(oc_d)
        else:
            s_ctx = gla_final_state(rev(kc), rev(vc), rev(lac[:, :, d]), s0)
        o_d, _ = gla_chunked(rev(q), rev(k), rev(v), rev(la[:, :, d]), s_ctx)
        o = o + rev(o_d)

    def out(o, g):
        B, L = o.shape[:2]
        return (rmsnorm(o.astype(g.dtype), gain) * jax.nn.silu(g)).reshape(B, L, GLA_V)

    return out(o, g), (out(oc, gc) if ctx_out else None)


def gqa_attend(q, keys, vals):
    B, L, hq, hd = q.shape
    hkv = keys.shape[2]
    qb = q.reshape(B, L // Q_BLOCK, Q_BLOCK, hkv, hq // hkv, hd).transpose(1, 0, 2, 3, 4, 5)

    def one(qblk):
        s = jnp.einsum('bqkgd,bskd->bkgqs', qblk, keys).astype(jnp.float32) * hd ** -0.5
        pr = jax.nn.softmax(s, axis=-1).astype(vals.dtype)
        return jnp.einsum('bkgqs,bskd->bqkgd', pr, vals)

    o = lax.map(one, qb)
    return o.transpose(1, 0, 2, 3, 4, 5).reshape(B, L, hq * hd)


def gqa_mixer(p, pc, qk_g, cos, sin, ctx_out):
    def heads(q, k, v):
        B, L, _ = q.shape
        q = rmsnorm(q.reshape(B, L, GQA_HEADS, HEAD_DIM), qk_g[0])
        k = rmsnorm(k.reshape(B, L, GQA_KV_HEADS, HEAD_DIM), qk_g[1])
        return q, k, v.reshape(B, L, GQA_KV_HEADS, HEAD_DIM)

    q, k, v = heads(*p)
    qc, kc, vc = heads(*pc)
    q, k = apply_rope(q, cos, sin), apply_rope(k, cos, sin)
    y = gqa_attend(q, jnp.concatenate([kc, k], axis=1), jnp.concatenate([vc, v], axis=1))
    yc = gqa_attend(qc, kc, vc) if ctx_out else None
    return y, yc


def even_mixer(h, hc, w_in, w_lr, b_lr, gla_g, qk_g, w_out, cos, sin, ctx_out):
    sizes = (GLA_QK, GLA_QK, GLA_V, GLA_V, 2 * GLA_RANK,
             GQA_HEADS * HEAD_DIM, GQA_KV_HEADS * HEAD_DIM, GQA_KV_HEADS * HEAD_DIM)
    p = split_cols(h @ w_in, sizes)
    pc = split_cols(hc @ w_in, sizes)
    y_a, yc_a = gla_mixer(p[:5], pc[:5], w_lr, b_lr, gla_g, ctx_out)
    y_b, yc_b = gqa_mixer(p[5:], pc[5:], qk_g, cos, sin, ctx_out)
    y = jnp.concatenate([y_a, y_b], axis=-1) @ w_out
    yc = (jnp.concatenate([yc_a, yc_b], axis=-1) @ w_out) if ctx_out else None
    return y, yc


def short_conv3(u, w, b):
    up = jnp.pad(u, ((0, 0), (1, 1), (0, 0)))
    return up[:, :-2] * w[0] + up[:, 1:-1] * w[1] + up[:, 2:] * w[2] + b


def hyena_filters(L, w1, b1, w2, b2, w3):
    pos = jnp.arange(L, dtype=jnp.float32)
    t = pos / max(L - 1, 1)
    w = 2 * math.pi * pos / L
    bands = jnp.linspace(1e-4, HY_BANDS - 1, HY_BANDS, dtype=jnp.float32)
    z = jnp.concatenate([t[:, None], jnp.cos(w[:, None] * bands), -jnp.sin(w[:, None] * bands)], axis=-1)
    z = z.astype(w1.dtype)
    hid = jnp.sin(z @ w1 + b1)
    hid = jnp.sin(hid @ w2 + b2)
    h = (hid @ w3).astype(jnp.float32).reshape(L, HY_ORDER, 2, HY_CH)
    deltas = jnp.abs(jnp.linspace(math.log(HY_TARGET) / HY_FAST_PCT, math.log(HY_TARGET) / HY_SLOW_PCT,
                                  HY_CH, dtype=jnp.float32))
    return h * jnp.exp(-t[:, None] * deltas)[:, None, None, :]


def bidir_fft_conv(u, h_fwd, h_bwd):
    L, C = h_fwd.shape
    k = jnp.concatenate([h_fwd, jnp.zeros((1, C), h_fwd.dtype), h_bwd[1:][::-1]], axis=0)
    uf = jnp.fft.rfft(u.astype(jnp.float32), n=2 * L, axis=1)
    kf = jnp.fft.rfft(k, n=2 * L, axis=0)
    y = jnp.fft.irfft(uf * kf[None], n=2 * L, axis=1)[:, :L]
    return y.astype(u.dtype)


def hyena_mixer(u, uc, conv_w, conv_b, f_w1, f_b1, f_w2, f_b2, f_w3, hy_bias, ctx_out):
    def run(u):
        L = u.shape[1]
        v, x1, x2 = jnp.split(short_conv3(u, conv_w, conv_b), HY_ORDER + 1, axis=-1)
        h = hyena_filters(L, f_w1, f_b1, f_w2, f_b2, f_w3)
        z = v
        for o, gate in enumerate((x1, x2)):
            z = gate * (bidir_fft_conv(z, h[:, o, 0], h[:, o, 1]) + z * hy_bias[o])
        return z

    return run(u), (run(uc) if ctx_out else None)


def diff_attend(q, keys, vals, lam):
    B, L, H, _, hd = q.shape
    qb = q.reshape(B, L // Q_BLOCK, Q_BLOCK, H, 2, hd).transpose(1, 0, 2, 3, 4, 5)

    def one(qblk):
        s = jnp.einsum('bqhcd,bshcd->bhcqs', qblk, keys).astype(jnp.float32) * hd ** -0.5
        pr = jax.nn.softmax(s, axis=-1)
        a = (pr[:, :, 0] - lam * pr[:, :, 1]).astype(vals.dtype)
        return jnp.einsum('bhqs,bshv->bqhv', a, vals)

    o = lax.map(one, qb)
    return o.transpose(1, 0, 2, 3, 4).reshape(B, L, H, vals.shape[-1])


def diff_mixer(p, pc, lam_p, gain, layer_idx, cos, sin, ctx_out):
    lam_init = 0.8 - 0.6 * math.exp(-0.3 * layer_idx)
    lp = lam_p.astype(jnp.float32)
    lam = jnp.exp(jnp.sum(lp[0] * lp[1])) - jnp.exp(jnp.sum(lp[2] * lp[3])) + lam_init

    def heads(q, k, v):
        B, L, _ = q.shape
        return (q.reshape(B, L, DIFF_HEADS, 2, HEAD_DIM), k.reshape(B, L, DIFF_HEADS, 2, HEAD_DIM),
                v.reshape(B, L, DIFF_HEADS, 2 * HEAD_DIM))

    q, k, v = heads(*p)
    qc, kc, vc = heads(*pc)
    q, k = apply_rope(q, cos, sin), apply_rope(k, cos, sin)

    def out(o):
        B, L = o.shape[:2]
        return (rmsnorm(o, gain) * (1 - lam_init)).reshape(B, L, DIFF_V)

    y = out(diff_attend(q, jnp.concatenate([kc, k], axis=1), jnp.concatenate([vc, v], axis=1), lam))
    yc = out(diff_attend(qc, kc, vc, lam)) if ctx_out else None
    return y, yc


def odd_mixer(h, hc, w_in, conv_w, conv_b, f_w1, f_b1, f_w2, f_b2, f_w3, hy_bias, lam_p, diff_g, w_out,
              cos, sin, layer_idx, ctx_out):
    sizes = (HY_COLS, DIFF_QK, DIFF_QK, DIFF_V)
    p = split_cols(h @ w_in, sizes)
    pc = split_cols(hc @ w_in, sizes)
    y_c, yc_c = hyena_mixer(p[0], pc[0], conv_w, conv_b, f_w1, f_b1, f_w2, f_b2, f_w3, hy_bias, ctx_out)
    y_d, yc_d = diff_mixer(p[1:], pc[1:], lam_p, diff_g, layer_idx, cos, sin, ctx_out)
    y = jnp.concatenate([y_c, y_d], axis=-1) @ w_out
    yc = (jnp.concatenate([yc_c, yc_d], axis=-1) @ w_out) if ctx_out else None
    return y, yc


def setup_inputs(seed: int = 0) -> dict:
    key = jax.random.key(seed)
    ks = jax.random.split(key, 28)
    ne, no = (DEPTH + 1) // 2, DEPTH // 2
    f32 = jnp.float32

    def nrm(k, shape, fan_in, scale=1.0):
        return jax.random.normal(k, shape, f32) * (scale * fan_in ** -0.5)

    def gain(k, shape):
        return 1.0 + 0.05 * jax.random.normal(k, shape, f32)

    def small(k, shape, s):
        return s * jax.random.normal(k, shape, f32)

    return {
        "x": jax.random.normal(ks[0], (BATCH, SEQ, D_MODEL), f32),
        "c": jax.random.normal(ks[1], (BATCH, D_MODEL), f32),
        "ctx": jax.random.normal(ks[2], (BATCH, CTX_LEN, D_MODEL), f32),
        "c_ctx": jax.random.normal(ks[3], (D_MODEL,), f32),
        "w_ada": nrm(ks[4], (DEPTH, D_MODEL, N_MOD * D_MODEL), D_MODEL),
        "b_ada": small(ks[5], (DEPTH, N_MOD * D_MODEL), 0.02),
        "norm_g": gain(ks[6], (DEPTH, 4, D_MODEL)),
        "w_mlp_in": nrm(ks[7], (DEPTH, D_MODEL, D_FF), D_MODEL),
        "w_mlp_out": nrm(ks[8], (DEPTH, D_FF, D_MODEL), D_FF),
        "ev_w_in": nrm(ks[9], (ne, D_MODEL, EVEN_COLS), D_MODEL),
        "ev_w_lr": nrm(ks[10], (ne, 2, GLA_RANK, GLA_QK), GLA_RANK),
        "ev_b_lr": small(ks[11], (ne, 2, GLA_QK), 0.1),
        "ev_gla_g": gain(ks[12], (ne, GLA_DV)),
        "ev_qk_g": gain(ks[13], (ne, 2, HEAD_DIM)),
        "ev_w_out": nrm(ks[14], (ne, EVEN_MIX, D_MODEL), EVEN_MIX),
        "od_w_in": nrm(ks[15], (no, D_MODEL, ODD_COLS), D_MODEL),
        "od_conv_w": nrm(ks[16], (no, HY_SHORT, HY_COLS), HY_SHORT),
        "od_conv_b": small(ks[17], (no, HY_COLS), 0.02),
        "od_f_w1": nrm(ks[18], (no, HY_EMB, HY_FFN), HY_EMB),
        "od_f_b1": small(ks[19], (no, HY_FFN), 0.1),
        "od_f_w2": nrm(ks[20], (no, HY_FFN, HY_FFN), HY_FFN),
        "od_f_b2": small(ks[21], (no, HY_FFN), 0.1),
        "od_f_w3": nrm(ks[22], (no, HY_FFN, HY_ORDER * 2 * HY_CH), HY_FFN, 0.05),
        "od_hy_bias": small(ks[23], (no, HY_ORDER, HY_CH), 0.5),
        "od_lam": small(ks[24], (no, 4, HEAD_DIM), 0.1),
        "od_diff_g": gain(ks[25], (no, 2 * HEAD_DIM)),
        "od_w_out": nrm(ks[26], (no, ODD_MIX, D_MODEL), ODD_MIX),
    }


def reference(x, c, ctx, c_ctx, w_ada, b_ada, norm_g, w_mlp_in, w_mlp_out,
              ev_w_in, ev_w_lr, ev_b_lr, ev_gla_g, ev_qk_g, ev_w_out,
              od_w_in, od_conv_w, od_conv_b, od_f_w1, od_f_b1, od_f_w2, od_f_b2, od_f_w3,
              od_hy_bias, od_lam, od_diff_g, od_w_out):
    ROWS = x.shape[1] // GRID_W
    cos, sin = axial_rope_tables(ROWS)
    sc = jax.nn.silu(c)
    scc = jax.nn.silu(c_ctx)
    for i in range(DEPTH):
        last = i == DEPTH - 1
        j = i // 2
        g = norm_g[i]
        m = [t[:, None, :] for t in jnp.split(sc @ w_ada[i] + b_ada[i], N_MOD, axis=-1)]
        mc = jnp.split(scc @ w_ada[i] + b_ada[i], N_MOD, axis=-1)
        h = modulate(rmsnorm(x, g[0]), m[0], m[1])
        hc = modulate(rmsnorm(ctx, g[0]), mc[0], mc[1])
        if i % 2 == 0:
            y, yc = even_mixer(h, hc, ev_w_in[j], ev_w_lr[j], ev_b_lr[j], ev_gla_g[j], ev_qk_g[j], ev_w_out[j],
                               cos, sin, not last)
        else:
            y, yc = odd_mixer(h, hc, od_w_in[j], od_conv_w[j], od_conv_b[j], od_f_w1[j], od_f_b1[j], od_f_w2[j],
                              od_f_b2[j], od_f_w3[j], od_hy_bias[j], od_lam[j], od_diff_g[j], od_w_out[j],
                              cos, sin, i, not last)
        x = x + m[2] * rmsnorm(y, g[1])
        x = x + m[5] * rmsnorm(sq_relu_mlp(modulate(rmsnorm(x, g[2]), m[3], m[4]), w_mlp_in[i], w_mlp_out[i]), g[3])
        if not last:
            ctx = ctx + mc[2] * rmsnorm(yc, g[1])
            ctx = ctx + mc[5] * rmsnorm(
                sq_relu_mlp(modulate(rmsnorm(ctx, g[2]), mc[3], mc[4]), w_mlp_in[i], w_mlp_out[i]), g[3])
    return x
```

```python
import math
import contextlib
import numpy as np
import concourse.bass as bass
import concourse.mybir as mybir
from concourse.bass_utils import run_bass_kernel_spmd

F32 = mybir.dt.float32
BF16 = mybir.dt.bfloat16
AF = mybir.ActivationFunctionType
ALU = mybir.AluOpType


class Buf:
    __slots__ = ("w", "r", "name")

    def __init__(self, name=""):
        self.w = None
        self.r = {}
        self.name = name


class View:
    __slots__ = ("ap", "bufs")

    def __init__(self, ap, bufs):
        self.ap = ap
        self.bufs = tuple(bufs)


class Tile:
    def __init__(self, t, name):
        self.t = t
        self.buf = Buf(name)

    def __getitem__(self, idx):
        return View(self.t[idx], (self.buf,))

    def v(self, ap):
        return View(ap, (self.buf,))


class DTile:
    def __init__(self, ap, name):
        self.t = ap
        self.name = name
        self.bufs = {}

    def b(self, key):
        if key not in self.bufs:
            self.bufs[key] = Buf(f"{self.name}:{key}")
        return self.bufs[key]

    def v(self, keys, ap):
        if not isinstance(keys, (list, tuple)):
            keys = [keys]
        return View(ap, [self.b(k) for k in keys])


class Eng:
    def __init__(self, name, h, semidx):
        self.name = name
        self.h = h
        self.semidx = semidx
        self.count = 0
        self.waited = {}


class K:
    WRITE_KEYS = ("out", "accum_out", "ap")

    def __init__(self, nc, es, n_dma=24):
        self.nc = nc
        self.es = es
        self.sems = []
        self.E = {}
        for name, h in [("pe", nc.tensor), ("act", nc.scalar), ("dve", nc.vector),
                        ("pool", nc.gpsimd), ("sp", nc.sync)]:
            self.sems.append(es.enter_context(nc.semaphore("s_" + name)))
            self.E[name] = Eng(name, h, len(self.sems) - 1)
        self.dma_sem = []
        self.dma_val = []
        for i in range(2 * n_dma):
            self.sems.append(es.enter_context(nc.semaphore(f"d{i}")))
            self.dma_sem.append(len(self.sems) - 1)
            self.dma_val.append(0)
        self.n_dma = n_dma
        self.dma_next = {"sp": 0, "pool": 0, "act": 0}
        self.ninst = 0

    def sbuf(self, es, name, shape, dtype):
        self.nalloc = getattr(self, "nalloc", 0) + 1
        name = f"sb{self.nalloc}_{name}"
        return Tile(es.enter_context(self.nc.sbuf_tensor(name, list(shape), dtype)), name)

    def psum(self, es, name, shape, dtype=F32):
        self.nalloc = getattr(self, "nalloc", 0) + 1
        name = f"ps{self.nalloc}_{name}"
        return Tile(es.enter_context(self.nc.psum_tensor(name, list(shape), dtype)), name)

    def _wait(self, E, toks):
        need = {}
        for s, v in toks:
            if v > need.get(s, 0):
                need[s] = v
        for s, v in need.items():
            if E.name == "pe" and s == E.semidx:
                continue
            if E.waited.get(s, 0) < v:
                E.h.wait_ge(self.sems[s], v)
                E.waited[s] = v

    @staticmethod
    def _deps(reads, writes):
        toks = []
        for b in reads:
            if b.w is not None:
                toks.append(b.w)
        for b in writes:
            if b.w is not None:
                toks.append(b.w)
            toks.extend(b.r.items())
        return toks

    @staticmethod
    def _commit(tok, reads, writes):
        s, v = tok
        for b in reads:
            if b.r.get(s, 0) < v:
                b.r[s] = v
        for b in writes:
            b.w = tok
            b.r = {}

    def op(self, eng, name, _r=(), _w=(), **kw):
        E = self.E[eng]
        reads, writes, real = [], [], {}
        for k, v in kw.items():
            if isinstance(v, View):
                (writes if k in self.WRITE_KEYS else reads).extend(v.bufs)
                real[k] = v.ap
            else:
                real[k] = v
        for v in _r:
            reads.extend(v.bufs if isinstance(v, View) else [v])
        for v in _w:
            writes.extend(v.bufs if isinstance(v, View) else [v])
        self._wait(E, self._deps(reads, writes))
        ins = getattr(E.h, name)(**real)
        E.count += 1
        ins.then_inc(self.sems[E.semidx], 1)
        self._commit((E.semidx, E.count), reads, writes)
        self.ninst += 1
        return ins

    def dma(self, q, out, in_, **kw):
        E = self.E[q]
        i0 = self.dma_next[q]
        self.dma_next[q] = (i0 + 1) % self.n_dma
        i = i0 + (self.n_dma if q == "pool" else 0)
        s = self.dma_sem[i]
        toks = self._deps(in_.bufs, out.bufs)
        if self.dma_val[i] > 0:
            toks.append((s, self.dma_val[i]))
        self._wait(E, toks)
        ins = E.h.dma_start(out=out.ap, in_=in_.ap, **kw)
        self.dma_val[i] += 16
        ins.then_inc(self.sems[s], 16)
        self._commit((s, self.dma_val[i]), in_.bufs, out.bufs)
        self.ninst += 1
        return ins

    def barrier(self):
        toks = []
        for e2 in self.E.values():
            if e2.count > 0:
                toks.append((e2.semidx, e2.count))
        for i, s in enumerate(self.dma_sem):
            if self.dma_val[i] > 0:
                toks.append((s, self.dma_val[i]))
        for E in self.E.values():
            pe_self = [(s, v) for (s, v) in toks if not (s == E.semidx)]
            self._wait(E, pe_self)

    def finish(self):
        E = self.E["sp"]
        for i, s in enumerate(self.dma_sem):
            if self.dma_val[i] > 0 and E.waited.get(s, 0) < self.dma_val[i]:
                E.h.wait_ge(self.sems[s], self.dma_val[i])
                E.waited[s] = self.dma_val[i]
        for name in ("pe", "act", "dve", "pool"):
            e2 = self.E[name]
            if e2.count > 0 and E.waited.get(e2.semidx, 0) < e2.count:
                E.h.wait_ge(self.sems[e2.semidx], e2.count)


D = 1024
DFF = 4096
C = 256
EPS = 1e-6
NMOD = 6


class Cfg:
    def __init__(self, L=8192, depth=4, debug=False, phases=None):
        self.L = L
        self.T = C + L
        self.depth = depth
        self.debug = debug
        self.phases = phases


def tiles(cfg, n):
    out = []
    for s in range(0, C, n):
        out.append((s, min(n, C - s), 1))
    for s in range(0, cfg.L, n):
        out.append((C + s, min(n, cfg.L - s), 0))
    return out


def host_consts():
    c = {}
    c["ident"] = np.eye(128, dtype=np.float32)
    c["ones"] = np.ones((128, 128), dtype=np.float32)
    return c


class M:
    def __init__(self, cfg):
        self.cfg = cfg
        nc = bass.Bass("TRN2", target_bir_lowering=False)
        self.nc = nc
        self.es = contextlib.ExitStack()
        self.k = K(nc, self.es)
        self.din = {}
        self.dbg_out = []

    def inp(self, name, shape, dtype=F32):
        ap = self.nc.dram_tensor(name, list(shape), dtype, kind="ExternalInput").ap()
        d = DTile(ap, name)
        self.din[name] = d
        return d

    def scratch(self, name, shape, dtype=F32, dbg=False):
        kind = "ExternalOutput" if (dbg and self.cfg.debug) else "Internal"
        ap = self.nc.dram_tensor(name, list(shape), dtype, kind=kind).ap()
        if kind == "ExternalOutput":
            self.dbg_out.append(name)
        return DTile(ap, name)

    def outp(self, name, shape, dtype=F32):
        ap = self.nc.dram_tensor(name, list(shape), dtype, kind="ExternalOutput").ap()
        return DTile(ap, name)

    def declare(self):
        cfg = self.cfg
        L, T, dp = cfg.L, cfg.T, cfg.depth
        ne, no = (dp + 1) // 2, dp // 2
        self.x = self.inp("x", [L, D])
        self.ctx = self.inp("ctx", [C, D])
        self.cc = self.inp("cc", [128, 8, 2])
        self.w_ada = self.inp("w_ada", [dp, D, NMOD * D])
        self.b_ada = self.inp("b_adaT", [dp, 128, 48])
        self.norm_g = self.inp("norm_gT", [dp, 128, 32])
        self.w_mlp_in = self.inp("w_mlp_in", [dp, D, DFF])
        self.w_mlp_out = self.inp("w_mlp_out", [dp, DFF, D])
        self.c_ident = self.inp("ident", [128, 128])
        self.c_ones = self.inp("ones", [128, 128])
        self.out = self.outp("out", [L, D])
        self.XT = self.scratch("XT", [D, T], dbg=True)

    def load_consts(self):
        k, es = self.k, self.es
        self.ident = k.sbuf(es, "ident", [128, 128], F32)
        k.dma("sp", self.ident[:], self.c_ident.v(0, self.c_ident.t[:, :]))
        stg = k.sbuf(es, "ones32", [128, 128], F32)
        k.dma("sp", stg[:], self.c_ones.v(0, self.c_ones.t[:, :]))
        self.ones_bf = k.sbuf(es, "ones_bf", [128, 128], BF16)
        k.op("dve", "tensor_copy", out=self.ones_bf[:], in_=stg[:])
        self.ones32 = stg
        self.sc = k.sbuf(es, "sc", [128, 8, 2], F32)
        tmp = k.sbuf(es, "sc_raw", [128, 8, 2], F32)
        k.dma("sp", tmp[:], self.cc.v(0, self.cc.t[:, :, :]))
        k.op("act", "activation", out=self.sc[:], in_=tmp[:], func=AF.Silu)
        self.mod = k.sbuf(es, "mod", [128, 48, 2], F32)
        self.gl = k.sbuf(es, "gl", [128, 32], F32)
        self.coef = k.sbuf(es, "coef", [128, 6, 8, 2], F32)

    def transpose_in(self):
        k, cfg = self.k, self.cfg
        with contextlib.ExitStack() as es:
            xin = [k.sbuf(es, f"ti_x{i}", [128, D], F32) for i in range(2)]
            stg = [k.sbuf(es, f"ti_s{i}", [128, 8, 512], F32) for i in range(2)]
            ps = [k.psum(es, f"ti_p{i}", [128, 512], F32) for i in range(4)]
            XTr = self.XT.t.rearrange("(c p) t -> p c t", p=128)
            it = 0
            ip = 0
            for gi, (t0, n, isctx) in enumerate(tiles(cfg, 512)):
                sg = stg[gi % 2]
                for s in range(n // 128):
                    xi = xin[it % 2]
                    it += 1
                    tt = t0 + s * 128
                    if isctx:
                        src = self.ctx.v(0, self.ctx.t[tt:tt + 128, :])
                    else:
                        src = self.x.v(0, self.x.t[tt - C:tt - C + 128, :])
                    k.dma("sp", xi[:], src)
                    for half in range(2):
                        p = ps[ip % 4]
                        ip += 1
                        for q in range(4):
                            c = half * 4 + q
                            k.op("pe", "transpose", out=p[:, q * 128:(q + 1) * 128],
                                 in_=xi[:, c * 128:(c + 1) * 128], identity=self.ident[:])
                        eng = "act" if half == 0 else "dve"
                        nm = "copy" if half == 0 else "tensor_copy"
                        k.op(eng, nm,
                             out=sg.v(sg.t[:, half * 4:half * 4 + 4, s * 128:(s + 1) * 128]),
                             in_=p.v(p.t[:, :].rearrange("p (q t) -> p q t", q=4)))
                k.dma("pool", self.XT.v(("t", t0), XTr[:, :, t0:t0 + n]), sg.v(sg.t[:, :, 0:n]))

    def transpose_out(self):
        k, cfg = self.k, self.cfg
        with contextlib.ExitStack() as es:
            xin = [k.sbuf(es, f"to_x{i}", [128, 8, 512], F32) for i in range(2)]
            stg = [k.sbuf(es, f"to_s{i}", [128, D], F32) for i in range(2)]
            ps = [k.psum(es, f"to_p{i}", [128, 512], F32) for i in range(4)]
            XTr = self.XT.t.rearrange("(c p) t -> p c t", p=128)
            it = 0
            ip = 0
            for gi, (t0, n, isctx) in enumerate(tiles(cfg, 512)):
                if isctx:
                    continue
                xi = xin[gi % 2]
                k.dma("sp", xi.v(xi.t[:, :, 0:n]), self.XT.v(("t", t0), XTr[:, :, t0:t0 + n]))
                for s in range(n // 128):
                    sg = stg[it % 2]
                    it += 1
                    for half in range(2):
                        p = ps[ip % 4]
                        ip += 1
                        for q in range(4):
                            c = half * 4 + q
                            k.op("pe", "transpose", out=p[:, q * 128:(q + 1) * 128],
                                 in_=xi.v(xi.t[:, c, s * 128:(s + 1) * 128]), identity=self.ident[:])
                        eng = "act" if half == 0 else "dve"
                        nm = "copy" if half == 0 else "tensor_copy"
                        k.op(eng, nm, out=sg[:, half * 512:(half + 1) * 512], in_=p[:, :])
                    tt = t0 - C + s * 128
                    k.dma("pool", self.out.v(("t", tt), self.out.t[tt:tt + 128, :]), sg[:])

    def mod_vectors(self, li):
        k = self.k
        with contextlib.ExitStack() as es:
            wst = [k.sbuf(es, f"mv_w{i}", [128, 8, 512], F32) for i in range(2)]
            ps = k.psum(es, "mv_ps", [128, 96], F32)
            bsb = k.sbuf(es, "mv_b", [128, 48], F32)
            tmp = k.sbuf(es, "mv_t", [128, 8, 2], F32)
            k.dma("sp", bsb[:], self.b_ada.v(0, self.b_ada.t[li, :, :]))
            k.dma("sp", self.gl[:], self.norm_g.v(0, self.norm_g.t[li, :, :]))
            War = self.w_ada.t[li].rearrange("(kc p) n -> p kc n", p=128)
            for blk in range(12):
                w = wst[blk % 2]
                k.dma("sp", w[:], self.w_ada.v(0, War[:, :, blk * 512:(blk + 1) * 512]))
                for jj in range(4):
                    j = blk * 4 + jj
                    for kc in range(8):
                        k.op("pe", "matmul", out=ps[:, 2 * j:2 * j + 2],
                             lhsT=w.v(w.t[:, kc, jj * 128:(jj + 1) * 128]),
                             rhs=self.sc.v(self.sc.t[:, kc, :]), start=(kc == 0), stop=(kc == 7))
            for t in range(2):
                k.op("dve", "tensor_tensor", out=self.mod.v(self.mod.t[:, :, t]),
                     in0=ps.v(ps.t[:, :].rearrange("p (j t) -> p j t", t=2)[:, :, t]),
                     in1=bsb[:, :], op=ALU.add)
            mod, gl, coef = self.mod, self.gl, self.coef

            def mv(m):
                return mod.v(mod.t[:, m * 8:(m + 1) * 8, :])

            def gv(g):
                return gl.v(gl.t[:, g * 8:(g + 1) * 8])

            for (dst, mscale, gidx) in ((0, 1, 0), (3, 4, 2)):
                k.op("dve", "tensor_scalar", out=tmp[:], in0=mv(mscale), scalar1=1.0, scalar2=None, op0=ALU.add)
                for t in range(2):
                    k.op("dve", "tensor_tensor", out=coef.v(coef.t[:, dst, :, t]),
                         in0=tmp.v(tmp.t[:, :, t]), in1=gv(gidx), op=ALU.mult)
            for (dst, mshift) in ((1, 0), (4, 3)):
                k.op("dve", "tensor_copy", out=coef.v(coef.t[:, dst, :, :]), in_=mv(mshift))
            for (dst, mgate, gidx) in ((2, 2, 1), (5, 5, 3)):
                for t in range(2):
                    k.op("dve", "tensor_tensor", out=coef.v(coef.t[:, dst, :, t]),
                         in0=mod.v(mod.t[:, mgate * 8:(mgate + 1) * 8, t]), in1=gv(gidx), op=ALU.mult)

    def rstd_bc(self, es_bufs, src_chunks, n, nch, dim):
        k = self.k
        ps = es_bufs["ps"]
        for c in range(nch):
            sq = es_bufs["sq"][c % 2]
            k.op("act", "activation", out=sq.v(sq.t[:, 0:n]), in_=src_chunks(c), func=AF.Square)
            k.op("pe", "matmul", out=ps.v(ps.t[:, 0:n]), lhsT=self.ones_bf[:], rhs=sq.v(sq.t[:, 0:n]),
                 start=(c == 0), stop=(c == nch - 1))
        rs, rstd = es_bufs["rs"], es_bufs["rstd"]
        k.op("act", "activation", out=rs.v(rs.t[:, 0:n]), in_=ps.v(ps.t[:, 0:n]), func=AF.Sqrt,
             scale=1.0 / dim, bias=self.epsb[:, 0:1])
        k.op("dve", "reciprocal", out=rstd.v(rstd.t[:, 0:n]), in_=rs.v(rs.t[:, 0:n]))
        return rstd.v(rstd.t[:, 0:n])

    def mlp_layer(self, li):
        k, cfg = self.k, self.cfg
        N = 256
        with contextlib.ExitStack() as es:
            W1 = k.sbuf(es, "W1", [128, 8, DFF], BF16)
            W2 = k.sbuf(es, "W2", [128, 32, D], BF16)
            stg = [k.sbuf(es, f"wstg{i}", [128, 2048], F32) for i in range(2)]
            cast_engs = [("act", "copy"), ("dve", "tensor_copy"), ("pool", "tensor_copy")]
            ic = 0
            for kc in range(8):
                for half in range(2):
                    s = stg[ic % 2]
                    k.dma("sp", s[:], self.w_mlp_in.v(0, self.w_mlp_in.t[li, kc * 128:(kc + 1) * 128,
                                                                         half * 2048:(half + 1) * 2048]))
                    e, nm = cast_engs[ic % 3]
                    k.op(e, nm, out=W1.v(W1.t[:, kc, half * 2048:(half + 1) * 2048]), in_=s[:])
                    ic += 1
            W2r = self.w_mlp_out.t[li].rearrange("(j p) n -> p j n", p=128)
            for jp in range(16):
                s = stg[ic % 2]
                k.dma("sp", s.v(s.t[:, :].rearrange("p (j n) -> p j n", j=2)),
                      self.w_mlp_out.v(0, W2r[:, 2 * jp:2 * jp + 2, :]))
                e, nm = cast_engs[ic % 3]
                k.op(e, nm, out=W2.v(W2.t[:, 2 * jp:2 * jp + 2, :]),
                     in_=s.v(s.t[:, :].rearrange("p (j n) -> p j n", j=2)))
                ic += 1
            xt = [k.sbuf(es, f"ml_x{i}", [128, 8, N], F32) for i in range(2)]
            h = k.sbuf(es, "ml_h", [128, 8, N], BF16)
            a = k.sbuf(es, "ml_a", [128, 32, N], BF16)
            ysb = k.sbuf(es, "ml_y", [128, 8, N], F32)
            tmp = [k.sbuf(es, f"ml_t{i}", [128, N], F32) for i in range(2)]
            nb = {"sq": [k.sbuf(es, f"ml_sq{i}", [128, N], BF16) for i in range(2)],
                  "ps": k.psum(es, "ml_pss", [128, N], F32),
                  "rs": k.sbuf(es, "ml_rs", [128, N], F32),
                  "rstd": k.sbuf(es, "ml_rstd", [128, N], F32)}
            ph = [k.psum(es, f"ml_ph{i}", [128, N], F32) for i in range(2)]
            py = [k.psum(es, f"ml_py{i}", [128, N], F32) for i in range(2)]
            XTr = self.XT.t.rearrange("(c p) t -> p c t", p=128)
            coef = self.coef
            tl = tiles(cfg, N)
            k.dma("sp", xt[0].v(xt[0].t[:, :, 0:tl[0][1]]),
                  self.XT.v(("t", tl[0][0]), XTr[:, :, tl[0][0]:tl[0][0] + tl[0][1]]))
            for ti, (t0, n, isctx) in enumerate(tl):
                x = xt[ti % 2]
                if ti + 1 < len(tl):
                    t1, n1, _ = tl[ti + 1]
                    xn = xt[(ti + 1) % 2]
                    k.dma("sp", xn.v(xn.t[:, :, 0:n1]), self.XT.v(("t", t1), XTr[:, :, t1:t1 + n1]))
                rstd = self.rstd_bc(nb, lambda c: x.v(x.t[:, c, 0:n]), n, 8, D)
                for c in range(8):
                    tp = tmp[c % 2]
                    k.op("dve", "scalar_tensor_tensor", out=tp.v(tp.t[:, 0:n]), in0=x.v(x.t[:, c, 0:n]),
                         scalar=coef.v(coef.t[:, 3, c, isctx:isctx + 1]), in1=rstd, op0=ALU.mult, op1=ALU.mult)
                    k.op("act", "activation", out=h.v(h.t[:, c, 0:n]), in_=tp.v(tp.t[:, 0:n]), func=AF.Identity,
                         bias=coef.v(coef.t[:, 4, c, isctx:isctx + 1]), scale=1.0)
                for j in range(32):
                    p = ph[j % 2]
                    for kc in range(8):
                        k.op("pe", "matmul", out=p.v(p.t[:, 0:n]), lhsT=W1.v(W1.t[:, kc, j * 128:(j + 1) * 128]),
                             rhs=h.v(h.t[:, kc, 0:n]), start=(kc == 0), stop=(kc == 7))
                    tp = tmp[j % 2]
                    k.op("act", "activation", out=tp.v(tp.t[:, 0:n]), in_=p.v(p.t[:, 0:n]), func=AF.Relu)
                    e = "dve" if j % 2 == 0 else "pool"
                    k.op(e, "tensor_tensor", out=a.v(a.t[:, j, 0:n]), in0=tp.v(tp.t[:, 0:n]),
                         in1=tp.v(tp.t[:, 0:n]), op=ALU.mult)
                for c in range(8):
                    p = py[c % 2]
                    for j in range(32):
                        k.op("pe", "matmul", out=p.v(p.t[:, 0:n]), lhsT=W2.v(W2.t[:, j, c * 128:(c + 1) * 128]),
                             rhs=a.v(a.t[:, j, 0:n]), start=(j == 0), stop=(j == 31))
                    k.op("act", "copy", out=ysb.v(ysb.t[:, c, 0:n]), in_=p.v(p.t[:, 0:n]))
                rstd = self.rstd_bc(nb, lambda c: ysb.v(ysb.t[:, c, 0:n]), n, 8, D)
                for c in range(8):
                    tp = tmp[c % 2]
                    k.op("dve", "scalar_tensor_tensor", out=tp.v(tp.t[:, 0:n]), in0=ysb.v(ysb.t[:, c, 0:n]),
                         scalar=coef.v(coef.t[:, 5, c, isctx:isctx + 1]), in1=rstd, op0=ALU.mult, op1=ALU.mult)
                    k.op("pool", "tensor_tensor", out=x.v(x.t[:, c, 0:n]), in0=x.v(x.t[:, c, 0:n]),
                         in1=tp.v(tp.t[:, 0:n]), op=ALU.add)
                k.dma("pool", self.XT.v(("t", t0), XTr[:, :, t0:t0 + n]), x.v(x.t[:, :, 0:n]))

    def build(self):
        cfg = self.cfg
        k = self.k
        self.declare()
        self.load_consts()
        self.epsb = k.sbuf(self.es, "epsb", [128, 1], F32)
        k.op("dve", "memset", ap=self.epsb[:], constant=EPS)
        self.transpose_in()
        k.barrier()
        for li in range(cfg.depth):
            self.mod_vectors(li)
            k.barrier()
            self.mlp_layer(li)
            k.barrier()
        self.transpose_out()
        k.finish()
        self.es.close()
        return self.nc


def in_proj(self, li, Wd, wl, ncols, fchunks, tgroups, PT, PK):
    k, cfg = self.k, self.cfg
    N = 512
    with contextlib.ExitStack() as es:
        W = k.sbuf(es, "ipW", [128, 8, ncols], BF16)
        stg = [k.sbuf(es, f"ipstg{i}", [128, ncols], F32) for i in range(2)]
        cast_engs = [("act", "copy"), ("dve", "tensor_copy"), ("pool", "tensor_copy")]
        for kc in range(8):
            s = stg[kc % 2]
            k.dma("sp", s[:], Wd.v(0, Wd.t[wl, kc * 128:(kc + 1) * 128, :]))
            e, nm = cast_engs[kc % 3]
            k.op(e, nm, out=W.v(W.t[:, kc, :]), in_=s[:])
        nf = len(fchunks)
        ktot = sum(m for (_, m, _) in tgroups)
        xt = [k.sbuf(es, f"ip_x{i}", [128, 8, N], F32) for i in range(2)]
        h = k.sbuf(es, "ip_h", [128, 8, N], BF16)
        outF = k.sbuf(es, "ip_oF", [128, nf, N], F32)
        outK = k.sbuf(es, "ip_oK", [128, 4, max(ktot, 1)], F32)
        tmp = [k.sbuf(es, f"ip_t{i}", [128, N], F32) for i in range(2)]
        nb = {"sq": [k.sbuf(es, f"ip_sq{i}", [128, N], BF16) for i in range(2)],
              "ps": k.psum(es, "ip_pss", [128, N], F32),
              "rs": k.sbuf(es, "ip_rs", [128, N], F32),
              "rstd": k.sbuf(es, "ip_rstd", [128, N], F32)}
        pp = [k.psum(es, f"ip_pp{i}", [128, N], F32) for i in range(4)]
        XTr = self.XT.t.rearrange("(c p) t -> p c t", p=128)
        PTr = PT.t.rearrange("(c p) t -> p c t", p=128)
        coef = self.coef
        tl = tiles(cfg, N)
        k.dma("sp", xt[0].v(xt[0].t[:, :, 0:tl[0][1]]),
              self.XT.v(("t", tl[0][0]), XTr[:, :, tl[0][0]:tl[0][0] + tl[0][1]]))
        ip = 0
        for ti, (t0, n, isctx) in enumerate(tl):
            x = xt[ti % 2]
            if ti + 1 < len(tl):
                t1, n1, _ = tl[ti + 1]
                xn = xt[(ti + 1) % 2]
                k.dma("sp", xn.v(xn.t[:, :, 0:n1]), self.XT.v(("t", t1), XTr[:, :, t1:t1 + n1]))
            rstd = self.rstd_bc(nb, lambda c: x.v(x.t[:, c, 0:n]), n, 8, D)
            for c in range(8):
                tp = tmp[c % 2]
                k.op("dve", "scalar_tensor_tensor", out=tp.v(tp.t[:, 0:n]), in0=x.v(x.t[:, c, 0:n]),
                     scalar=coef.v(coef.t[:, 0, c, isctx:isctx + 1]), in1=rstd, op0=ALU.mult, op1=ALU.mult)
                k.op("act", "activation", out=h.v(h.t[:, c, 0:n]), in_=tp.v(tp.t[:, 0:n]), func=AF.Identity,
                     bias=coef.v(coef.t[:, 1, c, isctx:isctx + 1]), scale=1.0)
            for fi, (c0, m) in enumerate(fchunks):
                p = pp[ip % 4]
                ip += 1
                for kc in range(8):
                    k.op("pe", "matmul", out=p.v(p.t[0:m, 0:n]), lhsT=W.v(W.t[:, kc, c0:c0 + m]),
                         rhs=h.v(h.t[:, kc, 0:n]), start=(kc == 0), stop=(kc == 7))
                if fi % 2 == 0:
                    k.op("act", "copy", out=outF.v(outF.t[0:m, fi, 0:n]), in_=p.v(p.t[0:m, 0:n]))
                else:
                    k.op("dve", "tensor_copy", out=outF.v(outF.t[0:m, fi, 0:n]), in_=p.v(p.t[0:m, 0:n]))
            k.dma("pool", PT.v(("t", t0), PTr[:, 0:nf, t0:t0 + n]), outF.v(outF.t[:, :, 0:n]))
            if tgroups:
                for s in range(n // 128):
                    for gi, (c0, m, dc) in enumerate(tgroups):
                        p = pp[ip % 4]
                        ip += 1
                        for kc in range(8):
                            k.op("pe", "matmul", out=p.v(p.t[:, 0:m]), lhsT=h.v(h.t[:, kc, s * 128:(s + 1) * 128]),
                                 rhs=W.v(W.t[:, kc, c0:c0 + m]), start=(kc == 0), stop=(kc == 7))
                        if (gi + s) % 2 == 0:
                            k.op("act", "copy", out=outK.v(outK.t[:, s, dc:dc + m]), in_=p.v(p.t[:, 0:m]))
                        else:
                            k.op("dve", "tensor_copy", out=outK.v(outK.t[:, s, dc:dc + m]), in_=p.v(p.t[:, 0:m]))
                k.dma("pool", PK.v(("t", t0), PK.t[t0:t0 + n, 0:ktot].rearrange("(s p) c -> p s c", p=128)),
                      outK.v(outK.t[:, 0:n // 128, :]))


M.in_proj = in_proj


def out_proj(self, li, Wd, wl, YM):
    k, cfg = self.k, self.cfg
    N = 512
    with contextlib.ExitStack() as es:
        W = k.sbuf(es, "opW", [128, 8, D], BF16)
        stg = [k.sbuf(es, f"opstg{i}", [128, D], F32) for i in range(2)]
        cast_engs = [("act", "copy"), ("dve", "tensor_copy"), ("pool", "tensor_copy")]
        for kc in range(8):
            s = stg[kc % 2]
            k.dma("sp", s[:], Wd.v(0, Wd.t[wl, kc * 128:(kc + 1) * 128, :]))
            e, nm = cast_engs[kc % 3]
            k.op(e, nm, out=W.v(W.t[:, kc, :]), in_=s[:])
        xt = [k.sbuf(es, f"op_x{i}", [128, 8, N], F32) for i in range(2)]
        ym = [k.sbuf(es, f"op_ym{i}", [128, 8, N], BF16) for i in range(2)]
        ysb = k.sbuf(es, "op_y", [128, 8, N], F32)
        tmp = [k.sbuf(es, f"op_t{i}", [128, N], F32) for i in range(2)]
        nb = {"sq": [k.sbuf(es, f"op_sq{i}", [128, N], BF16) for i in range(2)],
              "ps": k.psum(es, "op_pss", [128, N], F32),
              "rs": k.sbuf(es, "op_rs", [128, N], F32),
              "rstd": k.sbuf(es, "op_rstd", [128, N], F32)}
        py = [k.psum(es, f"op_py{i}", [128, N], F32) for i in range(2)]
        XTr = self.XT.t.rearrange("(c p) t -> p c t", p=128)
        YMr = YM.t.rearrange("(c p) t -> p c t", p=128)
        coef = self.coef
        tl = tiles(cfg, N)
        for ti, (t0, n, isctx) in enumerate(tl):
            x = xt[ti % 2]
            y_in = ym[ti % 2]
            k.dma("sp", x.v(x.t[:, :, 0:n]), self.XT.v(("t", t0), XTr[:, :, t0:t0 + n]))
            k.dma("sp", y_in.v(y_in.t[:, :, 0:n]), YM.v("all", YMr[:, :, t0:t0 + n]))
            for c in range(8):
                p = py[c % 2]
                for kc in range(8):
                    k.op("pe", "matmul", out=p.v(p.t[:, 0:n]), lhsT=W.v(W.t[:, kc, c * 128:(c + 1) * 128]),
                         rhs=y_in.v(y_in.t[:, kc, 0:n]), start=(kc == 0), stop=(kc == 7))
                k.op("act", "copy", out=ysb.v(ysb.t[:, c, 0:n]), in_=p.v(p.t[:, 0:n]))
            rstd = self.rstd_bc(nb, lambda c: ysb.v(ysb.t[:, c, 0:n]), n, 8, D)
            for c in range(8):
                tp = tmp[c % 2]
                k.op("dve", "scalar_tensor_tensor", out=tp.v(tp.t[:, 0:n]), in0=ysb.v(ysb.t[:, c, 0:n]),
                     scalar=coef.v(coef.t[:, 2, c, isctx:isctx + 1]), in1=rstd, op0=ALU.mult, op1=ALU.mult)
                k.op("pool", "tensor_tensor", out=x.v(x.t[:, c, 0:n]), in0=x.v(x.t[:, c, 0:n]),
                     in1=tp.v(tp.t[:, 0:n]), op=ALU.add)
            k.dma("pool", self.XT.v(("t", t0), XTr[:, :, t0:t0 + n]), x.v(x.t[:, :, 0:n]))


M.out_proj = out_proj


def rope_tables(L):
    GRID_W = 64
    t = np.arange(L)
    r = (t // GRID_W).astype(np.float32)
    col = (t % GRID_W).astype(np.float32)
    inv = (10000.0 ** (-np.arange(16, dtype=np.float32) / 16)).astype(np.float32)
    ang = np.concatenate([r[:, None] * inv, col[:, None] * inv], axis=-1).astype(np.float32)
    cos, sin = np.cos(ang).astype(np.float32), np.sin(ang).astype(np.float32)
    cosT = np.repeat(cos, 2, axis=1).T
    sinT = np.repeat(sin, 2, axis=1).T
    out = np.stack([np.tile(cosT, (2, 1)), np.tile(sinT, (2, 1))]).astype(np.float32)
    return np.ascontiguousarray(out)


def rot_matrix():
    R = np.zeros((128, 128), np.float32)
    for m in range(128):
        if m % 2 == 0:
            R[m + 1, m] = -1.0
        else:
            R[m - 1, m] = 1.0
    return R


def block_ones(bs):
    o = np.zeros((128, 128), np.float32)
    for b in range(128 // bs):
        o[b * bs:(b + 1) * bs, b * bs:(b + 1) * bs] = 1.0
    return o


def norm_rope(self, nb, src, n, out, g_ap, t_main0, do_norm, do_rope, blk, hd):
    k = self.k
    cur = src
    if do_norm:
        sq = nb["sq"][0]
        k.op("act", "activation", out=sq.v(sq.t[:, 0:n]), in_=src, func=AF.Square)
        ps = nb["ps"]
        k.op("pe", "matmul", out=ps.v(ps.t[:, 0:n]), lhsT=blk[:], rhs=sq.v(sq.t[:, 0:n]), start=True, stop=True)
        rs, rstd = nb["rs"], nb["rstd"]
        k.op("act", "activation", out=rs.v(rs.t[:, 0:n]), in_=ps.v(ps.t[:, 0:n]), func=AF.Sqrt,
             scale=1.0 / hd, bias=self.epsb[:, 0:1])
        k.op("dve", "reciprocal", out=rstd.v(rstd.t[:, 0:n]), in_=rs.v(rs.t[:, 0:n]))
        kn = nb["kn"]
        k.op("dve", "scalar_tensor_tensor", out=kn.v(kn.t[:, 0:n]), in0=src, scalar=g_ap,
             in1=rstd.v(rstd.t[:, 0:n]), op0=ALU.mult, op1=ALU.mult)
        cur = kn.v(kn.t[:, 0:n])
    if not do_rope:
        k.op("act", "copy", out=out, in_=cur)
        return
    cs = nb["cs"]
    k.dma("sp", cs.v(cs.t[:, :, 0:n]), self.c_rope.v(0, self.c_rope.t[:, :, t_main0:t_main0 + n].rearrange("a p t -> p a t")))
    pr = nb["pr"]
    k.op("pe", "matmul", out=pr.v(pr.t[:, 0:n]), lhsT=self.rotm[:], rhs=cur, start=True, stop=True)
    t1, t2 = nb["t1"], nb["t2"]
    k.op("pool", "tensor_tensor", out=t1.v(t1.t[:, 0:n]), in0=cur, in1=cs.v(cs.t[:, 0, 0:n]), op=ALU.mult)
    k.op("dve", "tensor_tensor", out=t2.v(t2.t[:, 0:n]), in0=pr.v(pr.t[:, 0:n]), in1=cs.v(cs.t[:, 1, 0:n]), op=ALU.mult)
    k.op("pool", "tensor_tensor", out=out, in0=t1.v(t1.t[:, 0:n]), in1=t2.v(t2.t[:, 0:n]), op=ALU.add)


M.norm_rope = norm_rope


def nr_bufs(self, es, N, pfx):
    k = self.k
    return {"sq": [k.sbuf(es, pfx + "sq", [128, N], BF16)],
            "ps": k.psum(es, pfx + "ps", [128, N], F32),
            "pr": k.psum(es, pfx + "pr", [128, N], F32),
            "rs": k.sbuf(es, pfx + "rs", [128, N], F32),
            "rstd": k.sbuf(es, pfx + "rstd", [128, N], F32),
            "kn": k.sbuf(es, pfx + "kn", [128, N], F32),
            "cs": k.sbuf(es, pfx + "cs", [128, 2, N], F32),
            "t1": k.sbuf(es, pfx + "t1", [128, N], F32),
            "t2": k.sbuf(es, pfx + "t2", [128, N], F32)}


M.nr_bufs = nr_bufs


def gqa(self, j, PT, PK, YM, qc0, kc, vcol, ym_row0, ctx_out):
    k, cfg = self.k, self.cfg
    T, L = cfg.T, cfg.L
    NT = T // 128
    N = 512
    with contextlib.ExitStack() as es:
        nb = self.nr_bufs(es, N, "gq_")
        blk = k.sbuf(es, "gq_blk", [128, 128], BF16)
        k.op("dve", "tensor_copy", out=blk[:], in_=self.blk64[:])
        gsb = k.sbuf(es, "gq_g", [128, 2], F32)
        k.dma("sp", gsb[:], self.ev_qk_g.v(0, self.ev_qk_g.t[j, :, :]))
        PTr = PT.t.rearrange("(c p) t -> p c t", p=128)
        KT = [k.sbuf(es, f"gq_KT{kv}", [128, T], BF16) for kv in range(2)]
        kraw = k.sbuf(es, "gq_kraw", [128, N], F32)
        for kv in range(2):
            for (t0, n, isctx) in tiles(cfg, N):
                for hf in range(2):
                    k.dma("sp", kraw.v(kraw.t[hf * 64:(hf + 1) * 64, 0:n]),
                          PT.v(("t", t0), PT.t[kc * 128 + kv * 64:kc * 128 + (kv + 1) * 64, t0:t0 + n]))
                self.norm_rope(nb, kraw.v(kraw.t[:, 0:n]), n, KT[kv].v(KT[kv].t[:, t0:t0 + n]),
                               gsb.v(gsb.t[:, 1:2]), t0 - C, True, not isctx, blk, 64)
        Va = k.sbuf(es, "gq_Va", [128, NT, 2, 65], BF16)
        k.op("pool", "memset", ap=Va[:], constant=1.0)
        vst = k.sbuf(es, "gq_vst", [128, 8, 128], F32)
        for b0 in range(0, NT, 8):
            nbk = min(8, NT - b0)
            k.dma("sp", vst.v(vst.t[:, 0:nbk, :]),
                  PK.v("all", PK.t[b0 * 128:(b0 + nbk) * 128, vcol:vcol + 128].rearrange("(s p) c -> p s c", p=128)))
            k.op("dve", "tensor_copy", out=Va.v(Va.t[:, b0:b0 + nbk, :, 0:64]),
                 in_=vst.v(vst.t[:, 0:nbk, :].rearrange("p s (kv d) -> p s kv d", kv=2)))
        qraw = [k.sbuf(es, f"gq_qraw{i}", [128, 4, N], F32) for i in range(2)]
        QT = [k.sbuf(es, f"gq_QT{i}", [128, 4, N], BF16) for i in range(2)]
        pS = [k.psum(es, f"gq_pS{i}", [128, N], F32) for i in range(3)]
        pO = [k.psum(es, f"gq_pO{i}", [128, N], F32) for i in range(2)]
        pB = k.psum(es, "gq_pB", [128, N], F32)
        pb = [k.sbuf(es, f"gq_pb{i}", [128, N], BF16) for i in range(3)]
        osb = [k.sbuf(es, f"gq_osb{i}", [128, N], F32) for i in range(2)]
        rsum = [k.sbuf(es, f"gq_rsum{i}", [128, N], F32) for i in range(2)]
        yst = [k.sbuf(es, f"gq_yst{i}", [64, N], BF16) for i in range(2)]
        iS = 0
        iH = 0
        for ti, (t0, n, isctx) in enumerate(tiles(cfg, N)):
            if isctx and not ctx_out:
                continue
            qr, qt = qraw[ti % 2], QT[ti % 2]
            k.dma("sp", qr.v(qr.t[:, :, 0:n]), PT.v(("t", t0), PTr[:, qc0:qc0 + 4, t0:t0 + n]))
            for c in range(4):
                self.norm_rope(nb, qr.v(qr.t[:, c, 0:n]), n, qt.v(qt.t[:, c, 0:n]),
                               gsb.v(gsb.t[:, 0:1]), t0 - C, True, not isctx, blk, 64)
            kts = list(range(C // 128)) if isctx else list(range(NT))
            for hq in range(8):
                c, r, kv = hq // 2, (hq % 2) * 64, hq // 4
                po = pO[iH % 2]
                ob, rsm, ys = osb[iH % 2], rsum[iH % 2], yst[iH % 2]
                iH += 1

                def qk(kt):
                    ps = pS[(iS + kt) % 3]
                    k.op("pe", "matmul", out=ps.v(ps.t[:, 0:n]),
                         lhsT=KT[kv].v(KT[kv].t[r:r + 64, kt * 128:(kt + 1) * 128]),
                         rhs=qt.v(qt.t[r:r + 64, c, 0:n]), start=True, stop=True)

                qk(kts[0])
                for ii, kt in enumerate(kts):
                    if ii + 1 < len(kts):
                        qk(kts[ii + 1])
                    ps = pS[(iS + kt) % 3]
                    pbuf = pb[(iS + kt) % 3]
                    k.op("act", "activation", out=pbuf.v(pbuf.t[:, 0:n]), in_=ps.v(ps.t[:, 0:n]),
                         func=AF.Exp, scale=0.125)
                    k.op("pe", "matmul", out=po.v(po.t[0:65, 0:n]), lhsT=Va.v(Va.t[:, kt, kv, :]),
                         rhs=pbuf.v(pbuf.t[:, 0:n]), start=(ii == 0), stop=(ii == len(kts) - 1))
                iS += len(kts)
                k.op("act", "copy", out=ob.v(ob.t[0:64, 0:n]), in_=po.v(po.t[0:64, 0:n]))
                k.op("dve", "reciprocal", out=rsm.v(rsm.t[64:65, 0:n]), in_=po.v(po.t[64:65, 0:n]))
                k.op("pe", "matmul", out=pB.v(pB.t[0:64, 0:n]), lhsT=self.ones32.v(self.ones32.t[64:65, 0:64]),
                     rhs=rsm.v(rsm.t[64:65, 0:n]), start=True, stop=True)
                k.op("dve", "tensor_tensor", out=ys.v(ys.t[:, 0:n]), in0=ob.v(ob.t[0:64, 0:n]),
                     in1=pB.v(pB.t[0:64, 0:n]), op=ALU.mult)
                row = ym_row0 + hq * 64
                k.dma("pool", YM.v("all", YM.t[row:row + 64, t0:t0 + n]), ys.v(ys.t[:, 0:n]))


M.gqa = gqa


EV_F = [(0, 128), (128, 128), (256, 128), (384, 128), (1024, 128), (1152, 128), (1280, 128), (1408, 128),
        (1536, 32), (1568, 128), (1696, 128), (1824, 128), (1952, 128), (2080, 128)]
EV_T = [(256, 512, 0), (768, 256, 512), (2208, 128, 768)]


def declare_mix(self):
    cfg = self.cfg
    dp, L, T = cfg.depth, cfg.L, cfg.T
    ne, no = (dp + 1) // 2, dp // 2
    self.ev_w_in = self.inp("ev_w_in", [ne, D, 2336])
    self.ev_w_out = self.inp("ev_w_out", [ne, D, D])
    self.ev_qk_g = self.inp("ev_qk_gT", [ne, 128, 2])
    self.c_rope = self.inp("rope", [2, 128, L])
    self.c_rotm = self.inp("rotm", [128, 128])
    self.c_blk64 = self.inp("blk64", [128, 128])
    self.PT = self.scratch("PT", [24 * 128, T], dbg=True)
    self.PK = self.scratch("PK", [T, 1024], dbg=True)
    self.YM = self.scratch("YM", [D, T], BF16, dbg=True)
    k, es = self.k, self.es
    self.rotm = k.sbuf(es, "rotm", [128, 128], F32)
    k.dma("sp", self.rotm[:], self.c_rotm.v(0, self.c_rotm.t[:, :]))
    self.blk64 = k.sbuf(es, "blk64", [128, 128], F32)
    k.dma("sp", self.blk64[:], self.c_blk64.v(0, self.c_blk64.t[:, :]))


M.declare_mix = declare_mix


def zero_ym(self, r0, r1):
    k, cfg = self.k, self.cfg
    with contextlib.ExitStack() as es:
        z = k.sbuf(es, "zz", [128, 2048], BF16)
        k.op("pool", "memset", ap=z[:], constant=0.0)
        for rr in range(r0, r1, 128):
            for t0 in range(0, cfg.T, 2048):
                n = min(2048, cfg.T - t0)
                k.dma("sp", self.YM.v("all", self.YM.t[rr:rr + 128, t0:t0 + n]), z.v(z.t[:, 0:n]))


M.zero_ym = zero_ym


def even_layer(self, li, last):
    k = self.k
    j = li // 2
    self.in_proj(li, self.ev_w_in, j, 2336, EV_F, EV_T, self.PT, self.PK)
    k.barrier()
    if self.cfg.phases is None or "gla" in self.cfg.phases:
        self.gla(j, self.PT, self.PK, self.YM, not last)
    else:
        self.zero_ym(0, 512)
    k.barrier()
    self.gqa(j, self.PT, self.PK, self.YM, 9, 13, 768, 512, not last)
    k.barrier()
    self.out_proj(li, self.ev_w_out, j, self.YM)
    k.barrier()


M.even_layer = even_layer


def build2(self):
    cfg = self.cfg
    k = self.k
    self.declare()
    self.declare_mix()
    self.load_consts()
    self.declare_gla()
    self.declare_odd()
    self.declare_hy()
    self.epsb = k.sbuf(self.es, "epsb", [128, 1], F32)
    k.op("dve", "memset", ap=self.epsb[:], constant=EPS)
    self.oneb = k.sbuf(self.es, "oneb", [128, 1], F32)
    k.op("dve", "memset", ap=self.oneb[:], constant=1.0)
    self.transpose_in()
    k.barrier()
    for li in range(cfg.depth):
        last = li == cfg.depth - 1
        self.mod_vectors(li)
        k.barrier()
        if li % 2 == 0:
            self.even_layer(li, last)
        else:
            self.odd_layer(li, last)
        self.mlp_layer(li)
        k.barrier()
    self.transpose_out()
    k.finish()
    self.es.close()
    return self.nc


M.build2 = build2


def gla_consts():
    s = np.arange(128)[:, None]
    t = np.arange(128)[None, :]
    sc = -1.0 / 16.0
    ucat = np.zeros((2, 128, 129), np.float32)
    ucat[0, :, :128] = (s <= t) * sc
    ucat[1, :, :128] = (s >= t) * sc
    ucat[:, :, 128] = sc
    ustr = np.zeros((2, 128, 128), np.float32)
    ustr[0] = (s > t) * sc
    ustr[1] = (s < t) * sc
    mask = np.zeros((2, 128, 128), np.float32)
    mask[0] = (s <= t)
    mask[1] = (s >= t)
    return {"gla_ucat": ucat, "gla_ustr": ustr, "gla_mask": mask}


def declare_gla(self):
    ne = (self.cfg.depth + 1) // 2
    self.ev_wlr = self.inp("ev_wlr_aug", [ne, 2, 17, 256])
    self.ev_gla_g = self.inp("ev_gla_gT", [ne, 128, 1])
    self.c_ucat = self.inp("gla_ucat", [2, 128, 129])
    self.c_ustr = self.inp("gla_ustr", [2, 128, 128])
    self.c_mask = self.inp("gla_mask", [2, 128, 128])
    self.OF = self.scratch("OF", [512, self.cfg.T], dbg=True)


M.declare_gla = declare_gla


def gla(self, j, PT, PK, YM, ctx_out):
    k, cfg = self.k, self.cfg
    T = cfg.T
    NT = T // 128
    nctx = C // 128
    PTr = PT.t.rearrange("(c p) t -> p c t", p=128)
    OFr = self.OF.t.rearrange("(c p) t -> p c t", p=128)
    YMr = YM.t.rearrange("(c p) t -> p c t", p=128)
    with contextlib.ExitStack() as es:
        sb = lambda nm, sh, dt=F32: k.sbuf(es, "gl_" + nm, sh, dt)
        lra = [sb(f"lra{d}", [32, T]) for d in range(2)]
        wlr = [sb(f"wlr{d}", [17, 256]) for d in range(2)]
        ucat = [sb(f"ucat{d}", [128, 129]) for d in range(2)]
        ustr = [sb(f"ustr{d}", [128, 128]) for d in range(2)]
        mask = [sb(f"mask{d}", [128, 128]) for d in range(2)]
        gain = sb("gain", [128, 1])
        k.dma("sp", gain[:], self.ev_gla_g.v(0, self.ev_gla_g.t[j, :, :]))
        for d in range(2):
            k.op("pool", "memset", ap=lra[d][:], constant=1.0)
            k.dma("sp", lra[d].v(lra[d].t[0:16, :]), PT.v("all", PT.t[8 * 128 + d * 16:8 * 128 + (d + 1) * 16, :]))
            k.dma("sp", wlr[d][:], self.ev_wlr.v(0, self.ev_wlr.t[j, d, :, :]))
            k.dma("sp", ucat[d][:], self.c_ucat.v(0, self.c_ucat.t[d, :, :]))
            k.dma("sp", ustr[d][:], self.c_ustr.v(0, self.c_ustr.t[d, :, :]))
            k.dma("sp", mask[d][:], self.c_mask.v(0, self.c_mask.t[d, :, :]))
        ldf = [sb(f"ldf{i}", [128, 4, 128]) for i in range(2)]
        ldk = [sb(f"ldk{i}", [128, 768]) for i in range(2)]
        e1 = sb("e1", [128, 256])
        lnv = sb("lnv", [128, 256])
        EqT = sb("EqT", [128, 2, 129])
        EkT = sb("EkT", [128, 2, 128])
        Eend = sb("Eend", [128, 256])
        qin = sb("qin", [128, 2, 128], BF16)
        kin = sb("kin", [128, 2, 128], BF16)
        kend = sb("kend", [128, 256], BF16)
        vbf = sb("vbf", [128, 512], BF16)
        ATm = [sb(f"ATm{i}", [128, 128], BF16) for i in range(2)]
        S = sb("S", [128, 2, 128])
        Sbf = sb("Sbf", [128, 2, 128], BF16)
        ofw = [sb(f"ofw{i}", [128, 4, 128]) for i in range(2)]
        ofl = [sb(f"ofl{i}", [128, 4, 128]) for i in range(2)]
        gT = [sb(f"gT{i}", [128, 4, 128]) for i in range(2)]
        otot = [sb(f"otot{i}", [128, 128]) for i in range(2)]
        sq = sb("sq", [128, 128], BF16)
        rs = sb("rs", [128, 128])
        rstd = sb("rstd", [128, 128])
        sg = sb("sg", [128, 128])
        y1 = sb("y1", [128, 128])
        yst = [sb(f"yst{i}", [128, 4, 128], BF16) for i in range(2)]
        p_gk = k.psum(es, "gl_pgk", [128, 512])
        p_bT = k.psum(es, "gl_pbT", [128, 2, 256])
        p_be = k.psum(es, "gl_pbe", [128, 512])
        p_AT = [k.psum(es, f"gl_pAT{i}", [128, 512]) for i in range(2)]
        p_o = [k.psum(es, f"gl_po{i}", [128, 512]) for i in range(2)]
        p_up = k.psum(es, "gl_pup", [128, 2, 128])
        ih = 0
        for d in range(2):
            k.op("dve", "memset", ap=S[:], constant=0.0)
            k.op("pool", "memset", ap=Sbf[:], constant=0.0)
            ctx_tiles = list(range(nctx))
            main_tiles = list(range(nctx, NT))
            order = ctx_tiles + main_tiles if d == 0 else ctx_tiles[::-1] + main_tiles[::-1]
            for it, tix in enumerate(order):
                t0 = tix * 128
                isctx = tix < nctx
                need_out = (not isctx) or ctx_out
                lf, lk = ldf[it % 2], ldk[it % 2]
                k.dma("sp", lf[:], PT.v("all", PTr[:, 0:4, t0:t0 + 128]))
                k.dma("sp", lk[:], PK.v("all", PK.t[t0:t0 + 128, 0:768]))
                if d == 1 and need_out:
                    k.dma("sp", gT[it % 2][:], PT.v("all", PTr[:, 4:8, t0:t0 + 128]))
                    k.dma("sp", ofl[it % 2][:], self.OF.v("all", OFr[:, :, t0:t0 + 128]))
                k.op("pe", "matmul", out=p_gk.v(p_gk.t[:, 0:256]), lhsT=lra[d].v(lra[d].t[0:17, t0:t0 + 128]),
                     rhs=wlr[d][:], start=True, stop=True)
                k.op("act", "activation", out=e1[:], in_=p_gk.v(p_gk.t[:, 0:256]), func=AF.Exp, scale=-1.0)
                k.op("act", "activation", out=lnv[:], in_=e1[:], func=AF.Ln, bias=self.oneb[:, 0:1], scale=1.0)
                for pr in range(2):
                    k.op("pe", "matmul", out=p_bT.v(p_bT.t[:, pr, 0:129]), lhsT=lnv.v(lnv.t[:, pr * 128:(pr + 1) * 128]),
                         rhs=ucat[d][:], start=True, stop=True)
                k.op("pe", "matmul", out=p_be.v(p_be.t[:, 0:256]), lhsT=ustr[d][:], rhs=lnv[:], start=True, stop=True)
                k.op("act", "activation", out=EqT[:], in_=p_bT.v(p_bT.t[:, :, 0:129]), func=AF.Exp)
                k.op("act", "activation", out=EkT[:], in_=p_bT.v(p_bT.t[:, :, 0:128]), func=AF.Exp, scale=-1.0)
                k.op("act", "activation", out=Eend[:], in_=p_be.v(p_be.t[:, 0:256]), func=AF.Exp)
                k.op("dve", "scalar_tensor_tensor", out=qin[:], in0=lf.v(lf.t[:, 0:2, :]), scalar=0.125,
                     in1=EqT.v(EqT.t[:, :, 0:128]), op0=ALU.mult, op1=ALU.mult)
                k.op("pool", "tensor_tensor", out=kin[:], in0=lf.v(lf.t[:, 2:4, :]), in1=EkT[:], op=ALU.mult)
                k.op("dve", "tensor_tensor", out=kend[:], in0=lk.v(lk.t[:, 0:256]), in1=Eend[:], op=ALU.mult)
                k.op("pool", "tensor_copy", out=vbf[:], in_=lk.v(lk.t[:, 256:768]))
                for h in range(4):
                    pr, r = h // 2, (h % 2) * 64
                    if need_out:
                        pa, po, am = p_AT[ih % 2], p_o[ih % 2], ATm[ih % 2]
                        k.op("pe", "matmul", out=pa.v(pa.t[:, 0:128]), lhsT=kin.v(kin.t[r:r + 64, pr, :]),
                             rhs=qin.v(qin.t[r:r + 64, pr, :]), start=True, stop=True)
                        k.op("dve", "tensor_tensor", out=am[:], in0=pa.v(pa.t[:, 0:128]), in1=mask[d][:], op=ALU.mult)
                        k.op("pe", "matmul", out=po.v(po.t[:, 0:128]), lhsT=vbf.v(vbf.t[:, h * 128:(h + 1) * 128]),
                             rhs=am[:], start=True, stop=False)
                        k.op("pe", "matmul", out=po.v(po.t[:, 0:128]), lhsT=Sbf.v(Sbf.t[r:r + 64, pr, :]),
                             rhs=qin.v(qin.t[r:r + 64, pr, :]), start=False, stop=True)
                        if d == 0:
                            k.op("act", "copy", out=ofw[it % 2].v(ofw[it % 2].t[:, h, :]), in_=po.v(po.t[:, 0:128]))
                        else:
                            ot = otot[ih % 2]
                            k.op("dve", "tensor_tensor", out=ot[:], in0=po.v(po.t[:, 0:128]),
                                 in1=ofl[it % 2].v(ofl[it % 2].t[:, h, :]), op=ALU.add)
                            k.op("act", "activation", out=sq[:], in_=ot[:], func=AF.Square)
                            k.op("pe", "matmul", out=p_gk.v(p_gk.t[:, 256:384]), lhsT=self.ones_bf[:], rhs=sq[:],
                                 start=True, stop=True)
                            k.op("act", "activation", out=rs[:], in_=p_gk.v(p_gk.t[:, 256:384]), func=AF.Sqrt,
                                 scale=1.0 / 128, bias=self.epsb[:, 0:1])
                            k.op("dve", "reciprocal", out=rstd[:], in_=rs[:])
                            k.op("act", "activation", out=sg[:], in_=gT[it % 2].v(gT[it % 2].t[:, h, :]), func=AF.Silu)
                            k.op("dve", "scalar_tensor_tensor", out=y1[:], in0=ot[:], scalar=gain[:, 0:1],
                                 in1=rstd[:], op0=ALU.mult, op1=ALU.mult)
                            k.op("pool", "tensor_tensor", out=yst[it % 2].v(yst[it % 2].t[:, h, :]), in0=y1[:],
                                 in1=sg[:], op=ALU.mult)
                        ih += 1
                    k.op("pe", "matmul", out=p_up.v(p_up.t[r:r + 64, pr, :]), lhsT=kend.v(kend.t[:, h * 64:(h + 1) * 64]),
                         rhs=vbf.v(vbf.t[:, h * 128:(h + 1) * 128]), start=True, stop=True)
                for pr in range(2):
                    k.op("dve", "scalar_tensor_tensor", out=S.v(S.t[:, pr, :]), in0=S.v(S.t[:, pr, :]),
                         scalar=EqT.v(EqT.t[:, pr, 128:129]), in1=p_up.v(p_up.t[:, pr, :]), op0=ALU.mult, op1=ALU.add)
                k.op("act", "copy", out=Sbf[:], in_=S[:])
                if need_out:
                    if d == 0:
                        k.dma("pool", self.OF.v("all", OFr[:, :, t0:t0 + 128]), ofw[it % 2][:])
                    else:
                        k.dma("pool", YM.v("all", YMr[:, 0:4, t0:t0 + 128]), yst[it % 2][:])
            k.barrier()


M.gla = gla


OD_F = [(i * 128, 128) for i in range(20)]
OD_T = [(2560, 512, 0)]


def declare_odd(self):
    cfg = self.cfg
    no = max(cfg.depth // 2, 1)
    self.od_w_in = self.inp("od_w_in", [no, D, 3072])
    self.od_w_out = self.inp("od_w_out", [no, D, D])
    self.od_lam = self.inp("od_lamB", [no, 128, 256])
    self.od_diff_g = self.inp("od_diff_gT", [no, 128, 1])


M.declare_odd = declare_odd


def diffattn(self, li, j, PT, PK, YM, qc0, kc0, ym_row0, ctx_out):
    k, cfg = self.k, self.cfg
    T, L = cfg.T, cfg.L
    NT = T // 128
    N = 512
    lam_init = 0.8 - 0.6 * math.exp(-0.3 * li)
    with contextlib.ExitStack() as es:
        nb = self.nr_bufs(es, N, "da_")
        PTr = PT.t.rearrange("(c p) t -> p c t", p=128)
        lp = k.sbuf(es, "da_lp", [128, 256], F32)
        k.dma("sp", lp[:], self.od_lam.v(0, self.od_lam.t[j, :, :]))
        pr2 = k.sbuf(es, "da_pr2", [128, 2, 64], F32)
        sm = k.sbuf(es, "da_sm", [128, 2], F32)
        ex = k.sbuf(es, "da_ex", [128, 2], F32)
        neglam = k.sbuf(es, "da_nl", [128, 1], F32)
        for q in range(2):
            k.op("dve", "tensor_tensor", out=pr2.v(pr2.t[:, q, :]), in0=lp.v(lp.t[:, q * 128:q * 128 + 64]),
                 in1=lp.v(lp.t[:, q * 128 + 64:q * 128 + 128]), op=ALU.mult)
        k.op("dve", "reduce_sum", out=sm[:], in_=pr2[:], axis=mybir.AxisListType.X)
        k.op("act", "activation", out=ex[:], in_=sm[:], func=AF.Exp)
        k.op("dve", "tensor_tensor", out=neglam[:], in0=ex.v(ex.t[:, 1:2]), in1=ex.v(ex.t[:, 0:1]), op=ALU.subtract)
        k.op("dve", "tensor_scalar", out=neglam[:], in0=neglam[:], scalar1=-lam_init, scalar2=None, op0=ALU.add)
        g2 = k.sbuf(es, "da_g2", [128, 1], F32)
        k.dma("sp", g2[:], self.od_diff_g.v(0, self.od_diff_g.t[j, :, :]))
        k.op("dve", "tensor_scalar", out=g2[:], in0=g2[:], scalar1=1.0 - lam_init, scalar2=None, op0=ALU.mult)
        KT = k.sbuf(es, "da_KT", [128, 4, T], BF16)
        kraw = k.sbuf(es, "da_kraw", [128, N], F32)
        for c in range(4):
            for (t0, n, isctx) in tiles(cfg, N):
                k.dma("sp", kraw.v(kraw.t[:, 0:n]), PT.v("all", PT.t[(kc0 + c) * 128:(kc0 + c + 1) * 128, t0:t0 + n]))
                self.norm_rope(nb, kraw.v(kraw.t[:, 0:n]), n, KT.v(KT.t[:, c, t0:t0 + n]),
                               None, t0 - C, False, not isctx, None, 64)
        V = k.sbuf(es, "da_V", [128, NT, 512], BF16)
        vst = k.sbuf(es, "da_vst", [128, 4, 512], F32)
        for b0 in range(0, NT, 4):
            nbk = min(4, NT - b0)
            k.dma("sp", vst.v(vst.t[:, 0:nbk, :]),
                  PK.v("all", PK.t[b0 * 128:(b0 + nbk) * 128, 0:512].rearrange("(s p) c -> p s c", p=128)))
            k.op("dve" if (b0 // 4) % 2 == 0 else "pool", "tensor_copy", out=V.v(V.t[:, b0:b0 + nbk, :]),
                 in_=vst.v(vst.t[:, 0:nbk, :]))
        qraw = [k.sbuf(es, f"da_qraw{i}", [128, 4, N], F32) for i in range(2)]
        QT = [k.sbuf(es, f"da_QT{i}", [128, 4, N], BF16) for i in range(2)]
        pS = [k.psum(es, f"da_pS{i}", [128, N], F32) for i in range(2)]
        pO = [k.psum(es, f"da_pO{i}", [128, N], F32) for i in range(2)]
        pZ = [k.psum(es, f"da_pZ{i}", [128, N], F32) for i in range(2)]
        pb = [k.sbuf(es, f"da_pb{i}", [128, N], BF16) for i in range(4)]
        rz = [k.sbuf(es, f"da_rz{i}", [128, N], F32) for i in range(2)]
        tt = [k.sbuf(es, f"da_tt{i}", [128, N], F32) for i in range(2)]
        osb = k.sbuf(es, "da_o", [128, N], F32)
        sq = k.sbuf(es, "da_sq", [128, N], BF16)
        y1 = k.sbuf(es, "da_y1", [128, N], BF16)
        ipb = 0
        for ti, (t0, n, isctx) in enumerate(tiles(cfg, N)):
            if isctx and not ctx_out:
                continue
            qr, qt = qraw[ti % 2], QT[ti % 2]
            k.dma("sp", qr.v(qr.t[:, :, 0:n]), PT.v("all", PTr[:, qc0:qc0 + 4, t0:t0 + n]))
            for c in range(4):
                self.norm_rope(nb, qr.v(qr.t[:, c, 0:n]), n, qt.v(qt.t[:, c, 0:n]),
                               None, t0 - C, False, not isctx, None, 64)
            kts = list(range(C // 128)) if isctx else list(range(NT))
            for h in range(4):
                for ii, kt in enumerate(kts):
                    for c in range(2):
                        r = c * 64
                        ps = pS[c]
                        k.op("pe", "matmul", out=ps.v(ps.t[:, 0:n]), lhsT=KT.v(KT.t[r:r + 64, h, kt * 128:(kt + 1) * 128]),
                             rhs=qt.v(qt.t[r:r + 64, h, 0:n]), start=True, stop=True)
                    pbs = []
                    for c in range(2):
                        pbuf = pb[ipb % 4]
                        ipb += 1
                        pbs.append(pbuf)
                        k.op("act", "activation", out=pbuf.v(pbuf.t[:, 0:n]), in_=pS[c].v(pS[c].t[:, 0:n]),
                             func=AF.Exp, scale=0.125)
                    for c in range(2):
                        k.op("pe", "matmul", out=pO[c].v(pO[c].t[:, 0:n]), lhsT=V.v(V.t[:, kt, h * 128:(h + 1) * 128]),
                             rhs=pbs[c].v(pbs[c].t[:, 0:n]), start=(ii == 0), stop=(ii == len(kts) - 1))
                        k.op("pe", "matmul", out=pZ[c].v(pZ[c].t[:, 0:n]), lhsT=self.ones_bf[:],
                             rhs=pbs[c].v(pbs[c].t[:, 0:n]), start=(ii == 0), stop=(ii == len(kts) - 1))
                for c in range(2):
                    k.op("dve", "reciprocal", out=rz[c].v(rz[c].t[:, 0:n]), in_=pZ[c].v(pZ[c].t[:, 0:n]))
                    k.op("dve", "tensor_tensor", out=tt[c].v(tt[c].t[:, 0:n]), in0=pO[c].v(pO[c].t[:, 0:n]),
                         in1=rz[c].v(rz[c].t[:, 0:n]), op=ALU.mult)
                k.op("dve", "scalar_tensor_tensor", out=osb.v(osb.t[:, 0:n]), in0=tt[1].v(tt[1].t[:, 0:n]),
                     scalar=neglam[:, 0:1], in1=tt[0].v(tt[0].t[:, 0:n]), op0=ALU.mult, op1=ALU.add)
                k.op("act", "activation", out=sq.v(sq.t[:, 0:n]), in_=osb.v(osb.t[:, 0:n]), func=AF.Square)
                ps = nb["ps"]
                k.op("pe", "matmul", out=ps.v(ps.t[:, 0:n]), lhsT=self.ones_bf[:], rhs=sq.v(sq.t[:, 0:n]),
                     start=True, stop=True)
                rs, rstd = nb["rs"], nb["rstd"]
                k.op("act", "activation", out=rs.v(rs.t[:, 0:n]), in_=ps.v(ps.t[:, 0:n]), func=AF.Sqrt,
                     scale=1.0 / 128, bias=self.epsb[:, 0:1])
                k.op("dve", "reciprocal", out=rstd.v(rstd.t[:, 0:n]), in_=rs.v(rs.t[:, 0:n]))
                k.op("dve", "scalar_tensor_tensor", out=y1.v(y1.t[:, 0:n]), in0=osb.v(osb.t[:, 0:n]),
                     scalar=g2[:, 0:1], in1=rstd.v(rstd.t[:, 0:n]), op0=ALU.mult, op1=ALU.mult)
                row = ym_row0 + h * 128
                k.dma("pool", YM.v("all", YM.t[row:row + 128, t0:t0 + n]), y1.v(y1.t[:, 0:n]))


M.diffattn = diffattn


def odd_layer(self, li, last):
    k = self.k
    j = li // 2
    self.in_proj(li, self.od_w_in, j, 3072, OD_F, OD_T, self.PT, self.PK)
    k.barrier()
    if self.cfg.phases is None or "hyena" in self.cfg.phases:
        self.hyena(j, self.PT, self.YM, not last)
    else:
        self.zero_ym(0, 512)
    k.barrier()
    self.diffattn(li, j, self.PT, self.PK, self.YM, 12, 16, 512, not last)
    k.barrier()
    self.out_proj(li, self.od_w_out, j, self.YM)
    k.barrier()


M.odd_layer = odd_layer


def hy_dims(Lseg):
    N = 2 * Lseg
    lg = int(round(math.log2(N)))
    N2 = 1 << ((lg + 1) // 2)
    N1 = N // N2
    H1 = N1 // 2
    NSQ = 256 // max(N1, N2)
    return N, N1, N2, H1, NSQ


def hy_consts(Lseg, pfx):
    N, N1, N2, H1, NSQ = hy_dims(Lseg)
    f64 = np.float64
    c = {}
    n1 = np.arange(H1, dtype=f64)[:, None]
    k1 = np.arange(N1, dtype=f64)[None, :]
    a = 2 * np.pi * n1 * k1 / N1
    c["F1cat"] = np.concatenate([np.cos(a), -np.sin(a)], 1)
    n2 = np.arange(N2, dtype=f64)[:, None]
    a = 2 * np.pi * n2 * k1 / N
    twc, tws = np.cos(a), -np.sin(a)
    c["TwA"] = np.tile(np.concatenate([twc, twc], 1)[:, None, :], (1, NSQ, 1))
    c["TwB"] = np.tile(np.concatenate([-tws, tws], 1)[:, None, :], (1, NSQ, 1))
    k2 = np.arange(N2, dtype=f64)[None, :]
    a = 2 * np.pi * n2 * k2 / N2
    c["F2c"], c["F2s"], c["F2sn"] = np.cos(a), -np.sin(a), np.sin(a)
    c["G2cat"] = np.concatenate([np.cos(a), np.sin(a)], 1)
    c["G2cat2"] = np.concatenate([-np.sin(a), np.cos(a)], 1)
    kk1 = np.arange(N1, dtype=f64)[:, None]
    nn2 = np.arange(N2, dtype=f64)[None, :]
    a = 2 * np.pi * kk1 * nn2 / N
    tc, ts = np.cos(a), np.sin(a)
    c["TwAi"] = np.tile(np.concatenate([tc, tc], 1)[:, None, :], (1, NSQ, 1))
    c["TwBi"] = np.tile(np.concatenate([-ts, ts], 1)[:, None, :], (1, NSQ, 1))
    nn1 = np.arange(H1, dtype=f64)[None, :]
    a = 2 * np.pi * kk1 * nn1 / N1
    c["G1c"], c["G1sn"] = np.cos(a) / N, -np.sin(a) / N
    pos = np.arange(Lseg, dtype=np.float32)
    t = pos / np.float32(max(Lseg - 1, 1))
    w = np.float32(2 * math.pi) * pos / np.float32(Lseg)
    bands = np.linspace(1e-4, 15, 16, dtype=np.float32)
    z = np.concatenate([t[:, None], np.cos(w[:, None] * bands), -np.sin(w[:, None] * bands)], -1).astype(np.float32)
    c["zT"] = z.T
    deltas = np.abs(np.linspace(math.log(1e-2) / 0.3, math.log(1e-2) / 1.5, 512, dtype=np.float32))
    c["decT"] = np.exp(-t[None, :] * deltas[:, None])
    return {pfx + k_: np.ascontiguousarray(v.astype(np.float32)) for k_, v in c.items()}


HY_SHAPES = lambda N, N1, N2, H1, NSQ, Lseg: {
    "F1cat": [H1, 2 * N1], "TwA": [N2, NSQ, 2 * N1], "TwB": [N2, NSQ, 2 * N1], "F2c": [N2, N2], "F2s": [N2, N2],
    "F2sn": [N2, N2], "G2cat": [N2, 2 * N2], "G2cat2": [N2, 2 * N2], "TwAi": [N1, NSQ, 2 * N2],
    "TwBi": [N1, NSQ, 2 * N2], "G1c": [N1, H1], "G1sn": [N1, H1], "zT": [33, Lseg], "decT": [512, Lseg]}


def declare_hy(self):
    cfg = self.cfg
    no = max(cfg.depth // 2, 1)
    self.hyc = {}
    for pfx, Lseg in (("hm_", cfg.L), ("hc_", C)):
        dims = hy_dims(Lseg)
        for nm, sh in HY_SHAPES(*dims, Lseg).items():
            self.hyc[pfx + nm] = self.inp(pfx + nm, sh)
    self.od_conv_w = self.inp("od_conv_wT", [no, 128, 12, 3])
    self.od_conv_b = self.inp("od_conv_bT", [no, 128, 12])
    self.od_f_w1 = self.inp("od_f_w1", [no, 33, 64])
    self.od_f_b1 = self.inp("od_f_b1T", [no, 64, 1])
    self.od_f_w2 = self.inp("od_f_w2", [no, 64, 64])
    self.od_f_b2 = self.inp("od_f_b2T", [no, 64, 1])
    self.od_f_w3 = self.inp("od_f_w3", [no, 64, 2048])
    self.od_hy_bias = self.inp("od_hy_biasB", [no, 128, 1024])
    self.UC = self.scratch("UC", [1536, cfg.T], dbg=True)
    self.HT = self.scratch("HT", [2048, cfg.L], dbg=True)
    self.HTc = self.scratch("HTc", [2048, C], dbg=True)


M.declare_hy = declare_hy


def hy_shortconv(self, j, PT, ctx_out):
    k, cfg = self.k, self.cfg
    BL = 2048
    with contextlib.ExitStack() as es:
        cw = k.sbuf(es, "sc_w", [128, 12, 3], F32)
        cb = k.sbuf(es, "sc_b", [128, 12], F32)
        k.dma("sp", cw[:], self.od_conv_w.v(0, self.od_conv_w.t[j, :, :, :]))
        k.dma("sp", cb[:], self.od_conv_b.v(0, self.od_conv_b.t[j, :, :]))
        ub = [k.sbuf(es, f"sc_u{i}", [128, BL + 2], F32) for i in range(2)]
        ac = [k.sbuf(es, f"sc_a{i}", [128, BL], F32) for i in range(2)]
        it = 0
        segs = [(C, cfg.L)] + ([(0, C)] if ctx_out else [])
        for (toff, Lseg) in segs:
            for c in range(12):
                for b0 in range(0, Lseg, BL):
                    n = min(BL, Lseg - b0)
                    u, a = ub[it % 2], ac[it % 2]
                    it += 1
                    lo = max(b0 - 1, 0)
                    hi = min(b0 + n + 1, Lseg)
                    if b0 == 0:
                        k.op("pool", "memset", ap=u.v(u.t[:, 0:1]), constant=0.0)
                    if b0 + n == Lseg:
                        k.op("pool", "memset", ap=u.v(u.t[:, n + 1:n + 2]), constant=0.0)
                    k.dma("sp", u.v(u.t[:, lo - b0 + 1:hi - b0 + 1]),
                          PT.v("all", PT.t[c * 128:(c + 1) * 128, toff + lo:toff + hi]))
                    k.op("dve", "tensor_scalar", out=a.v(a.t[:, 0:n]), in0=u.v(u.t[:, 0:n]), scalar1=cw.v(cw.t[:, c, 0:1]),
                         scalar2=cb.v(cb.t[:, c:c + 1]), op0=ALU.mult, op1=ALU.add)
                    k.op("dve", "scalar_tensor_tensor", out=a.v(a.t[:, 0:n]), in0=u.v(u.t[:, 1:n + 1]),
                         scalar=cw.v(cw.t[:, c, 1:2]), in1=a.v(a.t[:, 0:n]), op0=ALU.mult, op1=ALU.add)
                    k.op("dve", "scalar_tensor_tensor", out=a.v(a.t[:, 0:n]), in0=u.v(u.t[:, 2:n + 2]),
                         scalar=cw.v(cw.t[:, c, 2:3]), in1=a.v(a.t[:, 0:n]), op0=ALU.mult, op1=ALU.add)
                    k.dma("pool", self.UC.v("all", self.UC.t[c * 128:(c + 1) * 128, toff + b0:toff + b0 + n]),
                          a.v(a.t[:, 0:n]))


M.hy_shortconv = hy_shortconv


def hy_filters(self, j, pfx, Lseg, HT):
    k = self.k
    N = 512
    with contextlib.ExitStack() as es:
        sb = lambda nm, sh, dt=F32: k.sbuf(es, "hf_" + nm, sh, dt)
        w1, w2, w3 = sb("w1", [33, 64]), sb("w2", [64, 64]), sb("w3", [64, 2048])
        bb = sb("bb", [64, 2])
        bh, bq = sb("bh", [64, 2]), sb("bq", [64, 2])
        k.dma("sp", w1[:], self.od_f_w1.v(0, self.od_f_w1.t[j, :, :]))
        k.dma("sp", w2[:], self.od_f_w2.v(0, self.od_f_w2.t[j, :, :]))
        k.dma("sp", w3[:], self.od_f_w3.v(0, self.od_f_w3.t[j, :, :]))
        k.dma("sp", bb.v(bb.t[:, 0:1]), self.od_f_b1.v(0, self.od_f_b1.t[j, :, :]))
        k.dma("sp", bb.v(bb.t[:, 1:2]), self.od_f_b2.v(0, self.od_f_b2.t[j, :, :]))
        k.op("dve", "tensor_scalar", out=bh[:], in0=bb[:], scalar1=0.5, scalar2=None, op0=ALU.mult)
        k.op("dve", "tensor_scalar", out=bq[:], in0=bb[:], scalar1=0.25, scalar2=None, op0=ALU.mult)
        zT = [sb(f"zT{i}", [33, N]) for i in range(2)]
        dect = [sb(f"dec{i}", [128, 4, N]) for i in range(2)]
        a1, a2, tq = sb("a1", [64, N]), sb("a2", [64, N]), sb("tq", [64, N])
        hid = [sb(f"hid{i}", [64, N]) for i in range(2)]
        hsb = [sb(f"hsb{i}", [128, N]) for i in range(3)]
        p12 = [k.psum(es, f"hf_p{i}", [64, N]) for i in range(2)]
        ph = [k.psum(es, f"hf_ph{i}", [128, N]) for i in range(3)]
        zc, dc = self.hyc[pfx + "zT"], self.hyc[pfx + "decT"]
        dcr = dc.t.rearrange("(c p) t -> p c t", p=128)
        io = 0
        for ti, t0 in enumerate(range(0, Lseg, N)):
            n = min(N, Lseg - t0)
            z, de = zT[ti % 2], dect[ti % 2]
            k.dma("sp", z.v(z.t[:, 0:n]), zc.v(0, zc.t[:, t0:t0 + n]))
            k.dma("sp", de.v(de.t[:, :, 0:n]), dc.v(0, dcr[:, :, t0:t0 + n]))
            cur = z.v(z.t[:, 0:n])
            for layer, (w, kk) in enumerate(((w1, 33), (w2, 64))):
                p = p12[layer]
                k.op("pe", "matmul", out=p.v(p.t[:, 0:n]), lhsT=w.v(w.t[0:kk, :]), rhs=cur, start=True, stop=True)
                k.op("act", "activation", out=a1.v(a1.t[:, 0:n]), in_=p.v(p.t[:, 0:n]), func=AF.Sin,
                     bias=bh.v(bh.t[:, layer:layer + 1]), scale=0.5)
                k.op("act", "activation", out=a2.v(a2.t[:, 0:n]), in_=p.v(p.t[:, 0:n]), func=AF.Sin,
                     bias=bq.v(bq.t[:, layer:layer + 1]), scale=0.25)
                k.op("dve", "tensor_tensor", out=tq.v(tq.t[:, 0:n]), in0=a2.v(a2.t[:, 0:n]), in1=a2.v(a2.t[:, 0:n]),
                     op=ALU.mult)
                k.op("dve", "tensor_scalar", out=tq.v(tq.t[:, 0:n]), in0=tq.v(tq.t[:, 0:n]), scalar1=-2.0, scalar2=1.0,
                     op0=ALU.mult, op1=ALU.add)
                hd = hid[layer]
                k.op("dve", "scalar_tensor_tensor", out=hd.v(hd.t[:, 0:n]), in0=a1.v(a1.t[:, 0:n]), scalar=2.0,
                     in1=tq.v(tq.t[:, 0:n]), op0=ALU.mult, op1=ALU.mult)
                cur = hd.v(hd.t[:, 0:n])
            for cc in range(16):
                p, hs = ph[io % 3], hsb[io % 3]
                io += 1
                k.op("pe", "matmul", out=p.v(p.t[:, 0:n]), lhsT=w3.v(w3.t[:, cc * 128:(cc + 1) * 128]), rhs=cur,
                     start=True, stop=True)
                k.op("dve", "tensor_tensor", out=hs.v(hs.t[:, 0:n]), in0=p.v(p.t[:, 0:n]),
                     in1=de.v(de.t[:, cc % 4, 0:n]), op=ALU.mult)
                k.dma("pool", HT.v("all", HT.t[cc * 128:(cc + 1) * 128, t0:t0 + n]), hs.v(hs.t[:, 0:n]))


M.hy_filters = hy_filters


def hy_conv(self, j, pfx, Lseg, toff, HT, YM):
    k = self.k
    N, N1, N2, H1, NSQ = hy_dims(Lseg)
    with contextlib.ExitStack() as es:
        sb = lambda nm, sh, dt=F32: k.sbuf(es, "hv_" + nm, sh, dt)
        cst = {}
        for nm, sh in HY_SHAPES(N, N1, N2, H1, NSQ, Lseg).items():
            if nm in ("zT", "decT"):
                continue
            cst[nm] = sb(nm, sh)
            d = self.hyc[pfx + nm]
            k.dma("sp", cst[nm][:], d.v(0, d.t))
        hb = sb("hb", [128, 1024])
        k.dma("sp", hb[:], self.od_hy_bias.v(0, self.od_hy_bias.t[j, :, :]))
        hfl = [sb(f"hfl{i}", [H1, NSQ, N2]) for i in range(4)]
        dat = [[sb(f"dat{i}_{q}", [H1, NSQ, N2]) for q in range(3)] for i in range(2)]
        Xs = [sb(f"Xs{i}", [N2, NSQ, 2 * N1]) for i in range(2)]
        KA = [sb(f"KA{i}", [N2, NSQ, 2 * N1]) for i in range(2)]
        KB = [sb(f"KB{i}", [N2, NSQ, 2 * N1]) for i in range(2)]
        Bt = [sb(f"B{i}", [N2, NSQ, 2 * N1]) for i in range(2)]
        tmf = [sb(f"tmf{i}", [N2, NSQ, 2 * N1]) for i in range(2)]
        Yt = sb("Y", [N2, NSQ, 2 * N1])
        Dt = sb("D", [N1, NSQ, 2 * N2])
        tmi = sb("tmi", [N1, NSQ, 2 * N2])
        zmid = sb("zmid", [H1, NSQ, N2])
        zout = [sb(f"zout{i}", [H1, NSQ, N2], BF16) for i in range(2)]
        pA = [k.psum(es, f"hv_pA{i}", [128, 512]) for i in range(2)]
        pX = [k.psum(es, f"hv_pX{i}", [128, 512]) for i in range(2)]
        pC = k.psum(es, "hv_pC", [128, 512])
        pY = k.psum(es, "hv_pY", [128, 512])
        cnt = {"f": 0}

        def v3(tile_, P, W):
            return tile_.t[0:P, 0:NSQ * W].rearrange("p (s w) -> p s w", s=NSQ)

        def fwd_fft(zb):
            i = cnt["f"] % 2
            cnt["f"] += 1
            pa, px, B, tm = pA[i], pX[i], Bt[i], tmf[i]
            A3 = v3(pa, N2, 2 * N1)
            X3 = v3(px, N2, 2 * N1)
            for s in range(NSQ):
                k.op("pe", "matmul", out=pa.v(A3[:, s, :]), lhsT=zb.v(zb.t[:, s, :]), rhs=cst["F1cat"][:],
                     start=True, stop=True)
            k.op("dve", "tensor_tensor", out=B[:], in0=pa.v(A3), in1=cst["TwA"][:], op=ALU.mult)
            k.op("dve", "tensor_tensor", out=tm.v(tm.t[:, :, 0:N1]), in0=pa.v(A3[:, :, N1:2 * N1]),
                 in1=cst["TwB"].v(cst["TwB"].t[:, :, 0:N1]), op=ALU.mult)
            k.op("dve", "tensor_tensor", out=tm.v(tm.t[:, :, N1:2 * N1]), in0=pa.v(A3[:, :, 0:N1]),
                 in1=cst["TwB"].v(cst["TwB"].t[:, :, N1:2 * N1]), op=ALU.mult)
            k.op("pool", "tensor_tensor", out=B[:], in0=B[:], in1=tm[:], op=ALU.add)
            Br, Bi = B.v(B.t[:, :, 0:N1]), B.v(B.t[:, :, N1:2 * N1])
            k.op("pe", "matmul", out=px.v(X3[:, :, 0:N1]), lhsT=cst["F2c"][:], rhs=Br, start=True, stop=False)
            k.op("pe", "matmul", out=px.v(X3[:, :, 0:N1]), lhsT=cst["F2sn"][:], rhs=Bi, start=False, stop=True)
            k.op("pe", "matmul", out=px.v(X3[:, :, N1:2 * N1]), lhsT=cst["F2s"][:], rhs=Br, start=True, stop=False)
            k.op("pe", "matmul", out=px.v(X3[:, :, N1:2 * N1]), lhsT=cst["F2c"][:], rhs=Bi, start=False, stop=True)
            return px, X3

        def inv_fft(Y):
            C3 = v3(pC, N1, 2 * N2)
            for s in range(NSQ):
                k.op("pe", "matmul", out=pC.v(C3[:, s, :]), lhsT=Y.v(Y.t[:, s, 0:N1]), rhs=cst["G2cat"][:],
                     start=True, stop=False)
                k.op("pe", "matmul", out=pC.v(C3[:, s, :]), lhsT=Y.v(Y.t[:, s, N1:2 * N1]), rhs=cst["G2cat2"][:],
                     start=False, stop=True)
            k.op("dve", "tensor_tensor", out=Dt[:], in0=pC.v(C3), in1=cst["TwAi"][:], op=ALU.mult)
            k.op("dve", "tensor_tensor", out=tmi.v(tmi.t[:, :, 0:N2]), in0=pC.v(C3[:, :, N2:2 * N2]),
                 in1=cst["TwBi"].v(cst["TwBi"].t[:, :, 0:N2]), op=ALU.mult)
            k.op("dve", "tensor_tensor", out=tmi.v(tmi.t[:, :, N2:2 * N2]), in0=pC.v(C3[:, :, 0:N2]),
                 in1=cst["TwBi"].v(cst["TwBi"].t[:, :, N2:2 * N2]), op=ALU.mult)
            k.op("pool", "tensor_tensor", out=Dt[:], in0=Dt[:], in1=tmi[:], op=ALU.add)
            Y3 = pY.t[0:H1, 0:NSQ * N2].rearrange("p (s w) -> p s w", s=NSQ)
            k.op("pe", "matmul", out=pY.v(Y3), lhsT=cst["G1c"][:], rhs=Dt.v(Dt.t[:, :, 0:N2]), start=True, stop=False)
            k.op("pe", "matmul", out=pY.v(Y3), lhsT=cst["G1sn"][:], rhs=Dt.v(Dt.t[:, :, N2:2 * N2]), start=False, stop=True)
            return Y3

        def blk(dt_, row0):
            return dt_.t[row0:row0 + NSQ, :].rearrange("s (a b) -> a s b", b=N2)

        for g in range(512 // NSQ):
            ch0 = g * NSQ
            dd = dat[g % 2]
            for q in range(3):
                k.dma("sp", dd[q][:], self.UC.v("all", self.UC.t[q * 512 + ch0:q * 512 + ch0 + NSQ,
                                                                 toff:toff + Lseg].rearrange("s (a b) -> a s b", b=N2)))
            for o in range(2):
                for dr in range(2):
                    hf = hfl[o * 2 + dr]
                    k.dma("sp", hf[:], HT.v("all", blk(HT, o * 1024 + dr * 512 + ch0)))
                    if dr == 1:
                        k.op("pool", "memset", ap=hf.v(hf.t[0:1, :, 0:1]), constant=0.0)
                    px, X3 = fwd_fft(hf)
                    k.op("act", "copy", out=Xs[dr][:], in_=px.v(X3))
                ka, kb = KA[o], KB[o]
                k.op("pool", "tensor_tensor", out=ka.v(ka.t[:, :, 0:N1]), in0=Xs[0].v(Xs[0].t[:, :, 0:N1]),
                     in1=Xs[1].v(Xs[1].t[:, :, 0:N1]), op=ALU.add)
                for s in range(NSQ):
                    ci = o * 512 + ch0 + s
                    k.op("pool", "tensor_scalar", out=ka.v(ka.t[:, s, 0:N1]), in0=ka.v(ka.t[:, s, 0:N1]),
                         scalar1=hb.v(hb.t[0:N2, ci:ci + 1]), scalar2=None, op0=ALU.add)
                k.op("act", "copy", out=ka.v(ka.t[:, :, N1:2 * N1]), in_=ka.v(ka.t[:, :, 0:N1]))
                k.op("pool", "tensor_tensor", out=kb.v(kb.t[:, :, N1:2 * N1]), in0=Xs[0].v(Xs[0].t[:, :, N1:2 * N1]),
                     in1=Xs[1].v(Xs[1].t[:, :, N1:2 * N1]), op=ALU.subtract)
                k.op("pool", "tensor_tensor", out=kb.v(kb.t[:, :, 0:N1]), in0=Xs[1].v(Xs[1].t[:, :, N1:2 * N1]),
                     in1=Xs[0].v(Xs[0].t[:, :, N1:2 * N1]), op=ALU.subtract)
            zcur = dd[0]
            for o in range(2):
                px, X3 = fwd_fft(zcur)
                ka, kb = KA[o], KB[o]
                k.op("dve", "tensor_tensor", out=Yt[:], in0=px.v(X3), in1=ka[:], op=ALU.mult)
                tm = tmf[0]
                k.op("dve", "tensor_tensor", out=tm.v(tm.t[:, :, 0:N1]), in0=px.v(X3[:, :, N1:2 * N1]),
                     in1=kb.v(kb.t[:, :, 0:N1]), op=ALU.mult)
                k.op("dve", "tensor_tensor", out=tm.v(tm.t[:, :, N1:2 * N1]), in0=px.v(X3[:, :, 0:N1]),
                     in1=kb.v(kb.t[:, :, N1:2 * N1]), op=ALU.mult)
                k.op("pool", "tensor_tensor", out=Yt[:], in0=Yt[:], in1=tm[:], op=ALU.add)
                Y3 = inv_fft(Yt)
                if o == 0:
                    k.op("dve", "tensor_tensor", out=zmid[:], in0=pY.v(Y3), in1=dd[1][:], op=ALU.mult)
                    zcur = zmid
                else:
                    zo = zout[g % 2]
                    k.op("dve", "tensor_tensor", out=zo[:], in0=pY.v(Y3), in1=dd[2][:], op=ALU.mult)
                    k.dma("pool", YM.v("all", YM.t[ch0:ch0 + NSQ, toff:toff + Lseg].rearrange("s (a b) -> a s b", b=N2)),
                          zo[:])


M.hy_conv = hy_conv


def hyena(self, j, PT, YM, ctx_out):
    k, cfg = self.k, self.cfg
    self.hy_shortconv(j, PT, ctx_out)
    k.barrier()
    self.hy_filters(j, "hm_", cfg.L, self.HT)
    k.barrier()
    if ctx_out:
        self.hy_filters(j, "hc_", C, self.HTc)
        k.barrier()
    self.hy_conv(j, "hm_", cfg.L, C, self.HT, YM)
    k.barrier()
    if ctx_out:
        self.hy_conv(j, "hc_", C, 0, self.HTc, YM)
        k.barrier()


M.hyena = hyena


SEQ = 8192
DEPTH = 4
BATCH = 4


def host_inputs(inp, b, L, depth):
    d = {}
    d["x"] = np.ascontiguousarray(inp["x"][b])
    d["ctx"] = np.ascontiguousarray(inp["ctx"][b])
    d["cc"] = np.ascontiguousarray(np.stack([inp["c"][b], inp["c_ctx"]], -1).reshape(8, 128, 2).transpose(1, 0, 2))
    d["w_ada"] = inp["w_ada"]
    d["b_adaT"] = np.ascontiguousarray(inp["b_ada"].reshape(depth, 48, 128).transpose(0, 2, 1))
    d["norm_gT"] = np.ascontiguousarray(inp["norm_g"].reshape(depth, 32, 128).transpose(0, 2, 1))
    d["w_mlp_in"] = inp["w_mlp_in"]
    d["w_mlp_out"] = inp["w_mlp_out"]
    d["ev_w_in"] = inp["ev_w_in"]
    d["ev_w_out"] = inp["ev_w_out"]
    g = inp["ev_qk_g"]
    d["ev_qk_gT"] = np.ascontiguousarray(np.tile(g, (1, 1, 2)).transpose(0, 2, 1))
    d["od_w_in"] = inp["od_w_in"]
    d["od_w_out"] = inp["od_w_out"]
    no = inp["od_lam"].shape[0]
    d["od_lamB"] = np.ascontiguousarray(np.broadcast_to(inp["od_lam"].reshape(no, 1, 256), (no, 128, 256)))
    d["od_diff_gT"] = np.ascontiguousarray(inp["od_diff_g"][:, :, None])
    d["od_conv_wT"] = np.ascontiguousarray(
        inp["od_conv_w"].transpose(0, 2, 1).reshape(no, 12, 128, 3).transpose(0, 2, 1, 3))
    d["od_conv_bT"] = np.ascontiguousarray(inp["od_conv_b"].reshape(no, 12, 128).transpose(0, 2, 1))
    d["od_f_w1"] = inp["od_f_w1"]
    d["od_f_w2"] = inp["od_f_w2"]
    d["od_f_w3"] = inp["od_f_w3"]
    d["od_f_b1T"] = np.ascontiguousarray(inp["od_f_b1"][:, :, None])
    d["od_f_b2T"] = np.ascontiguousarray(inp["od_f_b2"][:, :, None])
    d["od_hy_biasB"] = np.ascontiguousarray(np.broadcast_to(inp["od_hy_bias"].reshape(no, 1, 1024), (no, 128, 1024)))
    d["ev_wlr_aug"] = np.ascontiguousarray(np.concatenate([inp["ev_w_lr"], inp["ev_b_lr"][:, :, None, :]], axis=2))
    d["ev_gla_gT"] = np.ascontiguousarray(inp["ev_gla_g"][:, :, None])
    return d


def const_inputs(L):
    d = {}
    d.update(hy_consts(L, "hm_"))
    d.update(hy_consts(C, "hc_"))
    d.update(gla_consts())
    d.update(host_consts())
    d["rope"] = rope_tables(L)
    d["rotm"] = rot_matrix()
    d["blk64"] = block_ones(64)
    return d


def kernel(**inputs):
    inp = {k_: np.asarray(v) for k_, v in inputs.items()}
    B, L, _ = inp["x"].shape
    depth = inp["w_ada"].shape[0]
    cfg = Cfg(L=L, depth=depth, debug=False)
    mm = M(cfg)
    nc = mm.build2()
    consts = const_inputs(L)
    n_cores = 8
    per_b = [dict(host_inputs(inp, b, L, depth), **consts) for b in range(B)]
    in_maps = [per_b[i % B] for i in range(n_cores)]
    res = run_bass_kernel_spmd(nc, in_maps, core_ids=list(range(n_cores)))
    out = np.stack([np.asarray(res.results[b]["out"]) for b in range(B)], axis=0)
    return out.astype(np.float32, copy=False)
```

```python
import math
import contextlib
import numpy as np
import concourse.bass as bass
import concourse.mybir as mybir
from concourse.bass_utils import run_bass_kernel_spmd

F32 = mybir.dt.float32
BF16 = mybir.dt.bfloat16
AF = mybir.ActivationFunctionType
ALU = mybir.AluOpType


class Buf:
    __slots__ = ("w", "r", "name")

    def __init__(self, name=""):
        self.w = None
        self.r = {}
        self.name = name


class View:
    __slots__ = ("ap", "bufs")

    def __init__(self, ap, bufs):
        self.ap = ap
        self.bufs = tuple(bufs)


class Tile:
    def __init__(self, t, name):
        self.t = t
        self.buf = Buf(name)

    def __getitem__(self, idx):
        return View(self.t[idx], (self.buf,))

    def v(self, ap):
        return View(ap, (self.buf,))


class DTile:
    def __init__(self, ap, name):
        self.t = ap
        self.name = name
        self.bufs = {}

    def b(self, key):
        if key not in self.bufs:
            self.bufs[key] = Buf(f"{self.name}:{key}")
        return self.bufs[key]

    def v(self, keys, ap):
        if not isinstance(keys, (list, tuple)):
            keys = [keys]
        return View(ap, [self.b(k) for k in keys])


class Eng:
    def __init__(self, name, h, semidx):
        self.name = name
        self.h = h
        self.semidx = semidx
        self.count = 0
        self.waited = {}


class K:
    WRITE_KEYS = ("out", "accum_out", "ap")

    def __init__(self, nc, es, n_dma=24):
        self.nc = nc
        self.es = es
        self.sems = []
        self.E = {}
        for name, h in [("pe", nc.tensor), ("act", nc.scalar), ("dve", nc.vector),
                        ("pool", nc.gpsimd), ("sp", nc.sync)]:
            self.sems.append(es.enter_context(nc.semaphore("s_" + name)))
            self.E[name] = Eng(name, h, len(self.sems) - 1)
        self.dma_sem = []
        self.dma_val = []
        for i in range(2 * n_dma):
            self.sems.append(es.enter_context(nc.semaphore(f"d{i}")))
            self.dma_sem.append(len(self.sems) - 1)
            self.dma_val.append(0)
        self.n_dma = n_dma
        self.dma_next = {"sp": 0, "pool": 0, "act": 0}
        self.ninst = 0

    def sbuf(self, es, name, shape, dtype):
        self.nalloc = getattr(self, "nalloc", 0) + 1
        name = f"sb{self.nalloc}_{name}"
        return Tile(es.enter_context(self.nc.sbuf_tensor(name, list(shape), dtype)), name)

    def psum(self, es, name, shape, dtype=F32):
        self.nalloc = getattr(self, "nalloc", 0) + 1
        name = f"ps{self.nalloc}_{name}"
        return Tile(es.enter_context(self.nc.psum_tensor(name, list(shape), dtype)), name)

    def _wait(self, E, toks):
        need = {}
        for s, v in toks:
            if v > need.get(s, 0):
                need[s] = v
        for s, v in need.items():
            if E.name == "pe" and s == E.semidx:
                continue
            if E.waited.get(s, 0) < v:
                E.h.wait_ge(self.sems[s], v)
                E.waited[s] = v

    @staticmethod
    def _deps(reads, writes):
        toks = []
        for b in reads:
            if b.w is not None:
                toks.append(b.w)
        for b in writes:
            if b.w is not None:
                toks.append(b.w)
            toks.extend(b.r.items())
        return toks

    @staticmethod
    def _commit(tok, reads, writes):
        s, v = tok
        for b in reads:
            if b.r.get(s, 0) < v:
                b.r[s] = v
        for b in writes:
            b.w = tok
            b.r = {}

    def op(self, eng, name, _r=(), _w=(), **kw):
        E = self.E[eng]
        reads, writes, real = [], [], {}
        for k, v in kw.items():
            if isinstance(v, View):
                (writes if k in self.WRITE_KEYS else reads).extend(v.bufs)
                real[k] = v.ap
            else:
                real[k] = v
        for v in _r:
            reads.extend(v.bufs if isinstance(v, View) else [v])
        for v in _w:
            writes.extend(v.bufs if isinstance(v, View) else [v])
        self._wait(E, self._deps(reads, writes))
        ins = getattr(E.h, name)(**real)
        E.count += 1
        ins.then_inc(self.sems[E.semidx], 1)
        self._commit((E.semidx, E.count), reads, writes)
        self.ninst += 1
        return ins

    def dma(self, q, out, in_, **kw):
        E = self.E[q]
        i0 = self.dma_next[q]
        self.dma_next[q] = (i0 + 1) % self.n_dma
        i = i0 + (self.n_dma if q == "pool" else 0)
        s = self.dma_sem[i]
        toks = self._deps(in_.bufs, out.bufs)
        if self.dma_val[i] > 0:
            toks.append((s, self.dma_val[i]))
        self._wait(E, toks)
        ins = E.h.dma_start(out=out.ap, in_=in_.ap, **kw)
        self.dma_val[i] += 16
        ins.then_inc(self.sems[s], 16)
        self._commit((s, self.dma_val[i]), in_.bufs, out.bufs)
        self.ninst += 1
        return ins

    def barrier(self):
        toks = []
        for e2 in self.E.values():
            if e2.count > 0:
                toks.append((e2.semidx, e2.count))
        for i, s in enumerate(self.dma_sem):
            if self.dma_val[i] > 0:
                toks.append((s, self.dma_val[i]))
        for E in self.E.values():
            pe_self = [(s, v) for (s, v) in toks if not (s == E.semidx)]
            self._wait(E, pe_self)

    def finish(self):
        E = self.E["sp"]
        for i, s in enumerate(self.dma_sem):
            if self.dma_val[i] > 0 and E.waited.get(s, 0) < self.dma_val[i]:
                E.h.wait_ge(self.sems[s], self.dma_val[i])
                E.waited[s] = self.dma_val[i]
        for name in ("pe", "act", "dve", "pool"):
            e2 = self.E[name]
            if e2.count > 0 and E.waited.get(e2.semidx, 0) < e2.count:
                E.h.wait_ge(self.sems[e2.semidx], e2.count)


D = 1024
DFF = 4096
C = 256
EPS = 1e-6
NMOD = 6


class Cfg:
    def __init__(self, L=8192, depth=4, debug=False, phases=None):
        self.L = L
        self.T = C + L
        self.depth = depth
        self.debug = debug
        self.phases = phases


def tiles(cfg, n):
    out = []
    for s in range(0, C, n):
        out.append((s, min(n, C - s), 1))
    for s in range(0, cfg.L, n):
        out.append((C + s, min(n, cfg.L - s), 0))
    return out


def host_consts():
    c = {}
    c["ident"] = np.eye(128, dtype=np.float32)
    c["ones"] = np.ones((128, 128), dtype=np.float32)
    return c


class M:
    def __init__(self, cfg):
        self.cfg = cfg
        nc = bass.Bass("TRN2", target_bir_lowering=False)
        self.nc = nc
        self.es = contextlib.ExitStack()
        self.k = K(nc, self.es)
        self.din = {}
        self.dbg_out = []

    def inp(self, name, shape, dtype=F32):
        ap = self.nc.dram_tensor(name, list(shape), dtype, kind="ExternalInput").ap()
        d = DTile(ap, name)
        self.din[name] = d
        return d

    def scratch(self, name, shape, dtype=F32, dbg=False):
        kind = "ExternalOutput" if (dbg and self.cfg.debug) else "Internal"
        ap = self.nc.dram_tensor(name, list(shape), dtype, kind=kind).ap()
        if kind == "ExternalOutput":
            self.dbg_out.append(name)
        return DTile(ap, name)

    def outp(self, name, shape, dtype=F32):
        ap = self.nc.dram_tensor(name, list(shape), dtype, kind="ExternalOutput").ap()
        return DTile(ap, name)

    def declare(self):
        cfg = self.cfg
        L, T, dp = cfg.L, cfg.T, cfg.depth
        ne, no = (dp + 1) // 2, dp // 2
        self.x = self.inp("x", [L, D])
        self.ctx = self.inp("ctx", [C, D])
        self.cc = self.inp("cc", [128, 8, 2])
        self.w_ada = self.inp("w_ada", [dp, D, NMOD * D])
        self.b_ada = self.inp("b_adaT", [dp, 128, 48])
        self.norm_g = self.inp("norm_gT", [dp, 128, 32])
        self.w_mlp_in = self.inp("w_mlp_in", [dp, D, DFF])
        self.w_mlp_out = self.inp("w_mlp_out", [dp, DFF, D])
        self.c_ident = self.inp("ident", [128, 128])
        self.c_ones = self.inp("ones", [128, 128])
        self.out = self.outp("out", [L, D])
        self.XT = self.scratch("XT", [D, T], dbg=True)

    def load_consts(self):
        k, es = self.k, self.es
        self.ident = k.sbuf(es, "ident", [128, 128], F32)
        k.dma("sp", self.ident[:], self.c_ident.v(0, self.c_ident.t[:, :]))
        stg = k.sbuf(es, "ones32", [128, 128], F32)
        k.dma("sp", stg[:], self.c_ones.v(0, self.c_ones.t[:, :]))
        self.ones_bf = k.sbuf(es, "ones_bf", [128, 128], BF16)
        k.op("dve", "tensor_copy", out=self.ones_bf[:], in_=stg[:])
        self.ones32 = stg
        self.sc = k.sbuf(es, "sc", [128, 8, 2], F32)
        tmp = k.sbuf(es, "sc_raw", [128, 8, 2], F32)
        k.dma("sp", tmp[:], self.cc.v(0, self.cc.t[:, :, :]))
        k.op("act", "activation", out=self.sc[:], in_=tmp[:], func=AF.Silu)
        self.mod = k.sbuf(es, "mod", [128, 48, 2], F32)
        self.gl = k.sbuf(es, "gl", [128, 32], F32)
        self.coef = k.sbuf(es, "coef", [128, 6, 8, 2], F32)

    def transpose_in(self):
        k, cfg = self.k, self.cfg
        with contextlib.ExitStack() as es:
            xin = [k.sbuf(es, f"ti_x{i}", [128, D], F32) for i in range(2)]
            stg = [k.sbuf(es, f"ti_s{i}", [128, 8, 512], F32) for i in range(2)]
            ps = [k.psum(es, f"ti_p{i}", [128, 512], F32) for i in range(4)]
            XTr = self.XT.t.rearrange("(c p) t -> p c t", p=128)
            it = 0
            ip = 0
            for gi, (t0, n, isctx) in enumerate(tiles(cfg, 512)):
                sg = stg[gi % 2]
                for s in range(n // 128):
                    xi = xin[it % 2]
                    it += 1
                    tt = t0 + s * 128
                    if isctx:
                        src = self.ctx.v(0, self.ctx.t[tt:tt + 128, :])
                    else:
                        src = self.x.v(0, self.x.t[tt - C:tt - C + 128, :])
                    k.dma("sp", xi[:], src)
                    for half in range(2):
                        p = ps[ip % 4]
                        ip += 1
                        for q in range(4):
                            c = half * 4 + q
                            k.op("pe", "transpose", out=p[:, q * 128:(q + 1) * 128],
                                 in_=xi[:, c * 128:(c + 1) * 128], identity=self.ident[:])
                        eng = "act" if half == 0 else "dve"
                        nm = "copy" if half == 0 else "tensor_copy"
                        k.op(eng, nm,
                             out=sg.v(sg.t[:, half * 4:half * 4 + 4, s * 128:(s + 1) * 128]),
                             in_=p.v(p.t[:, :].rearrange("p (q t) -> p q t", q=4)))
                k.dma("pool", self.XT.v(("t", t0), XTr[:, :, t0:t0 + n]), sg.v(sg.t[:, :, 0:n]))

    def transpose_out(self):
        k, cfg = self.k, self.cfg
        with contextlib.ExitStack() as es:
            xin = [k.sbuf(es, f"to_x{i}", [128, 8, 512], F32) for i in range(2)]
            stg = [k.sbuf(es, f"to_s{i}", [128, D], F32) for i in range(2)]
            ps = [k.psum(es, f"to_p{i}", [128, 512], F32) for i in range(4)]
            XTr = self.XT.t.rearrange("(c p) t -> p c t", p=128)
            it = 0
            ip = 0
            for gi, (t0, n, isctx) in enumerate(tiles(cfg, 512)):
                if isctx:
                    continue
                xi = xin[gi % 2]
                k.dma("sp", xi.v(xi.t[:, :, 0:n]), self.XT.v(("t", t0), XTr[:, :, t0:t0 + n]))
                for s in range(n // 128):
                    sg = stg[it % 2]
                    it += 1
                    for half in range(2):
                        p = ps[ip % 4]
                        ip += 1
                        for q in range(4):
                            c = half * 4 + q
                            k.op("pe", "transpose", out=p[:, q * 128:(q + 1) * 128],
                                 in_=xi.v(xi.t[:, c, s * 128:(s + 1) * 128]), identity=self.ident[:])
                        eng = "act" if half == 0 else "dve"
                        nm = "copy" if half == 0 else "tensor_copy"
                        k.op(eng, nm, out=sg[:, half * 512:(half + 1) * 512], in_=p[:, :])
                    tt = t0 - C + s * 128
                    k.dma("pool", self.out.v(("t", tt), self.out.t[tt:tt + 128, :]), sg[:])

    def mod_vectors(self, li):
        k = self.k
        with contextlib.ExitStack() as es:
            wst = [k.sbuf(es, f"mv_w{i}", [128, 8, 512], F32) for i in range(2)]
            ps = k.psum(es, "mv_ps", [128, 96], F32)
            bsb = k.sbuf(es, "mv_b", [128, 48], F32)
            tmp = k.sbuf(es, "mv_t", [128, 8, 2], F32)
            k.dma("sp", bsb[:], self.b_ada.v(0, self.b_ada.t[li, :, :]))
            k.dma("sp", self.gl[:], self.norm_g.v(0, self.norm_g.t[li, :, :]))
            War = self.w_ada.t[li].rearrange("(kc p) n -> p kc n", p=128)
            for blk in range(12):
                w = wst[blk % 2]
                k.dma("sp", w[:], self.w_ada.v(0, War[:, :, blk * 512:(blk + 1) * 512]))
                for jj in range(4):
                    j = blk * 4 + jj
                    for kc in range(8):
                        k.op("pe", "matmul", out=ps[:, 2 * j:2 * j + 2],
                             lhsT=w.v(w.t[:, kc, jj * 128:(jj + 1) * 128]),
                             rhs=self.sc.v(self.sc.t[:, kc, :]), start=(kc == 0), stop=(kc == 7))
            for t in range(2):
                k.op("dve", "tensor_tensor", out=self.mod.v(self.mod.t[:, :, t]),
                     in0=ps.v(ps.t[:, :].rearrange("p (j t) -> p j t", t=2)[:, :, t]),
                     in1=bsb[:, :], op=ALU.add)
            mod, gl, coef = self.mod, self.gl, self.coef

            def mv(m):
                return mod.v(mod.t[:, m * 8:(m + 1) * 8, :])

            def gv(g):
                return gl.v(gl.t[:, g * 8:(g + 1) * 8])

            for (dst, mscale, gidx) in ((0, 1, 0), (3, 4, 2)):
                k.op("dve", "tensor_scalar", out=tmp[:], in0=mv(mscale), scalar1=1.0, scalar2=None, op0=ALU.add)
                for t in range(2):
                    k.op("dve", "tensor_tensor", out=coef.v(coef.t[:, dst, :, t]),
                         in0=tmp.v(tmp.t[:, :, t]), in1=gv(gidx), op=ALU.mult)
            for (dst, mshift) in ((1, 0), (4, 3)):
                k.op("dve", "tensor_copy", out=coef.v(coef.t[:, dst, :, :]), in_=mv(mshift))
            for (dst, mgate, gidx) in ((2, 2, 1), (5, 5, 3)):
                for t in range(2):
                    k.op("dve", "tensor_tensor", out=coef.v(coef.t[:, dst, :, t]),
                         in0=mod.v(mod.t[:, mgate * 8:(mgate + 1) * 8, t]), in1=gv(gidx), op=ALU.mult)

    def rstd_bc(self, es_bufs, src_chunks, n, nch, dim):
        k = self.k
        ps = es_bufs["ps"]
        for c in range(nch):
            sq = es_bufs["sq"][c % 2]
            k.op("act", "activation", out=sq.v(sq.t[:, 0:n]), in_=src_chunks(c), func=AF.Square)
            k.op("pe", "matmul", out=ps.v(ps.t[:, 0:n]), lhsT=self.ones_bf[:], rhs=sq.v(sq.t[:, 0:n]),
                 start=(c == 0), stop=(c == nch - 1))
        rs, rstd = es_bufs["rs"], es_bufs["rstd"]
        k.op("act", "activation", out=rs.v(rs.t[:, 0:n]), in_=ps.v(ps.t[:, 0:n]), func=AF.Sqrt,
             scale=1.0 / dim, bias=self.epsb[:, 0:1])
        k.op("dve", "reciprocal", out=rstd.v(rstd.t[:, 0:n]), in_=rs.v(rs.t[:, 0:n]))
        return rstd.v(rstd.t[:, 0:n])

    def mlp_layer(self, li):
        k, cfg = self.k, self.cfg
        N = 256
        with contextlib.ExitStack() as es:
            W1 = k.sbuf(es, "W1", [128, 8, DFF], BF16)
            W2 = k.sbuf(es, "W2", [128, 32, D], BF16)
            stg = [k.sbuf(es, f"wstg{i}", [128, 2048], F32) for i in range(2)]
            cast_engs = [("act", "copy"), ("dve", "tensor_copy"), ("pool", "tensor_copy")]
            ic = 0
            for kc in range(8):
                for half in range(2):
                    s = stg[ic % 2]
                    k.dma("sp", s[:], self.w_mlp_in.v(0, self.w_mlp_in.t[li, kc * 128:(kc + 1) * 128,
                                                                         half * 2048:(half + 1) * 2048]))
                    e, nm = cast_engs[ic % 3]
                    k.op(e, nm, out=W1.v(W1.t[:, kc, half * 2048:(half + 1) * 2048]), in_=s[:])
                    ic += 1
            W2r = self.w_mlp_out.t[li].rearrange("(j p) n -> p j n", p=128)
            for jp in range(16):
                s = stg[ic % 2]
                k.dma("sp", s.v(s.t[:, :].rearrange("p (j n) -> p j n", j=2)),
                      self.w_mlp_out.v(0, W2r[:, 2 * jp:2 * jp + 2, :]))
                e, nm = cast_engs[ic % 3]
                k.op(e, nm, out=W2.v(W2.t[:, 2 * jp:2 * jp + 2, :]),
                     in_=s.v(s.t[:, :].rearrange("p (j n) -> p j n", j=2)))
                ic += 1
            xt = [k.sbuf(es, f"ml_x{i}", [128, 8, N], F32) for i in range(2)]
            h = k.sbuf(es, "ml_h", [128, 8, N], BF16)
            a = k.sbuf(es, "ml_a", [128, 32, N], BF16)
            ysb = k.sbuf(es, "ml_y", [128, 8, N], F32)
            tmp = [k.sbuf(es, f"ml_t{i}", [128, N], F32) for i in range(2)]
            nb = {"sq": [k.sbuf(es, f"ml_sq{i}", [128, N], BF16) for i in range(2)],
                  "ps": k.psum(es, "ml_pss", [128, N], F32),
                  "rs": k.sbuf(es, "ml_rs", [128, N], F32),
                  "rstd": k.sbuf(es, "ml_rstd", [128, N], F32)}
            ph = [k.psum(es, f"ml_ph{i}", [128, N], F32) for i in range(2)]
            py = [k.psum(es, f"ml_py{i}", [128, N], F32) for i in range(2)]
            XTr = self.XT.t.rearrange("(c p) t -> p c t", p=128)
            coef = self.coef
            tl = tiles(cfg, N)
            k.dma("sp", xt[0].v(xt[0].t[:, :, 0:tl[0][1]]),
                  self.XT.v(("t", tl[0][0]), XTr[:, :, tl[0][0]:tl[0][0] + tl[0][1]]))
            for ti, (t0, n, isctx) in enumerate(tl):
                x = xt[ti % 2]
                if ti + 1 < len(tl):
                    t1, n1, _ = tl[ti + 1]
                    xn = xt[(ti + 1) % 2]
                    k.dma("sp", xn.v(xn.t[:, :, 0:n1]), self.XT.v(("t", t1), XTr[:, :, t1:t1 + n1]))
                rstd = self.rstd_bc(nb, lambda c: x.v(x.t[:, c, 0:n]), n, 8, D)
                for c in range(8):
                    tp = tmp[c % 2]
                    k.op("dve", "scalar_tensor_tensor", out=tp.v(tp.t[:, 0:n]), in0=x.v(x.t[:, c, 0:n]),
                         scalar=coef.v(coef.t[:, 3, c, isctx:isctx + 1]), in1=rstd, op0=ALU.mult, op1=ALU.mult)
                    k.op("act", "activation", out=h.v(h.t[:, c, 0:n]), in_=tp.v(tp.t[:, 0:n]), func=AF.Identity,
                         bias=coef.v(coef.t[:, 4, c, isctx:isctx + 1]), scale=1.0)
                for j in range(32):
                    p = ph[j % 2]
                    for kc in range(8):
                        k.op("pe", "matmul", out=p.v(p.t[:, 0:n]), lhsT=W1.v(W1.t[:, kc, j * 128:(j + 1) * 128]),
                             rhs=h.v(h.t[:, kc, 0:n]), start=(kc == 0), stop=(kc == 7))
                    tp = tmp[j % 2]
                    k.op("act", "activation", out=tp.v(tp.t[:, 0:n]), in_=p.v(p.t[:, 0:n]), func=AF.Relu)
                    e = "dve" if j % 2 == 0 else "pool"
                    k.op(e, "tensor_tensor", out=a.v(a.t[:, j, 0:n]), in0=tp.v(tp.t[:, 0:n]),
                         in1=tp.v(tp.t[:, 0:n]), op=ALU.mult)
                for c in range(8):
                    p = py[c % 2]
                    for j in range(32):
                        k.op("pe", "matmul", out=p.v(p.t[:, 0:n]), lhsT=W2.v(W2.t[:, j, c * 128:(c + 1) * 128]),
                             rhs=a.v(a.t[:, j, 0:n]), start=(j == 0), stop=(j == 31))
                    k.op("act", "copy", out=ysb.v(ysb.t[:, c, 0:n]), in_=p.v(p.t[:, 0:n]))
                rstd = self.rstd_bc(nb, lambda c: ysb.v(ysb.t[:, c, 0:n]), n, 8, D)
                for c in range(8):
                    tp = tmp[c % 2]
                    k.op("dve", "scalar_tensor_tensor", out=tp.v(tp.t[:, 0:n]), in0=ysb.v(ysb.t[:, c, 0:n]),
                         scalar=coef.v(coef.t[:, 5, c, isctx:isctx + 1]), in1=rstd, op0=ALU.mult, op1=ALU.mult)
                    k.op("pool", "tensor_tensor", out=x.v(x.t[:, c, 0:n]), in0=x.v(x.t[:, c, 0:n]),
                         in1=tp.v(tp.t[:, 0:n]), op=ALU.add)
                k.dma("pool", self.XT.v(("t", t0), XTr[:, :, t0:t0 + n]), x.v(x.t[:, :, 0:n]))

    def build(self):
        cfg = self.cfg
        k = self.k
        self.declare()
        self.load_consts()
        self.epsb = k.sbuf(self.es, "epsb", [128, 1], F32)
        k.op("dve", "memset", ap=self.epsb[:], constant=EPS)
        self.transpose_in()
        k.barrier()
        for li in range(cfg.depth):
            self.mod_vectors(li)
            k.barrier()
            self.mlp_layer(li)
            k.barrier()
        self.transpose_out()
        k.finish()
        self.es.close()
        return self.nc


def in_proj(self, li, Wd, wl, ncols, fchunks, tgroups, PT, PK):
    k, cfg = self.k, self.cfg
    N = 512
    with contextlib.ExitStack() as es:
        W = k.sbuf(es, "ipW", [128, 8, ncols], BF16)
        stg = [k.sbuf(es, f"ipstg{i}", [128, ncols], F32) for i in range(2)]
        cast_engs = [("act", "copy"), ("dve", "tensor_copy"), ("pool", "tensor_copy")]
        for kc in range(8):
            s = stg[kc % 2]
            k.dma("sp", s[:], Wd.v(0, Wd.t[wl, kc * 128:(kc + 1) * 128, :]))
            e, nm = cast_engs[kc % 3]
            k.op(e, nm, out=W.v(W.t[:, kc, :]), in_=s[:])
        nf = len(fchunks)
        ktot = sum(m for (_, m, _) in tgroups)
        xt = [k.sbuf(es, f"ip_x{i}", [128, 8, N], F32) for i in range(2)]
        h = k.sbuf(es, "ip_h", [128, 8, N], BF16)
        outF = k.sbuf(es, "ip_oF", [128, nf, N], F32)
        outK = k.sbuf(es, "ip_oK", [128, 4, max(ktot, 1)], F32)
        tmp = [k.sbuf(es, f"ip_t{i}", [128, N], F32) for i in range(2)]
        nb = {"sq": [k.sbuf(es, f"ip_sq{i}", [128, N], BF16) for i in range(2)],
              "ps": k.psum(es, "ip_pss", [128, N], F32),
              "rs": k.sbuf(es, "ip_rs", [128, N], F32),
              "rstd": k.sbuf(es, "ip_rstd", [128, N], F32)}
        pp = [k.psum(es, f"ip_pp{i}", [128, N], F32) for i in range(4)]
        XTr = self.XT.t.rearrange("(c p) t -> p c t", p=128)
        PTr = PT.t.rearrange("(c p) t -> p c t", p=128)
        coef = self.coef
        tl = tiles(cfg, N)
        k.dma("sp", xt[0].v(xt[0].t[:, :, 0:tl[0][1]]),
              self.XT.v(("t", tl[0][0]), XTr[:, :, tl[0][0]:tl[0][0] + tl[0][1]]))
        ip = 0
        for ti, (t0, n, isctx) in enumerate(tl):
            x = xt[ti % 2]
            if ti + 1 < len(tl):
                t1, n1, _ = tl[ti + 1]
                xn = xt[(ti + 1) % 2]
                k.dma("sp", xn.v(xn.t[:, :, 0:n1]), self.XT.v(("t", t1), XTr[:, :, t1:t1 + n1]))
            rstd = self.rstd_bc(nb, lambda c: x.v(x.t[:, c, 0:n]), n, 8, D)
            for c in range(8):
                tp = tmp[c % 2]
                k.op("dve", "scalar_tensor_tensor", out=tp.v(tp.t[:, 0:n]), in0=x.v(x.t[:, c, 0:n]),
                     scalar=coef.v(coef.t[:, 0, c, isctx:isctx + 1]), in1=rstd, op0=ALU.mult, op1=ALU.mult)
                k.op("act", "activation", out=h.v(h.t[:, c, 0:n]), in_=tp.v(tp.t[:, 0:n]), func=AF.Identity,
                     bias=coef.v(coef.t[:, 1, c, isctx:isctx + 1]), scale=1.0)
            for fi, (c0, m) in enumerate(fchunks):
                p = pp[ip % 4]
                ip += 1
                for kc in range(8):
                    k.op("pe", "matmul", out=p.v(p.t[0:m, 0:n]), lhsT=W.v(W.t[:, kc, c0:c0 + m]),
                         rhs=h.v(h.t[:, kc, 0:n]), start=(kc == 0), stop=(kc == 7))
                if fi % 2 == 0:
                    k.op("act", "copy", out=outF.v(outF.t[0:m, fi, 0:n]), in_=p.v(p.t[0:m, 0:n]))
                else:
                    k.op("dve", "tensor_copy", out=outF.v(outF.t[0:m, fi, 0:n]), in_=p.v(p.t[0:m, 0:n]))
            k.dma("pool", PT.v(("t", t0), PTr[:, 0:nf, t0:t0 + n]), outF.v(outF.t[:, :, 0:n]))
            if tgroups:
                for s in range(n // 128):
                    for gi, (c0, m, dc) in enumerate(tgroups):
                        p = pp[ip % 4]
                        ip += 1
                        for kc in range(8):
                            k.op("pe", "matmul", out=p.v(p.t[:, 0:m]), lhsT=h.v(h.t[:, kc, s * 128:(s + 1) * 128]),
                                 rhs=W.v(W.t[:, kc, c0:c0 + m]), start=(kc == 0), stop=(kc == 7))
                        if (gi + s) % 2 == 0:
                            k.op("act", "copy", out=outK.v(outK.t[:, s, dc:dc + m]), in_=p.v(p.t[:, 0:m]))
                        else:
                            k.op("dve", "tensor_copy", out=outK.v(outK.t[:, s, dc:dc + m]), in_=p.v(p.t[:, 0:m]))
                k.dma("pool", PK.v(("t", t0), PK.t[t0:t0 + n, 0:ktot].rearrange("(s p) c -> p s c", p=128)),
                      outK.v(outK.t[:, 0:n // 128, :]))


M.in_proj = in_proj


def out_proj(self, li, Wd, wl, YM):
    k, cfg = self.k, self.cfg
    N = 512
    with contextlib.ExitStack() as es:
        W = k.sbuf(es, "opW", [128, 8, D], BF16)
        stg = [k.sbuf(es, f"opstg{i}", [128, D], F32) for i in range(2)]
        cast_engs = [("act", "copy"), ("dve", "tensor_copy"), ("pool", "tensor_copy")]
        for kc in range(8):
            s = stg[kc % 2]
            k.dma("sp", s[:], Wd.v(0, Wd.t[wl, kc * 128:(kc + 1) * 128, :]))
            e, nm = cast_engs[kc % 3]
            k.op(e, nm, out=W.v(W.t[:, kc, :]), in_=s[:])
        xt = [k.sbuf(es, f"op_x{i}", [128, 8, N], F32) for i in range(2)]
        ym = [k.sbuf(es, f"op_ym{i}", [128, 8, N], BF16) for i in range(2)]
        ysb = k.sbuf(es, "op_y", [128, 8, N], F32)
        tmp = [k.sbuf(es, f"op_t{i}", [128, N], F32) for i in range(2)]
        nb = {"sq": [k.sbuf(es, f"op_sq{i}", [128, N], BF16) for i in range(2)],
              "ps": k.psum(es, "op_pss", [128, N], F32),
              "rs": k.sbuf(es, "op_rs", [128, N], F32),
              "rstd": k.sbuf(es, "op_rstd", [128, N], F32)}
        py = [k.psum(es, f"op_py{i}", [128, N], F32) for i in range(2)]
        XTr = self.XT.t.rearrange("(c p) t -> p c t", p=128)
        YMr = YM.t.rearrange("(c p) t -> p c t", p=128)
        coef = self.coef
        tl = tiles(cfg, N)
        for ti, (t0, n, isctx) in enumerate(tl):
            x = xt[ti % 2]
            y_in = ym[ti % 2]
            k.dma("sp", x.v(x.t[:, :, 0:n]), self.XT.v(("t", t0), XTr[:, :, t0:t0 + n]))
            k.dma("sp", y_in.v(y_in.t[:, :, 0:n]), YM.v("all", YMr[:, :, t0:t0 + n]))
            for c in range(8):
                p = py[c % 2]
                for kc in range(8):
                    k.op("pe", "matmul", out=p.v(p.t[:, 0:n]), lhsT=W.v(W.t[:, kc, c * 128:(c + 1) * 128]),
                         rhs=y_in.v(y_in.t[:, kc, 0:n]), start=(kc == 0), stop=(kc == 7))
                k.op("act", "copy", out=ysb.v(ysb.t[:, c, 0:n]), in_=p.v(p.t[:, 0:n]))
            rstd = self.rstd_bc(nb, lambda c: ysb.v(ysb.t[:, c, 0:n]), n, 8, D)
            for c in range(8):
                tp = tmp[c % 2]
                k.op("dve", "scalar_tensor_tensor", out=tp.v(tp.t[:, 0:n]), in0=ysb.v(ysb.t[:, c, 0:n]),
                     scalar=coef.v(coef.t[:, 2, c, isctx:isctx + 1]), in1=rstd, op0=ALU.mult, op1=ALU.mult)
                k.op("pool", "tensor_tensor", out=x.v(x.t[:, c, 0:n]), in0=x.v(x.t[:, c, 0:n]),
                     in1=tp.v(tp.t[:, 0:n]), op=ALU.add)
            k.dma("pool", self.XT.v(("t", t0), XTr[:, :, t0:t0 + n]), x.v(x.t[:, :, 0:n]))


M.out_proj = out_proj


def rope_tables(L):
    GRID_W = 64
    t = np.arange(L)
    r = (t // GRID_W).astype(np.float32)
    col = (t % GRID_W).astype(np.float32)
    inv = (10000.0 ** (-np.arange(16, dtype=np.float32) / 16)).astype(np.float32)
    ang = np.concatenate([r[:, None] * inv, col[:, None] * inv], axis=-1).astype(np.float32)
    cos, sin = np.cos(ang).astype(np.float32), np.sin(ang).astype(np.float32)
    cosT = np.repeat(cos, 2, axis=1).T
    sinT = np.repeat(sin, 2, axis=1).T
    out = np.stack([np.tile(cosT, (2, 1)), np.tile(sinT, (2, 1))]).astype(np.float32)
    return np.ascontiguousarray(out)


def rot_matrix():
    R = np.zeros((128, 128), np.float32)
    for m in range(128):
        if m % 2 == 0:
            R[m + 1, m] = -1.0
        else:
            R[m - 1, m] = 1.0
    return R


def block_ones(bs):
    o = np.zeros((128, 128), np.float32)
    for b in range(128 // bs):
        o[b * bs:(b + 1) * bs, b * bs:(b + 1) * bs] = 1.0
    return o


def norm_rope(self, nb, src, n, out, g_ap, t_main0, do_norm, do_rope, blk, hd):
    k = self.k
    cur = src
    if do_norm:
        sq = nb["sq"][0]
        k.op("act", "activation", out=sq.v(sq.t[:, 0:n]), in_=src, func=AF.Square)
        ps = nb["ps"]
        k.op("pe", "matmul", out=ps.v(ps.t[:, 0:n]), lhsT=blk[:], rhs=sq.v(sq.t[:, 0:n]), start=True, stop=True)
        rs, rstd = nb["rs"], nb["rstd"]
        k.op("act", "activation", out=rs.v(rs.t[:, 0:n]), in_=ps.v(ps.t[:, 0:n]), func=AF.Sqrt,
             scale=1.0 / hd, bias=self.epsb[:, 0:1])
        k.op("dve", "reciprocal", out=rstd.v(rstd.t[:, 0:n]), in_=rs.v(rs.t[:, 0:n]))
        kn = nb["kn"]
        k.op("dve", "scalar_tensor_tensor", out=kn.v(kn.t[:, 0:n]), in0=src, scalar=g_ap,
             in1=rstd.v(rstd.t[:, 0:n]), op0=ALU.mult, op1=ALU.mult)
        cur = kn.v(kn.t[:, 0:n])
    def halves(t_, ncols):
        return [t_.v(t_.t[0:64, 0:ncols]), t_.v(t_.t[64:128, 0:ncols])]
    if not do_rope:
        if isinstance(out, list):
            if do_norm:
                src_h = halves(nb["kn"], n)
            else:
                src_h = src
            for o_, s_ in zip(out, src_h):
                k.op("act", "copy", out=o_, in_=s_)
        else:
            k.op("act", "copy", out=out, in_=cur)
        return
    if isinstance(cur, list):
        raise ValueError("rope path needs a full-view src")
    cs = nb["cs"]
    k.dma("sp", cs.v(cs.t[:, :, 0:n]), self.c_rope.v(0, self.c_rope.t[:, :, t_main0:t_main0 + n].rearrange("a p t -> p a t")))
    pr = nb["pr"]
    k.op("pe", "matmul", out=pr.v(pr.t[:, 0:n]), lhsT=self.rotm[:], rhs=cur, start=True, stop=True)
    t1, t2 = nb["t1"], nb["t2"]
    k.op("pool", "tensor_tensor", out=t1.v(t1.t[:, 0:n]), in0=cur, in1=cs.v(cs.t[:, 0, 0:n]), op=ALU.mult)
    k.op("dve", "tensor_tensor", out=t2.v(t2.t[:, 0:n]), in0=pr.v(pr.t[:, 0:n]), in1=cs.v(cs.t[:, 1, 0:n]), op=ALU.mult)
    if isinstance(out, list):
        for o_, a_, b_ in zip(out, halves(t1, n), halves(t2, n)):
            k.op("pool", "tensor_tensor", out=o_, in0=a_, in1=b_, op=ALU.add)
    else:
        k.op("pool", "tensor_tensor", out=out, in0=t1.v(t1.t[:, 0:n]), in1=t2.v(t2.t[:, 0:n]), op=ALU.add)


M.norm_rope = norm_rope


def nr_bufs(self, es, N, pfx):
    k = self.k
    return {"sq": [k.sbuf(es, pfx + "sq", [128, N], BF16)],
            "ps": k.psum(es, pfx + "ps", [128, N], F32),
            "pr": k.psum(es, pfx + "pr", [128, N], F32),
            "rs": k.sbuf(es, pfx + "rs", [128, N], F32),
            "rstd": k.sbuf(es, pfx + "rstd", [128, N], F32),
            "kn": k.sbuf(es, pfx + "kn", [128, N], F32),
            "cs": k.sbuf(es, pfx + "cs", [128, 2, N], F32),
            "t1": k.sbuf(es, pfx + "t1", [128, N], F32),
            "t2": k.sbuf(es, pfx + "t2", [128, N], F32)}


M.nr_bufs = nr_bufs


def gqa(self, j, PT, PK, YM, qc0, kc, vcol, ym_row0, ctx_out):
    k, cfg = self.k, self.cfg
    T, L = cfg.T, cfg.L
    NT = T // 128
    N = 512
    with contextlib.ExitStack() as es:
        nb = self.nr_bufs(es, N, "gq_")
        blk = k.sbuf(es, "gq_blk", [128, 128], BF16)
        k.op("dve", "tensor_copy", out=blk[:], in_=self.blk64[:])
        gsb = k.sbuf(es, "gq_g", [128, 2], F32)
        k.dma("sp", gsb[:], self.ev_qk_g.v(0, self.ev_qk_g.t[j, :, :]))
        PTr = PT.t.rearrange("(c p) t -> p c t", p=128)
        KT = [k.sbuf(es, f"gq_KT{kv}", [128, T], BF16) for kv in range(2)]
        kraw = k.sbuf(es, "gq_kraw", [128, N], F32)
        for kv in range(2):
            for (t0, n, isctx) in tiles(cfg, N):
                for hf in range(2):
                    k.dma("sp", kraw.v(kraw.t[hf * 64:(hf + 1) * 64, 0:n]),
                          PT.v(("t", t0), PT.t[kc * 128 + kv * 64:kc * 128 + (kv + 1) * 64, t0:t0 + n]))
                self.norm_rope(nb, kraw.v(kraw.t[:, 0:n]), n, KT[kv].v(KT[kv].t[:, t0:t0 + n]),
                               gsb.v(gsb.t[:, 1:2]), t0 - C, True, not isctx, blk, 64)
        Va = k.sbuf(es, "gq_Va", [128, NT, 2, 65], BF16)
        k.op("pool", "memset", ap=Va[:], constant=1.0)
        vst = k.sbuf(es, "gq_vst", [128, 8, 128], F32)
        for b0 in range(0, NT, 8):
            nbk = min(8, NT - b0)
            k.dma("sp", vst.v(vst.t[:, 0:nbk, :]),
                  PK.v("all", PK.t[b0 * 128:(b0 + nbk) * 128, vcol:vcol + 128].rearrange("(s p) c -> p s c", p=128)))
            k.op("dve", "tensor_copy", out=Va.v(Va.t[:, b0:b0 + nbk, :, 0:64]),
                 in_=vst.v(vst.t[:, 0:nbk, :].rearrange("p s (kv d) -> p s kv d", kv=2)))
        qraw = [k.sbuf(es, f"gq_qraw{i}", [128, 4, N], F32) for i in range(2)]
        QT = [k.sbuf(es, f"gq_QT{i}", [128, 8, N], BF16) for i in range(2)]
        for q_ in QT:
            k.op("pool", "memset", ap=q_[:], constant=0.0)
        pS = [k.psum(es, f"gq_pS{i}", [128, N], F32) for i in range(3)]
        pO = [k.psum(es, f"gq_pO{i}", [128, N], F32) for i in range(2)]
        pB = k.psum(es, "gq_pB", [128, N], F32)
        pb = [k.sbuf(es, f"gq_pb{i}", [128, N], BF16) for i in range(3)]
        osb = [k.sbuf(es, f"gq_osb{i}", [128, N], F32) for i in range(2)]
        rsum = [k.sbuf(es, f"gq_rsum{i}", [128, N], F32) for i in range(2)]
        yst = [k.sbuf(es, f"gq_yst{i}", [64, N], BF16) for i in range(2)]
        iS = 0
        iH = 0
        for ti, (t0, n, isctx) in enumerate(tiles(cfg, N)):
            if isctx and not ctx_out:
                continue
            qr, qt = qraw[ti % 2], QT[ti % 2]
            k.dma("sp", qr.v(qr.t[:, :, 0:n]), PT.v(("t", t0), PTr[:, qc0:qc0 + 4, t0:t0 + n]))
            for c in range(4):
                self.norm_rope(nb, qr.v(qr.t[:, c, 0:n]), n,
                               [qt.v(qt.t[0:64, 2 * c, 0:n]), qt.v(qt.t[64:128, 2 * c + 1, 0:n])],
                               gsb.v(gsb.t[:, 0:1]), t0 - C, True, not isctx, blk, 64)
            kts = list(range(C // 128)) if isctx else list(range(NT))
            for hq in range(8):
                c, r, kv = hq // 2, (hq % 2) * 64, hq // 4
                po = pO[iH % 2]
                ob, rsm, ys = osb[iH % 2], rsum[iH % 2], yst[iH % 2]
                iH += 1

                def qk(kt):
                    ps = pS[(iS + kt) % 3]
                    k.op("pe", "matmul", out=ps.v(ps.t[:, 0:n]),
                         lhsT=KT[kv].v(KT[kv].t[:, kt * 128:(kt + 1) * 128]),
                         rhs=qt.v(qt.t[:, hq, 0:n]), start=True, stop=True)

                qk(kts[0])
                for ii, kt in enumerate(kts):
                    if ii + 1 < len(kts):
                        qk(kts[ii + 1])
                    ps = pS[(iS + kt) % 3]
                    pbuf = pb[(iS + kt) % 3]
                    k.op("act", "activation", out=pbuf.v(pbuf.t[:, 0:n]), in_=ps.v(ps.t[:, 0:n]),
                         func=AF.Exp, scale=0.125)
                    k.op("pe", "matmul", out=po.v(po.t[0:65, 0:n]), lhsT=Va.v(Va.t[:, kt, kv, :]),
                         rhs=pbuf.v(pbuf.t[:, 0:n]), start=(ii == 0), stop=(ii == len(kts) - 1))
                iS += len(kts)
                k.op("act", "copy", out=ob.v(ob.t[0:64, 0:n]), in_=po.v(po.t[0:64, 0:n]))
                k.op("dve", "reciprocal", out=rsm.v(rsm.t[64:65, 0:n]), in_=po.v(po.t[64:65, 0:n]))
                k.op("pe", "matmul", out=pB.v(pB.t[0:64, 0:n]), lhsT=self.ones32.v(self.ones32.t[64:65, 0:64]),
                     rhs=rsm.v(rsm.t[64:65, 0:n]), start=True, stop=True)
                k.op("dve", "tensor_tensor", out=ys.v(ys.t[:, 0:n]), in0=ob.v(ob.t[0:64, 0:n]),
                     in1=pB.v(pB.t[0:64, 0:n]), op=ALU.mult)
                row = ym_row0 + hq * 64
                k.dma("pool", YM.v("all", YM.t[row:row + 64, t0:t0 + n]), ys.v(ys.t[:, 0:n]))


M.gqa = gqa


EV_F = [(0, 128), (128, 128), (256, 128), (384, 128), (1024, 128), (1152, 128), (1280, 128), (1408, 128),
        (1536, 32), (1568, 128), (1696, 128), (1824, 128), (1952, 128), (2080, 128)]
EV_T = [(256, 512, 0), (768, 256, 512), (2208, 128, 768)]


def declare_mix(self):
    cfg = self.cfg
    dp, L, T = cfg.depth, cfg.L, cfg.T
    ne, no = (dp + 1) // 2, dp // 2
    self.ev_w_in = self.inp("ev_w_in", [ne, D, 2336])
    self.ev_w_out = self.inp("ev_w_out", [ne, D, D])
    self.ev_qk_g = self.inp("ev_qk_gT", [ne, 128, 2])
    self.c_rope = self.inp("rope", [2, 128, L])
    self.c_rotm = self.inp("rotm", [128, 128])
    self.c_blk64 = self.inp("blk64", [128, 128])
    self.PT = self.scratch("PT", [24 * 128, T], dbg=True)
    self.PK = self.scratch("PK", [T, 1024], dbg=True)
    self.YM = self.scratch("YM", [D, T], BF16, dbg=True)
    k, es = self.k, self.es
    self.rotm = k.sbuf(es, "rotm", [128, 128], F32)
    k.dma("sp", self.rotm[:], self.c_rotm.v(0, self.c_rotm.t[:, :]))
    self.blk64 = k.sbuf(es, "blk64", [128, 128], F32)
    k.dma("sp", self.blk64[:], self.c_blk64.v(0, self.c_blk64.t[:, :]))


M.declare_mix = declare_mix


def zero_ym(self, r0, r1):
    k, cfg = self.k, self.cfg
    with contextlib.ExitStack() as es:
        z = k.sbuf(es, "zz", [128, 2048], BF16)
        k.op("pool", "memset", ap=z[:], constant=0.0)
        for rr in range(r0, r1, 128):
            for t0 in range(0, cfg.T, 2048):
                n = min(2048, cfg.T - t0)
                k.dma("sp", self.YM.v("all", self.YM.t[rr:rr + 128, t0:t0 + n]), z.v(z.t[:, 0:n]))


M.zero_ym = zero_ym


def even_layer(self, li, last):
    k = self.k
    j = li // 2
    self.in_proj(li, self.ev_w_in, j, 2336, EV_F, EV_T, self.PT, self.PK)
    k.barrier()
    if self.cfg.phases is None or "gla" in self.cfg.phases:
        self.gla(j, self.PT, self.PK, self.YM, not last)
    else:
        self.zero_ym(0, 512)
    k.barrier()
    self.gqa(j, self.PT, self.PK, self.YM, 9, 13, 768, 512, not last)
    k.barrier()
    self.out_proj(li, self.ev_w_out, j, self.YM)
    k.barrier()


M.even_layer = even_layer


def build2(self):
    cfg = self.cfg
    k = self.k
    self.declare()
    self.declare_mix()
    self.load_consts()
    self.declare_gla()
    self.declare_odd()
    self.declare_hy()
    self.epsb = k.sbuf(self.es, "epsb", [128, 1], F32)
    k.op("dve", "memset", ap=self.epsb[:], constant=EPS)
    self.oneb = k.sbuf(self.es, "oneb", [128, 1], F32)
    k.op("dve", "memset", ap=self.oneb[:], constant=1.0)
    self.transpose_in()
    k.barrier()
    for li in range(cfg.depth):
        last = li == cfg.depth - 1
        self.mod_vectors(li)
        k.barrier()
        if li % 2 == 0:
            self.even_layer(li, last)
        else:
            self.odd_layer(li, last)
        self.mlp_layer(li)
        k.barrier()
    self.transpose_out()
    k.finish()
    self.es.close()
    return self.nc


M.build2 = build2


def gla_consts():
    s = np.arange(128)[:, None]
    t = np.arange(128)[None, :]
    sc = -1.0 / 16.0
    ucat = np.zeros((2, 128, 129), np.float32)
    ucat[0, :, :128] = (s <= t) * sc
    ucat[1, :, :128] = (s >= t) * sc
    ucat[:, :, 128] = sc
    ustr = np.zeros((2, 128, 128), np.float32)
    ustr[0] = (s > t) * sc
    ustr[1] = (s < t) * sc
    mask = np.zeros((2, 128, 128), np.float32)
    mask[0] = (s <= t)
    mask[1] = (s >= t)
    return {"gla_ucat": ucat, "gla_ustr": ustr, "gla_mask": mask}


def declare_gla(self):
    ne = (self.cfg.depth + 1) // 2
    self.ev_wlr = self.inp("ev_wlr_aug", [ne, 2, 17, 256])
    self.ev_gla_g = self.inp("ev_gla_gT", [ne, 128, 1])
    self.c_ucat = self.inp("gla_ucat", [2, 128, 129])
    self.c_ustr = self.inp("gla_ustr", [2, 128, 128])
    self.c_mask = self.inp("gla_mask", [2, 128, 128])
    self.OF = self.scratch("OF", [512, self.cfg.T], dbg=True)


M.declare_gla = declare_gla


def gla(self, j, PT, PK, YM, ctx_out):
    k, cfg = self.k, self.cfg
    T = cfg.T
    NT = T // 128
    nctx = C // 128
    PTr = PT.t.rearrange("(c p) t -> p c t", p=128)
    OFr = self.OF.t.rearrange("(c p) t -> p c t", p=128)
    YMr = YM.t.rearrange("(c p) t -> p c t", p=128)
    with contextlib.ExitStack() as es:
        sb = lambda nm, sh, dt=F32: k.sbuf(es, "gl_" + nm, sh, dt)
        lra = [sb(f"lra{d}", [32, T]) for d in range(2)]
        wlr = [sb(f"wlr{d}", [17, 256]) for d in range(2)]
        ucat = [sb(f"ucat{d}", [128, 129]) for d in range(2)]
        ustr = [sb(f"ustr{d}", [128, 128]) for d in range(2)]
        mask = [sb(f"mask{d}", [128, 128]) for d in range(2)]
        gain = sb("gain", [128, 1])
        k.dma("sp", gain[:], self.ev_gla_g.v(0, self.ev_gla_g.t[j, :, :]))
        for d in range(2):
            k.op("pool", "memset", ap=lra[d][:], constant=1.0)
            k.dma("sp", lra[d].v(lra[d].t[0:16, :]), PT.v("all", PT.t[8 * 128 + d * 16:8 * 128 + (d + 1) * 16, :]))
            k.dma("sp", wlr[d][:], self.ev_wlr.v(0, self.ev_wlr.t[j, d, :, :]))
            k.dma("sp", ucat[d][:], self.c_ucat.v(0, self.c_ucat.t[d, :, :]))
            k.dma("sp", ustr[d][:], self.c_ustr.v(0, self.c_ustr.t[d, :, :]))
            k.dma("sp", mask[d][:], self.c_mask.v(0, self.c_mask.t[d, :, :]))
        ldf = [sb(f"ldf{i}", [128, 4, 128]) for i in range(2)]
        ldk = [sb(f"ldk{i}", [128, 768]) for i in range(2)]
        e1 = sb("e1", [128, 256])
        lnv = sb("lnv", [128, 256])
        EqT = sb("EqT", [128, 2, 129])
        EkT = sb("EkT", [128, 2, 128])
        Eend = sb("Eend", [128, 256])
        qin = sb("qin", [128, 2, 128], BF16)
        kin = sb("kin", [128, 2, 128], BF16)
        kend = sb("kend", [128, 256], BF16)
        vbf = sb("vbf", [128, 512], BF16)
        ATm = [sb(f"ATm{i}", [128, 128], BF16) for i in range(2)]
        S = sb("S", [128, 2, 128])
        Sbf = sb("Sbf", [128, 2, 128], BF16)
        ofw = [sb(f"ofw{i}", [128, 4, 128]) for i in range(2)]
        ofl = [sb(f"ofl{i}", [128, 4, 128]) for i in range(2)]
        gT = [sb(f"gT{i}", [128, 4, 128]) for i in range(2)]
        otot = [sb(f"otot{i}", [128, 128]) for i in range(2)]
        sq = sb("sq", [128, 128], BF16)
        rs = sb("rs", [128, 128])
        rstd = sb("rstd", [128, 128])
        sg = sb("sg", [128, 128])
        y1 = sb("y1", [128, 128])
        yst = [sb(f"yst{i}", [128, 4, 128], BF16) for i in range(2)]
        p_gk = k.psum(es, "gl_pgk", [128, 512])
        p_bT = k.psum(es, "gl_pbT", [128, 2, 256])
        p_be = k.psum(es, "gl_pbe", [128, 512])
        p_AT = [k.psum(es, f"gl_pAT{i}", [128, 512]) for i in range(2)]
        p_o = [k.psum(es, f"gl_po{i}", [128, 512]) for i in range(2)]
        p_up = k.psum(es, "gl_pup", [128, 2, 128])
        ih = 0
        for d in range(2):
            k.op("dve", "memset", ap=S[:], constant=0.0)
            k.op("pool", "memset", ap=Sbf[:], constant=0.0)
            ctx_tiles = list(range(nctx))
            main_tiles = list(range(nctx, NT))
            order = ctx_tiles + main_tiles if d == 0 else ctx_tiles[::-1] + main_tiles[::-1]
            for it, tix in enumerate(order):
                t0 = tix * 128
                isctx = tix < nctx
                need_out = (not isctx) or ctx_out
                lf, lk = ldf[it % 2], ldk[it % 2]
                k.dma("sp", lf[:], PT.v("all", PTr[:, 0:4, t0:t0 + 128]))
                k.dma("sp", lk[:], PK.v("all", PK.t[t0:t0 + 128, 0:768]))
                if d == 1 and need_out:
                    k.dma("sp", gT[it % 2][:], PT.v("all", PTr[:, 4:8, t0:t0 + 128]))
                    k.dma("sp", ofl[it % 2][:], self.OF.v("all", OFr[:, :, t0:t0 + 128]))
                k.op("pe", "matmul", out=p_gk.v(p_gk.t[:, 0:256]), lhsT=lra[d].v(lra[d].t[0:17, t0:t0 + 128]),
                     rhs=wlr[d][:], start=True, stop=True)
                k.op("act", "activation", out=e1[:], in_=p_gk.v(p_gk.t[:, 0:256]), func=AF.Exp, scale=-1.0)
                k.op("act", "activation", out=lnv[:], in_=e1[:], func=AF.Ln, bias=self.oneb[:, 0:1], scale=1.0)
                for pr in range(2):
                    k.op("pe", "matmul", out=p_bT.v(p_bT.t[:, pr, 0:129]), lhsT=lnv.v(lnv.t[:, pr * 128:(pr + 1) * 128]),
                         rhs=ucat[d][:], start=True, stop=True)
                k.op("pe", "matmul", out=p_be.v(p_be.t[:, 0:256]), lhsT=ustr[d][:], rhs=lnv[:], start=True, stop=True)
                k.op("act", "activation", out=EqT[:], in_=p_bT.v(p_bT.t[:, :, 0:129]), func=AF.Exp)
                k.op("act", "activation", out=EkT[:], in_=p_bT.v(p_bT.t[:, :, 0:128]), func=AF.Exp, scale=-1.0)
                k.op("act", "activation", out=Eend[:], in_=p_be.v(p_be.t[:, 0:256]), func=AF.Exp)
                k.op("dve", "scalar_tensor_tensor", out=qin[:], in0=lf.v(lf.t[:, 0:2, :]), scalar=0.125,
                     in1=EqT.v(EqT.t[:, :, 0:128]), op0=ALU.mult, op1=ALU.mult)
                k.op("pool", "tensor_tensor", out=kin[:], in0=lf.v(lf.t[:, 2:4, :]), in1=EkT[:], op=ALU.mult)
                k.op("dve", "tensor_tensor", out=kend[:], in0=lk.v(lk.t[:, 0:256]), in1=Eend[:], op=ALU.mult)
                k.op("pool", "tensor_copy", out=vbf[:], in_=lk.v(lk.t[:, 256:768]))
                for h in range(4):
                    pr, r = h // 2, (h % 2) * 64
                    if need_out:
                        pa, po, am = p_AT[ih % 2], p_o[ih % 2], ATm[ih % 2]
                        k.op("pe", "matmul", out=pa.v(pa.t[:, 0:128]), lhsT=kin.v(kin.t[r:r + 64, pr, :]),
                             rhs=qin.v(qin.t[r:r + 64, pr, :]), start=True, stop=True)
                        k.op("dve", "tensor_tensor", out=am[:], in0=pa.v(pa.t[:, 0:128]), in1=mask[d][:], op=ALU.mult)
                        k.op("pe", "matmul", out=po.v(po.t[:, 0:128]), lhsT=vbf.v(vbf.t[:, h * 128:(h + 1) * 128]),
                             rhs=am[:], start=True, stop=False)
                        k.op("pe", "matmul", out=po.v(po.t[:, 0:128]), lhsT=Sbf.v(Sbf.t[r:r + 64, pr, :]),
                             rhs=qin.v(qin.t[r:r + 64, pr, :]), start=False, stop=True)
                        if d == 0:
                            k.op("act", "copy", out=ofw[it % 2].v(ofw[it % 2].t[:, h, :]), in_=po.v(po.t[:, 0:128]))
                        else:
                            ot = otot[ih % 2]
                            k.op("dve", "tensor_tensor", out=ot[:], in0=po.v(po.t[:, 0:128]),
                                 in1=ofl[it % 2].v(ofl[it % 2].t[:, h, :]), op=ALU.add)
                            k.op("act", "activation", out=sq[:], in_=ot[:], func=AF.Square)
                            k.op("pe", "matmul", out=p_gk.v(p_gk.t[:, 256:384]), lhsT=self.ones_bf[:], rhs=sq[:],
                                 start=True, stop=True)
                            k.op("act", "activation", out=rs[:], in_=p_gk.v(p_gk.t[:, 256:384]), func=AF.Sqrt,
                                 scale=1.0 / 128, bias=self.epsb[:, 0:1])
                            k.op("dve", "reciprocal", out=rstd[:], in_=rs[:])
                            k.op("act", "activation", out=sg[:], in_=gT[it % 2].v(gT[it % 2].t[:, h, :]), func=AF.Silu)
                            k.op("dve", "scalar_tensor_tensor", out=y1[:], in0=ot[:], scalar=gain[:, 0:1],
                                 in1=rstd[:], op0=ALU.mult, op1=ALU.mult)
                            k.op("pool", "tensor_tensor", out=yst[it % 2].v(yst[it % 2].t[:, h, :]), in0=y1[:],
                                 in1=sg[:], op=ALU.mult)
                        ih += 1
                    k.op("pe", "matmul", out=p_up.v(p_up.t[r:r + 64, pr, :]), lhsT=kend.v(kend.t[:, h * 64:(h + 1) * 64]),
                         rhs=vbf.v(vbf.t[:, h * 128:(h + 1) * 128]), start=True, stop=True)
                for pr in range(2):
                    k.op("dve", "scalar_tensor_tensor", out=S.v(S.t[:, pr, :]), in0=S.v(S.t[:, pr, :]),
                         scalar=EqT.v(EqT.t[:, pr, 128:129]), in1=p_up.v(p_up.t[:, pr, :]), op0=ALU.mult, op1=ALU.add)
                k.op("act", "copy", out=Sbf[:], in_=S[:])
                if need_out:
                    if d == 0:
                        k.dma("pool", self.OF.v("all", OFr[:, :, t0:t0 + 128]), ofw[it % 2][:])
                    else:
                        k.dma("pool", YM.v("all", YMr[:, 0:4, t0:t0 + 128]), yst[it % 2][:])
            k.barrier()


M.gla = gla


OD_F = [(i * 128, 128) for i in range(20)]
OD_T = [(2560, 512, 0)]


def declare_odd(self):
    cfg = self.cfg
    no = max(cfg.depth // 2, 1)
    self.od_w_in = self.inp("od_w_in", [no, D, 3072])
    self.od_w_out = self.inp("od_w_out", [no, D, D])
    self.od_lam = self.inp("od_lamB", [no, 128, 256])
    self.od_diff_g = self.inp("od_diff_gT", [no, 128, 1])


M.declare_odd = declare_odd


def diffattn(self, li, j, PT, PK, YM, qc0, kc0, ym_row0, ctx_out):
    k, cfg = self.k, self.cfg
    T, L = cfg.T, cfg.L
    NT = T // 128
    N = 512
    lam_init = 0.8 - 0.6 * math.exp(-0.3 * li)
    with contextlib.ExitStack() as es:
        nb = self.nr_bufs(es, N, "da_")
        PTr = PT.t.rearrange("(c p) t -> p c t", p=128)
        lp = k.sbuf(es, "da_lp", [128, 256], F32)
        k.dma("sp", lp[:], self.od_lam.v(0, self.od_lam.t[j, :, :]))
        pr2 = k.sbuf(es, "da_pr2", [128, 2, 64], F32)
        sm = k.sbuf(es, "da_sm", [128, 2], F32)
        ex = k.sbuf(es, "da_ex", [128, 2], F32)
        neglam = k.sbuf(es, "da_nl", [128, 1], F32)
        for q in range(2):
            k.op("dve", "tensor_tensor", out=pr2.v(pr2.t[:, q, :]), in0=lp.v(lp.t[:, q * 128:q * 128 + 64]),
                 in1=lp.v(lp.t[:, q * 128 + 64:q * 128 + 128]), op=ALU.mult)
        k.op("dve", "reduce_sum", out=sm[:], in_=pr2[:], axis=mybir.AxisListType.X)
        k.op("act", "activation", out=ex[:], in_=sm[:], func=AF.Exp)
        k.op("dve", "tensor_tensor", out=neglam[:], in0=ex.v(ex.t[:, 1:2]), in1=ex.v(ex.t[:, 0:1]), op=ALU.subtract)
        k.op("dve", "tensor_scalar", out=neglam[:], in0=neglam[:], scalar1=-lam_init, scalar2=None, op0=ALU.add)
        g2 = k.sbuf(es, "da_g2", [128, 1], F32)
        k.dma("sp", g2[:], self.od_diff_g.v(0, self.od_diff_g.t[j, :, :]))
        k.op("dve", "tensor_scalar", out=g2[:], in0=g2[:], scalar1=1.0 - lam_init, scalar2=None, op0=ALU.mult)
        KT = k.sbuf(es, "da_KT", [128, 4, T], BF16)
        kraw = k.sbuf(es, "da_kraw", [128, N], F32)
        for c in range(4):
            for (t0, n, isctx) in tiles(cfg, N):
                k.dma("sp", kraw.v(kraw.t[:, 0:n]), PT.v("all", PT.t[(kc0 + c) * 128:(kc0 + c + 1) * 128, t0:t0 + n]))
                self.norm_rope(nb, kraw.v(kraw.t[:, 0:n]), n, KT.v(KT.t[:, c, t0:t0 + n]),
                               None, t0 - C, False, not isctx, None, 64)
        V = k.sbuf(es, "da_V", [128, NT, 512], BF16)
        vst = k.sbuf(es, "da_vst", [128, 2, 512], F32)
        for b0 in range(0, NT, 2):
            nbk = min(2, NT - b0)
            k.dma("sp", vst.v(vst.t[:, 0:nbk, :]),
                  PK.v("all", PK.t[b0 * 128:(b0 + nbk) * 128, 0:512].rearrange("(s p) c -> p s c", p=128)))
            k.op("dve" if (b0 // 2) % 2 == 0 else "pool", "tensor_copy", out=V.v(V.t[:, b0:b0 + nbk, :]),
                 in_=vst.v(vst.t[:, 0:nbk, :]))
        qraw = [k.sbuf(es, f"da_qraw{i}", [128, 4, N], F32) for i in range(2)]
        QT = [k.sbuf(es, f"da_QT{i}", [128, 4, 2, N], BF16) for i in range(2)]
        for q_ in QT:
            k.op("pool", "memset", ap=q_[:], constant=0.0)
        pS = [k.psum(es, f"da_pS{i}", [128, N], F32) for i in range(2)]
        pO = [k.psum(es, f"da_pO{i}", [128, N], F32) for i in range(2)]
        pZ = [k.psum(es, f"da_pZ{i}", [128, N], F32) for i in range(2)]
        pb = [k.sbuf(es, f"da_pb{i}", [128, N], BF16) for i in range(4)]
        rz = [k.sbuf(es, f"da_rz{i}", [128, N], F32) for i in range(2)]
        tt = [k.sbuf(es, f"da_tt{i}", [128, N], F32) for i in range(2)]
        osb = k.sbuf(es, "da_o", [128, N], F32)
        sq = k.sbuf(es, "da_sq", [128, N], BF16)
        y1 = k.sbuf(es, "da_y1", [128, N], BF16)
        ipb = 0
        for ti, (t0, n, isctx) in enumerate(tiles(cfg, N)):
            if isctx and not ctx_out:
                continue
            qr, qt = qraw[ti % 2], QT[ti % 2]
            k.dma("sp", qr.v(qr.t[:, :, 0:n]), PT.v("all", PTr[:, qc0:qc0 + 4, t0:t0 + n]))
            for c in range(4):
                src_ = qr.v(qr.t[:, c, 0:n]) if not isctx else [qr.v(qr.t[0:64, c, 0:n]), qr.v(qr.t[64:128, c, 0:n])]
                self.norm_rope(nb, src_, n,
                               [qt.v(qt.t[0:64, c, 0, 0:n]), qt.v(qt.t[64:128, c, 1, 0:n])],
                               None, t0 - C, False, not isctx, None, 64)
            kts = list(range(C // 128)) if isctx else list(range(NT))
            for h in range(4):
                for ii, kt in enumerate(kts):
                    for c in range(2):
                        r = c * 64
                        ps = pS[c]
                        k.op("pe", "matmul", out=ps.v(ps.t[:, 0:n]), lhsT=KT.v(KT.t[:, h, kt * 128:(kt + 1) * 128]),
                             rhs=qt.v(qt.t[:, h, c, 0:n]), start=True, stop=True)
                    pbs = []
                    for c in range(2):
                        pbuf = pb[ipb % 4]
                        ipb += 1
                        pbs.append(pbuf)
                        k.op("act", "activation", out=pbuf.v(pbuf.t[:, 0:n]), in_=pS[c].v(pS[c].t[:, 0:n]),
                             func=AF.Exp, scale=0.125)
                    for c in range(2):
                        k.op("pe", "matmul", out=pO[c].v(pO[c].t[:, 0:n]), lhsT=V.v(V.t[:, kt, h * 128:(h + 1) * 128]),
                             rhs=pbs[c].v(pbs[c].t[:, 0:n]), start=(ii == 0), stop=(ii == len(kts) - 1))
                        k.op("pe", "matmul", out=pZ[c].v(pZ[c].t[:, 0:n]), lhsT=self.ones_bf[:],
                             rhs=pbs[c].v(pbs[c].t[:, 0:n]), start=(ii == 0), stop=(ii == len(kts) - 1))
                for c in range(2):
                    k.op("dve", "reciprocal", out=rz[c].v(rz[c].t[:, 0:n]), in_=pZ[c].v(pZ[c].t[:, 0:n]))
                    k.op("dve", "tensor_tensor", out=tt[c].v(tt[c].t[:, 0:n]), in0=pO[c].v(pO[c].t[:, 0:n]),
                         in1=rz[c].v(rz[c].t[:, 0:n]), op=ALU.mult)
                k.op("dve", "scalar_tensor_tensor", out=osb.v(osb.t[:, 0:n]), in0=tt[1].v(tt[1].t[:, 0:n]),
                     scalar=neglam[:, 0:1], in1=tt[0].v(tt[0].t[:, 0:n]), op0=ALU.mult, op1=ALU.add)
                k.op("act", "activation", out=sq.v(sq.t[:, 0:n]), in_=osb.v(osb.t[:, 0:n]), func=AF.Square)
                ps = nb["ps"]
                k.op("pe", "matmul", out=ps.v(ps.t[:, 0:n]), lhsT=self.ones_bf[:], rhs=sq.v(sq.t[:, 0:n]),
                     start=True, stop=True)
                rs, rstd = nb["rs"], nb["rstd"]
                k.op("act", "activation", out=rs.v(rs.t[:, 0:n]), in_=ps.v(ps.t[:, 0:n]), func=AF.Sqrt,
                     scale=1.0 / 128, bias=self.epsb[:, 0:1])
                k.op("dve", "reciprocal", out=rstd.v(rstd.t[:, 0:n]), in_=rs.v(rs.t[:, 0:n]))
                k.op("dve", "scalar_tensor_tensor", out=y1.v(y1.t[:, 0:n]), in0=osb.v(osb.t[:, 0:n]),
                     scalar=g2[:, 0:1], in1=rstd.v(rstd.t[:, 0:n]), op0=ALU.mult, op1=ALU.mult)
                row = ym_row0 + h * 128
                k.dma("pool", YM.v("all", YM.t[row:row + 128, t0:t0 + n]), y1.v(y1.t[:, 0:n]))


M.diffattn = diffattn


def odd_layer(self, li, last):
    k = self.k
    j = li // 2
    self.in_proj(li, self.od_w_in, j, 3072, OD_F, OD_T, self.PT, self.PK)
    k.barrier()
    if self.cfg.phases is None or "hyena" in self.cfg.phases:
        self.hyena(j, self.PT, self.YM, not last)
    else:
        self.zero_ym(0, 512)
    k.barrier()
    self.diffattn(li, j, self.PT, self.PK, self.YM, 12, 16, 512, not last)
    k.barrier()
    self.out_proj(li, self.od_w_out, j, self.YM)
    k.barrier()


M.odd_layer = odd_layer


def hy_dims(Lseg):
    N = 2 * Lseg
    lg = int(round(math.log2(N)))
    N2 = 1 << ((lg + 1) // 2)
    N1 = N // N2
    H1 = N1 // 2
    NSQ = 256 // max(N1, N2)
    return N, N1, N2, H1, NSQ


def hy_consts(Lseg, pfx):
    N, N1, N2, H1, NSQ = hy_dims(Lseg)
    f64 = np.float64
    c = {}
    n1 = np.arange(H1, dtype=f64)[:, None]
    k1 = np.arange(N1, dtype=f64)[None, :]
    a = 2 * np.pi * n1 * k1 / N1
    c["F1cat"] = np.concatenate([np.cos(a), -np.sin(a)], 1)
    n2 = np.arange(N2, dtype=f64)[:, None]
    a = 2 * np.pi * n2 * k1 / N
    twc, tws = np.cos(a), -np.sin(a)
    c["TwA"] = np.tile(np.concatenate([twc, twc], 1)[:, None, :], (1, NSQ, 1))
    c["TwB"] = np.tile(np.concatenate([-tws, tws], 1)[:, None, :], (1, NSQ, 1))
    k2 = np.arange(N2, dtype=f64)[None, :]
    a = 2 * np.pi * n2 * k2 / N2
    c["F2c"], c["F2s"], c["F2sn"] = np.cos(a), -np.sin(a), np.sin(a)
    c["G2cat"] = np.concatenate([np.cos(a), np.sin(a)], 1)
    c["G2cat2"] = np.concatenate([-np.sin(a), np.cos(a)], 1)
    kk1 = np.arange(N1, dtype=f64)[:, None]
    nn2 = np.arange(N2, dtype=f64)[None, :]
    a = 2 * np.pi * kk1 * nn2 / N
    tc, ts = np.cos(a), np.sin(a)
    c["TwAi"] = np.tile(np.concatenate([tc, tc], 1)[:, None, :], (1, NSQ, 1))
    c["TwBi"] = np.tile(np.concatenate([-ts, ts], 1)[:, None, :], (1, NSQ, 1))
    nn1 = np.arange(H1, dtype=f64)[None, :]
    a = 2 * np.pi * kk1 * nn1 / N1
    c["G1c"], c["G1sn"] = np.cos(a) / N, -np.sin(a) / N
    pos = np.arange(Lseg, dtype=np.float32)
    t = pos / np.float32(max(Lseg - 1, 1))
    w = np.float32(2 * math.pi) * pos / np.float32(Lseg)
    bands = np.linspace(1e-4, 15, 16, dtype=np.float32)
    z = np.concatenate([t[:, None], np.cos(w[:, None] * bands), -np.sin(w[:, None] * bands)], -1).astype(np.float32)
    c["zT"] = z.T
    deltas = np.abs(np.linspace(math.log(1e-2) / 0.3, math.log(1e-2) / 1.5, 512, dtype=np.float32))
    c["decT"] = np.exp(-t[None, :] * deltas[:, None])
    return {pfx + k_: np.ascontiguousarray(v.astype(np.float32)) for k_, v in c.items()}


HY_SHAPES = lambda N, N1, N2, H1, NSQ, Lseg: {
    "F1cat": [H1, 2 * N1], "TwA": [N2, NSQ, 2 * N1], "TwB": [N2, NSQ, 2 * N1], "F2c": [N2, N2], "F2s": [N2, N2],
    "F2sn": [N2, N2], "G2cat": [N2, 2 * N2], "G2cat2": [N2, 2 * N2], "TwAi": [N1, NSQ, 2 * N2],
    "TwBi": [N1, NSQ, 2 * N2], "G1c": [N1, H1], "G1sn": [N1, H1], "zT": [33, Lseg], "decT": [512, Lseg]}


def declare_hy(self):
    cfg = self.cfg
    no = max(cfg.depth // 2, 1)
    self.hyc = {}
    for pfx, Lseg in (("hm_", cfg.L), ("hc_", C)):
        dims = hy_dims(Lseg)
        for nm, sh in HY_SHAPES(*dims, Lseg).items():
            self.hyc[pfx + nm] = self.inp(pfx + nm, sh)
    self.od_conv_w = self.inp("od_conv_wT", [no, 128, 12, 3])
    self.od_conv_b = self.inp("od_conv_bT", [no, 128, 12])
    self.od_f_w1 = self.inp("od_f_w1", [no, 33, 64])
    self.od_f_b1 = self.inp("od_f_b1T", [no, 64, 1])
    self.od_f_w2 = self.inp("od_f_w2", [no, 64, 64])
    self.od_f_b2 = self.inp("od_f_b2T", [no, 64, 1])
    self.od_f_w3 = self.inp("od_f_w3", [no, 64, 2048])
    self.od_hy_bias = self.inp("od_hy_biasB", [no, 128, 1024])
    self.UC = self.scratch("UC", [1536, cfg.T], dbg=True)
    self.HT = self.scratch("HT", [2048, cfg.L], dbg=True)
    self.HTc = self.scratch("HTc", [2048, C], dbg=True)


M.declare_hy = declare_hy


def hy_shortconv(self, j, PT, ctx_out):
    k, cfg = self.k, self.cfg
    BL = 2048
    with contextlib.ExitStack() as es:
        cw = k.sbuf(es, "sc_w", [128, 12, 3], F32)
        cb = k.sbuf(es, "sc_b", [128, 12], F32)
        k.dma("sp", cw[:], self.od_conv_w.v(0, self.od_conv_w.t[j, :, :, :]))
        k.dma("sp", cb[:], self.od_conv_b.v(0, self.od_conv_b.t[j, :, :]))
        ub = [k.sbuf(es, f"sc_u{i}", [128, BL + 2], F32) for i in range(2)]
        ac = [k.sbuf(es, f"sc_a{i}", [128, BL], F32) for i in range(2)]
        it = 0
        segs = [(C, cfg.L)] + ([(0, C)] if ctx_out else [])
        for (toff, Lseg) in segs:
            for c in range(12):
                for b0 in range(0, Lseg, BL):
                    n = min(BL, Lseg - b0)
                    u, a = ub[it % 2], ac[it % 2]
                    it += 1
                    lo = max(b0 - 1, 0)
                    hi = min(b0 + n + 1, Lseg)
                    if b0 == 0:
                        k.op("pool", "memset", ap=u.v(u.t[:, 0:1]), constant=0.0)
                    if b0 + n == Lseg:
                        k.op("pool", "memset", ap=u.v(u.t[:, n + 1:n + 2]), constant=0.0)
                    k.dma("sp", u.v(u.t[:, lo - b0 + 1:hi - b0 + 1]),
                          PT.v("all", PT.t[c * 128:(c + 1) * 128, toff + lo:toff + hi]))
                    k.op("dve", "tensor_scalar", out=a.v(a.t[:, 0:n]), in0=u.v(u.t[:, 0:n]), scalar1=cw.v(cw.t[:, c, 0:1]),
                         scalar2=cb.v(cb.t[:, c:c + 1]), op0=ALU.mult, op1=ALU.add)
                    k.op("dve", "scalar_tensor_tensor", out=a.v(a.t[:, 0:n]), in0=u.v(u.t[:, 1:n + 1]),
                         scalar=cw.v(cw.t[:, c, 1:2]), in1=a.v(a.t[:, 0:n]), op0=ALU.mult, op1=ALU.add)
                    k.op("dve", "scalar_tensor_tensor", out=a.v(a.t[:, 0:n]), in0=u.v(u.t[:, 2:n + 2]),
                         scalar=cw.v(cw.t[:, c, 2:3]), in1=a.v(a.t[:, 0:n]), op0=ALU.mult, op1=ALU.add)
                    k.dma("pool", self.UC.v("all", self.UC.t[c * 128:(c + 1) * 128, toff + b0:toff + b0 + n]),
                          a.v(a.t[:, 0:n]))


M.hy_shortconv = hy_shortconv


def hy_filters(self, j, pfx, Lseg, HT):
    k = self.k
    N = 512
    with contextlib.ExitStack() as es:
        sb = lambda nm, sh, dt=F32: k.sbuf(es, "hf_" + nm, sh, dt)
        w1, w2, w3 = sb("w1", [33, 64]), sb("w2", [64, 64]), sb("w3", [64, 2048])
        bb = sb("bb", [64, 2])
        bh, bq = sb("bh", [64, 2]), sb("bq", [64, 2])
        k.dma("sp", w1[:], self.od_f_w1.v(0, self.od_f_w1.t[j, :, :]))
        k.dma("sp", w2[:], self.od_f_w2.v(0, self.od_f_w2.t[j, :, :]))
        k.dma("sp", w3[:], self.od_f_w3.v(0, self.od_f_w3.t[j, :, :]))
        k.dma("sp", bb.v(bb.t[:, 0:1]), self.od_f_b1.v(0, self.od_f_b1.t[j, :, :]))
        k.dma("sp", bb.v(bb.t[:, 1:2]), self.od_f_b2.v(0, self.od_f_b2.t[j, :, :]))
        k.op("dve", "tensor_scalar", out=bh[:], in0=bb[:], scalar1=0.5, scalar2=None, op0=ALU.mult)
        k.op("dve", "tensor_scalar", out=bq[:], in0=bb[:], scalar1=0.25, scalar2=None, op0=ALU.mult)
        zT = [sb(f"zT{i}", [33, N]) for i in range(2)]
        dect = [sb(f"dec{i}", [128, 4, N]) for i in range(2)]
        a1, a2, tq = sb("a1", [64, N]), sb("a2", [64, N]), sb("tq", [64, N])
        hid = [sb(f"hid{i}", [64, N]) for i in range(2)]
        hsb = [sb(f"hsb{i}", [128, N]) for i in range(3)]
        p12 = [k.psum(es, f"hf_p{i}", [64, N]) for i in range(2)]
        ph = [k.psum(es, f"hf_ph{i}", [128, N]) for i in range(3)]
        zc, dc = self.hyc[pfx + "zT"], self.hyc[pfx + "decT"]
        dcr = dc.t.rearrange("(c p) t -> p c t", p=128)
        io = 0
        for ti, t0 in enumerate(range(0, Lseg, N)):
            n = min(N, Lseg - t0)
            z, de = zT[ti % 2], dect[ti % 2]
            k.dma("sp", z.v(z.t[:, 0:n]), zc.v(0, zc.t[:, t0:t0 + n]))
            k.dma("sp", de.v(de.t[:, :, 0:n]), dc.v(0, dcr[:, :, t0:t0 + n]))
            cur = z.v(z.t[:, 0:n])
            for layer, (w, kk) in enumerate(((w1, 33), (w2, 64))):
                p = p12[layer]
                k.op("pe", "matmul", out=p.v(p.t[:, 0:n]), lhsT=w.v(w.t[0:kk, :]), rhs=cur, start=True, stop=True)
                k.op("act", "activation", out=a1.v(a1.t[:, 0:n]), in_=p.v(p.t[:, 0:n]), func=AF.Sin,
                     bias=bh.v(bh.t[:, layer:layer + 1]), scale=0.5)
                k.op("act", "activation", out=a2.v(a2.t[:, 0:n]), in_=p.v(p.t[:, 0:n]), func=AF.Sin,
                     bias=bq.v(bq.t[:, layer:layer + 1]), scale=0.25)
                k.op("dve", "tensor_tensor", out=tq.v(tq.t[:, 0:n]), in0=a2.v(a2.t[:, 0:n]), in1=a2.v(a2.t[:, 0:n]),
                     op=ALU.mult)
                k.op("dve", "tensor_scalar", out=tq.v(tq.t[:, 0:n]), in0=tq.v(tq.t[:, 0:n]), scalar1=-2.0, scalar2=1.0,
                     op0=ALU.mult, op1=ALU.add)
                hd = hid[layer]
                k.op("dve", "scalar_tensor_tensor", out=hd.v(hd.t[:, 0:n]), in0=a1.v(a1.t[:, 0:n]), scalar=2.0,
                     in1=tq.v(tq.t[:, 0:n]), op0=ALU.mult, op1=ALU.mult)
                cur = hd.v(hd.t[:, 0:n])
            for cc in range(16):
                p, hs = ph[io % 3], hsb[io % 3]
                io += 1
                k.op("pe", "matmul", out=p.v(p.t[:, 0:n]), lhsT=w3.v(w3.t[:, cc * 128:(cc + 1) * 128]), rhs=cur,
                     start=True, stop=True)
                k.op("dve", "tensor_tensor", out=hs.v(hs.t[:, 0:n]), in0=p.v(p.t[:, 0:n]),
                     in1=de.v(de.t[:, cc % 4, 0:n]), op=ALU.mult)
                k.dma("pool", HT.v("all", HT.t[cc * 128:(cc + 1) * 128, t0:t0 + n]), hs.v(hs.t[:, 0:n]))


M.hy_filters = hy_filters


def hy_conv(self, j, pfx, Lseg, toff, HT, YM):
    k = self.k
    N, N1, N2, H1, NSQ = hy_dims(Lseg)
    NSLOT = 2
    with contextlib.ExitStack() as es:
        sb = lambda nm, sh, dt=F32: k.sbuf(es, "hv_" + nm, sh, dt)
        cst = {}
        for nm, sh in HY_SHAPES(N, N1, N2, H1, NSQ, Lseg).items():
            if nm in ("zT", "decT"):
                continue
            cst[nm] = sb(nm, sh)
            d = self.hyc[pfx + nm]
            k.dma("sp", cst[nm][:], d.v(0, d.t))
        hb = sb("hb", [128, 1024])
        k.dma("sp", hb[:], self.od_hy_bias.v(0, self.od_hy_bias.t[j, :, :]))
        slots = []
        for sl in range(NSLOT):
            B_ = {}
            B_["hfl"] = [sb(f"hfl{sl}_{i}", [H1, NSQ, N2]) for i in range(4)]
            B_["dat"] = [sb(f"dat{sl}_{q}", [H1, NSQ, N2]) for q in range(3)]
            B_["Xs"] = [sb(f"Xs{sl}_{i}", [N2, NSQ, 2 * N1]) for i in range(2)]
            B_["KA"] = [sb(f"KA{sl}_{i}", [N2, NSQ, 2 * N1]) for i in range(2)]
            B_["KB"] = [sb(f"KB{sl}_{i}", [N2, NSQ, 2 * N1]) for i in range(2)]
            B_["B"] = sb(f"B{sl}", [N2, NSQ, 2 * N1])
            B_["tmf"] = sb(f"tmf{sl}", [N2, NSQ, 2 * N1])
            B_["Y"] = sb(f"Y{sl}", [N2, NSQ, 2 * N1])
            B_["D"] = sb(f"D{sl}", [N1, NSQ, 2 * N2])
            B_["tmi"] = sb(f"tmi{sl}", [N1, NSQ, 2 * N2])
            B_["zmid"] = sb(f"zmid{sl}", [H1, NSQ, N2])
            B_["zout"] = sb(f"zout{sl}", [H1, NSQ, N2], BF16)
            B_["pA"] = k.psum(es, f"hv_pA{sl}", [128, 512])
            B_["pX"] = k.psum(es, f"hv_pX{sl}", [128, 512])
            B_["pC"] = k.psum(es, f"hv_pC{sl}", [128, 512])
            B_["pY"] = k.psum(es, f"hv_pY{sl}", [128, 512])
            B_["alt"] = 0
            slots.append(B_)

        def v3(tile_, P, W):
            return tile_.t[0:P, 0:NSQ * W].rearrange("p (s w) -> p s w", s=NSQ)

        def add_eng(S_):
            S_["alt"] += 1
            return "pool" if S_["alt"] % 3 else "dve"

        def fwd_fft(S_, zb):
            pa, px, B, tm = S_["pA"], S_["pX"], S_["B"], S_["tmf"]
            A3 = v3(pa, N2, 2 * N1)
            X3 = v3(px, N2, 2 * N1)
            for s in range(NSQ):
                k.op("pe", "matmul", out=pa.v(A3[:, s, :]), lhsT=zb.v(zb.t[:, s, :]), rhs=cst["F1cat"][:],
                     start=True, stop=True)
            yield
            k.op("dve", "tensor_tensor", out=B[:], in0=pa.v(A3), in1=cst["TwA"][:], op=ALU.mult)
            k.op("dve", "tensor_tensor", out=tm.v(tm.t[:, :, 0:N1]), in0=pa.v(A3[:, :, N1:2 * N1]),
                 in1=cst["TwB"].v(cst["TwB"].t[:, :, 0:N1]), op=ALU.mult)
            k.op("dve", "tensor_tensor", out=tm.v(tm.t[:, :, N1:2 * N1]), in0=pa.v(A3[:, :, 0:N1]),
                 in1=cst["TwB"].v(cst["TwB"].t[:, :, N1:2 * N1]), op=ALU.mult)
            yield
            k.op(add_eng(S_), "tensor_tensor", out=B[:], in0=B[:], in1=tm[:], op=ALU.add)
            yield
            Br, Bi = B.v(B.t[:, :, 0:N1]), B.v(B.t[:, :, N1:2 * N1])
            k.op("pe", "matmul", out=px.v(X3[:, :, 0:N1]), lhsT=cst["F2c"][:], rhs=Br, start=True, stop=False)
            k.op("pe", "matmul", out=px.v(X3[:, :, 0:N1]), lhsT=cst["F2sn"][:], rhs=Bi, start=False, stop=True)
            k.op("pe", "matmul", out=px.v(X3[:, :, N1:2 * N1]), lhsT=cst["F2s"][:], rhs=Br, start=True, stop=False)
            k.op("pe", "matmul", out=px.v(X3[:, :, N1:2 * N1]), lhsT=cst["F2c"][:], rhs=Bi, start=False, stop=True)
            yield

        def inv_fft(S_, Y):
            pC, pY, Dt, tmi = S_["pC"], S_["pY"], S_["D"], S_["tmi"]
            C3 = v3(pC, N1, 2 * N2)
            for s in range(NSQ):
                k.op("pe", "matmul", out=pC.v(C3[:, s, :]), lhsT=Y.v(Y.t[:, s, 0:N1]), rhs=cst["G2cat"][:],
                     start=True, stop=False)
                k.op("pe", "matmul", out=pC.v(C3[:, s, :]), lhsT=Y.v(Y.t[:, s, N1:2 * N1]), rhs=cst["G2cat2"][:],
                     start=False, stop=True)
            yield
            k.op("dve", "tensor_tensor", out=Dt[:], in0=pC.v(C3), in1=cst["TwAi"][:], op=ALU.mult)
            k.op("dve", "tensor_tensor", out=tmi.v(tmi.t[:, :, 0:N2]), in0=pC.v(C3[:, :, N2:2 * N2]),
                 in1=cst["TwBi"].v(cst["TwBi"].t[:, :, 0:N2]), op=ALU.mult)
            k.op("dve", "tensor_tensor", out=tmi.v(tmi.t[:, :, N2:2 * N2]), in0=pC.v(C3[:, :, 0:N2]),
                 in1=cst["TwBi"].v(cst["TwBi"].t[:, :, N2:2 * N2]), op=ALU.mult)
            yield
            k.op(add_eng(S_), "tensor_tensor", out=Dt[:], in0=Dt[:], in1=tmi[:], op=ALU.add)
            yield
            Y3 = pY.t[0:H1, 0:NSQ * N2].rearrange("p (s w) -> p s w", s=NSQ)
            k.op("pe", "matmul", out=pY.v(Y3), lhsT=cst["G1c"][:], rhs=Dt.v(Dt.t[:, :, 0:N2]), start=True, stop=False)
            k.op("pe", "matmul", out=pY.v(Y3), lhsT=cst["G1sn"][:], rhs=Dt.v(Dt.t[:, :, N2:2 * N2]), start=False, stop=True)
            yield

        def blk(dt_, row0):
            return dt_.t[row0:row0 + NSQ, :].rearrange("s (a b) -> a s b", b=N2)

        def group(g, S_):
            ch0 = g * NSQ
            dd = S_["dat"]
            Xs, KA, KB = S_["Xs"], S_["KA"], S_["KB"]
            px = S_["pX"]
            X3 = v3(px, N2, 2 * N1)
            pY = S_["pY"]
            Y3 = pY.t[0:H1, 0:NSQ * N2].rearrange("p (s w) -> p s w", s=NSQ)
            for q in range(3):
                k.dma("sp", dd[q][:], self.UC.v("all", self.UC.t[q * 512 + ch0:q * 512 + ch0 + NSQ,
                                                                 toff:toff + Lseg].rearrange("s (a b) -> a s b", b=N2)))
            for o in range(2):
                for dr in range(2):
                    hf = S_["hfl"][o * 2 + dr]
                    k.dma("sp", hf[:], HT.v("all", blk(HT, o * 1024 + dr * 512 + ch0)))
                    if dr == 1:
                        k.op("pool", "memset", ap=hf.v(hf.t[0:1, :, 0:1]), constant=0.0)
            yield
            for o in range(2):
                for dr in range(2):
                    hf = S_["hfl"][o * 2 + dr]
                    yield from fwd_fft(S_, hf)
                    k.op("act", "copy", out=Xs[dr][:], in_=px.v(X3))
                    yield
                ka, kb = KA[o], KB[o]
                k.op("pool", "tensor_tensor", out=ka.v(ka.t[:, :, 0:N1]), in0=Xs[0].v(Xs[0].t[:, :, 0:N1]),
                     in1=Xs[1].v(Xs[1].t[:, :, 0:N1]), op=ALU.add)
                for s in range(NSQ):
                    ci = o * 512 + ch0 + s
                    k.op("act", "activation", out=ka.v(ka.t[:, s, 0:N1]), in_=ka.v(ka.t[:, s, 0:N1]), func=AF.Identity,
                         bias=hb.v(hb.t[0:N2, ci:ci + 1]), scale=1.0)
                k.op("act", "copy", out=ka.v(ka.t[:, :, N1:2 * N1]), in_=ka.v(ka.t[:, :, 0:N1]))
                yield
                k.op("pool", "tensor_tensor", out=kb.v(kb.t[:, :, N1:2 * N1]), in0=Xs[0].v(Xs[0].t[:, :, N1:2 * N1]),
                     in1=Xs[1].v(Xs[1].t[:, :, N1:2 * N1]), op=ALU.subtract)
                k.op("act", "activation", out=kb.v(kb.t[:, :, 0:N1]), in_=kb.v(kb.t[:, :, N1:2 * N1]), func=AF.Copy,
                     scale=-1.0)
                yield
            zcur = dd[0]
            Yt, tm = S_["Y"], S_["tmf"]
            for o in range(2):
                yield from fwd_fft(S_, zcur)
                ka, kb = KA[o], KB[o]
                k.op("dve", "tensor_tensor", out=Yt[:], in0=px.v(X3), in1=ka[:], op=ALU.mult)
                k.op("dve", "tensor_tensor", out=tm.v(tm.t[:, :, 0:N1]), in0=px.v(X3[:, :, N1:2 * N1]),
                     in1=kb.v(kb.t[:, :, 0:N1]), op=ALU.mult)
                k.op("dve", "tensor_tensor", out=tm.v(tm.t[:, :, N1:2 * N1]), in0=px.v(X3[:, :, 0:N1]),
                     in1=kb.v(kb.t[:, :, N1:2 * N1]), op=ALU.mult)
                yield
                k.op(add_eng(S_), "tensor_tensor", out=Yt[:], in0=Yt[:], in1=tm[:], op=ALU.add)
                yield
                yield from inv_fft(S_, Yt)
                if o == 0:
                    k.op("dve", "tensor_tensor", out=S_["zmid"][:], in0=pY.v(Y3), in1=dd[1][:], op=ALU.mult)
                    zcur = S_["zmid"]
                else:
                    zo = S_["zout"]
                    k.op("dve", "tensor_tensor", out=zo[:], in0=pY.v(Y3), in1=dd[2][:], op=ALU.mult)
                    k.dma("pool", YM.v("all", YM.t[ch0:ch0 + NSQ, toff:toff + Lseg].rearrange("s (a b) -> a s b", b=N2)),
                          zo[:])
                yield

        ngroups = 512 // NSQ
        nxt = 0
        active = []
        for sl in range(NSLOT):
            if nxt < ngroups:
                active.append([group(nxt, slots[sl]), sl])
                nxt += 1
        while active:
            for ent in list(active):
                try:
                    next(ent[0])
                except StopIteration:
                    if nxt < ngroups:
                        ent[0] = group(nxt, slots[ent[1]])
                        nxt += 1
                    else:
                        active.remove(ent)


M.hy_conv = hy_conv


def hyena(self, j, PT, YM, ctx_out):
    k, cfg = self.k, self.cfg
    self.hy_shortconv(j, PT, ctx_out)
    k.barrier()
    self.hy_filters(j, "hm_", cfg.L, self.HT)
    k.barrier()
    if ctx_out:
        self.hy_filters(j, "hc_", C, self.HTc)
        k.barrier()
    self.hy_conv(j, "hm_", cfg.L, C, self.HT, YM)
    k.barrier()
    if ctx_out:
        self.hy_conv(j, "hc_", C, 0, self.HTc, YM)
        k.barrier()


M.hyena = hyena

SEQ = 8192
DEPTH = 4
BATCH = 4


def host_inputs(inp, b, L, depth):
    d = {}
    d["x"] = np.ascontiguousarray(inp["x"][b])
    d["ctx"] = np.ascontiguousarray(inp["ctx"][b])
    d["cc"] = np.ascontiguousarray(np.stack([inp["c"][b], inp["c_ctx"]], -1).reshape(8, 128, 2).transpose(1, 0, 2))
    d["w_ada"] = inp["w_ada"]
    d["b_adaT"] = np.ascontiguousarray(inp["b_ada"].reshape(depth, 48, 128).transpose(0, 2, 1))
    d["norm_gT"] = np.ascontiguousarray(inp["norm_g"].reshape(depth, 32, 128).transpose(0, 2, 1))
    d["w_mlp_in"] = inp["w_mlp_in"]
    d["w_mlp_out"] = inp["w_mlp_out"]
    d["ev_w_in"] = inp["ev_w_in"]
    d["ev_w_out"] = inp["ev_w_out"]
    g = inp["ev_qk_g"]
    d["ev_qk_gT"] = np.ascontiguousarray(np.tile(g, (1, 1, 2)).transpose(0, 2, 1))
    d["od_w_in"] = inp["od_w_in"]
    d["od_w_out"] = inp["od_w_out"]
    no = inp["od_lam"].shape[0]
    d["od_lamB"] = np.ascontiguousarray(np.broadcast_to(inp["od_lam"].reshape(no, 1, 256), (no, 128, 256)))
    d["od_diff_gT"] = np.ascontiguousarray(inp["od_diff_g"][:, :, None])
    d["od_conv_wT"] = np.ascontiguousarray(
        inp["od_conv_w"].transpose(0, 2, 1).reshape(no, 12, 128, 3).transpose(0, 2, 1, 3))
    d["od_conv_bT"] = np.ascontiguousarray(inp["od_conv_b"].reshape(no, 12, 128).transpose(0, 2, 1))
    d["od_f_w1"] = inp["od_f_w1"]
    d["od_f_w2"] = inp["od_f_w2"]
    d["od_f_w3"] = inp["od_f_w3"]
    d["od_f_b1T"] = np.ascontiguousarray(inp["od_f_b1"][:, :, None])
    d["od_f_b2T"] = np.ascontiguousarray(inp["od_f_b2"][:, :, None])
    d["od_hy_biasB"] = np.ascontiguousarray(np.broadcast_to(inp["od_hy_bias"].reshape(no, 1, 1024), (no, 128, 1024)))
    d["ev_wlr_aug"] = np.ascontiguousarray(np.concatenate([inp["ev_w_lr"], inp["ev_b_lr"][:, :, None, :]], axis=2))
    d["ev_gla_gT"] = np.ascontiguousarray(inp["ev_gla_g"][:, :, None])
    return d


def const_inputs(L):
    d = {}
    d.update(hy_consts(L, "hm_"))
    d.update(hy_consts(C, "hc_"))
    d.update(gla_consts())
    d.update(host_consts())
    d["rope"] = rope_tables(L)
    d["rotm"] = rot_matrix()
    d["blk64"] = block_ones(64)
    return d


def kernel(**inputs):
    inp = {k_: np.asarray(v) for k_, v in inputs.items()}
    B, L, _ = inp["x"].shape
    depth = inp["w_ada"].shape[0]
    cfg = Cfg(L=L, depth=depth, debug=False)
    mm = M(cfg)
    nc = mm.build2()
    consts = const_inputs(L)
    n_cores = 8
    per_b = [dict(host_inputs(inp, b, L, depth), **consts) for b in range(B)]
    in_maps = [per_b[i % B] for i in range(n_cores)]
    res = run_bass_kernel_spmd(nc, in_maps, core_ids=list(range(n_cores)))
    out = np.stack([np.asarray(res.results[b]["out"]) for b in range(B)], axis=0)
    return out.astype(np.float32, copy=False)
```

```python
import math
import contextlib
import numpy as np
import concourse.bass as bass
import concourse.mybir as mybir
from concourse.bass_utils import run_bass_kernel_spmd

F32 = mybir.dt.float32
BF16 = mybir.dt.bfloat16
AF = mybir.ActivationFunctionType
ALU = mybir.AluOpType


class Buf:
    __slots__ = ("w", "r", "name")

    def __init__(self, name=""):
        self.w = None
        self.r = {}
        self.name = name


class View:
    __slots__ = ("ap", "bufs")

    def __init__(self, ap, bufs):
        self.ap = ap
        self.bufs = tuple(bufs)


class Tile:
    def __init__(self, t, name, nsub=0):
        self.t = t
        self.buf = Buf(name)
        self.subs = [Buf(f"{name}.{i}") for i in range(nsub)]

    def __getitem__(self, idx):
        return View(self.t[idx], (self.buf,))

    def v(self, ap):
        return View(ap, (self.buf,))

    def vs(self, ap, idxs):
        return View(ap, [self.subs[i] for i in idxs])


class DTile:
    def __init__(self, ap, name):
        self.t = ap
        self.name = name
        self.bufs = {}

    def b(self, key):
        if key not in self.bufs:
            self.bufs[key] = Buf(f"{self.name}:{key}")
        return self.bufs[key]

    def v(self, keys, ap):
        if not isinstance(keys, (list, tuple)):
            keys = [keys]
        return View(ap, [self.b(k) for k in keys])


class Eng:
    def __init__(self, name, h, semidx):
        self.name = name
        self.h = h
        self.semidx = semidx
        self.count = 0
        self.waited = {}


class K:
    WRITE_KEYS = ("out", "accum_out", "ap")

    def __init__(self, nc, es, n_dma=24):
        self.nc = nc
        self.es = es
        self.sems = []
        self.E = {}
        for name, h in [("pe", nc.tensor), ("act", nc.scalar), ("dve", nc.vector),
                        ("pool", nc.gpsimd), ("sp", nc.sync)]:
            self.sems.append(es.enter_context(nc.semaphore("s_" + name)))
            self.E[name] = Eng(name, h, len(self.sems) - 1)
        self.dma_sem = []
        self.dma_val = []
        for i in range(2 * n_dma):
            self.sems.append(es.enter_context(nc.semaphore(f"d{i}")))
            self.dma_sem.append(len(self.sems) - 1)
            self.dma_val.append(0)
        self.n_dma = n_dma
        self.dma_next = {"sp": 0, "pool": 0, "act": 0}
        self.ninst = 0

    def sbuf(self, es, name, shape, dtype, nsub=0):
        self.nalloc = getattr(self, "nalloc", 0) + 1
        name = f"sb{self.nalloc}_{name}"
        return Tile(es.enter_context(self.nc.sbuf_tensor(name, list(shape), dtype)), name, nsub)

    def psum(self, es, name, shape, dtype=F32, nsub=0):
        self.nalloc = getattr(self, "nalloc", 0) + 1
        name = f"ps{self.nalloc}_{name}"
        return Tile(es.enter_context(self.nc.psum_tensor(name, list(shape), dtype)), name, nsub)

    def _wait(self, E, toks):
        need = {}
        for s, v in toks:
            if v > need.get(s, 0):
                need[s] = v
        for s, v in need.items():
            if E.name == "pe" and s == E.semidx:
                continue
            if E.waited.get(s, 0) < v:
                E.h.wait_ge(self.sems[s], v)
                E.waited[s] = v

    @staticmethod
    def _deps(reads, writes):
        toks = []
        for b in reads:
            if b.w is not None:
                toks.append(b.w)
        for b in writes:
            if b.w is not None:
                toks.append(b.w)
            toks.extend(b.r.items())
        return toks

    @staticmethod
    def _commit(tok, reads, writes):
        s, v = tok
        for b in reads:
            if b.r.get(s, 0) < v:
                b.r[s] = v
        for b in writes:
            b.w = tok
            b.r = {}

    def op(self, eng, name, _r=(), _w=(), **kw):
        E = self.E[eng]
        reads, writes, real = [], [], {}
        for k, v in kw.items():
            if isinstance(v, View):
                (writes if k in self.WRITE_KEYS else reads).extend(v.bufs)
                real[k] = v.ap
            else:
                real[k] = v
        for v in _r:
            reads.extend(v.bufs if isinstance(v, View) else [v])
        for v in _w:
            writes.extend(v.bufs if isinstance(v, View) else [v])
        self._wait(E, self._deps(reads, writes))
        ins = getattr(E.h, name)(**real)
        E.count += 1
        ins.then_inc(self.sems[E.semidx], 1)
        self._commit((E.semidx, E.count), reads, writes)
        self.ninst += 1
        return ins

    def dma(self, q, out, in_, **kw):
        E = self.E[q]
        i0 = self.dma_next[q]
        self.dma_next[q] = (i0 + 1) % self.n_dma
        i = i0 + (self.n_dma if q == "pool" else 0)
        s = self.dma_sem[i]
        toks = self._deps(in_.bufs, out.bufs)
        if self.dma_val[i] > 0:
            toks.append((s, self.dma_val[i]))
        self._wait(E, toks)
        ins = E.h.dma_start(out=out.ap, in_=in_.ap, **kw)
        self.dma_val[i] += 16
        ins.then_inc(self.sems[s], 16)
        self._commit((s, self.dma_val[i]), in_.bufs, out.bufs)
        self.ninst += 1
        return ins

    def barrier(self):
        toks = []
        for e2 in self.E.values():
            if e2.count > 0:
                toks.append((e2.semidx, e2.count))
        for i, s in enumerate(self.dma_sem):
            if self.dma_val[i] > 0:
                toks.append((s, self.dma_val[i]))
        for E in self.E.values():
            pe_self = [(s, v) for (s, v) in toks if not (s == E.semidx)]
            self._wait(E, pe_self)

    def finish(self):
        E = self.E["sp"]
        for i, s in enumerate(self.dma_sem):
            if self.dma_val[i] > 0 and E.waited.get(s, 0) < self.dma_val[i]:
                E.h.wait_ge(self.sems[s], self.dma_val[i])
                E.waited[s] = self.dma_val[i]
        for name in ("pe", "act", "dve", "pool"):
            e2 = self.E[name]
            if e2.count > 0 and E.waited.get(e2.semidx, 0) < e2.count:
                E.h.wait_ge(self.sems[e2.semidx], e2.count)


D = 1024
DFF = 4096
C = 256
EPS = 1e-6
NMOD = 6


class Cfg:
    def __init__(self, L=8192, depth=4, debug=False, phases=None):
        self.L = L
        self.T = C + L
        self.depth = depth
        self.debug = debug
        self.phases = phases


def tiles(cfg, n):
    out = []
    for s in range(0, C, n):
        out.append((s, min(n, C - s), 1))
    for s in range(0, cfg.L, n):
        out.append((C + s, min(n, cfg.L - s), 0))
    return out


def host_consts():
    c = {}
    c["ident"] = np.eye(128, dtype=np.float32)
    c["ones"] = np.ones((128, 128), dtype=np.float32)
    return c


class M:
    def __init__(self, cfg):
        self.cfg = cfg
        nc = bass.Bass("TRN2", target_bir_lowering=False)
        self.nc = nc
        self.es = contextlib.ExitStack()
        self.k = K(nc, self.es)
        self.din = {}
        self.dbg_out = []

    def inp(self, name, shape, dtype=F32):
        ap = self.nc.dram_tensor(name, list(shape), dtype, kind="ExternalInput").ap()
        d = DTile(ap, name)
        self.din[name] = d
        return d

    def scratch(self, name, shape, dtype=F32, dbg=False):
        kind = "ExternalOutput" if (dbg and self.cfg.debug) else "Internal"
        ap = self.nc.dram_tensor(name, list(shape), dtype, kind=kind).ap()
        if kind == "ExternalOutput":
            self.dbg_out.append(name)
        return DTile(ap, name)

    def outp(self, name, shape, dtype=F32):
        ap = self.nc.dram_tensor(name, list(shape), dtype, kind="ExternalOutput").ap()
        return DTile(ap, name)

    def declare(self):
        cfg = self.cfg
        L, T, dp = cfg.L, cfg.T, cfg.depth
        ne, no = (dp + 1) // 2, dp // 2
        self.x = self.inp("x", [L, D])
        self.ctx = self.inp("ctx", [C, D])
        self.cc = self.inp("cc", [128, 8, 2])
        self.w_ada = self.inp("w_ada", [dp, D, NMOD * D])
        self.b_ada = self.inp("b_adaT", [dp, 128, 48])
        self.norm_g = self.inp("norm_gT", [dp, 128, 32])
        self.w_mlp_in = self.inp("w_mlp_in", [dp, D, DFF])
        self.w_mlp_out = self.inp("w_mlp_out", [dp, DFF, D])
        self.c_ident = self.inp("ident", [128, 128])
        self.c_ones = self.inp("ones", [128, 128])
        self.out = self.outp("out", [L, D])
        self.XT = self.scratch("XT", [D, T], dbg=True)

    def load_consts(self):
        k, es = self.k, self.es
        self.ident = k.sbuf(es, "ident", [128, 128], F32)
        k.dma("sp", self.ident[:], self.c_ident.v(0, self.c_ident.t[:, :]))
        stg = k.sbuf(es, "ones32", [128, 128], F32)
        k.dma("sp", stg[:], self.c_ones.v(0, self.c_ones.t[:, :]))
        self.ones_bf = k.sbuf(es, "ones_bf", [128, 128], BF16)
        k.op("dve", "tensor_copy", out=self.ones_bf[:], in_=stg[:])
        self.ones32 = stg
        self.sc = k.sbuf(es, "sc", [128, 8, 2], F32)
        tmp = k.sbuf(es, "sc_raw", [128, 8, 2], F32)
        k.dma("sp", tmp[:], self.cc.v(0, self.cc.t[:, :, :]))
        k.op("act", "activation", out=self.sc[:], in_=tmp[:], func=AF.Silu)
        self.mod = k.sbuf(es, "mod", [128, 48, 2], F32)
        self.gl = k.sbuf(es, "gl", [128, 32], F32)
        self.coef = k.sbuf(es, "coef", [128, 6, 8, 2], F32)

    def transpose_in(self):
        k, cfg = self.k, self.cfg
        with contextlib.ExitStack() as es:
            xin = [k.sbuf(es, f"ti_x{i}", [128, D], F32) for i in range(2)]
            stg = [k.sbuf(es, f"ti_s{i}", [128, 8, 512], F32) for i in range(2)]
            ps = [k.psum(es, f"ti_p{i}", [128, 512], F32) for i in range(4)]
            XTr = self.XT.t.rearrange("(c p) t -> p c t", p=128)
            it = 0
            ip = 0
            for gi, (t0, n, isctx) in enumerate(tiles(cfg, 512)):
                sg = stg[gi % 2]
                for s in range(n // 128):
                    xi = xin[it % 2]
                    it += 1
                    tt = t0 + s * 128
                    if isctx:
                        src = self.ctx.v(0, self.ctx.t[tt:tt + 128, :])
                    else:
                        src = self.x.v(0, self.x.t[tt - C:tt - C + 128, :])
                    k.dma("sp", xi[:], src)
                    for half in range(2):
                        p = ps[ip % 4]
                        ip += 1
                        for q in range(4):
                            c = half * 4 + q
                            k.op("pe", "transpose", out=p[:, q * 128:(q + 1) * 128],
                                 in_=xi[:, c * 128:(c + 1) * 128], identity=self.ident[:])
                        eng = "act" if half == 0 else "dve"
                        nm = "copy" if half == 0 else "tensor_copy"
                        k.op(eng, nm,
                             out=sg.v(sg.t[:, half * 4:half * 4 + 4, s * 128:(s + 1) * 128]),
                             in_=p.v(p.t[:, :].rearrange("p (q t) -> p q t", q=4)))
                k.dma("pool", self.XT.v(("t", t0), XTr[:, :, t0:t0 + n]), sg.v(sg.t[:, :, 0:n]))

    def transpose_out(self):
        k, cfg = self.k, self.cfg
        with contextlib.ExitStack() as es:
            xin = [k.sbuf(es, f"to_x{i}", [128, 8, 512], F32) for i in range(2)]
            stg = [k.sbuf(es, f"to_s{i}", [128, D], F32) for i in range(2)]
            ps = [k.psum(es, f"to_p{i}", [128, 512], F32) for i in range(4)]
            XTr = self.XT.t.rearrange("(c p) t -> p c t", p=128)
            it = 0
            ip = 0
            for gi, (t0, n, isctx) in enumerate(tiles(cfg, 512)):
                if isctx:
                    continue
                xi = xin[gi % 2]
                k.dma("sp", xi.v(xi.t[:, :, 0:n]), self.XT.v(("t", t0), XTr[:, :, t0:t0 + n]))
                for s in range(n // 128):
                    sg = stg[it % 2]
                    it += 1
                    for half in range(2):
                        p = ps[ip % 4]
                        ip += 1
                        for q in range(4):
                            c = half * 4 + q
                            k.op("pe", "transpose", out=p[:, q * 128:(q + 1) * 128],
                                 in_=xi.v(xi.t[:, c, s * 128:(s + 1) * 128]), identity=self.ident[:])
                        eng = "act" if half == 0 else "dve"
                        nm = "copy" if half == 0 else "tensor_copy"
                        k.op(eng, nm, out=sg[:, half * 512:(half + 1) * 512], in_=p[:, :])
                    tt = t0 - C + s * 128
                    k.dma("pool", self.out.v(("t", tt), self.out.t[tt:tt + 128, :]), sg[:])

    def mod_vectors(self, li):
        k = self.k
        with contextlib.ExitStack() as es:
            wst = [k.sbuf(es, f"mv_w{i}", [128, 8, 512], F32) for i in range(2)]
            ps = k.psum(es, "mv_ps", [128, 96], F32)
            bsb = k.sbuf(es, "mv_b", [128, 48], F32)
            tmp = k.sbuf(es, "mv_t", [128, 8, 2], F32)
            k.dma("sp", bsb[:], self.b_ada.v(0, self.b_ada.t[li, :, :]))
            k.dma("sp", self.gl[:], self.norm_g.v(0, self.norm_g.t[li, :, :]))
            War = self.w_ada.t[li].rearrange("(kc p) n -> p kc n", p=128)
            for blk in range(12):
                w = wst[blk % 2]
                k.dma("sp", w[:], self.w_ada.v(0, War[:, :, blk * 512:(blk + 1) * 512]))
                for jj in range(4):
                    j = blk * 4 + jj
                    for kc in range(8):
                        k.op("pe", "matmul", out=ps[:, 2 * j:2 * j + 2],
                             lhsT=w.v(w.t[:, kc, jj * 128:(jj + 1) * 128]),
                             rhs=self.sc.v(self.sc.t[:, kc, :]), start=(kc == 0), stop=(kc == 7))
            for t in range(2):
                k.op("dve", "tensor_tensor", out=self.mod.v(self.mod.t[:, :, t]),
                     in0=ps.v(ps.t[:, :].rearrange("p (j t) -> p j t", t=2)[:, :, t]),
                     in1=bsb[:, :], op=ALU.add)
            mod, gl, coef = self.mod, self.gl, self.coef

            def mv(m):
                return mod.v(mod.t[:, m * 8:(m + 1) * 8, :])

            def gv(g):
                return gl.v(gl.t[:, g * 8:(g + 1) * 8])

            for (dst, mscale, gidx) in ((0, 1, 0), (3, 4, 2)):
                k.op("dve", "tensor_scalar", out=tmp[:], in0=mv(mscale), scalar1=1.0, scalar2=None, op0=ALU.add)
                for t in range(2):
                    k.op("dve", "tensor_tensor", out=coef.v(coef.t[:, dst, :, t]),
                         in0=tmp.v(tmp.t[:, :, t]), in1=gv(gidx), op=ALU.mult)
            for (dst, mshift) in ((1, 0), (4, 3)):
                k.op("dve", "tensor_copy", out=coef.v(coef.t[:, dst, :, :]), in_=mv(mshift))
            for (dst, mgate, gidx) in ((2, 2, 1), (5, 5, 3)):
                for t in range(2):
                    k.op("dve", "tensor_tensor", out=coef.v(coef.t[:, dst, :, t]),
                         in0=mod.v(mod.t[:, mgate * 8:(mgate + 1) * 8, t]), in1=gv(gidx), op=ALU.mult)

    def rstd_bc(self, es_bufs, src_chunks, n, nch, dim):
        k = self.k
        ps = es_bufs["ps"]
        for c in range(nch):
            sq = es_bufs["sq"][c % 2]
            k.op("act", "activation", out=sq.v(sq.t[:, 0:n]), in_=src_chunks(c), func=AF.Square)
            k.op("pe", "matmul", out=ps.v(ps.t[:, 0:n]), lhsT=self.ones_bf[:], rhs=sq.v(sq.t[:, 0:n]),
                 start=(c == 0), stop=(c == nch - 1))
        rs, rstd = es_bufs["rs"], es_bufs["rstd"]
        k.op("act", "activation", out=rs.v(rs.t[:, 0:n]), in_=ps.v(ps.t[:, 0:n]), func=AF.Sqrt,
             scale=1.0 / dim, bias=self.epsb[:, 0:1])
        k.op("dve", "reciprocal", out=rstd.v(rstd.t[:, 0:n]), in_=rs.v(rs.t[:, 0:n]))
        return rstd.v(rstd.t[:, 0:n])

    def mlp_layer(self, li):
        k, cfg = self.k, self.cfg
        N = 256
        with contextlib.ExitStack() as es:
            W1 = k.sbuf(es, "W1", [128, 8, DFF], BF16)
            W2 = k.sbuf(es, "W2", [128, 32, D], BF16)
            stg = [k.sbuf(es, f"wstg{i}", [128, 2048], F32) for i in range(2)]
            cast_engs = [("act", "copy"), ("dve", "tensor_copy"), ("pool", "tensor_copy")]
            ic = 0
            for kc in range(8):
                for half in range(2):
                    s = stg[ic % 2]
                    k.dma("sp", s[:], self.w_mlp_in.v(0, self.w_mlp_in.t[li, kc * 128:(kc + 1) * 128,
                                                                         half * 2048:(half + 1) * 2048]))
                    e, nm = cast_engs[ic % 3]
                    k.op(e, nm, out=W1.v(W1.t[:, kc, half * 2048:(half + 1) * 2048]), in_=s[:])
                    ic += 1
            W2r = self.w_mlp_out.t[li].rearrange("(j p) n -> p j n", p=128)
            for jp in range(16):
                s = stg[ic % 2]
                k.dma("sp", s.v(s.t[:, :].rearrange("p (j n) -> p j n", j=2)),
                      self.w_mlp_out.v(0, W2r[:, 2 * jp:2 * jp + 2, :]))
                e, nm = cast_engs[ic % 3]
                k.op(e, nm, out=W2.v(W2.t[:, 2 * jp:2 * jp + 2, :]),
                     in_=s.v(s.t[:, :].rearrange("p (j n) -> p j n", j=2)))
                ic += 1
            xt = [k.sbuf(es, f"ml_x{i}", [128, 8, N], F32) for i in range(2)]
            h = k.sbuf(es, "ml_h", [128, 8, N], BF16)
            a = k.sbuf(es, "ml_a", [128, 32, N], BF16)
            ysb = k.sbuf(es, "ml_y", [128, 8, N], F32)
            tmp = [k.sbuf(es, f"ml_t{i}", [128, N], F32) for i in range(2)]
            nb = {"sq": [k.sbuf(es, f"ml_sq{i}", [128, N], BF16) for i in range(2)],
                  "ps": k.psum(es, "ml_pss", [128, N], F32),
                  "rs": k.sbuf(es, "ml_rs", [128, N], F32),
                  "rstd": k.sbuf(es, "ml_rstd", [128, N], F32)}
            ph = [k.psum(es, f"ml_ph{i}", [128, N], F32) for i in range(2)]
            py = [k.psum(es, f"ml_py{i}", [128, N], F32) for i in range(2)]
            XTr = self.XT.t.rearrange("(c p) t -> p c t", p=128)
            coef = self.coef
            tl = tiles(cfg, N)
            k.dma("sp", xt[0].v(xt[0].t[:, :, 0:tl[0][1]]),
                  self.XT.v(("t", tl[0][0]), XTr[:, :, tl[0][0]:tl[0][0] + tl[0][1]]))
            for ti, (t0, n, isctx) in enumerate(tl):
                x = xt[ti % 2]
                if ti + 1 < len(tl):
                    t1, n1, _ = tl[ti + 1]
                    xn = xt[(ti + 1) % 2]
                    k.dma("sp", xn.v(xn.t[:, :, 0:n1]), self.XT.v(("t", t1), XTr[:, :, t1:t1 + n1]))
                rstd = self.rstd_bc(nb, lambda c: x.v(x.t[:, c, 0:n]), n, 8, D)
                for c in range(8):
                    tp = tmp[c % 2]
                    k.op("dve", "scalar_tensor_tensor", out=tp.v(tp.t[:, 0:n]), in0=x.v(x.t[:, c, 0:n]),
                         scalar=coef.v(coef.t[:, 3, c, isctx:isctx + 1]), in1=rstd, op0=ALU.mult, op1=ALU.mult)
                    k.op("act", "activation", out=h.v(h.t[:, c, 0:n]), in_=tp.v(tp.t[:, 0:n]), func=AF.Identity,
                         bias=coef.v(coef.t[:, 4, c, isctx:isctx + 1]), scale=1.0)
                for j in range(32):
                    p = ph[j % 2]
                    for kc in range(8):
                        k.op("pe", "matmul", out=p.v(p.t[:, 0:n]), lhsT=W1.v(W1.t[:, kc, j * 128:(j + 1) * 128]),
                             rhs=h.v(h.t[:, kc, 0:n]), start=(kc == 0), stop=(kc == 7))
                    tp = tmp[j % 2]
                    k.op("act", "activation", out=tp.v(tp.t[:, 0:n]), in_=p.v(p.t[:, 0:n]), func=AF.Relu)
                    e = "dve" if j % 2 == 0 else "pool"
                    k.op(e, "tensor_tensor", out=a.v(a.t[:, j, 0:n]), in0=tp.v(tp.t[:, 0:n]),
                         in1=tp.v(tp.t[:, 0:n]), op=ALU.mult)
                for c in range(8):
                    p = py[c % 2]
                    for j in range(32):
                        k.op("pe", "matmul", out=p.v(p.t[:, 0:n]), lhsT=W2.v(W2.t[:, j, c * 128:(c + 1) * 128]),
                             rhs=a.v(a.t[:, j, 0:n]), start=(j == 0), stop=(j == 31))
                    k.op("act", "copy", out=ysb.v(ysb.t[:, c, 0:n]), in_=p.v(p.t[:, 0:n]))
                rstd = self.rstd_bc(nb, lambda c: ysb.v(ysb.t[:, c, 0:n]), n, 8, D)
                for c in range(8):
                    tp = tmp[c % 2]
                    k.op("dve", "scalar_tensor_tensor", out=tp.v(tp.t[:, 0:n]), in0=ysb.v(ysb.t[:, c, 0:n]),
                         scalar=coef.v(coef.t[:, 5, c, isctx:isctx + 1]), in1=rstd, op0=ALU.mult, op1=ALU.mult)
                    k.op("pool", "tensor_tensor", out=x.v(x.t[:, c, 0:n]), in0=x.v(x.t[:, c, 0:n]),
                         in1=tp.v(tp.t[:, 0:n]), op=ALU.add)
                k.dma("pool", self.XT.v(("t", t0), XTr[:, :, t0:t0 + n]), x.v(x.t[:, :, 0:n]))

    def build(self):
        cfg = self.cfg
        k = self.k
        self.declare()
        self.load_consts()
        self.epsb = k.sbuf(self.es, "epsb", [128, 1], F32)
        k.op("dve", "memset", ap=self.epsb[:], constant=EPS)
        self.transpose_in()
        k.barrier()
        for li in range(cfg.depth):
            self.mod_vectors(li)
            k.barrier()
            self.mlp_layer(li)
            k.barrier()
        self.transpose_out()
        k.finish()
        self.es.close()
        return self.nc


def in_proj(self, li, Wd, wl, ncols, fchunks, tgroups, PT, PK):
    k, cfg = self.k, self.cfg
    N = 512
    with contextlib.ExitStack() as es:
        W = k.sbuf(es, "ipW", [128, 8, ncols], BF16)
        stg = [k.sbuf(es, f"ipstg{i}", [128, ncols], F32) for i in range(2)]
        cast_engs = [("act", "copy"), ("dve", "tensor_copy"), ("pool", "tensor_copy")]
        for kc in range(8):
            s = stg[kc % 2]
            k.dma("sp", s[:], Wd.v(0, Wd.t[wl, kc * 128:(kc + 1) * 128, :]))
            e, nm = cast_engs[kc % 3]
            k.op(e, nm, out=W.v(W.t[:, kc, :]), in_=s[:])
        nf = len(fchunks)
        ktot = sum(m for (_, m, _) in tgroups)
        xt = [k.sbuf(es, f"ip_x{i}", [128, 8, N], F32) for i in range(2)]
        h = k.sbuf(es, "ip_h", [128, 8, N], BF16)
        outF = k.sbuf(es, "ip_oF", [128, nf, N], F32)
        k.op("pool", "memset", ap=outF[:], constant=0.0)
        outK = k.sbuf(es, "ip_oK", [128, 4, max(ktot, 1)], F32)
        tmp = [k.sbuf(es, f"ip_t{i}", [128, N], F32) for i in range(2)]
        nb = {"sq": [k.sbuf(es, f"ip_sq{i}", [128, N], BF16) for i in range(2)],
              "ps": k.psum(es, "ip_pss", [128, N], F32),
              "rs": k.sbuf(es, "ip_rs", [128, N], F32),
              "rstd": k.sbuf(es, "ip_rstd", [128, N], F32)}
        pp = [k.psum(es, f"ip_pp{i}", [128, N], F32) for i in range(4)]
        XTr = self.XT.t.rearrange("(c p) t -> p c t", p=128)
        PTr = PT.t.rearrange("(c p) t -> p c t", p=128)
        coef = self.coef
        tl = tiles(cfg, N)
        k.dma("sp", xt[0].v(xt[0].t[:, :, 0:tl[0][1]]),
              self.XT.v(("t", tl[0][0]), XTr[:, :, tl[0][0]:tl[0][0] + tl[0][1]]))
        ip = 0
        for ti, (t0, n, isctx) in enumerate(tl):
            x = xt[ti % 2]
            if ti + 1 < len(tl):
                t1, n1, _ = tl[ti + 1]
                xn = xt[(ti + 1) % 2]
                k.dma("sp", xn.v(xn.t[:, :, 0:n1]), self.XT.v(("t", t1), XTr[:, :, t1:t1 + n1]))
            rstd = self.rstd_bc(nb, lambda c: x.v(x.t[:, c, 0:n]), n, 8, D)
            for c in range(8):
                tp = tmp[c % 2]
                k.op("dve", "scalar_tensor_tensor", out=tp.v(tp.t[:, 0:n]), in0=x.v(x.t[:, c, 0:n]),
                     scalar=coef.v(coef.t[:, 0, c, isctx:isctx + 1]), in1=rstd, op0=ALU.mult, op1=ALU.mult)
                k.op("act", "activation", out=h.v(h.t[:, c, 0:n]), in_=tp.v(tp.t[:, 0:n]), func=AF.Identity,
                     bias=coef.v(coef.t[:, 1, c, isctx:isctx + 1]), scale=1.0)
            for fi, (c0, m) in enumerate(fchunks):
                p = pp[ip % 4]
                ip += 1
                for kc in range(8):
                    k.op("pe", "matmul", out=p.v(p.t[0:m, 0:n]), lhsT=W.v(W.t[:, kc, c0:c0 + m]),
                         rhs=h.v(h.t[:, kc, 0:n]), start=(kc == 0), stop=(kc == 7))
                if fi % 2 == 0:
                    k.op("act", "copy", out=outF.v(outF.t[0:m, fi, 0:n]), in_=p.v(p.t[0:m, 0:n]))
                else:
                    k.op("dve", "tensor_copy", out=outF.v(outF.t[0:m, fi, 0:n]), in_=p.v(p.t[0:m, 0:n]))
            k.dma("pool", PT.v(("t", t0), PTr[:, 0:nf, t0:t0 + n]), outF.v(outF.t[:, :, 0:n]))
            if tgroups:
                for s in range(n // 128):
                    for gi, (c0, m, dc) in enumerate(tgroups):
                        p = pp[ip % 4]
                        ip += 1
                        for kc in range(8):
                            k.op("pe", "matmul", out=p.v(p.t[:, 0:m]), lhsT=h.v(h.t[:, kc, s * 128:(s + 1) * 128]),
                                 rhs=W.v(W.t[:, kc, c0:c0 + m]), start=(kc == 0), stop=(kc == 7))
                        if (gi + s) % 2 == 0:
                            k.op("act", "copy", out=outK.v(outK.t[:, s, dc:dc + m]), in_=p.v(p.t[:, 0:m]))
                        else:
                            k.op("dve", "tensor_copy", out=outK.v(outK.t[:, s, dc:dc + m]), in_=p.v(p.t[:, 0:m]))
                k.dma("pool", PK.v(("t", t0), PK.t[t0:t0 + n, 0:ktot].rearrange("(s p) c -> p s c", p=128)),
                      outK.v(outK.t[:, 0:n // 128, :]))


M.in_proj = in_proj


def out_proj(self, li, Wd, wl, YM):
    k, cfg = self.k, self.cfg
    N = 512
    with contextlib.ExitStack() as es:
        W = k.sbuf(es, "opW", [128, 8, D], BF16)
        stg = [k.sbuf(es, f"opstg{i}", [128, D], F32) for i in range(2)]
        cast_engs = [("act", "copy"), ("dve", "tensor_copy"), ("pool", "tensor_copy")]
        for kc in range(8):
            s = stg[kc % 2]
            k.dma("sp", s[:], Wd.v(0, Wd.t[wl, kc * 128:(kc + 1) * 128, :]))
            e, nm = cast_engs[kc % 3]
            k.op(e, nm, out=W.v(W.t[:, kc, :]), in_=s[:])
        xt = [k.sbuf(es, f"op_x{i}", [128, 8, N], F32) for i in range(2)]
        ym = [k.sbuf(es, f"op_ym{i}", [128, 8, N], BF16) for i in range(2)]
        ysb = k.sbuf(es, "op_y", [128, 8, N], F32)
        tmp = [k.sbuf(es, f"op_t{i}", [128, N], F32) for i in range(2)]
        nb = {"sq": [k.sbuf(es, f"op_sq{i}", [128, N], BF16) for i in range(2)],
              "ps": k.psum(es, "op_pss", [128, N], F32),
              "rs": k.sbuf(es, "op_rs", [128, N], F32),
              "rstd": k.sbuf(es, "op_rstd", [128, N], F32)}
        py = [k.psum(es, f"op_py{i}", [128, N], F32) for i in range(2)]
        XTr = self.XT.t.rearrange("(c p) t -> p c t", p=128)
        YMr = YM.t.rearrange("(c p) t -> p c t", p=128)
        coef = self.coef
        tl = tiles(cfg, N)
        for ti, (t0, n, isctx) in enumerate(tl):
            x = xt[ti % 2]
            y_in = ym[ti % 2]
            k.dma("sp", x.v(x.t[:, :, 0:n]), self.XT.v(("t", t0), XTr[:, :, t0:t0 + n]))
            k.dma("sp", y_in.v(y_in.t[:, :, 0:n]), YM.v("all", YMr[:, :, t0:t0 + n]))
            for c in range(8):
                p = py[c % 2]
                for kc in range(8):
                    k.op("pe", "matmul", out=p.v(p.t[:, 0:n]), lhsT=W.v(W.t[:, kc, c * 128:(c + 1) * 128]),
                         rhs=y_in.v(y_in.t[:, kc, 0:n]), start=(kc == 0), stop=(kc == 7))
                k.op("act", "copy", out=ysb.v(ysb.t[:, c, 0:n]), in_=p.v(p.t[:, 0:n]))
            rstd = self.rstd_bc(nb, lambda c: ysb.v(ysb.t[:, c, 0:n]), n, 8, D)
            for c in range(8):
                tp = tmp[c % 2]
                k.op("dve", "scalar_tensor_tensor", out=tp.v(tp.t[:, 0:n]), in0=ysb.v(ysb.t[:, c, 0:n]),
                     scalar=coef.v(coef.t[:, 2, c, isctx:isctx + 1]), in1=rstd, op0=ALU.mult, op1=ALU.mult)
                k.op("pool", "tensor_tensor", out=x.v(x.t[:, c, 0:n]), in0=x.v(x.t[:, c, 0:n]),
                     in1=tp.v(tp.t[:, 0:n]), op=ALU.add)
            k.dma("pool", self.XT.v(("t", t0), XTr[:, :, t0:t0 + n]), x.v(x.t[:, :, 0:n]))


M.out_proj = out_proj


def rope_tables(L):
    GRID_W = 64
    t = np.arange(L)
    r = (t // GRID_W).astype(np.float32)
    col = (t % GRID_W).astype(np.float32)
    inv = (10000.0 ** (-np.arange(16, dtype=np.float32) / 16)).astype(np.float32)
    ang = np.concatenate([r[:, None] * inv, col[:, None] * inv], axis=-1).astype(np.float32)
    cos, sin = np.cos(ang).astype(np.float32), np.sin(ang).astype(np.float32)
    cosT = np.repeat(cos, 2, axis=1).T
    sinT = np.repeat(sin, 2, axis=1).T
    out = np.stack([np.tile(cosT, (2, 1)), np.tile(sinT, (2, 1))]).astype(np.float32)
    return np.ascontiguousarray(out)


def rot_matrix():
    R = np.zeros((128, 128), np.float32)
    for m in range(128):
        if m % 2 == 0:
            R[m + 1, m] = -1.0
        else:
            R[m - 1, m] = 1.0
    return R


def block_ones(bs):
    o = np.zeros((128, 128), np.float32)
    for b in range(128 // bs):
        o[b * bs:(b + 1) * bs, b * bs:(b + 1) * bs] = 1.0
    return o


def norm_rope(self, nb, src, n, out, g_ap, t_main0, do_norm, do_rope, blk, hd):
    k = self.k
    cur = src
    if do_norm:
        sq = nb["sq"][0]
        k.op("act", "activation", out=sq.v(sq.t[:, 0:n]), in_=src, func=AF.Square)
        ps = nb["ps"]
        k.op("pe", "matmul", out=ps.v(ps.t[:, 0:n]), lhsT=blk[:], rhs=sq.v(sq.t[:, 0:n]), start=True, stop=True)
        rs, rstd = nb["rs"], nb["rstd"]
        k.op("act", "activation", out=rs.v(rs.t[:, 0:n]), in_=ps.v(ps.t[:, 0:n]), func=AF.Sqrt,
             scale=1.0 / hd, bias=self.epsb[:, 0:1])
        k.op("dve", "reciprocal", out=rstd.v(rstd.t[:, 0:n]), in_=rs.v(rs.t[:, 0:n]))
        kn = nb["kn"]
        k.op("dve", "scalar_tensor_tensor", out=kn.v(kn.t[:, 0:n]), in0=src, scalar=g_ap,
             in1=rstd.v(rstd.t[:, 0:n]), op0=ALU.mult, op1=ALU.mult)
        cur = kn.v(kn.t[:, 0:n])
    def halves(t_, ncols):
        return [t_.v(t_.t[0:64, 0:ncols]), t_.v(t_.t[64:128, 0:ncols])]
    if not do_rope:
        if isinstance(out, list):
            if do_norm:
                src_h = halves(nb["kn"], n)
            else:
                src_h = src
            for o_, s_ in zip(out, src_h):
                k.op("act", "copy", out=o_, in_=s_)
        else:
            k.op("act", "copy", out=out, in_=cur)
        return
    if isinstance(cur, list):
        raise ValueError("rope path needs a full-view src")
    cs = nb["cs"]
    k.dma("sp", cs.v(cs.t[:, :, 0:n]), self.c_rope.v(0, self.c_rope.t[:, :, t_main0:t_main0 + n].rearrange("a p t -> p a t")))
    pr = nb["pr"]
    k.op("pe", "matmul", out=pr.v(pr.t[:, 0:n]), lhsT=self.rotm[:], rhs=cur, start=True, stop=True)
    t1, t2 = nb["t1"], nb["t2"]
    k.op("pool", "tensor_tensor", out=t1.v(t1.t[:, 0:n]), in0=cur, in1=cs.v(cs.t[:, 0, 0:n]), op=ALU.mult)
    k.op("dve", "tensor_tensor", out=t2.v(t2.t[:, 0:n]), in0=pr.v(pr.t[:, 0:n]), in1=cs.v(cs.t[:, 1, 0:n]), op=ALU.mult)
    if isinstance(out, list):
        for o_, a_, b_ in zip(out, halves(t1, n), halves(t2, n)):
            k.op("pool", "tensor_tensor", out=o_, in0=a_, in1=b_, op=ALU.add)
    else:
        k.op("pool", "tensor_tensor", out=out, in0=t1.v(t1.t[:, 0:n]), in1=t2.v(t2.t[:, 0:n]), op=ALU.add)


M.norm_rope = norm_rope


def nr_bufs(self, es, N, pfx):
    k = self.k
    return {"sq": [k.sbuf(es, pfx + "sq", [128, N], BF16)],
            "ps": k.psum(es, pfx + "ps", [128, N], F32),
            "pr": k.psum(es, pfx + "pr", [128, N], F32),
            "rs": k.sbuf(es, pfx + "rs", [128, N], F32),
            "rstd": k.sbuf(es, pfx + "rstd", [128, N], F32),
            "kn": k.sbuf(es, pfx + "kn", [128, N], F32),
            "cs": k.sbuf(es, pfx + "cs", [128, 2, N], F32),
            "t1": k.sbuf(es, pfx + "t1", [128, N], F32),
            "t2": k.sbuf(es, pfx + "t2", [128, N], F32)}


M.nr_bufs = nr_bufs


def gqa(self, j, PT, PK, YM, qc0, kc, vcol, ym_row0, ctx_out):
    k, cfg = self.k, self.cfg
    T, L = cfg.T, cfg.L
    NT = T // 128
    N = 512
    with contextlib.ExitStack() as es:
        nb = self.nr_bufs(es, N, "gq_")
        blk = k.sbuf(es, "gq_blk", [128, 128], BF16)
        k.op("dve", "tensor_copy", out=blk[:], in_=self.blk64[:])
        gsb = k.sbuf(es, "gq_g", [128, 2], F32)
        k.dma("sp", gsb[:], self.ev_qk_g.v(0, self.ev_qk_g.t[j, :, :]))
        PTr = PT.t.rearrange("(c p) t -> p c t", p=128)
        KT = [k.sbuf(es, f"gq_KT{kv}", [128, T], BF16) for kv in range(2)]
        kraw = k.sbuf(es, "gq_kraw", [128, N], F32)
        for kv in range(2):
            for (t0, n, isctx) in tiles(cfg, N):
                for hf in range(2):
                    k.dma("sp", kraw.v(kraw.t[hf * 64:(hf + 1) * 64, 0:n]),
                          PT.v(("t", t0), PT.t[kc * 128 + kv * 64:kc * 128 + (kv + 1) * 64, t0:t0 + n]))
                self.norm_rope(nb, kraw.v(kraw.t[:, 0:n]), n, KT[kv].v(KT[kv].t[:, t0:t0 + n]),
                               gsb.v(gsb.t[:, 1:2]), t0 - C, True, not isctx, blk, 64)
        Va = k.sbuf(es, "gq_Va", [128, NT, 2, 65], BF16)
        k.op("pool", "memset", ap=Va[:], constant=1.0)
        vst = k.sbuf(es, "gq_vst", [128, 8, 128], F32)
        for b0 in range(0, NT, 8):
            nbk = min(8, NT - b0)
            k.dma("sp", vst.v(vst.t[:, 0:nbk, :]),
                  PK.v("all", PK.t[b0 * 128:(b0 + nbk) * 128, vcol:vcol + 128].rearrange("(s p) c -> p s c", p=128)))
            k.op("dve", "tensor_copy", out=Va.v(Va.t[:, b0:b0 + nbk, :, 0:64]),
                 in_=vst.v(vst.t[:, 0:nbk, :].rearrange("p s (kv d) -> p s kv d", kv=2)))
        qraw = [k.sbuf(es, f"gq_qraw{i}", [128, 4, N], F32) for i in range(2)]
        QT = [k.sbuf(es, f"gq_QT{i}", [128, 8, N], BF16) for i in range(2)]
        for q_ in QT:
            k.op("pool", "memset", ap=q_[:], constant=0.0)
        pS = k.psum(es, "gq_pS", [128, 4 * N], F32, nsub=4)
        pO = [k.psum(es, f"gq_pO{i}", [128, N], F32) for i in range(2)]
        pB = nb["pr"]
        pb = [k.sbuf(es, f"gq_pb{i}", [128, 2, N], BF16) for i in range(2)]
        osb = [k.sbuf(es, f"gq_osb{i}", [128, N], F32) for i in range(2)]
        rsum = [k.sbuf(es, f"gq_rsum{i}", [128, N], F32) for i in range(2)]
        yst = [k.sbuf(es, f"gq_yst{i}", [64, N], BF16) for i in range(2)]
        iP = 0
        iH = 0
        for ti, (t0, n, isctx) in enumerate(tiles(cfg, N)):
            if isctx and not ctx_out:
                continue
            qr, qt = qraw[ti % 2], QT[ti % 2]
            k.dma("sp", qr.v(qr.t[:, :, 0:n]), PT.v(("t", t0), PTr[:, qc0:qc0 + 4, t0:t0 + n]))
            for c in range(4):
                self.norm_rope(nb, qr.v(qr.t[:, c, 0:n]), n,
                               [qt.v(qt.t[0:64, 2 * c, 0:n]), qt.v(qt.t[64:128, 2 * c + 1, 0:n])],
                               gsb.v(gsb.t[:, 0:1]), t0 - C, True, not isctx, blk, 64)
            kts = list(range(C // 128)) if isctx else list(range(NT))
            pairs = [kts[i:i + 2] for i in range(0, len(kts), 2)]
            for hq in range(8):
                kv = hq // 4
                po = pO[iH % 2]
                ob, rsm, ys = osb[iH % 2], rsum[iH % 2], yst[iH % 2]
                iH += 1

                def qk(pi):
                    base = ((iP + pi) % 2) * 2
                    for jj, kt in enumerate(pairs[pi]):
                        bk = base + jj
                        k.op("pe", "matmul", out=pS.vs(pS.t[:, bk * N:bk * N + n], [bk]),
                             lhsT=KT[kv].v(KT[kv].t[:, kt * 128:(kt + 1) * 128]),
                             rhs=qt.v(qt.t[:, hq, 0:n]), start=True, stop=True)

                qk(0)
                for pi, pr_ in enumerate(pairs):
                    if pi + 1 < len(pairs):
                        qk(pi + 1)
                    base = ((iP + pi) % 2) * 2
                    pbuf = pb[(iP + pi) % 2]
                    np_ = len(pr_)
                    src3 = pS.t[:, base * N:(base + np_) * N].rearrange("p (j t) -> p j t", j=np_)[:, :, 0:n]
                    k.op("act", "activation", out=pbuf.v(pbuf.t[:, 0:np_, 0:n]),
                         in_=pS.vs(src3, list(range(base, base + np_))), func=AF.Exp, scale=0.125)
                    for jj, kt in enumerate(pr_):
                        first = (pi == 0 and jj == 0)
                        last_ = (pi == len(pairs) - 1 and jj == np_ - 1)
                        k.op("pe", "matmul", out=po.v(po.t[0:65, 0:n]), lhsT=Va.v(Va.t[:, kt, kv, :]),
                             rhs=pbuf.v(pbuf.t[:, jj, 0:n]), start=first, stop=last_)
                iP += len(pairs)
                k.op("act", "copy", out=ob.v(ob.t[0:64, 0:n]), in_=po.v(po.t[0:64, 0:n]))
                k.op("dve", "reciprocal", out=rsm.v(rsm.t[64:65, 0:n]), in_=po.v(po.t[64:65, 0:n]))
                k.op("pe", "matmul", out=pB.v(pB.t[0:64, 0:n]), lhsT=self.ones32.v(self.ones32.t[64:65, 0:64]),
                     rhs=rsm.v(rsm.t[64:65, 0:n]), start=True, stop=True)
                k.op("dve", "tensor_tensor", out=ys.v(ys.t[:, 0:n]), in0=ob.v(ob.t[0:64, 0:n]),
                     in1=pB.v(pB.t[0:64, 0:n]), op=ALU.mult)
                row = ym_row0 + hq * 64
                k.dma("pool", YM.v("all", YM.t[row:row + 64, t0:t0 + n]), ys.v(ys.t[:, 0:n]))


M.gqa = gqa


EV_F = [(0, 128), (128, 128), (256, 128), (384, 128), (1024, 128), (1152, 128), (1280, 128), (1408, 128),
        (1536, 32), (1568, 128), (1696, 128), (1824, 128), (1952, 128), (2080, 128)]
EV_T = [(256, 512, 0), (768, 256, 512), (2208, 128, 768)]


def declare_mix(self):
    cfg = self.cfg
    dp, L, T = cfg.depth, cfg.L, cfg.T
    ne, no = (dp + 1) // 2, dp // 2
    self.ev_w_in = self.inp("ev_w_in", [ne, D, 2336])
    self.ev_w_out = self.inp("ev_w_out", [ne, D, D])
    self.ev_qk_g = self.inp("ev_qk_gT", [ne, 128, 2])
    self.c_rope = self.inp("rope", [2, 128, L])
    self.c_rotm = self.inp("rotm", [128, 128])
    self.c_blk64 = self.inp("blk64", [128, 128])
    self.PT = self.scratch("PT", [24 * 128, T], dbg=True)
    self.PK = self.scratch("PK", [T, 1024], dbg=True)
    self.YM = self.scratch("YM", [D, T], BF16, dbg=True)
    k, es = self.k, self.es
    self.rotm = k.sbuf(es, "rotm", [128, 128], F32)
    k.dma("sp", self.rotm[:], self.c_rotm.v(0, self.c_rotm.t[:, :]))
    self.blk64 = k.sbuf(es, "blk64", [128, 128], F32)
    k.dma("sp", self.blk64[:], self.c_blk64.v(0, self.c_blk64.t[:, :]))


M.declare_mix = declare_mix


def zero_ym(self, r0, r1):
    k, cfg = self.k, self.cfg
    with contextlib.ExitStack() as es:
        z = k.sbuf(es, "zz", [128, 2048], BF16)
        k.op("pool", "memset", ap=z[:], constant=0.0)
        for rr in range(r0, r1, 128):
            for t0 in range(0, cfg.T, 2048):
                n = min(2048, cfg.T - t0)
                k.dma("sp", self.YM.v("all", self.YM.t[rr:rr + 128, t0:t0 + n]), z.v(z.t[:, 0:n]))


M.zero_ym = zero_ym


def even_layer(self, li, last):
    k = self.k
    j = li // 2
    self.in_proj(li, self.ev_w_in, j, 2336, EV_F, EV_T, self.PT, self.PK)
    k.barrier()
    if self.cfg.phases is None or "gla" in self.cfg.phases:
        self.gla(j, self.PT, self.PK, self.YM, not last)
    else:
        self.zero_ym(0, 512)
    k.barrier()
    self.gqa(j, self.PT, self.PK, self.YM, 9, 13, 768, 512, not last)
    k.barrier()
    self.out_proj(li, self.ev_w_out, j, self.YM)
    k.barrier()


M.even_layer = even_layer


def build2(self):
    cfg = self.cfg
    k = self.k
    self.declare()
    self.declare_mix()
    self.load_consts()
    self.declare_gla()
    self.declare_odd()
    self.declare_hy()
    self.epsb = k.sbuf(self.es, "epsb", [128, 1], F32)
    k.op("dve", "memset", ap=self.epsb[:], constant=EPS)
    self.oneb = k.sbuf(self.es, "oneb", [128, 1], F32)
    k.op("dve", "memset", ap=self.oneb[:], constant=1.0)
    self.transpose_in()
    k.barrier()
    for li in range(cfg.depth):
        last = li == cfg.depth - 1
        self.mod_vectors(li)
        k.barrier()
        if li % 2 == 0:
            self.even_layer(li, last)
        else:
            self.odd_layer(li, last)
        self.mlp_layer(li)
        k.barrier()
    self.transpose_out()
    k.finish()
    self.es.close()
    return self.nc


M.build2 = build2


def gla_consts():
    s = np.arange(128)[:, None]
    t = np.arange(128)[None, :]
    sc = -1.0 / 16.0
    ucat = np.zeros((2, 128, 129), np.float32)
    ucat[0, :, :128] = (s <= t) * sc
    ucat[1, :, :128] = (s >= t) * sc
    ucat[:, :, 128] = sc
    ustr = np.zeros((2, 128, 128), np.float32)
    ustr[0] = (s > t) * sc
    ustr[1] = (s < t) * sc
    mask = np.zeros((2, 128, 128), np.float32)
    mask[0] = (s <= t)
    mask[1] = (s >= t)
    return {"gla_ucat": ucat, "gla_ustr": ustr, "gla_mask": mask}


def declare_gla(self):
    ne = (self.cfg.depth + 1) // 2
    self.ev_wlr = self.inp("ev_wlr_aug", [ne, 2, 17, 256])
    self.ev_gla_g = self.inp("ev_gla_gT", [ne, 128, 1])
    self.c_ucat = self.inp("gla_ucat", [2, 128, 129])
    self.c_ustr = self.inp("gla_ustr", [2, 128, 128])
    self.c_mask = self.inp("gla_mask", [2, 128, 128])
    self.OF = self.scratch("OF", [512, self.cfg.T], dbg=True)


M.declare_gla = declare_gla


def gla(self, j, PT, PK, YM, ctx_out):
    k, cfg = self.k, self.cfg
    T = cfg.T
    NT = T // 128
    nctx = C // 128
    PTr = PT.t.rearrange("(c p) t -> p c t", p=128)
    OFr = self.OF.t.rearrange("(c p) t -> p c t", p=128)
    YMr = YM.t.rearrange("(c p) t -> p c t", p=128)
    with contextlib.ExitStack() as es:
        sb = lambda nm, sh, dt=F32: k.sbuf(es, "gl_" + nm, sh, dt)
        lra = [sb(f"lra{d}", [32, T]) for d in range(2)]
        wlr = [sb(f"wlr{d}", [17, 256]) for d in range(2)]
        ucat = [sb(f"ucat{d}", [128, 129]) for d in range(2)]
        ustr = [sb(f"ustr{d}", [128, 128]) for d in range(2)]
        mask = [sb(f"mask{d}", [128, 128]) for d in range(2)]
        gain = sb("gain", [128, 1])
        k.dma("sp", gain[:], self.ev_gla_g.v(0, self.ev_gla_g.t[j, :, :]))
        for d in range(2):
            k.op("pool", "memset", ap=lra[d][:], constant=1.0)
            k.dma("sp", lra[d].v(lra[d].t[0:16, :]), PT.v("all", PT.t[8 * 128 + d * 16:8 * 128 + (d + 1) * 16, :]))
            k.dma("sp", wlr[d][:], self.ev_wlr.v(0, self.ev_wlr.t[j, d, :, :]))
            k.dma("sp", ucat[d][:], self.c_ucat.v(0, self.c_ucat.t[d, :, :]))
            k.dma("sp", ustr[d][:], self.c_ustr.v(0, self.c_ustr.t[d, :, :]))
            k.dma("sp", mask[d][:], self.c_mask.v(0, self.c_mask.t[d, :, :]))
        ldf = [sb(f"ldf{i}", [128, 4, 128]) for i in range(2)]
        ldk = [sb(f"ldk{i}", [128, 768]) for i in range(2)]
        e1 = sb("e1", [128, 256])
        lnv = sb("lnv", [128, 256])
        EqT = sb("EqT", [128, 2, 129])
        EkT = sb("EkT", [128, 2, 128])
        Eend = sb("Eend", [128, 256])
        qin = sb("qin", [128, 2, 128], BF16)
        kin = sb("kin", [128, 2, 128], BF16)
        kend = sb("kend", [128, 256], BF16)
        vbf = sb("vbf", [128, 512], BF16)
        ATm = [sb(f"ATm{i}", [128, 128], BF16) for i in range(2)]
        S = sb("S", [128, 2, 128])
        Sbf = sb("Sbf", [128, 2, 128], BF16)
        ofw = [sb(f"ofw{i}", [128, 4, 128]) for i in range(2)]
        ofl = [sb(f"ofl{i}", [128, 4, 128]) for i in range(2)]
        gT = [sb(f"gT{i}", [128, 4, 128]) for i in range(2)]
        otot = [sb(f"otot{i}", [128, 128]) for i in range(2)]
        sq = sb("sq", [128, 128], BF16)
        rs = sb("rs", [128, 128])
        rstd = sb("rstd", [128, 128])
        sg = sb("sg", [128, 128])
        y1 = sb("y1", [128, 128])
        yst = [sb(f"yst{i}", [128, 4, 128], BF16) for i in range(2)]
        p_gk = k.psum(es, "gl_pgk", [128, 512])
        p_bT = k.psum(es, "gl_pbT", [128, 2, 256])
        p_be = k.psum(es, "gl_pbe", [128, 512])
        p_AT = [k.psum(es, f"gl_pAT{i}", [128, 512]) for i in range(2)]
        p_o = [k.psum(es, f"gl_po{i}", [128, 512]) for i in range(2)]
        p_up = k.psum(es, "gl_pup", [128, 2, 128])
        ih = 0
        for d in range(2):
            k.op("dve", "memset", ap=S[:], constant=0.0)
            k.op("pool", "memset", ap=Sbf[:], constant=0.0)
            ctx_tiles = list(range(nctx))
            main_tiles = list(range(nctx, NT))
            order = ctx_tiles + main_tiles if d == 0 else ctx_tiles[::-1] + main_tiles[::-1]
            for it, tix in enumerate(order):
                t0 = tix * 128
                isctx = tix < nctx
                need_out = (not isctx) or ctx_out
                lf, lk = ldf[it % 2], ldk[it % 2]
                k.dma("sp", lf[:], PT.v("all", PTr[:, 0:4, t0:t0 + 128]))
                k.dma("sp", lk[:], PK.v("all", PK.t[t0:t0 + 128, 0:768]))
                if d == 1 and need_out:
                    k.dma("sp", gT[it % 2][:], PT.v("all", PTr[:, 4:8, t0:t0 + 128]))
                    k.dma("sp", ofl[it % 2][:], self.OF.v("all", OFr[:, :, t0:t0 + 128]))
                k.op("pe", "matmul", out=p_gk.v(p_gk.t[:, 0:256]), lhsT=lra[d].v(lra[d].t[0:17, t0:t0 + 128]),
                     rhs=wlr[d][:], start=True, stop=True)
                k.op("act", "activation", out=e1[:], in_=p_gk.v(p_gk.t[:, 0:256]), func=AF.Exp, scale=-1.0)
                k.op("act", "activation", out=lnv[:], in_=e1[:], func=AF.Ln, bias=self.oneb[:, 0:1], scale=1.0)
                for pr in range(2):
                    k.op("pe", "matmul", out=p_bT.v(p_bT.t[:, pr, 0:129]), lhsT=lnv.v(lnv.t[:, pr * 128:(pr + 1) * 128]),
                         rhs=ucat[d][:], start=True, stop=True)
                k.op("pe", "matmul", out=p_be.v(p_be.t[:, 0:256]), lhsT=ustr[d][:], rhs=lnv[:], start=True, stop=True)
                k.op("act", "activation", out=EqT[:], in_=p_bT.v(p_bT.t[:, :, 0:129]), func=AF.Exp)
                k.op("act", "activation", out=EkT[:], in_=p_bT.v(p_bT.t[:, :, 0:128]), func=AF.Exp, scale=-1.0)
                k.op("act", "activation", out=Eend[:], in_=p_be.v(p_be.t[:, 0:256]), func=AF.Exp)
                k.op("dve", "scalar_tensor_tensor", out=qin[:], in0=lf.v(lf.t[:, 0:2, :]), scalar=0.125,
                     in1=EqT.v(EqT.t[:, :, 0:128]), op0=ALU.mult, op1=ALU.mult)
                k.op("pool", "tensor_tensor", out=kin[:], in0=lf.v(lf.t[:, 2:4, :]), in1=EkT[:], op=ALU.mult)
                k.op("dve", "tensor_tensor", out=kend[:], in0=lk.v(lk.t[:, 0:256]), in1=Eend[:], op=ALU.mult)
                k.op("pool", "tensor_copy", out=vbf[:], in_=lk.v(lk.t[:, 256:768]))
                for h in range(4):
                    pr, r = h // 2, (h % 2) * 64
                    if need_out:
                        pa, po, am = p_AT[ih % 2], p_o[ih % 2], ATm[ih % 2]
                        k.op("pe", "matmul", out=pa.v(pa.t[:, 0:128]), lhsT=kin.v(kin.t[r:r + 64, pr, :]),
                             rhs=qin.v(qin.t[r:r + 64, pr, :]), start=True, stop=True)
                        k.op("dve", "tensor_tensor", out=am[:], in0=pa.v(pa.t[:, 0:128]), in1=mask[d][:], op=ALU.mult)
                        k.op("pe", "matmul", out=po.v(po.t[:, 0:128]), lhsT=vbf.v(vbf.t[:, h * 128:(h + 1) * 128]),
                             rhs=am[:], start=True, stop=False)
                        k.op("pe", "matmul", out=po.v(po.t[:, 0:128]), lhsT=Sbf.v(Sbf.t[r:r + 64, pr, :]),
                             rhs=qin.v(qin.t[r:r + 64, pr, :]), start=False, stop=True)
                        if d == 0:
                            k.op("act", "copy", out=ofw[it % 2].v(ofw[it % 2].t[:, h, :]), in_=po.v(po.t[:, 0:128]))
                        else:
                            ot = otot[ih % 2]
                            k.op("dve", "tensor_tensor", out=ot[:], in0=po.v(po.t[:, 0:128]),
                                 in1=ofl[it % 2].v(ofl[it % 2].t[:, h, :]), op=ALU.add)
                            k.op("act", "activation", out=sq[:], in_=ot[:], func=AF.Square)
                            k.op("pe", "matmul", out=p_gk.v(p_gk.t[:, 256:384]), lhsT=self.ones_bf[:], rhs=sq[:],
                                 start=True, stop=True)
                            k.op("act", "activation", out=rs[:], in_=p_gk.v(p_gk.t[:, 256:384]), func=AF.Sqrt,
                                 scale=1.0 / 128, bias=self.epsb[:, 0:1])
                            k.op("dve", "reciprocal", out=rstd[:], in_=rs[:])
                            k.op("act", "activation", out=sg[:], in_=gT[it % 2].v(gT[it % 2].t[:, h, :]), func=AF.Silu)
                            k.op("dve", "scalar_tensor_tensor", out=y1[:], in0=ot[:], scalar=gain[:, 0:1],
                                 in1=rstd[:], op0=ALU.mult, op1=ALU.mult)
                            k.op("pool", "tensor_tensor", out=yst[it % 2].v(yst[it % 2].t[:, h, :]), in0=y1[:],
                                 in1=sg[:], op=ALU.mult)
                        ih += 1
                    k.op("pe", "matmul", out=p_up.v(p_up.t[r:r + 64, pr, :]), lhsT=kend.v(kend.t[:, h * 64:(h + 1) * 64]),
                         rhs=vbf.v(vbf.t[:, h * 128:(h + 1) * 128]), start=True, stop=True)
                for pr in range(2):
                    k.op("dve", "scalar_tensor_tensor", out=S.v(S.t[:, pr, :]), in0=S.v(S.t[:, pr, :]),
                         scalar=EqT.v(EqT.t[:, pr, 128:129]), in1=p_up.v(p_up.t[:, pr, :]), op0=ALU.mult, op1=ALU.add)
                k.op("act", "copy", out=Sbf[:], in_=S[:])
                if need_out:
                    if d == 0:
                        k.dma("pool", self.OF.v("all", OFr[:, :, t0:t0 + 128]), ofw[it % 2][:])
                    else:
                        k.dma("pool", YM.v("all", YMr[:, 0:4, t0:t0 + 128]), yst[it % 2][:])
            k.barrier()


M.gla = gla


OD_F = [(i * 128, 128) for i in range(20)]
OD_T = [(2560, 512, 0)]


def declare_odd(self):
    cfg = self.cfg
    no = max(cfg.depth // 2, 1)
    self.od_w_in = self.inp("od_w_in", [no, D, 3072])
    self.od_w_out = self.inp("od_w_out", [no, D, D])
    self.od_lam = self.inp("od_lamB", [no, 128, 256])
    self.od_diff_g = self.inp("od_diff_gT", [no, 128, 1])


M.declare_odd = declare_odd


def diffattn(self, li, j, PT, PK, YM, qc0, kc0, ym_row0, ctx_out):
    k, cfg = self.k, self.cfg
    T, L = cfg.T, cfg.L
    NT = T // 128
    N = 512
    lam_init = 0.8 - 0.6 * math.exp(-0.3 * li)
    with contextlib.ExitStack() as es:
        nb = self.nr_bufs(es, N, "da_")
        PTr = PT.t.rearrange("(c p) t -> p c t", p=128)
        lp = k.sbuf(es, "da_lp", [128, 256], F32)
        k.dma("sp", lp[:], self.od_lam.v(0, self.od_lam.t[j, :, :]))
        pr2 = k.sbuf(es, "da_pr2", [128, 2, 64], F32)
        sm = k.sbuf(es, "da_sm", [128, 2], F32)
        ex = k.sbuf(es, "da_ex", [128, 2], F32)
        neglam = k.sbuf(es, "da_nl", [128, 1], F32)
        for q in range(2):
            k.op("dve", "tensor_tensor", out=pr2.v(pr2.t[:, q, :]), in0=lp.v(lp.t[:, q * 128:q * 128 + 64]),
                 in1=lp.v(lp.t[:, q * 128 + 64:q * 128 + 128]), op=ALU.mult)
        k.op("dve", "reduce_sum", out=sm[:], in_=pr2[:], axis=mybir.AxisListType.X)
        k.op("act", "activation", out=ex[:], in_=sm[:], func=AF.Exp)
        k.op("dve", "tensor_tensor", out=neglam[:], in0=ex.v(ex.t[:, 1:2]), in1=ex.v(ex.t[:, 0:1]), op=ALU.subtract)
        k.op("dve", "tensor_scalar", out=neglam[:], in0=neglam[:], scalar1=-lam_init, scalar2=None, op0=ALU.add)
        g2 = k.sbuf(es, "da_g2", [128, 1], F32)
        k.dma("sp", g2[:], self.od_diff_g.v(0, self.od_diff_g.t[j, :, :]))
        k.op("dve", "tensor_scalar", out=g2[:], in0=g2[:], scalar1=1.0 - lam_init, scalar2=None, op0=ALU.mult)
        KT = k.sbuf(es, "da_KT", [128, 4, T], BF16)
        kraw = k.sbuf(es, "da_kraw", [128, N], F32)
        for c in range(4):
            for (t0, n, isctx) in tiles(cfg, N):
                k.dma("sp", kraw.v(kraw.t[:, 0:n]), PT.v("all", PT.t[(kc0 + c) * 128:(kc0 + c + 1) * 128, t0:t0 + n]))
                self.norm_rope(nb, kraw.v(kraw.t[:, 0:n]), n, KT.v(KT.t[:, c, t0:t0 + n]),
                               None, t0 - C, False, not isctx, None, 64)
        V = k.sbuf(es, "da_V", [128, NT, 512], BF16)
        vst = k.sbuf(es, "da_vst", [128, 1, 512], F32)
        for b0 in range(0, NT, 1):
            nbk = 1
            k.dma("sp", vst.v(vst.t[:, 0:nbk, :]),
                  PK.v("all", PK.t[b0 * 128:(b0 + nbk) * 128, 0:512].rearrange("(s p) c -> p s c", p=128)))
            k.op("dve" if (b0 // 2) % 2 == 0 else "pool", "tensor_copy", out=V.v(V.t[:, b0:b0 + nbk, :]),
                 in_=vst.v(vst.t[:, 0:nbk, :]))
        qraw = [k.sbuf(es, "da_qraw0", [128, 4, N], F32)]
        QT = [k.sbuf(es, f"da_QT{i}", [128, 4, 2, N], BF16) for i in range(2)]
        for q_ in QT:
            k.op("pool", "memset", ap=q_[:], constant=0.0)
        pS = k.psum(es, "da_pS", [128, 4 * N], F32, nsub=4)
        pO = [k.psum(es, f"da_pO{i}", [128, N], F32) for i in range(2)]
        pZ = nb["pr"]
        pb = [k.sbuf(es, f"da_pb{i}", [128, 2, N], BF16) for i in range(3)]
        acc = [k.sbuf(es, f"da_acc{i}", [128, 2, N], F32) for i in range(2)]
        rz1 = k.sbuf(es, "da_rz", [128, N], F32)
        rz = [rz1, rz1]
        tt = [k.sbuf(es, f"da_tt{i}", [128, N], F32) for i in range(2)]
        osb = k.sbuf(es, "da_o", [128, N], F32)
        sq = k.sbuf(es, "da_sq", [128, N], BF16)
        y1 = k.sbuf(es, "da_y1", [128, N], BF16)
        iP = 0
        for ti, (t0, n, isctx) in enumerate(tiles(cfg, N)):
            if isctx and not ctx_out:
                continue
            qr, qt = qraw[0], QT[ti % 2]
            k.dma("sp", qr.v(qr.t[:, :, 0:n]), PT.v("all", PTr[:, qc0:qc0 + 4, t0:t0 + n]))
            for c in range(4):
                src_ = qr.v(qr.t[:, c, 0:n]) if not isctx else [qr.v(qr.t[0:64, c, 0:n]), qr.v(qr.t[64:128, c, 0:n])]
                self.norm_rope(nb, src_, n,
                               [qt.v(qt.t[0:64, c, 0, 0:n]), qt.v(qt.t[64:128, c, 1, 0:n])],
                               None, t0 - C, False, not isctx, None, 64)
            kts = list(range(C // 128)) if isctx else list(range(NT))
            for h in range(4):
                k.op("dve", "memset", ap=acc[0][:], constant=0.0)
                k.op("pool", "memset", ap=acc[1][:], constant=0.0)

                def qk(ii):
                    base = ((iP + ii) % 2) * 2
                    kt = kts[ii]
                    for c in range(2):
                        bk = base + c
                        k.op("pe", "matmul", out=pS.vs(pS.t[:, bk * N:bk * N + n], [bk]),
                             lhsT=KT.v(KT.t[:, h, kt * 128:(kt + 1) * 128]),
                             rhs=qt.v(qt.t[:, h, c, 0:n]), start=True, stop=True)

                qk(0)
                for ii, kt in enumerate(kts):
                    if ii + 1 < len(kts):
                        qk(ii + 1)
                    base = ((iP + ii) % 2) * 2
                    pbuf = pb[(iP + ii) % 3]
                    src3 = pS.t[:, base * N:(base + 2) * N].rearrange("p (j t) -> p j t", j=2)[:, :, 0:n]
                    k.op("act", "activation", out=pbuf.v(pbuf.t[:, :, 0:n]), in_=pS.vs(src3, [base, base + 1]),
                         func=AF.Exp, scale=0.125)
                    for c in range(2):
                        k.op("pe", "matmul", out=pO[c].v(pO[c].t[:, 0:n]), lhsT=V.v(V.t[:, kt, h * 128:(h + 1) * 128]),
                             rhs=pbuf.v(pbuf.t[:, c, 0:n]), start=(ii == 0), stop=(ii == len(kts) - 1))
                    ae = ii % 2
                    k.op("dve" if ae == 0 else "pool", "tensor_tensor", out=acc[ae].v(acc[ae].t[:, :, 0:n]),
                         in0=acc[ae].v(acc[ae].t[:, :, 0:n]), in1=pbuf.v(pbuf.t[:, :, 0:n]), op=ALU.add)
                iP += len(kts)
                k.op("dve", "tensor_tensor", out=acc[0].v(acc[0].t[:, :, 0:n]), in0=acc[0].v(acc[0].t[:, :, 0:n]),
                     in1=acc[1].v(acc[1].t[:, :, 0:n]), op=ALU.add)
                for c in range(2):
                    k.op("pe", "matmul", out=pZ.v(pZ.t[:, 0:n]), lhsT=self.ones32[:], rhs=acc[0].v(acc[0].t[:, c, 0:n]),
                         start=True, stop=True)
                    k.op("dve", "reciprocal", out=rz[c].v(rz[c].t[:, 0:n]), in_=pZ.v(pZ.t[:, 0:n]))
                    k.op("dve", "tensor_tensor", out=tt[c].v(tt[c].t[:, 0:n]), in0=pO[c].v(pO[c].t[:, 0:n]),
                         in1=rz[c].v(rz[c].t[:, 0:n]), op=ALU.mult)
                k.op("dve", "scalar_tensor_tensor", out=osb.v(osb.t[:, 0:n]), in0=tt[1].v(tt[1].t[:, 0:n]),
                     scalar=neglam[:, 0:1], in1=tt[0].v(tt[0].t[:, 0:n]), op0=ALU.mult, op1=ALU.add)
                k.op("act", "activation", out=sq.v(sq.t[:, 0:n]), in_=osb.v(osb.t[:, 0:n]), func=AF.Square)
                ps = nb["ps"]
                k.op("pe", "matmul", out=ps.v(ps.t[:, 0:n]), lhsT=self.ones_bf[:], rhs=sq.v(sq.t[:, 0:n]),
                     start=True, stop=True)
                rs, rstd = nb["rs"], nb["rstd"]
                k.op("act", "activation", out=rs.v(rs.t[:, 0:n]), in_=ps.v(ps.t[:, 0:n]), func=AF.Sqrt,
                     scale=1.0 / 128, bias=self.epsb[:, 0:1])
                k.op("dve", "reciprocal", out=rstd.v(rstd.t[:, 0:n]), in_=rs.v(rs.t[:, 0:n]))
                k.op("dve", "scalar_tensor_tensor", out=y1.v(y1.t[:, 0:n]), in0=osb.v(osb.t[:, 0:n]),
                     scalar=g2[:, 0:1], in1=rstd.v(rstd.t[:, 0:n]), op0=ALU.mult, op1=ALU.mult)
                row = ym_row0 + h * 128
                k.dma("pool", YM.v("all", YM.t[row:row + 128, t0:t0 + n]), y1.v(y1.t[:, 0:n]))


M.diffattn = diffattn


def odd_layer(self, li, last):
    k = self.k
    j = li // 2
    self.in_proj(li, self.od_w_in, j, 3072, OD_F, OD_T, self.PT, self.PK)
    k.barrier()
    if self.cfg.phases is None or "hyena" in self.cfg.phases:
        self.hyena(j, self.PT, self.YM, not last)
    else:
        self.zero_ym(0, 512)
    k.barrier()
    self.diffattn(li, j, self.PT, self.PK, self.YM, 12, 16, 512, not last)
    k.barrier()
    self.out_proj(li, self.od_w_out, j, self.YM)
    k.barrier()


M.odd_layer = odd_layer


def hy_dims(Lseg):
    N = 2 * Lseg
    lg = int(round(math.log2(N)))
    N2 = 1 << ((lg + 1) // 2)
    N1 = N // N2
    H1 = N1 // 2
    NSQ = 256 // max(N1, N2)
    return N, N1, N2, H1, NSQ


def hy_consts(Lseg, pfx):
    N, N1, N2, H1, NSQ = hy_dims(Lseg)
    f64 = np.float64
    c = {}
    n1 = np.arange(H1, dtype=f64)[:, None]
    k1 = np.arange(N1, dtype=f64)[None, :]
    a = 2 * np.pi * n1 * k1 / N1
    c["F1cat"] = np.concatenate([np.cos(a), -np.sin(a)], 1)
    n1f = np.arange(N1, dtype=f64)[:, None]
    af = 2 * np.pi * n1f * k1 / N1
    c["F1cat"] = np.concatenate([np.cos(af), -np.sin(af)], 1)
    n2 = np.arange(N2, dtype=f64)[:, None]
    a = 2 * np.pi * n2 * k1 / N
    twc, tws = np.cos(a), -np.sin(a)
    c["TwA"] = np.tile(np.concatenate([twc, twc], 1)[:, None, :], (1, NSQ, 1))
    c["TwB"] = np.tile(np.concatenate([-tws, tws], 1)[:, None, :], (1, NSQ, 1))
    k2 = np.arange(N2, dtype=f64)[None, :]
    a = 2 * np.pi * n2 * k2 / N2
    c["F2c"], c["F2s"], c["F2sn"] = np.cos(a), -np.sin(a), np.sin(a)
    c["G2cat"] = np.concatenate([np.cos(a), np.sin(a)], 1)
    c["G2cat2"] = np.concatenate([-np.sin(a), np.cos(a)], 1)
    kk1 = np.arange(N1, dtype=f64)[:, None]
    nn2 = np.arange(N2, dtype=f64)[None, :]
    a = 2 * np.pi * kk1 * nn2 / N
    tc, ts = np.cos(a), np.sin(a)
    c["TwAi"] = np.tile(np.concatenate([tc, tc], 1)[:, None, :], (1, NSQ, 1))
    c["TwBi"] = np.tile(np.concatenate([-ts, ts], 1)[:, None, :], (1, NSQ, 1))
    nn1 = np.arange(H1, dtype=f64)[None, :]
    a = 2 * np.pi * kk1 * nn1 / N1
    c["G1c"], c["G1sn"] = np.cos(a) / N, -np.sin(a) / N
    pos = np.arange(Lseg, dtype=np.float32)
    t = pos / np.float32(max(Lseg - 1, 1))
    w = np.float32(2 * math.pi) * pos / np.float32(Lseg)
    bands = np.linspace(1e-4, 15, 16, dtype=np.float32)
    z = np.concatenate([t[:, None], np.cos(w[:, None] * bands), -np.sin(w[:, None] * bands)], -1).astype(np.float32)
    deltas = np.abs(np.linspace(math.log(1e-2) / 0.3, math.log(1e-2) / 1.5, 512, dtype=np.float32))
    dec = np.exp(-t[None, :] * deltas[:, None])
    idx = (Lseg - np.arange(Lseg)) % Lseg
    zrev = z[idx]
    decrev = dec[:, idx].copy()
    decrev[:, 0] = 0.0
    c["zT"] = np.concatenate([z.T, zrev.T], 1)
    c["decT"] = np.concatenate([dec, decrev], 1)
    return {pfx + k_: np.ascontiguousarray(v.astype(np.float32)) for k_, v in c.items()}


HY_SHAPES = lambda N, N1, N2, H1, NSQ, Lseg: {
    "F1cat": [N1, 2 * N1], "TwA": [N2, NSQ, 2 * N1], "TwB": [N2, NSQ, 2 * N1], "F2c": [N2, N2], "F2s": [N2, N2],
    "F2sn": [N2, N2], "G2cat": [N2, 2 * N2], "G2cat2": [N2, 2 * N2], "TwAi": [N1, NSQ, 2 * N2],
    "TwBi": [N1, NSQ, 2 * N2], "G1c": [N1, H1], "G1sn": [N1, H1], "zT": [33, 2 * Lseg], "decT": [512, 2 * Lseg]}


def declare_hy(self):
    cfg = self.cfg
    no = max(cfg.depth // 2, 1)
    self.hyc = {}
    for pfx, Lseg in (("hm_", cfg.L), ("hc_", C)):
        dims = hy_dims(Lseg)
        for nm, sh in HY_SHAPES(*dims, Lseg).items():
            self.hyc[pfx + nm] = self.inp(pfx + nm, sh)
    self.od_conv_w = self.inp("od_conv_wT", [no, 128, 12, 3])
    self.od_conv_b = self.inp("od_conv_bT", [no, 128, 12])
    self.od_f_w1 = self.inp("od_f_w1", [no, 33, 64])
    self.od_f_b1 = self.inp("od_f_b1T", [no, 64, 1])
    self.od_f_w2 = self.inp("od_f_w2", [no, 64, 64])
    self.od_f_b2 = self.inp("od_f_b2T", [no, 64, 1])
    self.od_f_w3 = self.inp("od_f_w3", [no, 64, 2048])
    self.od_hy_bias = self.inp("od_hy_biasB", [no, 128, 1024])
    self.UC = self.scratch("UC", [1536, cfg.T], dbg=True)
    self.HT = self.scratch("HT", [1024, 2 * cfg.L], dbg=True)
    self.HTc = self.scratch("HTc", [1024, 2 * C], dbg=True)


M.declare_hy = declare_hy


def hy_shortconv(self, j, PT, ctx_out):
    k, cfg = self.k, self.cfg
    BL = 2048
    with contextlib.ExitStack() as es:
        cw = k.sbuf(es, "sc_w", [128, 12, 3], F32)
        cb = k.sbuf(es, "sc_b", [128, 12], F32)
        k.dma("sp", cw[:], self.od_conv_w.v(0, self.od_conv_w.t[j, :, :, :]))
        k.dma("sp", cb[:], self.od_conv_b.v(0, self.od_conv_b.t[j, :, :]))
        ub = [k.sbuf(es, f"sc_u{i}", [128, BL + 2], F32) for i in range(2)]
        ac = [k.sbuf(es, f"sc_a{i}", [128, BL], F32) for i in range(2)]
        it = 0
        segs = [(C, cfg.L)] + ([(0, C)] if ctx_out else [])
        for (toff, Lseg) in segs:
            for c in range(12):
                for b0 in range(0, Lseg, BL):
                    n = min(BL, Lseg - b0)
                    u, a = ub[it % 2], ac[it % 2]
                    it += 1
                    lo = max(b0 - 1, 0)
                    hi = min(b0 + n + 1, Lseg)
                    if b0 == 0:
                        k.op("pool", "memset", ap=u.v(u.t[:, 0:1]), constant=0.0)
                    if b0 + n == Lseg:
                        k.op("pool", "memset", ap=u.v(u.t[:, n + 1:n + 2]), constant=0.0)
                    k.dma("sp", u.v(u.t[:, lo - b0 + 1:hi - b0 + 1]),
                          PT.v("all", PT.t[c * 128:(c + 1) * 128, toff + lo:toff + hi]))
                    k.op("dve", "tensor_scalar", out=a.v(a.t[:, 0:n]), in0=u.v(u.t[:, 0:n]), scalar1=cw.v(cw.t[:, c, 0:1]),
                         scalar2=cb.v(cb.t[:, c:c + 1]), op0=ALU.mult, op1=ALU.add)
                    k.op("dve", "scalar_tensor_tensor", out=a.v(a.t[:, 0:n]), in0=u.v(u.t[:, 1:n + 1]),
                         scalar=cw.v(cw.t[:, c, 1:2]), in1=a.v(a.t[:, 0:n]), op0=ALU.mult, op1=ALU.add)
                    k.op("dve", "scalar_tensor_tensor", out=a.v(a.t[:, 0:n]), in0=u.v(u.t[:, 2:n + 2]),
                         scalar=cw.v(cw.t[:, c, 2:3]), in1=a.v(a.t[:, 0:n]), op0=ALU.mult, op1=ALU.add)
                    k.dma("pool", self.UC.v("all", self.UC.t[c * 128:(c + 1) * 128, toff + b0:toff + b0 + n]),
                          a.v(a.t[:, 0:n]))


M.hy_shortconv = hy_shortconv


def hy_filters(self, j, pfx, Lseg, HT):
    k = self.k
    N = 512
    with contextlib.ExitStack() as es:
        sb = lambda nm, sh, dt=F32: k.sbuf(es, "hf_" + nm, sh, dt)
        w1, w2, w3 = sb("w1", [33, 64]), sb("w2", [64, 64]), sb("w3", [64, 2048])
        bb = sb("bb", [64, 2])
        bh, bq = sb("bh", [64, 2]), sb("bq", [64, 2])
        k.dma("sp", w1[:], self.od_f_w1.v(0, self.od_f_w1.t[j, :, :]))
        k.dma("sp", w2[:], self.od_f_w2.v(0, self.od_f_w2.t[j, :, :]))
        k.dma("sp", w3[:], self.od_f_w3.v(0, self.od_f_w3.t[j, :, :]))
        k.dma("sp", bb.v(bb.t[:, 0:1]), self.od_f_b1.v(0, self.od_f_b1.t[j, :, :]))
        k.dma("sp", bb.v(bb.t[:, 1:2]), self.od_f_b2.v(0, self.od_f_b2.t[j, :, :]))
        k.op("dve", "tensor_scalar", out=bh[:], in0=bb[:], scalar1=0.5, scalar2=None, op0=ALU.mult)
        k.op("dve", "tensor_scalar", out=bq[:], in0=bb[:], scalar1=0.25, scalar2=None, op0=ALU.mult)
        zT = [sb(f"zT{i}", [33, N]) for i in range(2)]
        dect = [sb(f"dec{i}", [128, 4, N]) for i in range(2)]
        a1, a2, tq = sb("a1", [64, N]), sb("a2", [64, N]), sb("tq", [64, N])
        hid = [sb(f"hid{i}", [64, N]) for i in range(2)]
        hsb = [sb(f"hsb{i}", [128, N]) for i in range(3)]
        p12 = [k.psum(es, f"hf_p{i}", [64, N]) for i in range(2)]
        ph = [k.psum(es, f"hf_ph{i}", [128, N]) for i in range(3)]
        zc, dc = self.hyc[pfx + "zT"], self.hyc[pfx + "decT"]
        dcr = dc.t.rearrange("(c p) t -> p c t", p=128)
        io = 0
        for ti, t0 in enumerate(range(0, 2 * Lseg, min(N, Lseg))):
            n = min(N, Lseg)
            dr_ = t0 // Lseg
            z, de = zT[ti % 2], dect[ti % 2]
            k.dma("sp", z.v(z.t[:, 0:n]), zc.v(0, zc.t[:, t0:t0 + n]))
            k.dma("sp", de.v(de.t[:, :, 0:n]), dc.v(0, dcr[:, :, t0:t0 + n]))
            cur = z.v(z.t[:, 0:n])
            for layer, (w, kk) in enumerate(((w1, 33), (w2, 64))):
                p = p12[layer]
                k.op("pe", "matmul", out=p.v(p.t[:, 0:n]), lhsT=w.v(w.t[0:kk, :]), rhs=cur, start=True, stop=True)
                k.op("act", "activation", out=a1.v(a1.t[:, 0:n]), in_=p.v(p.t[:, 0:n]), func=AF.Sin,
                     bias=bh.v(bh.t[:, layer:layer + 1]), scale=0.5)
                k.op("act", "activation", out=a2.v(a2.t[:, 0:n]), in_=p.v(p.t[:, 0:n]), func=AF.Sin,
                     bias=bq.v(bq.t[:, layer:layer + 1]), scale=0.25)
                k.op("dve", "tensor_tensor", out=tq.v(tq.t[:, 0:n]), in0=a2.v(a2.t[:, 0:n]), in1=a2.v(a2.t[:, 0:n]),
                     op=ALU.mult)
                k.op("dve", "tensor_scalar", out=tq.v(tq.t[:, 0:n]), in0=tq.v(tq.t[:, 0:n]), scalar1=-2.0, scalar2=1.0,
                     op0=ALU.mult, op1=ALU.add)
                hd = hid[layer]
                k.op("dve", "scalar_tensor_tensor", out=hd.v(hd.t[:, 0:n]), in0=a1.v(a1.t[:, 0:n]), scalar=2.0,
                     in1=tq.v(tq.t[:, 0:n]), op0=ALU.mult, op1=ALU.mult)
                cur = hd.v(hd.t[:, 0:n])
            for o_ in range(2):
                for c4 in range(4):
                    p, hs = ph[io % 3], hsb[io % 3]
                    io += 1
                    wc = o_ * 1024 + dr_ * 512 + c4 * 128
                    k.op("pe", "matmul", out=p.v(p.t[:, 0:n]), lhsT=w3.v(w3.t[:, wc:wc + 128]), rhs=cur,
                         start=True, stop=True)
                    k.op("dve", "tensor_tensor", out=hs.v(hs.t[:, 0:n]), in0=p.v(p.t[:, 0:n]),
                         in1=de.v(de.t[:, c4, 0:n]), op=ALU.mult)
                    rr = o_ * 512 + c4 * 128
                    k.dma("pool", HT.v("all", HT.t[rr:rr + 128, t0:t0 + n]), hs.v(hs.t[:, 0:n]))


M.hy_filters = hy_filters


def hy_conv(self, j, pfx, Lseg, toff, HT, YM):
    k = self.k
    N, N1, N2, H1, NSQ = hy_dims(Lseg)
    NSLOT = 2
    with contextlib.ExitStack() as es:
        sb = lambda nm, sh, dt=F32: k.sbuf(es, "hv_" + nm, sh, dt)
        cst = {}
        for nm, sh in HY_SHAPES(N, N1, N2, H1, NSQ, Lseg).items():
            if nm in ("zT", "decT"):
                continue
            cst[nm] = sb(nm, sh)
            d = self.hyc[pfx + nm]
            k.dma("sp", cst[nm][:], d.v(0, d.t))
        hb = sb("hb", [128, 1024])
        k.dma("sp", hb[:], self.od_hy_bias.v(0, self.od_hy_bias.t[j, :, :]))
        slots = []
        for sl in range(NSLOT):
            B_ = {}
            B_["hfl"] = [sb(f"hfl{sl}_{i}", [N1, NSQ, N2]) for i in range(2)]
            B_["dat"] = [sb(f"dat{sl}_{q}", [H1, NSQ, N2]) for q in range(3)]
            B_["Xs"] = [sb(f"Xs{sl}_{i}", [N2, NSQ, 2 * N1]) for i in range(2)]
            B_["KA"] = [sb(f"KA{sl}_{i}", [N2, NSQ, 2 * N1]) for i in range(2)]
            B_["KB"] = [sb(f"KB{sl}_{i}", [N2, NSQ, 2 * N1]) for i in range(2)]
            B_["B"] = sb(f"B{sl}", [N2, NSQ, 2 * N1])
            B_["tmf"] = sb(f"tmf{sl}", [N2, NSQ, 2 * N1])
            B_["Y"] = sb(f"Y{sl}", [N2, NSQ, 2 * N1])
            B_["D"] = sb(f"D{sl}", [N1, NSQ, 2 * N2])
            B_["tmi"] = sb(f"tmi{sl}", [N1, NSQ, 2 * N2])
            B_["zmid"] = sb(f"zmid{sl}", [H1, NSQ, N2])
            B_["zout"] = sb(f"zout{sl}", [H1, NSQ, N2], BF16)
            B_["pA"] = k.psum(es, f"hv_pA{sl}", [128, 512])
            B_["pX"] = k.psum(es, f"hv_pX{sl}", [128, 512])
            B_["pC"] = k.psum(es, f"hv_pC{sl}", [128, 512])
            B_["pY"] = k.psum(es, f"hv_pY{sl}", [128, 512])
            B_["alt"] = 0
            slots.append(B_)

        def v3(tile_, P, W):
            return tile_.t[0:P, 0:NSQ * W].rearrange("p (s w) -> p s w", s=NSQ)

        def add_eng(S_):
            S_["alt"] += 1
            return "pool" if S_["alt"] % 3 else "dve"

        def fwd_fft(S_, zb, rows=H1):
            pa, px, B, tm = S_["pA"], S_["pX"], S_["B"], S_["tmf"]
            A3 = v3(pa, N2, 2 * N1)
            X3 = v3(px, N2, 2 * N1)
            f1 = cst["F1cat"]
            for s in range(NSQ):
                k.op("pe", "matmul", out=pa.v(A3[:, s, :]), lhsT=zb.v(zb.t[0:rows, s, :]), rhs=f1.v(f1.t[0:rows, :]),
                     start=True, stop=True)
            yield
            k.op("dve", "tensor_tensor", out=B[:], in0=pa.v(A3), in1=cst["TwA"][:], op=ALU.mult)
            k.op("dve", "tensor_tensor", out=tm.v(tm.t[:, :, 0:N1]), in0=pa.v(A3[:, :, N1:2 * N1]),
                 in1=cst["TwB"].v(cst["TwB"].t[:, :, 0:N1]), op=ALU.mult)
            k.op("dve", "tensor_tensor", out=tm.v(tm.t[:, :, N1:2 * N1]), in0=pa.v(A3[:, :, 0:N1]),
                 in1=cst["TwB"].v(cst["TwB"].t[:, :, N1:2 * N1]), op=ALU.mult)
            yield
            k.op(add_eng(S_), "tensor_tensor", out=B[:], in0=B[:], in1=tm[:], op=ALU.add)
            yield
            Br, Bi = B.v(B.t[:, :, 0:N1]), B.v(B.t[:, :, N1:2 * N1])
            k.op("pe", "matmul", out=px.v(X3[:, :, 0:N1]), lhsT=cst["F2c"][:], rhs=Br, start=True, stop=False)
            k.op("pe", "matmul", out=px.v(X3[:, :, 0:N1]), lhsT=cst["F2sn"][:], rhs=Bi, start=False, stop=True)
            k.op("pe", "matmul", out=px.v(X3[:, :, N1:2 * N1]), lhsT=cst["F2s"][:], rhs=Br, start=True, stop=False)
            k.op("pe", "matmul", out=px.v(X3[:, :, N1:2 * N1]), lhsT=cst["F2c"][:], rhs=Bi, start=False, stop=True)
            yield

        def inv_fft(S_, Y):
            pC, pY, Dt, tmi = S_["pC"], S_["pY"], S_["D"], S_["tmi"]
            C3 = v3(pC, N1, 2 * N2)
            for s in range(NSQ):
                k.op("pe", "matmul", out=pC.v(C3[:, s, :]), lhsT=Y.v(Y.t[:, s, 0:N1]), rhs=cst["G2cat"][:],
                     start=True, stop=False)
                k.op("pe", "matmul", out=pC.v(C3[:, s, :]), lhsT=Y.v(Y.t[:, s, N1:2 * N1]), rhs=cst["G2cat2"][:],
                     start=False, stop=True)
            yield
            k.op("dve", "tensor_tensor", out=Dt[:], in0=pC.v(C3), in1=cst["TwAi"][:], op=ALU.mult)
            k.op("dve", "tensor_tensor", out=tmi.v(tmi.t[:, :, 0:N2]), in0=pC.v(C3[:, :, N2:2 * N2]),
                 in1=cst["TwBi"].v(cst["TwBi"].t[:, :, 0:N2]), op=ALU.mult)
            k.op("dve", "tensor_tensor", out=tmi.v(tmi.t[:, :, N2:2 * N2]), in0=pC.v(C3[:, :, 0:N2]),
                 in1=cst["TwBi"].v(cst["TwBi"].t[:, :, N2:2 * N2]), op=ALU.mult)
            yield
            k.op(add_eng(S_), "tensor_tensor", out=Dt[:], in0=Dt[:], in1=tmi[:], op=ALU.add)
            yield
            Y3 = pY.t[0:H1, 0:NSQ * N2].rearrange("p (s w) -> p s w", s=NSQ)
            k.op("pe", "matmul", out=pY.v(Y3), lhsT=cst["G1c"][:], rhs=Dt.v(Dt.t[:, :, 0:N2]), start=True, stop=False)
            k.op("pe", "matmul", out=pY.v(Y3), lhsT=cst["G1sn"][:], rhs=Dt.v(Dt.t[:, :, N2:2 * N2]), start=False, stop=True)
            yield

        def blk(dt_, row0):
            return dt_.t[row0:row0 + NSQ, :].rearrange("s (a b) -> a s b", b=N2)

        def group(g, S_):
            ch0 = g * NSQ
            dd = S_["dat"]
            Xs, KA, KB = S_["Xs"], S_["KA"], S_["KB"]
            px = S_["pX"]
            X3 = v3(px, N2, 2 * N1)
            pY = S_["pY"]
            Y3 = pY.t[0:H1, 0:NSQ * N2].rearrange("p (s w) -> p s w", s=NSQ)
            for q in range(3):
                k.dma("sp", dd[q][:], self.UC.v("all", self.UC.t[q * 512 + ch0:q * 512 + ch0 + NSQ,
                                                                 toff:toff + Lseg].rearrange("s (a b) -> a s b", b=N2)))
            for o in range(2):
                hf = S_["hfl"][o]
                k.dma("sp", hf[:], HT.v("all", blk(HT, o * 512 + ch0)))
            yield
            for o in range(2):
                hf = S_["hfl"][o]
                yield from fwd_fft(S_, hf, N1)
                ka, kb = KA[o], KB[o]
                for s in range(NSQ):
                    ci = o * 512 + ch0 + s
                    k.op("act", "activation", out=ka.v(ka.t[:, s, 0:N1]), in_=px.v(X3[:, s, 0:N1]), func=AF.Identity,
                         bias=hb.v(hb.t[0:N2, ci:ci + 1]), scale=1.0)
                k.op("act", "copy", out=kb.v(kb.t[:, :, N1:2 * N1]), in_=px.v(X3[:, :, N1:2 * N1]))
                k.op("act", "activation", out=kb.v(kb.t[:, :, 0:N1]), in_=px.v(X3[:, :, N1:2 * N1]), func=AF.Copy,
                     scale=-1.0)
                yield
                k.op("pool", "tensor_copy", out=ka.v(ka.t[:, :, N1:2 * N1]), in_=ka.v(ka.t[:, :, 0:N1]))
                yield
            zcur = dd[0]
            Yt, tm = S_["Y"], S_["tmf"]
            for o in range(2):
                yield from fwd_fft(S_, zcur)
                ka, kb = KA[o], KB[o]
                k.op("dve", "tensor_tensor", out=Yt[:], in0=px.v(X3), in1=ka[:], op=ALU.mult)
                k.op("dve", "tensor_tensor", out=tm.v(tm.t[:, :, 0:N1]), in0=px.v(X3[:, :, N1:2 * N1]),
                     in1=kb.v(kb.t[:, :, 0:N1]), op=ALU.mult)
                k.op("dve", "tensor_tensor", out=tm.v(tm.t[:, :, N1:2 * N1]), in0=px.v(X3[:, :, 0:N1]),
                     in1=kb.v(kb.t[:, :, N1:2 * N1]), op=ALU.mult)
                yield
                k.op(add_eng(S_), "tensor_tensor", out=Yt[:], in0=Yt[:], in1=tm[:], op=ALU.add)
                yield
                yield from inv_fft(S_, Yt)
                if o == 0:
                    k.op("dve", "tensor_tensor", out=S_["zmid"][:], in0=pY.v(Y3), in1=dd[1][:], op=ALU.mult)
                    zcur = S_["zmid"]
                else:
                    zo = S_["zout"]
                    k.op("dve", "tensor_tensor", out=zo[:], in0=pY.v(Y3), in1=dd[2][:], op=ALU.mult)
                    k.dma("pool", YM.v("all", YM.t[ch0:ch0 + NSQ, toff:toff + Lseg].rearrange("s (a b) -> a s b", b=N2)),
                          zo[:])
                yield

        ngroups = 512 // NSQ
        nxt = 0
        active = []
        for sl in range(NSLOT):
            if nxt < ngroups:
                active.append([group(nxt, slots[sl]), sl])
                nxt += 1
        while active:
            for ent in list(active):
                try:
                    next(ent[0])
                except StopIteration:
                    if nxt < ngroups:
                        ent[0] = group(nxt, slots[ent[1]])
                        nxt += 1
                    else:
                        active.remove(ent)


M.hy_conv = hy_conv


def hyena(self, j, PT, YM, ctx_out):
    k, cfg = self.k, self.cfg
    self.hy_shortconv(j, PT, ctx_out)
    k.barrier()
    self.hy_filters(j, "hm_", cfg.L, self.HT)
    k.barrier()
    if ctx_out:
        self.hy_filters(j, "hc_", C, self.HTc)
        k.barrier()
    self.hy_conv(j, "hm_", cfg.L, C, self.HT, YM)
    k.barrier()
    if ctx_out:
        self.hy_conv(j, "hc_", C, 0, self.HTc, YM)
        k.barrier()


M.hyena = hyena

SEQ = 8192
DEPTH = 4
BATCH = 4


def host_inputs(inp, b, L, depth):
    d = {}
    d["x"] = np.ascontiguousarray(inp["x"][b])
    d["ctx"] = np.ascontiguousarray(inp["ctx"][b])
    d["cc"] = np.ascontiguousarray(np.stack([inp["c"][b], inp["c_ctx"]], -1).reshape(8, 128, 2).transpose(1, 0, 2))
    d["w_ada"] = inp["w_ada"]
    d["b_adaT"] = np.ascontiguousarray(inp["b_ada"].reshape(depth, 48, 128).transpose(0, 2, 1))
    d["norm_gT"] = np.ascontiguousarray(inp["norm_g"].reshape(depth, 32, 128).transpose(0, 2, 1))
    d["w_mlp_in"] = inp["w_mlp_in"]
    d["w_mlp_out"] = inp["w_mlp_out"]
    d["ev_w_in"] = inp["ev_w_in"]
    d["ev_w_out"] = inp["ev_w_out"]
    g = inp["ev_qk_g"]
    d["ev_qk_gT"] = np.ascontiguousarray(np.tile(g, (1, 1, 2)).transpose(0, 2, 1))
    d["od_w_in"] = inp["od_w_in"]
    d["od_w_out"] = inp["od_w_out"]
    no = inp["od_lam"].shape[0]
    d["od_lamB"] = np.ascontiguousarray(np.broadcast_to(inp["od_lam"].reshape(no, 1, 256), (no, 128, 256)))
    d["od_diff_gT"] = np.ascontiguousarray(inp["od_diff_g"][:, :, None])
    d["od_conv_wT"] = np.ascontiguousarray(
        inp["od_conv_w"].transpose(0, 2, 1).reshape(no, 12, 128, 3).transpose(0, 2, 1, 3))
    d["od_conv_bT"] = np.ascontiguousarray(inp["od_conv_b"].reshape(no, 12, 128).transpose(0, 2, 1))
    d["od_f_w1"] = inp["od_f_w1"]
    d["od_f_w2"] = inp["od_f_w2"]
    d["od_f_w3"] = inp["od_f_w3"]
    d["od_f_b1T"] = np.ascontiguousarray(inp["od_f_b1"][:, :, None])
    d["od_f_b2T"] = np.ascontiguousarray(inp["od_f_b2"][:, :, None])
    d["od_hy_biasB"] = np.ascontiguousarray(np.broadcast_to(inp["od_hy_bias"].reshape(no, 1, 1024), (no, 128, 1024)))
    d["ev_wlr_aug"] = np.ascontiguousarray(np.concatenate([inp["ev_w_lr"], inp["ev_b_lr"][:, :, None, :]], axis=2))
    d["ev_gla_gT"] = np.ascontiguousarray(inp["ev_gla_g"][:, :, None])
    return d


def const_inputs(L):
    d = {}
    d.update(hy_consts(L, "hm_"))
    d.update(hy_consts(C, "hc_"))
    d.update(gla_consts())
    d.update(host_consts())
    d["rope"] = rope_tables(L)
    d["rotm"] = rot_matrix()
    d["blk64"] = block_ones(64)
    return d


def kernel(**inputs):
    inp = {k_: np.asarray(v) for k_, v in inputs.items()}
    B, L, _ = inp["x"].shape
    depth = inp["w_ada"].shape[0]
    cfg = Cfg(L=L, depth=depth, debug=False)
    mm = M(cfg)
    nc = mm.build2()
    consts = const_inputs(L)
    n_cores = 8
    per_b = [dict(host_inputs(inp, b, L, depth), **consts) for b in range(B)]
    in_maps = [per_b[i % B] for i in range(n_cores)]
    res = run_bass_kernel_spmd(nc, in_maps, core_ids=list(range(n_cores)))
    out = np.stack([np.asarray(res.results[b]["out"]) for b in range(B)], axis=0)
    return out.astype(np.float32, copy=False)
```

```python
import math
import contextlib
import numpy as np
import concourse.bass as bass
import concourse.mybir as mybir
from concourse.bass_utils import run_bass_kernel_spmd

F32 = mybir.dt.float32
BF16 = mybir.dt.bfloat16
AF = mybir.ActivationFunctionType
ALU = mybir.AluOpType


class Buf:
    __slots__ = ("w", "r", "name")

    def __init__(self, name=""):
        self.w = None
        self.r = {}
        self.name = name


class View:
    __slots__ = ("ap", "bufs")

    def __init__(self, ap, bufs):
        self.ap = ap
        self.bufs = tuple(bufs)


class Tile:
    def __init__(self, t, name, nsub=0):
        self.t = t
        self.buf = Buf(name)
        self.subs = [Buf(f"{name}.{i}") for i in range(nsub)]

    def __getitem__(self, idx):
        return View(self.t[idx], (self.buf,))

    def v(self, ap):
        return View(ap, (self.buf,))

    def vs(self, ap, idxs):
        return View(ap, [self.subs[i] for i in idxs])


class DTile:
    def __init__(self, ap, name):
        self.t = ap
        self.name = name
        self.bufs = {}

    def b(self, key):
        if key not in self.bufs:
            self.bufs[key] = Buf(f"{self.name}:{key}")
        return self.bufs[key]

    def v(self, keys, ap):
        if not isinstance(keys, (list, tuple)):
            keys = [keys]
        return View(ap, [self.b(k) for k in keys])


class Eng:
    def __init__(self, name, h, semidx):
        self.name = name
        self.h = h
        self.semidx = semidx
        self.count = 0
        self.waited = {}


class K:
    WRITE_KEYS = ("out", "accum_out", "ap")

    def __init__(self, nc, es, n_dma=24):
        self.nc = nc
        self.es = es
        self.sems = []
        self.E = {}
        for name, h in [("pe", nc.tensor), ("act", nc.scalar), ("dve", nc.vector),
                        ("pool", nc.gpsimd), ("sp", nc.sync)]:
            self.sems.append(es.enter_context(nc.semaphore("s_" + name)))
            self.E[name] = Eng(name, h, len(self.sems) - 1)
        self.dma_sem = []
        self.dma_val = []
        for i in range(2 * n_dma):
            self.sems.append(es.enter_context(nc.semaphore(f"d{i}")))
            self.dma_sem.append(len(self.sems) - 1)
            self.dma_val.append(0)
        self.n_dma = n_dma
        self.dma_next = {"sp": 0, "pool": 0, "act": 0}
        self.ninst = 0

    def sbuf(self, es, name, shape, dtype, nsub=0):
        self.nalloc = getattr(self, "nalloc", 0) + 1
        name = f"sb{self.nalloc}_{name}"
        return Tile(es.enter_context(self.nc.sbuf_tensor(name, list(shape), dtype)), name, nsub)

    def psum(self, es, name, shape, dtype=F32, nsub=0):
        self.nalloc = getattr(self, "nalloc", 0) + 1
        name = f"ps{self.nalloc}_{name}"
        return Tile(es.enter_context(self.nc.psum_tensor(name, list(shape), dtype)), name, nsub)

    def _wait(self, E, toks):
        need = {}
        for s, v in toks:
            if v > need.get(s, 0):
                need[s] = v
        for s, v in need.items():
            if E.name == "pe" and s == E.semidx:
                continue
            if E.waited.get(s, 0) < v:
                E.h.wait_ge(self.sems[s], v)
                E.waited[s] = v

    @staticmethod
    def _deps(reads, writes):
        toks = []
        for b in reads:
            if b.w is not None:
                toks.append(b.w)
        for b in writes:
            if b.w is not None:
                toks.append(b.w)
            toks.extend(b.r.items())
        return toks

    @staticmethod
    def _commit(tok, reads, writes):
        s, v = tok
        for b in reads:
            if b.r.get(s, 0) < v:
                b.r[s] = v
        for b in writes:
            b.w = tok
            b.r = {}

    def op(self, eng, name, _r=(), _w=(), **kw):
        E = self.E[eng]
        reads, writes, real = [], [], {}
        for k, v in kw.items():
            if isinstance(v, View):
                (writes if k in self.WRITE_KEYS else reads).extend(v.bufs)
                real[k] = v.ap
            else:
                real[k] = v
        for v in _r:
            reads.extend(v.bufs if isinstance(v, View) else [v])
        for v in _w:
            writes.extend(v.bufs if isinstance(v, View) else [v])
        self._wait(E, self._deps(reads, writes))
        ins = getattr(E.h, name)(**real)
        E.count += 1
        ins.then_inc(self.sems[E.semidx], 1)
        self._commit((E.semidx, E.count), reads, writes)
        self.ninst += 1
        return ins

    def dma(self, q, out, in_, **kw):
        E = self.E[q]
        i0 = self.dma_next[q]
        self.dma_next[q] = (i0 + 1) % self.n_dma
        i = i0 + (self.n_dma if q == "pool" else 0)
        s = self.dma_sem[i]
        toks = self._deps(in_.bufs, out.bufs)
        if self.dma_val[i] > 0:
            toks.append((s, self.dma_val[i]))
        self._wait(E, toks)
        ins = E.h.dma_start(out=out.ap, in_=in_.ap, **kw)
        self.dma_val[i] += 16
        ins.then_inc(self.sems[s], 16)
        self._commit((s, self.dma_val[i]), in_.bufs, out.bufs)
        self.ninst += 1
        return ins

    def barrier(self):
        toks = []
        for e2 in self.E.values():
            if e2.count > 0:
                toks.append((e2.semidx, e2.count))
        for i, s in enumerate(self.dma_sem):
            if self.dma_val[i] > 0:
                toks.append((s, self.dma_val[i]))
        for E in self.E.values():
            pe_self = [(s, v) for (s, v) in toks if not (s == E.semidx)]
            self._wait(E, pe_self)

    def finish(self):
        E = self.E["sp"]
        for i, s in enumerate(self.dma_sem):
            if self.dma_val[i] > 0 and E.waited.get(s, 0) < self.dma_val[i]:
                E.h.wait_ge(self.sems[s], self.dma_val[i])
                E.waited[s] = self.dma_val[i]
        for name in ("pe", "act", "dve", "pool"):
            e2 = self.E[name]
            if e2.count > 0 and E.waited.get(e2.semidx, 0) < e2.count:
                E.h.wait_ge(self.sems[e2.semidx], e2.count)


D = 1024
DFF = 4096
C = 256
EPS = 1e-6
NMOD = 6


class Cfg:
    def __init__(self, L=8192, depth=4, debug=False, phases=None):
        self.L = L
        self.T = C + L
        self.depth = depth
        self.debug = debug
        self.phases = phases


def tiles(cfg, n):
    out = []
    for s in range(0, C, n):
        out.append((s, min(n, C - s), 1))
    for s in range(0, cfg.L, n):
        out.append((C + s, min(n, cfg.L - s), 0))
    return out


def host_consts():
    c = {}
    c["ident"] = np.eye(128, dtype=np.float32)
    c["ones"] = np.ones((128, 128), dtype=np.float32)
    return c


class M:
    def __init__(self, cfg):
        self.cfg = cfg
        nc = bass.Bass("TRN2", target_bir_lowering=False)
        self.nc = nc
        self.es = contextlib.ExitStack()
        self.k = K(nc, self.es)
        self.din = {}
        self.dbg_out = []

    def inp(self, name, shape, dtype=F32):
        ap = self.nc.dram_tensor(name, list(shape), dtype, kind="ExternalInput").ap()
        d = DTile(ap, name)
        self.din[name] = d
        return d

    def scratch(self, name, shape, dtype=F32, dbg=False):
        kind = "ExternalOutput" if (dbg and self.cfg.debug) else "Internal"
        ap = self.nc.dram_tensor(name, list(shape), dtype, kind=kind).ap()
        if kind == "ExternalOutput":
            self.dbg_out.append(name)
        return DTile(ap, name)

    def outp(self, name, shape, dtype=F32):
        ap = self.nc.dram_tensor(name, list(shape), dtype, kind="ExternalOutput").ap()
        return DTile(ap, name)

    def declare(self):
        cfg = self.cfg
        L, T, dp = cfg.L, cfg.T, cfg.depth
        ne, no = (dp + 1) // 2, dp // 2
        self.x = self.inp("x", [L, D])
        self.ctx = self.inp("ctx", [C, D])
        self.cc = self.inp("cc", [128, 8, 2])
        self.w_ada = self.inp("w_ada", [dp, D, NMOD * D])
        self.b_ada = self.inp("b_adaT", [dp, 128, 48])
        self.norm_g = self.inp("norm_gT", [dp, 128, 32])
        self.w_mlp_in = self.inp("w_mlp_in", [dp, D, DFF])
        self.w_mlp_out = self.inp("w_mlp_out", [dp, DFF, D])
        self.c_ident = self.inp("ident", [128, 128])
        self.c_ones = self.inp("ones", [128, 128])
        self.out = self.outp("out", [L, D])
        self.XT = self.scratch("XT", [D, T], dbg=True)

    def load_consts(self):
        k, es = self.k, self.es
        self.ident = k.sbuf(es, "ident", [128, 128], F32)
        k.dma("sp", self.ident[:], self.c_ident.v(0, self.c_ident.t[:, :]))
        stg = k.sbuf(es, "ones32", [128, 128], F32)
        k.dma("sp", stg[:], self.c_ones.v(0, self.c_ones.t[:, :]))
        self.ones_bf = k.sbuf(es, "ones_bf", [128, 128], BF16)
        k.op("dve", "tensor_copy", out=self.ones_bf[:], in_=stg[:])
        self.ones32 = stg
        self.sc = k.sbuf(es, "sc", [128, 8, 2], F32)
        tmp = k.sbuf(es, "sc_raw", [128, 8, 2], F32)
        k.dma("sp", tmp[:], self.cc.v(0, self.cc.t[:, :, :]))
        k.op("act", "activation", out=self.sc[:], in_=tmp[:], func=AF.Silu)
        self.mod = k.sbuf(es, "mod", [128, 48, 2], F32)
        self.gl = k.sbuf(es, "gl", [128, 32], F32)
        self.coef = k.sbuf(es, "coef", [128, 6, 8, 2], F32)

    def transpose_in(self):
        k, cfg = self.k, self.cfg
        with contextlib.ExitStack() as es:
            xin = [k.sbuf(es, f"ti_x{i}", [128, D], F32) for i in range(2)]
            stg = [k.sbuf(es, f"ti_s{i}", [128, 8, 512], F32) for i in range(2)]
            ps = [k.psum(es, f"ti_p{i}", [128, 512], F32) for i in range(4)]
            XTr = self.XT.t.rearrange("(c p) t -> p c t", p=128)
            it = 0
            ip = 0
            for gi, (t0, n, isctx) in enumerate(tiles(cfg, 512)):
                sg = stg[gi % 2]
                for s in range(n // 128):
                    xi = xin[it % 2]
                    it += 1
                    tt = t0 + s * 128
                    if isctx:
                        src = self.ctx.v(0, self.ctx.t[tt:tt + 128, :])
                    else:
                        src = self.x.v(0, self.x.t[tt - C:tt - C + 128, :])
                    k.dma("sp", xi[:], src)
                    for half in range(2):
                        p = ps[ip % 4]
                        ip += 1
                        for q in range(4):
                            c = half * 4 + q
                            k.op("pe", "transpose", out=p[:, q * 128:(q + 1) * 128],
                                 in_=xi[:, c * 128:(c + 1) * 128], identity=self.ident[:])
                        eng = "act" if half == 0 else "dve"
                        nm = "copy" if half == 0 else "tensor_copy"
                        k.op(eng, nm,
                             out=sg.v(sg.t[:, half * 4:half * 4 + 4, s * 128:(s + 1) * 128]),
                             in_=p.v(p.t[:, :].rearrange("p (q t) -> p q t", q=4)))
                k.dma("pool", self.XT.v(("t", t0), XTr[:, :, t0:t0 + n]), sg.v(sg.t[:, :, 0:n]))

    def transpose_out(self):
        k, cfg = self.k, self.cfg
        with contextlib.ExitStack() as es:
            xin = [k.sbuf(es, f"to_x{i}", [128, 8, 512], F32) for i in range(2)]
            stg = [k.sbuf(es, f"to_s{i}", [128, D], F32) for i in range(2)]
            ps = [k.psum(es, f"to_p{i}", [128, 512], F32) for i in range(4)]
            XTr = self.XT.t.rearrange("(c p) t -> p c t", p=128)
            it = 0
            ip = 0
            for gi, (t0, n, isctx) in enumerate(tiles(cfg, 512)):
                if isctx:
                    continue
                xi = xin[gi % 2]
                k.dma("sp", xi.v(xi.t[:, :, 0:n]), self.XT.v(("t", t0), XTr[:, :, t0:t0 + n]))
                for s in range(n // 128):
                    sg = stg[it % 2]
                    it += 1
                    for half in range(2):
                        p = ps[ip % 4]
                        ip += 1
                        for q in range(4):
                            c = half * 4 + q
                            k.op("pe", "transpose", out=p[:, q * 128:(q + 1) * 128],
                                 in_=xi.v(xi.t[:, c, s * 128:(s + 1) * 128]), identity=self.ident[:])
                        eng = "act" if half == 0 else "dve"
                        nm = "copy" if half == 0 else "tensor_copy"
                        k.op(eng, nm, out=sg[:, half * 512:(half + 1) * 512], in_=p[:, :])
                    tt = t0 - C + s * 128
                    k.dma("pool", self.out.v(("t", tt), self.out.t[tt:tt + 128, :]), sg[:])

    def mod_vectors(self, li):
        k = self.k
        with contextlib.ExitStack() as es:
            wst = [k.sbuf(es, f"mv_w{i}", [128, 8, 512], F32) for i in range(2)]
            ps = k.psum(es, "mv_ps", [128, 96], F32)
            bsb = k.sbuf(es, "mv_b", [128, 48], F32)
            tmp = k.sbuf(es, "mv_t", [128, 8, 2], F32)
            k.dma("sp", bsb[:], self.b_ada.v(0, self.b_ada.t[li, :, :]))
            k.dma("sp", self.gl[:], self.norm_g.v(0, self.norm_g.t[li, :, :]))
            War = self.w_ada.t[li].rearrange("(kc p) n -> p kc n", p=128)
            for blk in range(12):
                w = wst[blk % 2]
                k.dma("sp", w[:], self.w_ada.v(0, War[:, :, blk * 512:(blk + 1) * 512]))
                for jj in range(4):
                    j = blk * 4 + jj
                    for kc in range(8):
                        k.op("pe", "matmul", out=ps[:, 2 * j:2 * j + 2],
                             lhsT=w.v(w.t[:, kc, jj * 128:(jj + 1) * 128]),
                             rhs=self.sc.v(self.sc.t[:, kc, :]), start=(kc == 0), stop=(kc == 7))
            for t in range(2):
                k.op("dve", "tensor_tensor", out=self.mod.v(self.mod.t[:, :, t]),
                     in0=ps.v(ps.t[:, :].rearrange("p (j t) -> p j t", t=2)[:, :, t]),
                     in1=bsb[:, :], op=ALU.add)
            mod, gl, coef = self.mod, self.gl, self.coef

            def mv(m):
                return mod.v(mod.t[:, m * 8:(m + 1) * 8, :])

            def gv(g):
                return gl.v(gl.t[:, g * 8:(g + 1) * 8])

            for (dst, mscale, gidx) in ((0, 1, 0), (3, 4, 2)):
                k.op("dve", "tensor_scalar", out=tmp[:], in0=mv(mscale), scalar1=1.0, scalar2=None, op0=ALU.add)
                for t in range(2):
                    k.op("dve", "tensor_tensor", out=coef.v(coef.t[:, dst, :, t]),
                         in0=tmp.v(tmp.t[:, :, t]), in1=gv(gidx), op=ALU.mult)
            for (dst, mshift) in ((1, 0), (4, 3)):
                k.op("dve", "tensor_copy", out=coef.v(coef.t[:, dst, :, :]), in_=mv(mshift))
            for (dst, mgate, gidx) in ((2, 2, 1), (5, 5, 3)):
                for t in range(2):
                    k.op("dve", "tensor_tensor", out=coef.v(coef.t[:, dst, :, t]),
                         in0=mod.v(mod.t[:, mgate * 8:(mgate + 1) * 8, t]), in1=gv(gidx), op=ALU.mult)

    def rstd_bc(self, es_bufs, src_chunks, n, nch, dim):
        k = self.k
        ps = es_bufs["ps"]
        for c in range(nch):
            sq = es_bufs["sq"][c % 2]
            k.op("act", "activation", out=sq.v(sq.t[:, 0:n]), in_=src_chunks(c), func=AF.Square)
            k.op("pe", "matmul", out=ps.v(ps.t[:, 0:n]), lhsT=self.ones_bf[:], rhs=sq.v(sq.t[:, 0:n]),
                 start=(c == 0), stop=(c == nch - 1))
        rs, rstd = es_bufs["rs"], es_bufs["rstd"]
        k.op("act", "activation", out=rs.v(rs.t[:, 0:n]), in_=ps.v(ps.t[:, 0:n]), func=AF.Sqrt,
             scale=1.0 / dim, bias=self.epsb[:, 0:1])
        k.op("dve", "reciprocal", out=rstd.v(rstd.t[:, 0:n]), in_=rs.v(rs.t[:, 0:n]))
        return rstd.v(rstd.t[:, 0:n])

    def mlp_layer(self, li):
        k, cfg = self.k, self.cfg
        N = 256
        with contextlib.ExitStack() as es:
            W1 = k.sbuf(es, "W1", [128, 8, DFF], BF16)
            W2 = k.sbuf(es, "W2", [128, 32, D], BF16)
            stg = [k.sbuf(es, f"wstg{i}", [128, 2048], F32) for i in range(2)]
            cast_engs = [("act", "copy"), ("dve", "tensor_copy"), ("pool", "tensor_copy")]
            ic = 0
            for kc in range(8):
                for half in range(2):
                    s = stg[ic % 2]
                    k.dma("sp", s[:], self.w_mlp_in.v(0, self.w_mlp_in.t[li, kc * 128:(kc + 1) * 128,
                                                                         half * 2048:(half + 1) * 2048]))
                    e, nm = cast_engs[ic % 3]
                    k.op(e, nm, out=W1.v(W1.t[:, kc, half * 2048:(half + 1) * 2048]), in_=s[:])
                    ic += 1
            W2r = self.w_mlp_out.t[li].rearrange("(j p) n -> p j n", p=128)
            for jp in range(16):
                s = stg[ic % 2]
                k.dma("sp", s.v(s.t[:, :].rearrange("p (j n) -> p j n", j=2)),
                      self.w_mlp_out.v(0, W2r[:, 2 * jp:2 * jp + 2, :]))
                e, nm = cast_engs[ic % 3]
                k.op(e, nm, out=W2.v(W2.t[:, 2 * jp:2 * jp + 2, :]),
                     in_=s.v(s.t[:, :].rearrange("p (j n) -> p j n", j=2)))
                ic += 1
            xt = [k.sbuf(es, f"ml_x{i}", [128, 8, N], F32) for i in range(2)]
            h = k.sbuf(es, "ml_h", [128, 8, N], BF16)
            a = k.sbuf(es, "ml_a", [128, 32, N], BF16)
            ysb = k.sbuf(es, "ml_y", [128, 8, N], F32)
            tmp = [k.sbuf(es, f"ml_t{i}", [128, N], F32) for i in range(2)]
            nb = {"sq": [k.sbuf(es, f"ml_sq{i}", [128, N], BF16) for i in range(2)],
                  "ps": k.psum(es, "ml_pss", [128, N], F32),
                  "rs": k.sbuf(es, "ml_rs", [128, N], F32),
                  "rstd": k.sbuf(es, "ml_rstd", [128, N], F32)}
            ph = [k.psum(es, f"ml_ph{i}", [128, N], F32) for i in range(2)]
            py = [k.psum(es, f"ml_py{i}", [128, N], F32) for i in range(2)]
            XTr = self.XT.t.rearrange("(c p) t -> p c t", p=128)
            coef = self.coef
            tl = tiles(cfg, N)
            k.dma("sp", xt[0].v(xt[0].t[:, :, 0:tl[0][1]]),
                  self.XT.v(("t", tl[0][0]), XTr[:, :, tl[0][0]:tl[0][0] + tl[0][1]]))
            for ti, (t0, n, isctx) in enumerate(tl):
                x = xt[ti % 2]
                if ti + 1 < len(tl):
                    t1, n1, _ = tl[ti + 1]
                    xn = xt[(ti + 1) % 2]
                    k.dma("sp", xn.v(xn.t[:, :, 0:n1]), self.XT.v(("t", t1), XTr[:, :, t1:t1 + n1]))
                rstd = self.rstd_bc(nb, lambda c: x.v(x.t[:, c, 0:n]), n, 8, D)
                for c in range(8):
                    tp = tmp[c % 2]
                    k.op("dve", "scalar_tensor_tensor", out=tp.v(tp.t[:, 0:n]), in0=x.v(x.t[:, c, 0:n]),
                         scalar=coef.v(coef.t[:, 3, c, isctx:isctx + 1]), in1=rstd, op0=ALU.mult, op1=ALU.mult)
                    k.op("act", "activation", out=h.v(h.t[:, c, 0:n]), in_=tp.v(tp.t[:, 0:n]), func=AF.Identity,
                         bias=coef.v(coef.t[:, 4, c, isctx:isctx + 1]), scale=1.0)
                for j in range(32):
                    p = ph[j % 2]
                    for kc in range(8):
                        k.op("pe", "matmul", out=p.v(p.t[:, 0:n]), lhsT=W1.v(W1.t[:, kc, j * 128:(j + 1) * 128]),
                             rhs=h.v(h.t[:, kc, 0:n]), start=(kc == 0), stop=(kc == 7))
                    tp = tmp[j % 2]
                    k.op("act", "activation", out=tp.v(tp.t[:, 0:n]), in_=p.v(p.t[:, 0:n]), func=AF.Relu)
                    e = "dve" if j % 2 == 0 else "pool"
                    k.op(e, "tensor_tensor", out=a.v(a.t[:, j, 0:n]), in0=tp.v(tp.t[:, 0:n]),
                         in1=tp.v(tp.t[:, 0:n]), op=ALU.mult)
                for c in range(8):
                    p = py[c % 2]
                    for j in range(32):
                        k.op("pe", "matmul", out=p.v(p.t[:, 0:n]), lhsT=W2.v(W2.t[:, j, c * 128:(c + 1) * 128]),
                             rhs=a.v(a.t[:, j, 0:n]), start=(j == 0), stop=(j == 31))
                    k.op("act", "copy", out=ysb.v(ysb.t[:, c, 0:n]), in_=p.v(p.t[:, 0:n]))
                rstd = self.rstd_bc(nb, lambda c: ysb.v(ysb.t[:, c, 0:n]), n, 8, D)
                for c in range(8):
                    tp = tmp[c % 2]
                    k.op("dve", "scalar_tensor_tensor", out=tp.v(tp.t[:, 0:n]), in0=ysb.v(ysb.t[:, c, 0:n]),
                         scalar=coef.v(coef.t[:, 5, c, isctx:isctx + 1]), in1=rstd, op0=ALU.mult, op1=ALU.mult)
                    k.op("pool", "tensor_tensor", out=x.v(x.t[:, c, 0:n]), in0=x.v(x.t[:, c, 0:n]),
                         in1=tp.v(tp.t[:, 0:n]), op=ALU.add)
                k.dma("pool", self.XT.v(("t", t0), XTr[:, :, t0:t0 + n]), x.v(x.t[:, :, 0:n]))

    def build(self):
        cfg = self.cfg
        k = self.k
        self.declare()
        self.load_consts()
        self.epsb = k.sbuf(self.es, "epsb", [128, 1], F32)
        k.op("dve", "memset", ap=self.epsb[:], constant=EPS)
        self.transpose_in()
        k.barrier()
        for li in range(cfg.depth):
            self.mod_vectors(li)
            k.barrier()
            self.mlp_layer(li)
            k.barrier()
        self.transpose_out()
        k.finish()
        self.es.close()
        return self.nc


def in_proj(self, li, Wd, wl, ncols, fchunks, tgroups, PT, PK):
    k, cfg = self.k, self.cfg
    N = 512
    with contextlib.ExitStack() as es:
        W = k.sbuf(es, "ipW", [128, 8, ncols], BF16)
        stg = [k.sbuf(es, f"ipstg{i}", [128, ncols], F32) for i in range(2)]
        cast_engs = [("act", "copy"), ("dve", "tensor_copy"), ("pool", "tensor_copy")]
        for kc in range(8):
            s = stg[kc % 2]
            k.dma("sp", s[:], Wd.v(0, Wd.t[wl, kc * 128:(kc + 1) * 128, :]))
            e, nm = cast_engs[kc % 3]
            k.op(e, nm, out=W.v(W.t[:, kc, :]), in_=s[:])
        nf = len(fchunks)
        ktot = sum(m for (_, m, _) in tgroups)
        xt = [k.sbuf(es, f"ip_x{i}", [128, 8, N], F32) for i in range(2)]
        h = k.sbuf(es, "ip_h", [128, 8, N], BF16)
        outF = k.sbuf(es, "ip_oF", [128, nf, N], F32)
        k.op("pool", "memset", ap=outF[:], constant=0.0)
        outK = k.sbuf(es, "ip_oK", [128, 4, max(ktot, 1)], F32)
        tmp = [k.sbuf(es, f"ip_t{i}", [128, N], F32) for i in range(2)]
        nb = {"sq": [k.sbuf(es, f"ip_sq{i}", [128, N], BF16) for i in range(2)],
              "ps": k.psum(es, "ip_pss", [128, N], F32),
              "rs": k.sbuf(es, "ip_rs", [128, N], F32),
              "rstd": k.sbuf(es, "ip_rstd", [128, N], F32)}
        pp = [k.psum(es, f"ip_pp{i}", [128, N], F32) for i in range(4)]
        XTr = self.XT.t.rearrange("(c p) t -> p c t", p=128)
        PTr = PT.t.rearrange("(c p) t -> p c t", p=128)
        coef = self.coef
        tl = tiles(cfg, N)
        k.dma("sp", xt[0].v(xt[0].t[:, :, 0:tl[0][1]]),
              self.XT.v(("t", tl[0][0]), XTr[:, :, tl[0][0]:tl[0][0] + tl[0][1]]))
        ip = 0
        for ti, (t0, n, isctx) in enumerate(tl):
            x = xt[ti % 2]
            if ti + 1 < len(tl):
                t1, n1, _ = tl[ti + 1]
                xn = xt[(ti + 1) % 2]
                k.dma("sp", xn.v(xn.t[:, :, 0:n1]), self.XT.v(("t", t1), XTr[:, :, t1:t1 + n1]))
            rstd = self.rstd_bc(nb, lambda c: x.v(x.t[:, c, 0:n]), n, 8, D)
            for c in range(8):
                tp = tmp[c % 2]
                k.op("dve", "scalar_tensor_tensor", out=tp.v(tp.t[:, 0:n]), in0=x.v(x.t[:, c, 0:n]),
                     scalar=coef.v(coef.t[:, 0, c, isctx:isctx + 1]), in1=rstd, op0=ALU.mult, op1=ALU.mult)
                k.op("act", "activation", out=h.v(h.t[:, c, 0:n]), in_=tp.v(tp.t[:, 0:n]), func=AF.Identity,
                     bias=coef.v(coef.t[:, 1, c, isctx:isctx + 1]), scale=1.0)
            for fi, (c0, m) in enumerate(fchunks):
                p = pp[ip % 4]
                ip += 1
                for kc in range(8):
                    k.op("pe", "matmul", out=p.v(p.t[0:m, 0:n]), lhsT=W.v(W.t[:, kc, c0:c0 + m]),
                         rhs=h.v(h.t[:, kc, 0:n]), start=(kc == 0), stop=(kc == 7))
                if fi % 2 == 0:
                    k.op("act", "copy", out=outF.v(outF.t[0:m, fi, 0:n]), in_=p.v(p.t[0:m, 0:n]))
                else:
                    k.op("dve", "tensor_copy", out=outF.v(outF.t[0:m, fi, 0:n]), in_=p.v(p.t[0:m, 0:n]))
            k.dma("pool", PT.v(("t", t0), PTr[:, 0:nf, t0:t0 + n]), outF.v(outF.t[:, :, 0:n]))
            if tgroups:
                for s in range(n // 128):
                    for gi, (c0, m, dc) in enumerate(tgroups):
                        p = pp[ip % 4]
                        ip += 1
                        for kc in range(8):
                            k.op("pe", "matmul", out=p.v(p.t[:, 0:m]), lhsT=h.v(h.t[:, kc, s * 128:(s + 1) * 128]),
                                 rhs=W.v(W.t[:, kc, c0:c0 + m]), start=(kc == 0), stop=(kc == 7))
                        if (gi + s) % 2 == 0:
                            k.op("act", "copy", out=outK.v(outK.t[:, s, dc:dc + m]), in_=p.v(p.t[:, 0:m]))
                        else:
                            k.op("dve", "tensor_copy", out=outK.v(outK.t[:, s, dc:dc + m]), in_=p.v(p.t[:, 0:m]))
                k.dma("pool", PK.v(("t", t0), PK.t[t0:t0 + n, 0:ktot].rearrange("(s p) c -> p s c", p=128)),
                      outK.v(outK.t[:, 0:n // 128, :]))


M.in_proj = in_proj


def out_proj(self, li, Wd, wl, YM):
    k, cfg = self.k, self.cfg
    N = 512
    with contextlib.ExitStack() as es:
        W = k.sbuf(es, "opW", [128, 8, D], BF16)
        stg = [k.sbuf(es, f"opstg{i}", [128, D], F32) for i in range(2)]
        cast_engs = [("act", "copy"), ("dve", "tensor_copy"), ("pool", "tensor_copy")]
        for kc in range(8):
            s = stg[kc % 2]
            k.dma("sp", s[:], Wd.v(0, Wd.t[wl, kc * 128:(kc + 1) * 128, :]))
            e, nm = cast_engs[kc % 3]
            k.op(e, nm, out=W.v(W.t[:, kc, :]), in_=s[:])
        xt = [k.sbuf(es, f"op_x{i}", [128, 8, N], F32) for i in range(2)]
        ym = [k.sbuf(es, f"op_ym{i}", [128, 8, N], BF16) for i in range(2)]
        ysb = k.sbuf(es, "op_y", [128, 8, N], F32)
        tmp = [k.sbuf(es, f"op_t{i}", [128, N], F32) for i in range(2)]
        nb = {"sq": [k.sbuf(es, f"op_sq{i}", [128, N], BF16) for i in range(2)],
              "ps": k.psum(es, "op_pss", [128, N], F32),
              "rs": k.sbuf(es, "op_rs", [128, N], F32),
              "rstd": k.sbuf(es, "op_rstd", [128, N], F32)}
        py = [k.psum(es, f"op_py{i}", [128, N], F32) for i in range(2)]
        XTr = self.XT.t.rearrange("(c p) t -> p c t", p=128)
        YMr = YM.t.rearrange("(c p) t -> p c t", p=128)
        coef = self.coef
        tl = tiles(cfg, N)
        for ti, (t0, n, isctx) in enumerate(tl):
            x = xt[ti % 2]
            y_in = ym[ti % 2]
            k.dma("sp", x.v(x.t[:, :, 0:n]), self.XT.v(("t", t0), XTr[:, :, t0:t0 + n]))
            k.dma("sp", y_in.v(y_in.t[:, :, 0:n]), YM.v("all", YMr[:, :, t0:t0 + n]))
            for c in range(8):
                p = py[c % 2]
                for kc in range(8):
                    k.op("pe", "matmul", out=p.v(p.t[:, 0:n]), lhsT=W.v(W.t[:, kc, c * 128:(c + 1) * 128]),
                         rhs=y_in.v(y_in.t[:, kc, 0:n]), start=(kc == 0), stop=(kc == 7))
                k.op("act", "copy", out=ysb.v(ysb.t[:, c, 0:n]), in_=p.v(p.t[:, 0:n]))
            rstd = self.rstd_bc(nb, lambda c: ysb.v(ysb.t[:, c, 0:n]), n, 8, D)
            for c in range(8):
                tp = tmp[c % 2]
                k.op("dve", "scalar_tensor_tensor", out=tp.v(tp.t[:, 0:n]), in0=ysb.v(ysb.t[:, c, 0:n]),
                     scalar=coef.v(coef.t[:, 2, c, isctx:isctx + 1]), in1=rstd, op0=ALU.mult, op1=ALU.mult)
                k.op("pool", "tensor_tensor", out=x.v(x.t[:, c, 0:n]), in0=x.v(x.t[:, c, 0:n]),
                     in1=tp.v(tp.t[:, 0:n]), op=ALU.add)
            k.dma("pool", self.XT.v(("t", t0), XTr[:, :, t0:t0 + n]), x.v(x.t[:, :, 0:n]))


M.out_proj = out_proj


def rope_tables(L):
    GRID_W = 64
    t = np.arange(L)
    r = (t // GRID_W).astype(np.float32)
    col = (t % GRID_W).astype(np.float32)
    inv = (10000.0 ** (-np.arange(16, dtype=np.float32) / 16)).astype(np.float32)
    ang = np.concatenate([r[:, None] * inv, col[:, None] * inv], axis=-1).astype(np.float32)
    cos, sin = np.cos(ang).astype(np.float32), np.sin(ang).astype(np.float32)
    cosT = np.repeat(cos, 2, axis=1).T
    sinT = np.repeat(sin, 2, axis=1).T
    out = np.stack([np.tile(cosT, (2, 1)), np.tile(sinT, (2, 1))]).astype(np.float32)
    return np.ascontiguousarray(out)


def rot_matrix():
    R = np.zeros((128, 128), np.float32)
    for m in range(128):
        if m % 2 == 0:
            R[m + 1, m] = -1.0
        else:
            R[m - 1, m] = 1.0
    return R


def block_ones(bs):
    o = np.zeros((128, 128), np.float32)
    for b in range(128 // bs):
        o[b * bs:(b + 1) * bs, b * bs:(b + 1) * bs] = 1.0
    return o


def norm_rope(self, nb, src, n, out, g_ap, t_main0, do_norm, do_rope, blk, hd):
    k = self.k
    cur = src
    if do_norm:
        sq = nb["sq"][0]
        k.op("act", "activation", out=sq.v(sq.t[:, 0:n]), in_=src, func=AF.Square)
        ps = nb["ps"]
        k.op("pe", "matmul", out=ps.v(ps.t[:, 0:n]), lhsT=blk[:], rhs=sq.v(sq.t[:, 0:n]), start=True, stop=True)
        rs, rstd = nb["rs"], nb["rstd"]
        k.op("act", "activation", out=rs.v(rs.t[:, 0:n]), in_=ps.v(ps.t[:, 0:n]), func=AF.Sqrt,
             scale=1.0 / hd, bias=self.epsb[:, 0:1])
        k.op("dve", "reciprocal", out=rstd.v(rstd.t[:, 0:n]), in_=rs.v(rs.t[:, 0:n]))
        kn = nb["kn"]
        k.op("dve", "scalar_tensor_tensor", out=kn.v(kn.t[:, 0:n]), in0=src, scalar=g_ap,
             in1=rstd.v(rstd.t[:, 0:n]), op0=ALU.mult, op1=ALU.mult)
        cur = kn.v(kn.t[:, 0:n])
    def halves(t_, ncols):
        return [t_.v(t_.t[0:64, 0:ncols]), t_.v(t_.t[64:128, 0:ncols])]
    if not do_rope:
        if isinstance(out, list):
            if do_norm:
                src_h = halves(nb["kn"], n)
            else:
                src_h = src
            for o_, s_ in zip(out, src_h):
                k.op("act", "copy", out=o_, in_=s_)
        else:
            k.op("act", "copy", out=out, in_=cur)
        return
    if isinstance(cur, list):
        raise ValueError("rope path needs a full-view src")
    cs = nb["cs"]
    k.dma("sp", cs.v(cs.t[:, :, 0:n]), self.c_rope.v(0, self.c_rope.t[:, :, t_main0:t_main0 + n].rearrange("a p t -> p a t")))
    pr = nb["pr"]
    k.op("pe", "matmul", out=pr.v(pr.t[:, 0:n]), lhsT=self.rotm[:], rhs=cur, start=True, stop=True)
    t1, t2 = nb["t1"], nb["t2"]
    k.op("pool", "tensor_tensor", out=t1.v(t1.t[:, 0:n]), in0=cur, in1=cs.v(cs.t[:, 0, 0:n]), op=ALU.mult)
    k.op("dve", "tensor_tensor", out=t2.v(t2.t[:, 0:n]), in0=pr.v(pr.t[:, 0:n]), in1=cs.v(cs.t[:, 1, 0:n]), op=ALU.mult)
    if isinstance(out, list):
        for o_, a_, b_ in zip(out, halves(t1, n), halves(t2, n)):
            k.op("pool", "tensor_tensor", out=o_, in0=a_, in1=b_, op=ALU.add)
    else:
        k.op("pool", "tensor_tensor", out=out, in0=t1.v(t1.t[:, 0:n]), in1=t2.v(t2.t[:, 0:n]), op=ALU.add)


M.norm_rope = norm_rope


def nr_bufs(self, es, N, pfx):
    k = self.k
    return {"sq": [k.sbuf(es, pfx + "sq", [128, N], BF16)],
            "ps": k.psum(es, pfx + "ps", [128, N], F32),
            "pr": k.psum(es, pfx + "pr", [128, N], F32),
            "rs": k.sbuf(es, pfx + "rs", [128, N], F32),
            "rstd": k.sbuf(es, pfx + "rstd", [128, N], F32),
            "kn": k.sbuf(es, pfx + "kn", [128, N], F32),
            "cs": k.sbuf(es, pfx + "cs", [128, 2, N], F32),
            "t1": k.sbuf(es, pfx + "t1", [128, N], F32),
            "t2": k.sbuf(es, pfx + "t2", [128, N], F32)}


M.nr_bufs = nr_bufs


def gqa(self, j, PT, PK, YM, qc0, kc, vcol, ym_row0, ctx_out):
    k, cfg = self.k, self.cfg
    T, L = cfg.T, cfg.L
    NT = T // 128
    N = 512
    with contextlib.ExitStack() as es:
        nb = self.nr_bufs(es, N, "gq_")
        blk = k.sbuf(es, "gq_blk", [128, 128], BF16)
        k.op("dve", "tensor_copy", out=blk[:], in_=self.blk64[:])
        gsb = k.sbuf(es, "gq_g", [128, 2], F32)
        k.dma("sp", gsb[:], self.ev_qk_g.v(0, self.ev_qk_g.t[j, :, :]))
        PTr = PT.t.rearrange("(c p) t -> p c t", p=128)
        KT = [k.sbuf(es, f"gq_KT{kv}", [128, T], BF16) for kv in range(2)]
        kraw = k.sbuf(es, "gq_kraw", [128, N], F32)
        for kv in range(2):
            for (t0, n, isctx) in tiles(cfg, N):
                for hf in range(2):
                    k.dma("sp", kraw.v(kraw.t[hf * 64:(hf + 1) * 64, 0:n]),
                          PT.v(("t", t0), PT.t[kc * 128 + kv * 64:kc * 128 + (kv + 1) * 64, t0:t0 + n]))
                self.norm_rope(nb, kraw.v(kraw.t[:, 0:n]), n, KT[kv].v(KT[kv].t[:, t0:t0 + n]),
                               gsb.v(gsb.t[:, 1:2]), t0 - C, True, not isctx, blk, 64)
        Va = k.sbuf(es, "gq_Va", [128, NT, 2, 65], BF16)
        k.op("pool", "memset", ap=Va[:], constant=1.0)
        vst = k.sbuf(es, "gq_vst", [128, 8, 128], F32)
        for b0 in range(0, NT, 8):
            nbk = min(8, NT - b0)
            k.dma("sp", vst.v(vst.t[:, 0:nbk, :]),
                  PK.v("all", PK.t[b0 * 128:(b0 + nbk) * 128, vcol:vcol + 128].rearrange("(s p) c -> p s c", p=128)))
            k.op("dve", "tensor_copy", out=Va.v(Va.t[:, b0:b0 + nbk, :, 0:64]),
                 in_=vst.v(vst.t[:, 0:nbk, :].rearrange("p s (kv d) -> p s kv d", kv=2)))
        qraw = [k.sbuf(es, f"gq_qraw{i}", [128, 4, N], F32) for i in range(2)]
        QT = [k.sbuf(es, f"gq_QT{i}", [128, 8, N], BF16) for i in range(2)]
        for q_ in QT:
            k.op("pool", "memset", ap=q_[:], constant=0.0)
        pS = k.psum(es, "gq_pS", [128, 4 * N], F32, nsub=4)
        pO = [k.psum(es, f"gq_pO{i}", [128, N], F32) for i in range(2)]
        pB = nb["pr"]
        pb = [k.sbuf(es, f"gq_pb{i}", [128, 2, N], BF16) for i in range(2)]
        osb = [k.sbuf(es, f"gq_osb{i}", [128, N], F32) for i in range(2)]
        rsum = [k.sbuf(es, f"gq_rsum{i}", [128, N], F32) for i in range(2)]
        yst = [k.sbuf(es, f"gq_yst{i}", [64, N], BF16) for i in range(2)]
        iP = 0
        iH = 0
        for ti, (t0, n, isctx) in enumerate(tiles(cfg, N)):
            if isctx and not ctx_out:
                continue
            qr, qt = qraw[ti % 2], QT[ti % 2]
            k.dma("sp", qr.v(qr.t[:, :, 0:n]), PT.v(("t", t0), PTr[:, qc0:qc0 + 4, t0:t0 + n]))
            for c in range(4):
                self.norm_rope(nb, qr.v(qr.t[:, c, 0:n]), n,
                               [qt.v(qt.t[0:64, 2 * c, 0:n]), qt.v(qt.t[64:128, 2 * c + 1, 0:n])],
                               gsb.v(gsb.t[:, 0:1]), t0 - C, True, not isctx, blk, 64)
            kts = list(range(C // 128)) if isctx else list(range(NT))
            pairs = [kts[i:i + 2] for i in range(0, len(kts), 2)]
            for hq in range(8):
                kv = hq // 4
                po = pO[iH % 2]
                ob, rsm, ys = osb[iH % 2], rsum[iH % 2], yst[iH % 2]
                iH += 1

                def qk(pi):
                    base = ((iP + pi) % 2) * 2
                    for jj, kt in enumerate(pairs[pi]):
                        bk = base + jj
                        k.op("pe", "matmul", out=pS.vs(pS.t[:, bk * N:bk * N + n], [bk]),
                             lhsT=KT[kv].v(KT[kv].t[:, kt * 128:(kt + 1) * 128]),
                             rhs=qt.v(qt.t[:, hq, 0:n]), start=True, stop=True)

                qk(0)
                for pi, pr_ in enumerate(pairs):
                    if pi + 1 < len(pairs):
                        qk(pi + 1)
                    base = ((iP + pi) % 2) * 2
                    pbuf = pb[(iP + pi) % 2]
                    np_ = len(pr_)
                    src3 = pS.t[:, base * N:(base + np_) * N].rearrange("p (j t) -> p j t", j=np_)[:, :, 0:n]
                    k.op("act", "activation", out=pbuf.v(pbuf.t[:, 0:np_, 0:n]),
                         in_=pS.vs(src3, list(range(base, base + np_))), func=AF.Exp, scale=0.125)
                    for jj, kt in enumerate(pr_):
                        first = (pi == 0 and jj == 0)
                        last_ = (pi == len(pairs) - 1 and jj == np_ - 1)
                        k.op("pe", "matmul", out=po.v(po.t[0:65, 0:n]), lhsT=Va.v(Va.t[:, kt, kv, :]),
                             rhs=pbuf.v(pbuf.t[:, jj, 0:n]), start=first, stop=last_)
                iP += len(pairs)
                k.op("act", "copy", out=ob.v(ob.t[0:64, 0:n]), in_=po.v(po.t[0:64, 0:n]))
                k.op("dve", "reciprocal", out=rsm.v(rsm.t[64:65, 0:n]), in_=po.v(po.t[64:65, 0:n]))
                k.op("pe", "matmul", out=pB.v(pB.t[0:64, 0:n]), lhsT=self.ones32.v(self.ones32.t[64:65, 0:64]),
                     rhs=rsm.v(rsm.t[64:65, 0:n]), start=True, stop=True)
                k.op("dve", "tensor_tensor", out=ys.v(ys.t[:, 0:n]), in0=ob.v(ob.t[0:64, 0:n]),
                     in1=pB.v(pB.t[0:64, 0:n]), op=ALU.mult)
                row = ym_row0 + hq * 64
                k.dma("pool", YM.v("all", YM.t[row:row + 64, t0:t0 + n]), ys.v(ys.t[:, 0:n]))


M.gqa = gqa


EV_F = [(0, 128), (128, 128), (256, 128), (384, 128), (1024, 128), (1152, 128), (1280, 128), (1408, 128),
        (1536, 32), (1568, 128), (1696, 128), (1824, 128), (1952, 128), (2080, 128)]
EV_T = [(256, 512, 0), (768, 256, 512), (2208, 128, 768)]


def declare_mix(self):
    cfg = self.cfg
    dp, L, T = cfg.depth, cfg.L, cfg.T
    ne, no = (dp + 1) // 2, dp // 2
    self.ev_w_in = self.inp("ev_w_in", [ne, D, 2336])
    self.ev_w_out = self.inp("ev_w_out", [ne, D, D])
    self.ev_qk_g = self.inp("ev_qk_gT", [ne, 128, 2])
    self.c_rope = self.inp("rope", [2, 128, L])
    self.c_rotm = self.inp("rotm", [128, 128])
    self.c_blk64 = self.inp("blk64", [128, 128])
    self.PT = self.scratch("PT", [24 * 128, T], dbg=True)
    self.PK = self.scratch("PK", [T, 1024], dbg=True)
    self.YM = self.scratch("YM", [D, T], BF16, dbg=True)
    k, es = self.k, self.es
    self.rotm = k.sbuf(es, "rotm", [128, 128], F32)
    k.dma("sp", self.rotm[:], self.c_rotm.v(0, self.c_rotm.t[:, :]))
    self.blk64 = k.sbuf(es, "blk64", [128, 128], F32)
    k.dma("sp", self.blk64[:], self.c_blk64.v(0, self.c_blk64.t[:, :]))


M.declare_mix = declare_mix


def zero_ym(self, r0, r1):
    k, cfg = self.k, self.cfg
    with contextlib.ExitStack() as es:
        z = k.sbuf(es, "zz", [128, 2048], BF16)
        k.op("pool", "memset", ap=z[:], constant=0.0)
        for rr in range(r0, r1, 128):
            for t0 in range(0, cfg.T, 2048):
                n = min(2048, cfg.T - t0)
                k.dma("sp", self.YM.v("all", self.YM.t[rr:rr + 128, t0:t0 + n]), z.v(z.t[:, 0:n]))


M.zero_ym = zero_ym


def even_layer(self, li, last):
    k = self.k
    j = li // 2
    self.in_proj(li, self.ev_w_in, j, 2336, EV_F, EV_T, self.PT, self.PK)
    k.barrier()
    if self.cfg.phases is None or "gla" in self.cfg.phases:
        self.gla(j, self.PT, self.PK, self.YM, not last)
    else:
        self.zero_ym(0, 512)
    k.barrier()
    self.gqa(j, self.PT, self.PK, self.YM, 9, 13, 768, 512, not last)
    k.barrier()
    self.out_proj(li, self.ev_w_out, j, self.YM)
    k.barrier()


M.even_layer = even_layer


def build2(self):
    cfg = self.cfg
    k = self.k
    self.declare()
    self.declare_mix()
    self.load_consts()
    self.declare_gla()
    self.declare_odd()
    self.declare_hy()
    self.epsb = k.sbuf(self.es, "epsb", [128, 1], F32)
    k.op("dve", "memset", ap=self.epsb[:], constant=EPS)
    self.oneb = k.sbuf(self.es, "oneb", [128, 1], F32)
    k.op("dve", "memset", ap=self.oneb[:], constant=1.0)
    self.transpose_in()
    k.barrier()
    for li in range(cfg.depth):
        last = li == cfg.depth - 1
        self.mod_vectors(li)
        k.barrier()
        if li % 2 == 0:
            self.even_layer(li, last)
        else:
            self.odd_layer(li, last)
        self.mlp_layer(li)
        k.barrier()
    self.transpose_out()
    k.finish()
    self.es.close()
    return self.nc


M.build2 = build2


def gla_consts():
    s = np.arange(128)[:, None]
    t = np.arange(128)[None, :]
    sc = -1.0 / 16.0
    ucat = np.zeros((2, 128, 129), np.float32)
    ucat[0, :, :128] = (s <= t) * sc
    ucat[1, :, :128] = (s >= t) * sc
    ucat[:, :, 128] = sc
    ustr = np.zeros((2, 128, 128), np.float32)
    ustr[0] = (s > t) * sc
    ustr[1] = (s < t) * sc
    mask = np.zeros((2, 128, 128), np.float32)
    mask[0] = (s <= t)
    mask[1] = (s >= t)
    return {"gla_ucat": ucat, "gla_ustr": ustr, "gla_mask": mask}


def declare_gla(self):
    ne = (self.cfg.depth + 1) // 2
    self.ev_wlr = self.inp("ev_wlr_aug", [ne, 2, 17, 256])
    self.ev_gla_g = self.inp("ev_gla_gT", [ne, 128, 1])
    self.c_ucat = self.inp("gla_ucat", [2, 128, 129])
    self.c_ustr = self.inp("gla_ustr", [2, 128, 128])
    self.c_mask = self.inp("gla_mask", [2, 128, 128])
    self.OF = self.scratch("OF", [512, self.cfg.T], dbg=True)


M.declare_gla = declare_gla


def gla(self, j, PT, PK, YM, ctx_out):
    k, cfg = self.k, self.cfg
    T = cfg.T
    NT = T // 128
    nctx = C // 128
    PTr = PT.t.rearrange("(c p) t -> p c t", p=128)
    OFr = self.OF.t.rearrange("(c p) t -> p c t", p=128)
    YMr = YM.t.rearrange("(c p) t -> p c t", p=128)
    with contextlib.ExitStack() as es:
        sb = lambda nm, sh, dt=F32: k.sbuf(es, "gl_" + nm, sh, dt)
        lra = [sb(f"lra{d}", [32, T]) for d in range(2)]
        wlr = [sb(f"wlr{d}", [17, 256]) for d in range(2)]
        ucat = [sb(f"ucat{d}", [128, 129]) for d in range(2)]
        ustr = [sb(f"ustr{d}", [128, 128]) for d in range(2)]
        mask = [sb(f"mask{d}", [128, 128]) for d in range(2)]
        gain = sb("gain", [128, 1])
        k.dma("sp", gain[:], self.ev_gla_g.v(0, self.ev_gla_g.t[j, :, :]))
        for d in range(2):
            k.op("pool", "memset", ap=lra[d][:], constant=1.0)
            k.dma("sp", lra[d].v(lra[d].t[0:16, :]), PT.v("all", PT.t[8 * 128 + d * 16:8 * 128 + (d + 1) * 16, :]))
            k.dma("sp", wlr[d][:], self.ev_wlr.v(0, self.ev_wlr.t[j, d, :, :]))
            k.dma("sp", ucat[d][:], self.c_ucat.v(0, self.c_ucat.t[d, :, :]))
            k.dma("sp", ustr[d][:], self.c_ustr.v(0, self.c_ustr.t[d, :, :]))
            k.dma("sp", mask[d][:], self.c_mask.v(0, self.c_mask.t[d, :, :]))
        ldf = [sb(f"ldf{i}", [128, 4, 128]) for i in range(2)]
        ldk = [sb(f"ldk{i}", [128, 768]) for i in range(2)]
        e1 = sb("e1", [128, 256])
        lnv = sb("lnv", [128, 256])
        EqT = sb("EqT", [128, 2, 129])
        EkT = sb("EkT", [128, 2, 128])
        Eend = sb("Eend", [128, 256])
        qin = sb("qin", [128, 2, 128], BF16)
        kin = sb("kin", [128, 2, 128], BF16)
        kend = sb("kend", [128, 256], BF16)
        vbf = sb("vbf", [128, 512], BF16)
        ATm = [sb(f"ATm{i}", [128, 128], BF16) for i in range(2)]
        S = sb("S", [128, 2, 128])
        Sbf = sb("Sbf", [128, 2, 128], BF16)
        ofw = [sb(f"ofw{i}", [128, 4, 128]) for i in range(2)]
        ofl = [sb(f"ofl{i}", [128, 4, 128]) for i in range(2)]
        gT = [sb(f"gT{i}", [128, 4, 128]) for i in range(2)]
        otot = [sb(f"otot{i}", [128, 128]) for i in range(2)]
        sq = sb("sq", [128, 128], BF16)
        rs = sb("rs", [128, 128])
        rstd = sb("rstd", [128, 128])
        sg = sb("sg", [128, 128])
        y1 = sb("y1", [128, 128])
        yst = [sb(f"yst{i}", [128, 4, 128], BF16) for i in range(2)]
        p_gk = k.psum(es, "gl_pgk", [128, 512])
        p_bT = k.psum(es, "gl_pbT", [128, 2, 256])
        p_be = k.psum(es, "gl_pbe", [128, 512])
        p_AT = [k.psum(es, f"gl_pAT{i}", [128, 512]) for i in range(2)]
        p_o = [k.psum(es, f"gl_po{i}", [128, 512]) for i in range(2)]
        p_up = k.psum(es, "gl_pup", [128, 2, 128])
        ih = 0
        for d in range(2):
            k.op("dve", "memset", ap=S[:], constant=0.0)
            k.op("pool", "memset", ap=Sbf[:], constant=0.0)
            ctx_tiles = list(range(nctx))
            main_tiles = list(range(nctx, NT))
            order = ctx_tiles + main_tiles if d == 0 else ctx_tiles[::-1] + main_tiles[::-1]
            for it, tix in enumerate(order):
                t0 = tix * 128
                isctx = tix < nctx
                need_out = (not isctx) or ctx_out
                lf, lk = ldf[it % 2], ldk[it % 2]
                k.dma("sp", lf[:], PT.v("all", PTr[:, 0:4, t0:t0 + 128]))
                k.dma("sp", lk[:], PK.v("all", PK.t[t0:t0 + 128, 0:768]))
                if d == 1 and need_out:
                    k.dma("sp", gT[it % 2][:], PT.v("all", PTr[:, 4:8, t0:t0 + 128]))
                    k.dma("sp", ofl[it % 2][:], self.OF.v("all", OFr[:, :, t0:t0 + 128]))
                k.op("pe", "matmul", out=p_gk.v(p_gk.t[:, 0:256]), lhsT=lra[d].v(lra[d].t[0:17, t0:t0 + 128]),
                     rhs=wlr[d][:], start=True, stop=True)
                k.op("act", "activation", out=e1[:], in_=p_gk.v(p_gk.t[:, 0:256]), func=AF.Exp, scale=-1.0)
                k.op("act", "activation", out=lnv[:], in_=e1[:], func=AF.Ln, bias=self.oneb[:, 0:1], scale=1.0)
                for pr in range(2):
                    k.op("pe", "matmul", out=p_bT.v(p_bT.t[:, pr, 0:129]), lhsT=lnv.v(lnv.t[:, pr * 128:(pr + 1) * 128]),
                         rhs=ucat[d][:], start=True, stop=True)
                k.op("pe", "matmul", out=p_be.v(p_be.t[:, 0:256]), lhsT=ustr[d][:], rhs=lnv[:], start=True, stop=True)
                k.op("act", "activation", out=EqT[:], in_=p_bT.v(p_bT.t[:, :, 0:129]), func=AF.Exp)
                k.op("act", "activation", out=EkT[:], in_=p_bT.v(p_bT.t[:, :, 0:128]), func=AF.Exp, scale=-1.0)
                k.op("act", "activation", out=Eend[:], in_=p_be.v(p_be.t[:, 0:256]), func=AF.Exp)
                k.op("dve", "scalar_tensor_tensor", out=qin[:], in0=lf.v(lf.t[:, 0:2, :]), scalar=0.125,
                     in1=EqT.v(EqT.t[:, :, 0:128]), op0=ALU.mult, op1=ALU.mult)
                k.op("pool", "tensor_tensor", out=kin[:], in0=lf.v(lf.t[:, 2:4, :]), in1=EkT[:], op=ALU.mult)
                k.op("dve", "tensor_tensor", out=kend[:], in0=lk.v(lk.t[:, 0:256]), in1=Eend[:], op=ALU.mult)
                k.op("pool", "tensor_copy", out=vbf[:], in_=lk.v(lk.t[:, 256:768]))
                for h in range(4):
                    pr, r = h // 2, (h % 2) * 64
                    if need_out:
                        pa, po, am = p_AT[ih % 2], p_o[ih % 2], ATm[ih % 2]
                        k.op("pe", "matmul", out=pa.v(pa.t[:, 0:128]), lhsT=kin.v(kin.t[r:r + 64, pr, :]),
                             rhs=qin.v(qin.t[r:r + 64, pr, :]), start=True, stop=True)
                        k.op("dve", "tensor_tensor", out=am[:], in0=pa.v(pa.t[:, 0:128]), in1=mask[d][:], op=ALU.mult)
                        k.op("pe", "matmul", out=po.v(po.t[:, 0:128]), lhsT=vbf.v(vbf.t[:, h * 128:(h + 1) * 128]),
                             rhs=am[:], start=True, stop=False)
                        k.op("pe", "matmul", out=po.v(po.t[:, 0:128]), lhsT=Sbf.v(Sbf.t[r:r + 64, pr, :]),
                             rhs=qin.v(qin.t[r:r + 64, pr, :]), start=False, stop=True)
                        if d == 0:
                            k.op("act", "copy", out=ofw[it % 2].v(ofw[it % 2].t[:, h, :]), in_=po.v(po.t[:, 0:128]))
                        else:
                            ot = otot[ih % 2]
                            k.op("dve", "tensor_tensor", out=ot[:], in0=po.v(po.t[:, 0:128]),
                                 in1=ofl[it % 2].v(ofl[it % 2].t[:, h, :]), op=ALU.add)
                            k.op("act", "activation", out=sq[:], in_=ot[:], func=AF.Square)
                            k.op("pe", "matmul", out=p_gk.v(p_gk.t[:, 256:384]), lhsT=self.ones_bf[:], rhs=sq[:],
                                 start=True, stop=True)
                            k.op("act", "activation", out=rs[:], in_=p_gk.v(p_gk.t[:, 256:384]), func=AF.Sqrt,
                                 scale=1.0 / 128, bias=self.epsb[:, 0:1])
                            k.op("dve", "reciprocal", out=rstd[:], in_=rs[:])
                            k.op("act", "activation", out=sg[:], in_=gT[it % 2].v(gT[it % 2].t[:, h, :]), func=AF.Silu)
                            k.op("dve", "scalar_tensor_tensor", out=y1[:], in0=ot[:], scalar=gain[:, 0:1],
                                 in1=rstd[:], op0=ALU.mult, op1=ALU.mult)
                            k.op("pool", "tensor_tensor", out=yst[it % 2].v(yst[it % 2].t[:, h, :]), in0=y1[:],
                                 in1=sg[:], op=ALU.mult)
                        ih += 1
                    k.op("pe", "matmul", out=p_up.v(p_up.t[r:r + 64, pr, :]), lhsT=kend.v(kend.t[:, h * 64:(h + 1) * 64]),
                         rhs=vbf.v(vbf.t[:, h * 128:(h + 1) * 128]), start=True, stop=True)
                for pr in range(2):
                    k.op("dve", "scalar_tensor_tensor", out=S.v(S.t[:, pr, :]), in0=S.v(S.t[:, pr, :]),
                         scalar=EqT.v(EqT.t[:, pr, 128:129]), in1=p_up.v(p_up.t[:, pr, :]), op0=ALU.mult, op1=ALU.add)
                k.op("act", "copy", out=Sbf[:], in_=S[:])
                if need_out:
                    if d == 0:
                        k.dma("pool", self.OF.v("all", OFr[:, :, t0:t0 + 128]), ofw[it % 2][:])
                    else:
                        k.dma("pool", YM.v("all", YMr[:, 0:4, t0:t0 + 128]), yst[it % 2][:])
            k.barrier()


M.gla = gla


OD_F = [(i * 128, 128) for i in range(20)]
OD_T = [(2560, 512, 0)]


def declare_odd(self):
    cfg = self.cfg
    no = max(cfg.depth // 2, 1)
    self.od_w_in = self.inp("od_w_in", [no, D, 3072])
    self.od_w_out = self.inp("od_w_out", [no, D, D])
    self.od_lam = self.inp("od_lamB", [no, 128, 256])
    self.od_diff_g = self.inp("od_diff_gT", [no, 128, 1])


M.declare_odd = declare_odd


def diffattn(self, li, j, PT, PK, YM, qc0, kc0, ym_row0, ctx_out):
    k, cfg = self.k, self.cfg
    T, L = cfg.T, cfg.L
    NT = T // 128
    N = 512
    lam_init = 0.8 - 0.6 * math.exp(-0.3 * li)
    with contextlib.ExitStack() as es:
        nb = self.nr_bufs(es, N, "da_")
        PTr = PT.t.rearrange("(c p) t -> p c t", p=128)
        lp = k.sbuf(es, "da_lp", [128, 256], F32)
        k.dma("sp", lp[:], self.od_lam.v(0, self.od_lam.t[j, :, :]))
        pr2 = k.sbuf(es, "da_pr2", [128, 2, 64], F32)
        sm = k.sbuf(es, "da_sm", [128, 2], F32)
        ex = k.sbuf(es, "da_ex", [128, 2], F32)
        neglam = k.sbuf(es, "da_nl", [128, 1], F32)
        for q in range(2):
            k.op("dve", "tensor_tensor", out=pr2.v(pr2.t[:, q, :]), in0=lp.v(lp.t[:, q * 128:q * 128 + 64]),
                 in1=lp.v(lp.t[:, q * 128 + 64:q * 128 + 128]), op=ALU.mult)
        k.op("dve", "reduce_sum", out=sm[:], in_=pr2[:], axis=mybir.AxisListType.X)
        k.op("act", "activation", out=ex[:], in_=sm[:], func=AF.Exp)
        k.op("dve", "tensor_tensor", out=neglam[:], in0=ex.v(ex.t[:, 1:2]), in1=ex.v(ex.t[:, 0:1]), op=ALU.subtract)
        k.op("dve", "tensor_scalar", out=neglam[:], in0=neglam[:], scalar1=-lam_init, scalar2=None, op0=ALU.add)
        g2 = k.sbuf(es, "da_g2", [128, 1], F32)
        k.dma("sp", g2[:], self.od_diff_g.v(0, self.od_diff_g.t[j, :, :]))
        k.op("dve", "tensor_scalar", out=g2[:], in0=g2[:], scalar1=1.0 - lam_init, scalar2=None, op0=ALU.mult)
        KT = k.sbuf(es, "da_KT", [128, 4, T], BF16)
        kraw = k.sbuf(es, "da_kraw", [128, N], F32)
        for c in range(4):
            for (t0, n, isctx) in tiles(cfg, N):
                k.dma("sp", kraw.v(kraw.t[:, 0:n]), PT.v("all", PT.t[(kc0 + c) * 128:(kc0 + c + 1) * 128, t0:t0 + n]))
                self.norm_rope(nb, kraw.v(kraw.t[:, 0:n]), n, KT.v(KT.t[:, c, t0:t0 + n]),
                               None, t0 - C, False, not isctx, None, 64)
        V = k.sbuf(es, "da_V", [128, NT, 512], BF16)
        vst = k.sbuf(es, "da_vst", [128, 1, 512], F32)
        for b0 in range(0, NT, 1):
            nbk = 1
            k.dma("sp", vst.v(vst.t[:, 0:nbk, :]),
                  PK.v("all", PK.t[b0 * 128:(b0 + nbk) * 128, 0:512].rearrange("(s p) c -> p s c", p=128)))
            k.op("dve" if (b0 // 2) % 2 == 0 else "pool", "tensor_copy", out=V.v(V.t[:, b0:b0 + nbk, :]),
                 in_=vst.v(vst.t[:, 0:nbk, :]))
        qraw = [k.sbuf(es, "da_qraw0", [128, 4, N], F32)]
        QT = [k.sbuf(es, f"da_QT{i}", [128, 4, 2, N], BF16) for i in range(2)]
        for q_ in QT:
            k.op("pool", "memset", ap=q_[:], constant=0.0)
        pS = k.psum(es, "da_pS", [128, 4 * N], F32, nsub=4)
        pO = [k.psum(es, f"da_pO{i}", [128, N], F32) for i in range(2)]
        pZ = nb["pr"]
        pb = [k.sbuf(es, f"da_pb{i}", [128, 2, N], BF16) for i in range(3)]
        acc = [k.sbuf(es, f"da_acc{i}", [128, 2, N], F32) for i in range(2)]
        rz1 = k.sbuf(es, "da_rz", [128, N], F32)
        rz = [rz1, rz1]
        tt = [k.sbuf(es, f"da_tt{i}", [128, N], F32) for i in range(2)]
        osb = k.sbuf(es, "da_o", [128, N], F32)
        sq = k.sbuf(es, "da_sq", [128, N], BF16)
        y1 = k.sbuf(es, "da_y1", [128, N], BF16)
        iP = 0
        for ti, (t0, n, isctx) in enumerate(tiles(cfg, N)):
            if isctx and not ctx_out:
                continue
            qr, qt = qraw[0], QT[ti % 2]
            k.dma("sp", qr.v(qr.t[:, :, 0:n]), PT.v("all", PTr[:, qc0:qc0 + 4, t0:t0 + n]))
            for c in range(4):
                src_ = qr.v(qr.t[:, c, 0:n]) if not isctx else [qr.v(qr.t[0:64, c, 0:n]), qr.v(qr.t[64:128, c, 0:n])]
                self.norm_rope(nb, src_, n,
                               [qt.v(qt.t[0:64, c, 0, 0:n]), qt.v(qt.t[64:128, c, 1, 0:n])],
                               None, t0 - C, False, not isctx, None, 64)
            kts = list(range(C // 128)) if isctx else list(range(NT))
            for h in range(4):
                k.op("dve", "memset", ap=acc[0][:], constant=0.0)
                k.op("pool", "memset", ap=acc[1][:], constant=0.0)

                def qk(ii):
                    base = ((iP + ii) % 2) * 2
                    kt = kts[ii]
                    for c in range(2):
                        bk = base + c
                        k.op("pe", "matmul", out=pS.vs(pS.t[:, bk * N:bk * N + n], [bk]),
                             lhsT=KT.v(KT.t[:, h, kt * 128:(kt + 1) * 128]),
                             rhs=qt.v(qt.t[:, h, c, 0:n]), start=True, stop=True)

                qk(0)
                for ii, kt in enumerate(kts):
                    if ii + 1 < len(kts):
                        qk(ii + 1)
                    base = ((iP + ii) % 2) * 2
                    pbuf = pb[(iP + ii) % 3]
                    src3 = pS.t[:, base * N:(base + 2) * N].rearrange("p (j t) -> p j t", j=2)[:, :, 0:n]
                    k.op("act", "activation", out=pbuf.v(pbuf.t[:, :, 0:n]), in_=pS.vs(src3, [base, base + 1]),
                         func=AF.Exp, scale=0.125)
                    for c in range(2):
                        k.op("pe", "matmul", out=pO[c].v(pO[c].t[:, 0:n]), lhsT=V.v(V.t[:, kt, h * 128:(h + 1) * 128]),
                             rhs=pbuf.v(pbuf.t[:, c, 0:n]), start=(ii == 0), stop=(ii == len(kts) - 1))
                    if ii % 2 == 0:
                        k.op("dve", "tensor_tensor", out=acc[0].v(acc[0].t[:, :, 0:n]),
                             in0=acc[0].v(acc[0].t[:, :, 0:n]), in1=pbuf.v(pbuf.t[:, :, 0:n]), op=ALU.add)
                    else:
                        k.op("pool", "tensor_tensor", out=acc[1].v(acc[1].t[:, 0, 0:n]),
                             in0=acc[1].v(acc[1].t[:, 0, 0:n]), in1=pbuf.v(pbuf.t[:, 0, 0:n]), op=ALU.add)
                        k.op("pe", "matmul", out=pZ.v(pZ.t[:, 0:n]), lhsT=self.ones_bf[:], rhs=pbuf.v(pbuf.t[:, 1, 0:n]),
                             start=(ii == 1), stop=False)
                iP += len(kts)
                k.op("dve", "tensor_tensor", out=acc[0].v(acc[0].t[:, 0, 0:n]), in0=acc[0].v(acc[0].t[:, 0, 0:n]),
                     in1=acc[1].v(acc[1].t[:, 0, 0:n]), op=ALU.add)
                k.op("pe", "matmul", out=pZ.v(pZ.t[:, 0:n]), lhsT=self.ones32[:], rhs=acc[0].v(acc[0].t[:, 1, 0:n]),
                     start=(len(kts) < 2), stop=True)
                k.op("dve", "reciprocal", out=rz[1].v(rz[1].t[:, 0:n]), in_=pZ.v(pZ.t[:, 0:n]))
                k.op("dve", "tensor_tensor", out=tt[1].v(tt[1].t[:, 0:n]), in0=pO[1].v(pO[1].t[:, 0:n]),
                     in1=rz[1].v(rz[1].t[:, 0:n]), op=ALU.mult)
                pz0 = nb["ps"]
                k.op("pe", "matmul", out=pz0.v(pz0.t[:, 0:n]), lhsT=self.ones32[:], rhs=acc[0].v(acc[0].t[:, 0, 0:n]),
                     start=True, stop=True)
                k.op("dve", "reciprocal", out=rz[0].v(rz[0].t[:, 0:n]), in_=pz0.v(pz0.t[:, 0:n]))
                k.op("dve", "tensor_tensor", out=tt[0].v(tt[0].t[:, 0:n]), in0=pO[0].v(pO[0].t[:, 0:n]),
                     in1=rz[0].v(rz[0].t[:, 0:n]), op=ALU.mult)
                k.op("dve", "scalar_tensor_tensor", out=osb.v(osb.t[:, 0:n]), in0=tt[1].v(tt[1].t[:, 0:n]),
                     scalar=neglam[:, 0:1], in1=tt[0].v(tt[0].t[:, 0:n]), op0=ALU.mult, op1=ALU.add)
                k.op("act", "activation", out=sq.v(sq.t[:, 0:n]), in_=osb.v(osb.t[:, 0:n]), func=AF.Square)
                ps = nb["ps"]
                k.op("pe", "matmul", out=ps.v(ps.t[:, 0:n]), lhsT=self.ones_bf[:], rhs=sq.v(sq.t[:, 0:n]),
                     start=True, stop=True)
                rs, rstd = nb["rs"], nb["rstd"]
                k.op("act", "activation", out=rs.v(rs.t[:, 0:n]), in_=ps.v(ps.t[:, 0:n]), func=AF.Sqrt,
                     scale=1.0 / 128, bias=self.epsb[:, 0:1])
                k.op("dve", "reciprocal", out=rstd.v(rstd.t[:, 0:n]), in_=rs.v(rs.t[:, 0:n]))
                k.op("dve", "scalar_tensor_tensor", out=y1.v(y1.t[:, 0:n]), in0=osb.v(osb.t[:, 0:n]),
                     scalar=g2[:, 0:1], in1=rstd.v(rstd.t[:, 0:n]), op0=ALU.mult, op1=ALU.mult)
                row = ym_row0 + h * 128
                k.dma("pool", YM.v("all", YM.t[row:row + 128, t0:t0 + n]), y1.v(y1.t[:, 0:n]))


M.diffattn = diffattn


def odd_layer(self, li, last):
    k = self.k
    j = li // 2
    self.in_proj(li, self.od_w_in, j, 3072, OD_F, OD_T, self.PT, self.PK)
    k.barrier()
    if self.cfg.phases is None or "hyena" in self.cfg.phases:
        self.hyena(j, self.PT, self.YM, not last)
    else:
        self.zero_ym(0, 512)
    k.barrier()
    self.diffattn(li, j, self.PT, self.PK, self.YM, 12, 16, 512, not last)
    k.barrier()
    self.out_proj(li, self.od_w_out, j, self.YM)
    k.barrier()


M.odd_layer = odd_layer


def hy_dims(Lseg):
    N = 2 * Lseg
    lg = int(round(math.log2(N)))
    N2 = 1 << ((lg + 1) // 2)
    N1 = N // N2
    H1 = N1 // 2
    NSQ = 256 // max(N1, N2)
    return N, N1, N2, H1, NSQ


def hy_consts(Lseg, pfx):
    N, N1, N2, H1, NSQ = hy_dims(Lseg)
    f64 = np.float64
    c = {}
    n1 = np.arange(H1, dtype=f64)[:, None]
    k1 = np.arange(N1, dtype=f64)[None, :]
    a = 2 * np.pi * n1 * k1 / N1
    c["F1cat"] = np.concatenate([np.cos(a), -np.sin(a)], 1)
    n1f = np.arange(N1, dtype=f64)[:, None]
    af = 2 * np.pi * n1f * k1 / N1
    c["F1cat"] = np.concatenate([np.cos(af), -np.sin(af)], 1)
    n2 = np.arange(N2, dtype=f64)[:, None]
    a = 2 * np.pi * n2 * k1 / N
    twc, tws = np.cos(a), -np.sin(a)
    c["TwA"] = np.tile(np.concatenate([twc, twc], 1)[:, None, :], (1, NSQ, 1))
    c["TwB"] = np.tile(np.concatenate([-tws, tws], 1)[:, None, :], (1, NSQ, 1))
    k2 = np.arange(N2, dtype=f64)[None, :]
    a = 2 * np.pi * n2 * k2 / N2
    c["F2c"], c["F2s"], c["F2sn"] = np.cos(a), -np.sin(a), np.sin(a)
    c["G2cat"] = np.concatenate([np.cos(a), np.sin(a)], 1)
    c["G2cat2"] = np.concatenate([-np.sin(a), np.cos(a)], 1)
    kk1 = np.arange(N1, dtype=f64)[:, None]
    nn2 = np.arange(N2, dtype=f64)[None, :]
    a = 2 * np.pi * kk1 * nn2 / N
    tc, ts = np.cos(a), np.sin(a)
    c["TwAi"] = np.tile(np.concatenate([tc, tc], 1)[:, None, :], (1, NSQ, 1))
    c["TwBi"] = np.tile(np.concatenate([-ts, ts], 1)[:, None, :], (1, NSQ, 1))
    nn1 = np.arange(H1, dtype=f64)[None, :]
    a = 2 * np.pi * kk1 * nn1 / N1
    c["G1c"], c["G1sn"] = np.cos(a) / N, -np.sin(a) / N
    pos = np.arange(Lseg, dtype=np.float32)
    t = pos / np.float32(max(Lseg - 1, 1))
    w = np.float32(2 * math.pi) * pos / np.float32(Lseg)
    bands = np.linspace(1e-4, 15, 16, dtype=np.float32)
    z = np.concatenate([t[:, None], np.cos(w[:, None] * bands), -np.sin(w[:, None] * bands)], -1).astype(np.float32)
    deltas = np.abs(np.linspace(math.log(1e-2) / 0.3, math.log(1e-2) / 1.5, 512, dtype=np.float32))
    dec = np.exp(-t[None, :] * deltas[:, None])
    idx = (Lseg - np.arange(Lseg)) % Lseg
    zrev = z[idx]
    decrev = dec[:, idx].copy()
    decrev[:, 0] = 0.0
    c["zT"] = np.concatenate([z.T, zrev.T], 1)
    c["decT"] = np.concatenate([dec, decrev], 1)
    return {pfx + k_: np.ascontiguousarray(v.astype(np.float32)) for k_, v in c.items()}


HY_SHAPES = lambda N, N1, N2, H1, NSQ, Lseg: {
    "F1cat": [N1, 2 * N1], "TwA": [N2, NSQ, 2 * N1], "TwB": [N2, NSQ, 2 * N1], "F2c": [N2, N2], "F2s": [N2, N2],
    "F2sn": [N2, N2], "G2cat": [N2, 2 * N2], "G2cat2": [N2, 2 * N2], "TwAi": [N1, NSQ, 2 * N2],
    "TwBi": [N1, NSQ, 2 * N2], "G1c": [N1, H1], "G1sn": [N1, H1], "zT": [33, 2 * Lseg], "decT": [512, 2 * Lseg]}


def declare_hy(self):
    cfg = self.cfg
    no = max(cfg.depth // 2, 1)
    self.hyc = {}
    for pfx, Lseg in (("hm_", cfg.L), ("hc_", C)):
        dims = hy_dims(Lseg)
        for nm, sh in HY_SHAPES(*dims, Lseg).items():
            self.hyc[pfx + nm] = self.inp(pfx + nm, sh)
    self.od_conv_w = self.inp("od_conv_wT", [no, 128, 12, 3])
    self.od_conv_b = self.inp("od_conv_bT", [no, 128, 12])
    self.od_f_w1 = self.inp("od_f_w1", [no, 33, 64])
    self.od_f_b1 = self.inp("od_f_b1T", [no, 64, 1])
    self.od_f_w2 = self.inp("od_f_w2", [no, 64, 64])
    self.od_f_b2 = self.inp("od_f_b2T", [no, 64, 1])
    self.od_f_w3 = self.inp("od_f_w3", [no, 64, 2048])
    self.od_hy_bias = self.inp("od_hy_biasB", [no, 128, 1024])
    self.UC = self.scratch("UC", [1536, cfg.T], dbg=True)
    self.HT = self.scratch("HT", [1024, 2 * cfg.L], dbg=True)
    self.HTc = self.scratch("HTc", [1024, 2 * C], dbg=True)


M.declare_hy = declare_hy


def hy_shortconv(self, j, PT, ctx_out):
    k, cfg = self.k, self.cfg
    BL = 2048
    with contextlib.ExitStack() as es:
        cw = k.sbuf(es, "sc_w", [128, 12, 3], F32)
        cb = k.sbuf(es, "sc_b", [128, 12], F32)
        k.dma("sp", cw[:], self.od_conv_w.v(0, self.od_conv_w.t[j, :, :, :]))
        k.dma("sp", cb[:], self.od_conv_b.v(0, self.od_conv_b.t[j, :, :]))
        ub = [k.sbuf(es, f"sc_u{i}", [128, BL + 2], F32) for i in range(2)]
        ac = [k.sbuf(es, f"sc_a{i}", [128, BL], F32) for i in range(2)]
        it = 0
        segs = [(C, cfg.L)] + ([(0, C)] if ctx_out else [])
        for (toff, Lseg) in segs:
            for c in range(12):
                for b0 in range(0, Lseg, BL):
                    n = min(BL, Lseg - b0)
                    u, a = ub[it % 2], ac[it % 2]
                    it += 1
                    lo = max(b0 - 1, 0)
                    hi = min(b0 + n + 1, Lseg)
                    if b0 == 0:
                        k.op("pool", "memset", ap=u.v(u.t[:, 0:1]), constant=0.0)
                    if b0 + n == Lseg:
                        k.op("pool", "memset", ap=u.v(u.t[:, n + 1:n + 2]), constant=0.0)
                    k.dma("sp", u.v(u.t[:, lo - b0 + 1:hi - b0 + 1]),
                          PT.v("all", PT.t[c * 128:(c + 1) * 128, toff + lo:toff + hi]))
                    k.op("dve", "tensor_scalar", out=a.v(a.t[:, 0:n]), in0=u.v(u.t[:, 0:n]), scalar1=cw.v(cw.t[:, c, 0:1]),
                         scalar2=cb.v(cb.t[:, c:c + 1]), op0=ALU.mult, op1=ALU.add)
                    k.op("dve", "scalar_tensor_tensor", out=a.v(a.t[:, 0:n]), in0=u.v(u.t[:, 1:n + 1]),
                         scalar=cw.v(cw.t[:, c, 1:2]), in1=a.v(a.t[:, 0:n]), op0=ALU.mult, op1=ALU.add)
                    k.op("dve", "scalar_tensor_tensor", out=a.v(a.t[:, 0:n]), in0=u.v(u.t[:, 2:n + 2]),
                         scalar=cw.v(cw.t[:, c, 2:3]), in1=a.v(a.t[:, 0:n]), op0=ALU.mult, op1=ALU.add)
                    k.dma("pool", self.UC.v("all", self.UC.t[c * 128:(c + 1) * 128, toff + b0:toff + b0 + n]),
                          a.v(a.t[:, 0:n]))


M.hy_shortconv = hy_shortconv


def hy_filters(self, j, pfx, Lseg, HT):
    k = self.k
    N = 512
    with contextlib.ExitStack() as es:
        sb = lambda nm, sh, dt=F32: k.sbuf(es, "hf_" + nm, sh, dt)
        w1, w2, w3 = sb("w1", [33, 64]), sb("w2", [64, 64]), sb("w3", [64, 2048])
        bb = sb("bb", [64, 2])
        bh, bq = sb("bh", [64, 2]), sb("bq", [64, 2])
        k.dma("sp", w1[:], self.od_f_w1.v(0, self.od_f_w1.t[j, :, :]))
        k.dma("sp", w2[:], self.od_f_w2.v(0, self.od_f_w2.t[j, :, :]))
        k.dma("sp", w3[:], self.od_f_w3.v(0, self.od_f_w3.t[j, :, :]))
        k.dma("sp", bb.v(bb.t[:, 0:1]), self.od_f_b1.v(0, self.od_f_b1.t[j, :, :]))
        k.dma("sp", bb.v(bb.t[:, 1:2]), self.od_f_b2.v(0, self.od_f_b2.t[j, :, :]))
        k.op("dve", "tensor_scalar", out=bh[:], in0=bb[:], scalar1=0.5, scalar2=None, op0=ALU.mult)
        k.op("dve", "tensor_scalar", out=bq[:], in0=bb[:], scalar1=0.25, scalar2=None, op0=ALU.mult)
        zT = [sb(f"zT{i}", [33, N]) for i in range(2)]
        dect = [sb(f"dec{i}", [128, 4, N]) for i in range(2)]
        a1, a2, tq = sb("a1", [64, N]), sb("a2", [64, N]), sb("tq", [64, N])
        hid = [sb(f"hid{i}", [64, N]) for i in range(2)]
        hsb = [sb(f"hsb{i}", [128, N]) for i in range(3)]
        p12 = [k.psum(es, f"hf_p{i}", [64, N]) for i in range(2)]
        ph = [k.psum(es, f"hf_ph{i}", [128, N]) for i in range(3)]
        zc, dc = self.hyc[pfx + "zT"], self.hyc[pfx + "decT"]
        dcr = dc.t.rearrange("(c p) t -> p c t", p=128)
        io = 0
        for ti, t0 in enumerate(range(0, 2 * Lseg, min(N, Lseg))):
            n = min(N, Lseg)
            dr_ = t0 // Lseg
            z, de = zT[ti % 2], dect[ti % 2]
            k.dma("sp", z.v(z.t[:, 0:n]), zc.v(0, zc.t[:, t0:t0 + n]))
            k.dma("sp", de.v(de.t[:, :, 0:n]), dc.v(0, dcr[:, :, t0:t0 + n]))
            cur = z.v(z.t[:, 0:n])
            for layer, (w, kk) in enumerate(((w1, 33), (w2, 64))):
                p = p12[layer]
                k.op("pe", "matmul", out=p.v(p.t[:, 0:n]), lhsT=w.v(w.t[0:kk, :]), rhs=cur, start=True, stop=True)
                k.op("act", "activation", out=a1.v(a1.t[:, 0:n]), in_=p.v(p.t[:, 0:n]), func=AF.Sin,
                     bias=bh.v(bh.t[:, layer:layer + 1]), scale=0.5)
                k.op("act", "activation", out=a2.v(a2.t[:, 0:n]), in_=p.v(p.t[:, 0:n]), func=AF.Sin,
                     bias=bq.v(bq.t[:, layer:layer + 1]), scale=0.25)
                k.op("dve", "tensor_tensor", out=tq.v(tq.t[:, 0:n]), in0=a2.v(a2.t[:, 0:n]), in1=a2.v(a2.t[:, 0:n]),
                     op=ALU.mult)
                k.op("dve", "tensor_scalar", out=tq.v(tq.t[:, 0:n]), in0=tq.v(tq.t[:, 0:n]), scalar1=-2.0, scalar2=1.0,
                     op0=ALU.mult, op1=ALU.add)
                hd = hid[layer]
                k.op("dve", "scalar_tensor_tensor", out=hd.v(hd.t[:, 0:n]), in0=a1.v(a1.t[:, 0:n]), scalar=2.0,
                     in1=tq.v(tq.t[:, 0:n]), op0=ALU.mult, op1=ALU.mult)
                cur = hd.v(hd.t[:, 0:n])
            for o_ in range(2):
                for c4 in range(4):
                    p, hs = ph[io % 3], hsb[io % 3]
                    io += 1
                    wc = o_ * 1024 + dr_ * 512 + c4 * 128
                    k.op("pe", "matmul", out=p.v(p.t[:, 0:n]), lhsT=w3.v(w3.t[:, wc:wc + 128]), rhs=cur,
                         start=True, stop=True)
                    k.op("dve", "tensor_tensor", out=hs.v(hs.t[:, 0:n]), in0=p.v(p.t[:, 0:n]),
                         in1=de.v(de.t[:, c4, 0:n]), op=ALU.mult)
                    rr = o_ * 512 + c4 * 128
                    k.dma("pool", HT.v("all", HT.t[rr:rr + 128, t0:t0 + n]), hs.v(hs.t[:, 0:n]))


M.hy_filters = hy_filters


def hy_conv(self, j, pfx, Lseg, toff, HT, YM):
    k = self.k
    N, N1, N2, H1, NSQ = hy_dims(Lseg)
    NSLOT = 4
    with contextlib.ExitStack() as es:
        sb = lambda nm, sh, dt=F32: k.sbuf(es, "hv_" + nm, sh, dt)
        cst = {}
        for nm, sh in HY_SHAPES(N, N1, N2, H1, NSQ, Lseg).items():
            if nm in ("zT", "decT"):
                continue
            cst[nm] = sb(nm, sh)
            d = self.hyc[pfx + nm]
            k.dma("sp", cst[nm][:], d.v(0, d.t))
        hb = sb("hb", [128, 1024])
        k.dma("sp", hb[:], self.od_hy_bias.v(0, self.od_hy_bias.t[j, :, :]))
        slots = []
        for sl in range(NSLOT):
            B_ = {}
            B_["hfl"] = [sb(f"hfl{sl}_{i}", [N1, NSQ, N2]) for i in range(2)]
            B_["dat"] = [sb(f"dat{sl}_{q}", [H1, NSQ, N2]) for q in range(3)]
            B_["Xs"] = [sb(f"Xs{sl}_{i}", [N2, NSQ, 2 * N1]) for i in range(2)]
            B_["KA"] = [sb(f"KA{sl}_{i}", [N2, NSQ, 2 * N1]) for i in range(2)]
            B_["KB"] = [sb(f"KB{sl}_{i}", [N2, NSQ, 2 * N1]) for i in range(2)]
            B_["B"] = sb(f"B{sl}", [N2, NSQ, 2 * N1])
            B_["tmf"] = sb(f"tmf{sl}", [N2, NSQ, 2 * N1])
            B_["Y"] = sb(f"Y{sl}", [N2, NSQ, 2 * N1])
            B_["D"] = sb(f"D{sl}", [N1, NSQ, 2 * N2])
            B_["tmi"] = sb(f"tmi{sl}", [N1, NSQ, 2 * N2])
            B_["zmid"] = sb(f"zmid{sl}", [H1, NSQ, N2])
            B_["zout"] = sb(f"zout{sl}", [H1, NSQ, N2], BF16)
            B_["pA"] = k.psum(es, f"hv_pA{sl}", [128, 512])
            B_["pX"] = k.psum(es, f"hv_pX{sl}", [128, 512])
            B_["pC"] = B_["pA"]
            B_["pY"] = B_["pX"]
            B_["alt"] = 0
            slots.append(B_)

        def v3(tile_, P, W):
            return tile_.t[0:P, 0:NSQ * W].rearrange("p (s w) -> p s w", s=NSQ)

        def add_eng(S_):
            S_["alt"] += 1
            return "pool" if S_["alt"] % 3 else "dve"

        def fwd_fft(S_, zb, rows=H1):
            pa, px, B, tm = S_["pA"], S_["pX"], S_["B"], S_["tmf"]
            A3 = v3(pa, N2, 2 * N1)
            X3 = v3(px, N2, 2 * N1)
            f1 = cst["F1cat"]
            for s in range(NSQ):
                k.op("pe", "matmul", out=pa.v(A3[:, s, :]), lhsT=zb.v(zb.t[0:rows, s, :]), rhs=f1.v(f1.t[0:rows, :]),
                     start=True, stop=True)
            yield
            k.op("dve", "tensor_tensor", out=B[:], in0=pa.v(A3), in1=cst["TwA"][:], op=ALU.mult)
            k.op("dve", "tensor_tensor", out=tm.v(tm.t[:, :, 0:N1]), in0=pa.v(A3[:, :, N1:2 * N1]),
                 in1=cst["TwB"].v(cst["TwB"].t[:, :, 0:N1]), op=ALU.mult)
            k.op("dve", "tensor_tensor", out=tm.v(tm.t[:, :, N1:2 * N1]), in0=pa.v(A3[:, :, 0:N1]),
                 in1=cst["TwB"].v(cst["TwB"].t[:, :, N1:2 * N1]), op=ALU.mult)
            yield
            k.op(add_eng(S_), "tensor_tensor", out=B[:], in0=B[:], in1=tm[:], op=ALU.add)
            yield
            Br, Bi = B.v(B.t[:, :, 0:N1]), B.v(B.t[:, :, N1:2 * N1])
            k.op("pe", "matmul", out=px.v(X3[:, :, 0:N1]), lhsT=cst["F2c"][:], rhs=Br, start=True, stop=False)
            k.op("pe", "matmul", out=px.v(X3[:, :, 0:N1]), lhsT=cst["F2sn"][:], rhs=Bi, start=False, stop=True)
            k.op("pe", "matmul", out=px.v(X3[:, :, N1:2 * N1]), lhsT=cst["F2s"][:], rhs=Br, start=True, stop=False)
            k.op("pe", "matmul", out=px.v(X3[:, :, N1:2 * N1]), lhsT=cst["F2c"][:], rhs=Bi, start=False, stop=True)
            yield

        def inv_fft(S_, Y):
            pC, pY, Dt, tmi = S_["pC"], S_["pY"], S_["D"], S_["tmi"]
            C3 = v3(pC, N1, 2 * N2)
            for s in range(NSQ):
                k.op("pe", "matmul", out=pC.v(C3[:, s, :]), lhsT=Y.v(Y.t[:, s, 0:N1]), rhs=cst["G2cat"][:],
                     start=True, stop=False)
                k.op("pe", "matmul", out=pC.v(C3[:, s, :]), lhsT=Y.v(Y.t[:, s, N1:2 * N1]), rhs=cst["G2cat2"][:],
                     start=False, stop=True)
            yield
            k.op("dve", "tensor_tensor", out=Dt[:], in0=pC.v(C3), in1=cst["TwAi"][:], op=ALU.mult)
            k.op("dve", "tensor_tensor", out=tmi.v(tmi.t[:, :, 0:N2]), in0=pC.v(C3[:, :, N2:2 * N2]),
                 in1=cst["TwBi"].v(cst["TwBi"].t[:, :, 0:N2]), op=ALU.mult)
            k.op("dve", "tensor_tensor", out=tmi.v(tmi.t[:, :, N2:2 * N2]), in0=pC.v(C3[:, :, 0:N2]),
                 in1=cst["TwBi"].v(cst["TwBi"].t[:, :, N2:2 * N2]), op=ALU.mult)
            yield
            k.op(add_eng(S_), "tensor_tensor", out=Dt[:], in0=Dt[:], in1=tmi[:], op=ALU.add)
            yield
            Y3 = pY.t[0:H1, 0:NSQ * N2].rearrange("p (s w) -> p s w", s=NSQ)
            k.op("pe", "matmul", out=pY.v(Y3), lhsT=cst["G1c"][:], rhs=Dt.v(Dt.t[:, :, 0:N2]), start=True, stop=False)
            k.op("pe", "matmul", out=pY.v(Y3), lhsT=cst["G1sn"][:], rhs=Dt.v(Dt.t[:, :, N2:2 * N2]), start=False, stop=True)
            yield

        def blk(dt_, row0):
            return dt_.t[row0:row0 + NSQ, :].rearrange("s (a b) -> a s b", b=N2)

        def group(g, S_):
            ch0 = g * NSQ
            dd = S_["dat"]
            Xs, KA, KB = S_["Xs"], S_["KA"], S_["KB"]
            px = S_["pX"]
            X3 = v3(px, N2, 2 * N1)
            pY = S_["pY"]
            Y3 = pY.t[0:H1, 0:NSQ * N2].rearrange("p (s w) -> p s w", s=NSQ)
            for q in range(3):
                k.dma("sp", dd[q][:], self.UC.v("all", self.UC.t[q * 512 + ch0:q * 512 + ch0 + NSQ,
                                                                 toff:toff + Lseg].rearrange("s (a b) -> a s b", b=N2)))
            for o in range(2):
                hf = S_["hfl"][o]
                k.dma("sp", hf[:], HT.v("all", blk(HT, o * 512 + ch0)))
            yield
            for o in range(2):
                hf = S_["hfl"][o]
                yield from fwd_fft(S_, hf, N1)
                ka, kb = KA[o], KB[o]
                for s in range(NSQ):
                    ci = o * 512 + ch0 + s
                    k.op("act", "activation", out=ka.v(ka.t[:, s, 0:N1]), in_=px.v(X3[:, s, 0:N1]), func=AF.Identity,
                         bias=hb.v(hb.t[0:N2, ci:ci + 1]), scale=1.0)
                k.op("act", "copy", out=kb.v(kb.t[:, :, N1:2 * N1]), in_=px.v(X3[:, :, N1:2 * N1]))
                k.op("act", "activation", out=kb.v(kb.t[:, :, 0:N1]), in_=px.v(X3[:, :, N1:2 * N1]), func=AF.Copy,
                     scale=-1.0)
                yield
                k.op("pool", "tensor_copy", out=ka.v(ka.t[:, :, N1:2 * N1]), in_=ka.v(ka.t[:, :, 0:N1]))
                yield
            zcur = dd[0]
            Yt, tm = S_["Y"], S_["tmf"]
            for o in range(2):
                yield from fwd_fft(S_, zcur)
                ka, kb = KA[o], KB[o]
                k.op("dve", "tensor_tensor", out=Yt[:], in0=px.v(X3), in1=ka[:], op=ALU.mult)
                k.op("dve", "tensor_tensor", out=tm.v(tm.t[:, :, 0:N1]), in0=px.v(X3[:, :, N1:2 * N1]),
                     in1=kb.v(kb.t[:, :, 0:N1]), op=ALU.mult)
                k.op("dve", "tensor_tensor", out=tm.v(tm.t[:, :, N1:2 * N1]), in0=px.v(X3[:, :, 0:N1]),
                     in1=kb.v(kb.t[:, :, N1:2 * N1]), op=ALU.mult)
                yield
                k.op(add_eng(S_), "tensor_tensor", out=Yt[:], in0=Yt[:], in1=tm[:], op=ALU.add)
                yield
                yield from inv_fft(S_, Yt)
                if o == 0:
                    k.op("dve", "tensor_tensor", out=S_["zmid"][:], in0=pY.v(Y3), in1=dd[1][:], op=ALU.mult)
                    zcur = S_["zmid"]
                else:
                    zo = S_["zout"]
                    k.op("dve", "tensor_tensor", out=zo[:], in0=pY.v(Y3), in1=dd[2][:], op=ALU.mult)
                    k.dma("pool", YM.v("all", YM.t[ch0:ch0 + NSQ, toff:toff + Lseg].rearrange("s (a b) -> a s b", b=N2)),
                          zo[:])
                yield

        ngroups = 512 // NSQ
        nxt = 0
        active = []
        for sl in range(NSLOT):
            if nxt < ngroups:
                active.append([group(nxt, slots[sl]), sl])
                nxt += 1
        while active:
            for ent in list(active):
                try:
                    next(ent[0])
                except StopIteration:
                    if nxt < ngroups:
                        ent[0] = group(nxt, slots[ent[1]])
                        nxt += 1
                    else:
                        active.remove(ent)


M.hy_conv = hy_conv


def hyena(self, j, PT, YM, ctx_out):
    k, cfg = self.k, self.cfg
    self.hy_shortconv(j, PT, ctx_out)
    k.barrier()
    self.hy_filters(j, "hm_", cfg.L, self.HT)
    k.barrier()
    if ctx_out:
        self.hy_filters(j, "hc_", C, self.HTc)
        k.barrier()
    self.hy_conv(j, "hm_", cfg.L, C, self.HT, YM)
    k.barrier()
    if ctx_out:
        self.hy_conv(j, "hc_", C, 0, self.HTc, YM)
        k.barrier()


M.hyena = hyena

SEQ = 8192
DEPTH = 4
BATCH = 4


def host_inputs(inp, b, L, depth):
    d = {}
    d["x"] = np.ascontiguousarray(inp["x"][b])
    d["ctx"] = np.ascontiguousarray(inp["ctx"][b])
    d["cc"] = np.ascontiguousarray(np.stack([inp["c"][b], inp["c_ctx"]], -1).reshape(8, 128, 2).transpose(1, 0, 2))
    d["w_ada"] = inp["w_ada"]
    d["b_adaT"] = np.ascontiguousarray(inp["b_ada"].reshape(depth, 48, 128).transpose(0, 2, 1))
    d["norm_gT"] = np.ascontiguousarray(inp["norm_g"].reshape(depth, 32, 128).transpose(0, 2, 1))
    d["w_mlp_in"] = inp["w_mlp_in"]
    d["w_mlp_out"] = inp["w_mlp_out"]
    d["ev_w_in"] = inp["ev_w_in"]
    d["ev_w_out"] = inp["ev_w_out"]
    g = inp["ev_qk_g"]
    d["ev_qk_gT"] = np.ascontiguousarray(np.tile(g, (1, 1, 2)).transpose(0, 2, 1))
    d["od_w_in"] = inp["od_w_in"]
    d["od_w_out"] = inp["od_w_out"]
    no = inp["od_lam"].shape[0]
    d["od_lamB"] = np.ascontiguousarray(np.broadcast_to(inp["od_lam"].reshape(no, 1, 256), (no, 128, 256)))
    d["od_diff_gT"] = np.ascontiguousarray(inp["od_diff_g"][:, :, None])
    d["od_conv_wT"] = np.ascontiguousarray(
        inp["od_conv_w"].transpose(0, 2, 1).reshape(no, 12, 128, 3).transpose(0, 2, 1, 3))
    d["od_conv_bT"] = np.ascontiguousarray(inp["od_conv_b"].reshape(no, 12, 128).transpose(0, 2, 1))
    d["od_f_w1"] = inp["od_f_w1"]
    d["od_f_w2"] = inp["od_f_w2"]
    d["od_f_w3"] = inp["od_f_w3"]
    d["od_f_b1T"] = np.ascontiguousarray(inp["od_f_b1"][:, :, None])
    d["od_f_b2T"] = np.ascontiguousarray(inp["od_f_b2"][:, :, None])
    d["od_hy_biasB"] = np.ascontiguousarray(np.broadcast_to(inp["od_hy_bias"].reshape(no, 1, 1024), (no, 128, 1024)))
    d["ev_wlr_aug"] = np.ascontiguousarray(np.concatenate([inp["ev_w_lr"], inp["ev_b_lr"][:, :, None, :]], axis=2))
    d["ev_gla_gT"] = np.ascontiguousarray(inp["ev_gla_g"][:, :, None])
    return d


def const_inputs(L):
    d = {}
    d.update(hy_consts(L, "hm_"))
    d.update(hy_consts(C, "hc_"))
    d.update(gla_consts())
    d.update(host_consts())
    d["rope"] = rope_tables(L)
    d["rotm"] = rot_matrix()
    d["blk64"] = block_ones(64)
    return d


def kernel(**inputs):
    inp = {k_: np.asarray(v) for k_, v in inputs.items()}
    B, L, _ = inp["x"].shape
    depth = inp["w_ada"].shape[0]
    cfg = Cfg(L=L, depth=depth, debug=False)
    mm = M(cfg)
    nc = mm.build2()
    consts = const_inputs(L)
    n_cores = 8
    per_b = [dict(host_inputs(inp, b, L, depth), **consts) for b in range(B)]
    in_maps = [per_b[i % B] for i in range(n_cores)]
    res = run_bass_kernel_spmd(nc, in_maps, core_ids=list(range(n_cores)))
    out = np.stack([np.asarray(res.results[b]["out"]) for b in range(B)], axis=0)
    return out.astype(np.float32, copy=False)
```
